# Optimizing a Trainium2 kernel written in Bass

```python
import math
import jax, jax.numpy as jnp
from jax import lax
import numpy as np

D_MODEL = 2048
BATCH = 8
SEQ = 2048
DEPTH = 1

CHUNK = 64
Q_BLOCK = 128
EPS = 1e-6

A_HEADS = 8
A_HEAD_DIM = 64
A_V_DIM = 2 * A_HEAD_DIM
A_WIDTH = A_HEADS * A_V_DIM
QK_COLS = A_HEADS * 2 * A_HEAD_DIM

G_GROUPS = 8
G_CHUNK = 128
G_GROUP_DIM = 128
G_WIDTH = G_GROUPS * G_GROUP_DIM

N_BRANCHES = 2

IN_COLS = 2 * QK_COLS + A_WIDTH + 2 * G_WIDTH + N_BRANCHES * D_MODEL
SPLITS = [QK_COLS, 2 * QK_COLS, 2 * QK_COLS + A_WIDTH, 2 * QK_COLS + A_WIDTH + 2 * G_WIDTH]

P_HEADS = 8
P_NKEYS = 128
P_NEXPERTS = P_NKEYS * P_NKEYS
P_KEY_DIM = 256
P_HALF = P_KEY_DIM // 2
P_TOPK = 16
P_TOKEN_BLOCK = 128

kernel_name = "hybrid_diffattn_gmlp_peer_block"


def rmsnorm(x, g):
    xf = x.astype(jnp.float32)
    y = xf * lax.rsqrt(jnp.mean(xf * xf, axis=-1, keepdims=True) + EPS)
    return (y * g.astype(jnp.float32)).astype(x.dtype)


def alibi_slopes(n_heads):
    return jnp.asarray(2.0 ** (-8.0 * np.arange(1, n_heads + 1) / n_heads), dtype=jnp.float32)


def diff_attention(q, k, v, lam, subln_gain, lam_init):
    S = q.shape[1]
    scale = A_HEAD_DIM ** -0.5
    slopes = alibi_slopes(A_HEADS)
    outs = []
    for qb in range(S // Q_BLOCK):
        q0 = qb * Q_BLOCK
        kv = q0 + Q_BLOCK
        qi = q[:, q0:kv]
        kj = k[:, :kv]
        vj = v[:, :kv]
        s = jnp.einsum('bqhmd,bkhmd->bhmqk', qi, kj).astype(jnp.float32) * scale
        tpos = jnp.arange(q0, kv)
        spos = jnp.arange(kv)
        dist = jnp.abs(tpos[:, None] - spos[None, :]).astype(jnp.float32)
        bias = -slopes[:, None, None, None] * dist
        mask = (spos[None, :] // CHUNK) <= (tpos[:, None] // CHUNK)
        s = jnp.where(mask, s + bias, -jnp.inf)
        p = jax.nn.softmax(s, axis=-1)
        attn = p[:, :, 0] - lam * p[:, :, 1]
        outs.append(jnp.einsum('bhqk,bkhd->bqhd', attn.astype(v.dtype), vj))
    o = jnp.concatenate(outs, axis=1)
    return rmsnorm(o, subln_gain) * (1.0 - lam_init)


def spatial_gating(z, v_gain, w_s, b_s):
    B, S, _ = z.shape
    u, vv = z[..., :G_WIDTH], z[..., G_WIDTH:]
    vv = rmsnorm(vv, v_gain).reshape(B, S // G_CHUNK, G_CHUNK, G_GROUPS, G_GROUP_DIM)
    tri = jnp.tril(jnp.ones((G_CHUNK, G_CHUNK), dtype=bool))
    ws = jnp.where(tri[None], w_s, jnp.zeros_like(w_s))
    sv = jnp.einsum('gts,bnsgc->bntgc', ws, vv) + b_s.T[None, None, :, :, None]
    return u * sv.reshape(B, S, G_WIDTH)


def peer(h, w_q, sub_keys, u_emb, v_emb):
    B, S, D = h.shape
    T = B * S
    ht = h.reshape(T, D)
    q = (ht @ w_q).reshape(T, P_HEADS, 2, P_HALF)
    s = jnp.einsum('thpd,pnd->thpn', q, sub_keys).astype(jnp.float32)
    v1, i1 = lax.top_k(s[:, :, 0], P_TOPK)
    v2, i2 = lax.top_k(s[:, :, 1], P_TOPK)
    cand = (v1[..., :, None] + v2[..., None, :]).reshape(T, P_HEADS, P_TOPK * P_TOPK)
    cidx = (i1[..., :, None] * P_NKEYS + i2[..., None, :]).reshape(T, P_HEADS, P_TOPK * P_TOPK)
    top, pos = lax.top_k(cand, P_TOPK)
    eidx = jnp.take_along_axis(cidx, pos, axis=-1)
    gate = jax.nn.softmax(top, axis=-1)
    nb = T // P_TOKEN_BLOCK
    xs = ht.reshape(nb, P_TOKEN_BLOCK, D)
    ids = eidx.reshape(nb, P_TOKEN_BLOCK, P_HEADS * P_TOPK)
    gs = gate.reshape(nb, P_TOKEN_BLOCK, P_HEADS * P_TOPK)

    def block(args):
        xb, ib, gb = args
        u = u_emb[ib]
        a = jnp.einsum('td,tkd->tk', xb, u).astype(jnp.float32)
        act = jax.nn.gelu(a, approximate=False) * gb
        v = v_emb[ib]
        return jnp.einsum('tk,tkd->td', act.astype(v.dtype), v)

    out = lax.map(block, (xs, ids, gs))
    return out.reshape(B, S, D)


def setup_inputs(seed: int = 0) -> dict:
    key = jax.random.key(seed)
    ks = jax.random.split(key, 20)
    f32 = jnp.float32
    nrm = lambda k, shape, s: jax.random.normal(k, shape, f32) * s
    L = DEPTH
    return {
        "x": nrm(ks[0], (BATCH, SEQ, D_MODEL), 1.0),
        "attn_norm": 1.0 + nrm(ks[1], (L, D_MODEL), 0.02),
        "w_in": nrm(ks[2], (L, D_MODEL, IN_COLS), D_MODEL ** -0.5),
        "diff_lambda": nrm(ks[3], (L, 4, A_HEAD_DIM), 0.1),
        "diff_subln": 1.0 + nrm(ks[4], (L, A_V_DIM), 0.02),
        "gmlp_norm": 1.0 + nrm(ks[5], (L, G_WIDTH), 0.02),
        "gmlp_ws": nrm(ks[6], (L, G_GROUPS, G_CHUNK, G_CHUNK), G_CHUNK ** -0.5),
        "gmlp_bs": 1.0 + nrm(ks[7], (L, G_GROUPS, G_CHUNK), 0.01),
        "w_branch_a": nrm(ks[8], (L, A_WIDTH, D_MODEL), A_WIDTH ** -0.5),
        "w_branch_b": nrm(ks[9], (L, G_WIDTH, D_MODEL), G_WIDTH ** -0.5),
        "w_out": nrm(ks[10], (L, D_MODEL, D_MODEL), D_MODEL ** -0.5),
        "ffn_norm": 1.0 + nrm(ks[11], (L, D_MODEL), 0.02),
        "peer_wq": nrm(ks[12], (L, D_MODEL, P_HEADS * P_KEY_DIM), D_MODEL ** -0.5),
        "peer_subkeys": nrm(ks[13], (L, 2, P_NKEYS, P_HALF), P_HALF ** -0.5),
        "peer_u": nrm(ks[14], (L, P_NEXPERTS, D_MODEL), D_MODEL ** -0.5),
        "peer_v": nrm(ks[15], (L, P_NEXPERTS, D_MODEL), P_HEADS ** -0.5),
        "final_norm": 1.0 + nrm(ks[16], (D_MODEL,), 0.02),
    }


def reference(x, attn_norm, w_in, diff_lambda, diff_subln, gmlp_norm, gmlp_ws, gmlp_bs,
              w_branch_a, w_branch_b, w_out, ffn_norm, peer_wq, peer_subkeys, peer_u, peer_v,
              final_norm):
    B, S, _ = x.shape
    for l in range(DEPTH):
        h = rmsnorm(x, attn_norm[l])
        proj = h @ w_in[l]
        q, k, v, z, gate = jnp.split(proj, SPLITS, axis=-1)
        q = q.reshape(B, S, A_HEADS, 2, A_HEAD_DIM)
        k = k.reshape(B, S, A_HEADS, 2, A_HEAD_DIM)
        v = v.reshape(B, S, A_HEADS, A_V_DIM)
        lam_init = 0.8 - 0.6 * math.exp(-0.3 * l)
        lp = diff_lambda[l].astype(jnp.float32)
        lam = jnp.exp(jnp.sum(lp[0] * lp[1])) - jnp.exp(jnp.sum(lp[2] * lp[3])) + lam_init
        o_a = diff_attention(q, k, v, lam, diff_subln[l], lam_init).reshape(B, S, A_WIDTH)
        o_b = spatial_gating(jax.nn.gelu(z, approximate=False), gmlp_norm[l], gmlp_ws[l], gmlp_bs[l])
        g = jax.nn.sigmoid(gate.astype(jnp.float32)).astype(x.dtype).reshape(B, S, N_BRANCHES, D_MODEL)
        merged = g[:, :, 0] * (o_a @ w_branch_a[l]) + g[:, :, 1] * (o_b @ w_branch_b[l])
        x = x + merged @ w_out[l]
        x = x + peer(rmsnorm(x, ffn_norm[l]), peer_wq[l], peer_subkeys[l], peer_u[l], peer_v[l])
    return rmsnorm(x, final_norm)
```

```python
import numpy as np
import ml_dtypes
import concourse.bass as bass
import concourse.mybir as mybir
from concourse.bass_utils import run_bass_kernel_spmd
from contextlib import ExitStack

F32 = mybir.dt.float32
BF16 = mybir.dt.bfloat16
ALU = mybir.AluOpType
AF = mybir.ActivationFunctionType
AX = mybir.AxisListType

S = 2048
D = 2048
NT = S // 128
EPS = 1e-6
IN_COLS = 9216
LAM_INIT = 0.8 - 0.6 * 1.0
NEG = -1.0e30


class Buf:
    __slots__ = ("name", "last_w", "readers")

    def __init__(self, name="b"):
        self.name = name
        self.last_w = None
        self.readers = []


class Op:
    __slots__ = ("eng", "fn", "dma", "deps", "signal", "sem", "semval", "prewait")

    def __init__(self, eng, fn, dma):
        self.eng = eng
        self.fn = fn
        self.dma = dma
        self.deps = []
        self.signal = False
        self.sem = None
        self.semval = None
        self.prewait = None


class Prog:
    ENGS = ("pe", "act", "dve", "pool", "sp")
    NDMA = {"sp": 12, "pool": 12}

    def __init__(self, nc, es):
        self.nc = nc
        self.es = es
        self.cur = es
        self.ops = {e: [] for e in self.ENGS}
        self.nops = 0
        self._bar_idx = {e: 0 for e in self.ENGS}
        self._pending = {e: [] for e in self.ENGS}

    def sb(self, name, shape, dtype):
        self.nops += 0
        self._uid = getattr(self, "_uid", 0) + 1
        return self.cur.enter_context(self.nc.sbuf_tensor(f"{name}_u{self._uid}", list(shape), dtype))

    def ps(self, name, shape, dtype=F32):
        self._uid = getattr(self, "_uid", 0) + 1
        return self.cur.enter_context(self.nc.psum_tensor(f"{name}_u{self._uid}", list(shape), dtype))

    def bufs(self, n, name="b"):
        return [Buf(name) for _ in range(n)]

    def barrier(self):
        deps = []
        for e in self.ENGS:
            ops = self.ops[e]
            for op in reversed(ops):
                if not op.dma:
                    deps.append(op)
                    break
            deps += [op for op in ops[self._bar_idx[e]:] if op.dma]
            self._bar_idx[e] = len(ops)
        for e in self.ENGS:
            self._pending[e] = self._pending[e] + deps

    def add(self, eng, fn, reads=(), writes=(), dma=False):
        op = Op(eng, fn, dma)
        deps = {}
        for b in reads:
            w = b.last_w
            if w is not None:
                deps[id(w)] = (w, 0)
        for b in writes:
            w = b.last_w
            if w is not None and id(w) not in deps:
                deps[id(w)] = (w, 1)
            for r in b.readers:
                if id(r) not in deps:
                    deps[id(r)] = (r, 2)
        for d, kind in deps.values():
            if d is op:
                continue
            if not d.dma and not op.dma and d.eng == eng:
                if eng == "pe" or kind == 2:
                    continue
            op.deps.append(d)
            d.signal = True
        if self._pending[eng]:
            for d in self._pending[eng]:
                op.deps.append(d)
                d.signal = True
            self._pending[eng] = []
        for b in reads:
            b.readers.append(op)
        for b in writes:
            b.last_w = op
            b.readers = []
        self.ops[eng].append(op)
        self.nops += 1
        return op

    def emit(self, final_waits=()):
        nc = self.nc
        es = self.es
        esem = {e: es.enter_context(nc.semaphore(f"s_{e}")) for e in self.ENGS if e != "sp"}
        dsem = {e: [es.enter_context(nc.semaphore(f"d_{e}{i}")) for i in range(n)] for e, n in self.NDMA.items()}
        for e in self.ENGS:
            tick = 0
            k = 0
            cnt = [0] * self.NDMA.get(e, 0)
            for op in self.ops[e]:
                if op.dma:
                    n = self.NDMA[e]
                    j = k % n
                    k += 1
                    op.prewait = (dsem[e][j], 16 * cnt[j]) if cnt[j] > 0 else None
                    cnt[j] += 1
                    op.sem = dsem[e][j]
                    op.semval = 16 * cnt[j]
                elif op.signal:
                    tick += 1
                    op.sem = esem[e]
                    op.semval = tick
        block = es.enter_context(nc.Block())
        engfn = {"pe": block.tensor, "act": block.scalar, "dve": block.vector, "pool": block.gpsimd, "sp": block.sync}

        def make(e):
            ops = self.ops[e]

            def body(eng):
                waited = {}

                def flush(need):
                    for key, (sem, val) in need.items():
                        if waited.get(key, 0) >= val:
                            continue
                        waited[key] = val
                        eng.wait_ge(sem, val)

                for op in ops:
                    need = {}
                    for d in op.deps:
                        key = id(d.sem)
                        if key not in need or need[key][1] < d.semval:
                            need[key] = (d.sem, d.semval)
                    if op.prewait is not None:
                        key = id(op.prewait[0])
                        if key not in need or need[key][1] < op.prewait[1]:
                            need[key] = op.prewait
                    flush(need)
                    ins = op.fn(eng)
                    if op.dma:
                        ins.then_inc(op.sem, 16)
                    elif op.signal:
                        ins.then_inc(op.sem, 1)
                if e == "sp":
                    need = {}
                    for d in final_waits:
                        key = id(d.sem)
                        if key not in need or need[key][1] < d.semval:
                            need[key] = (d.sem, d.semval)
                    flush(need)

            return body

        for e in self.ENGS:
            engfn[e](make(e))


def build(stop_after=99, dbg=()):
    nc = bass.Bass("TRN2", target_bir_lowering=False)

    def din(name, shape, dt=F32):
        return nc.dram_tensor(name, list(shape), dt, kind="ExternalInput").ap()

    def dscr(name, shape, dt):
        kind = "ExternalOutput" if name in dbg else "Internal"
        return nc.dram_tensor(name, list(shape), dt, kind=kind).ap()

    x = din("x", [S, D])
    attn_norm = din("attn_norm", [1, D])
    w_in = din("w_in", [D, IN_COLS])
    diff_lambda = din("diff_lambda", [1, 256])
    diff_subln = din("diff_subln", [1, 128])
    gmlp_norm = din("gmlp_norm", [1, 1024])
    gmlp_ws = din("gmlp_ws", [8, 128, 128])
    gmlp_bs = din("gmlp_bs", [8, 128])
    w_a = din("w_branch_a", [1024, D])
    w_b = din("w_branch_b", [1024, D])
    w_out = din("w_out", [D, D])
    ffn_norm = din("ffn_norm", [1, D])
    peer_wq = din("peer_wq", [D, 2048])
    peer_sk = din("peer_subkeys", [2, 128, 128])
    peer_u = din("peer_u", [16384, D])
    peer_v = din("peer_v", [16384, D])
    final_norm = din("final_norm", [1, D])
    c_identb = din("c_identb", [128, 128], BF16)
    c_identf = din("c_identf", [128, 128], F32)
    c_kaug = din("c_kaug", [8, 4, S], BF16)
    c_qaug = din("c_qaug", [8, 4, S], BF16)
    c_bdiag = din("c_bdiag", [128, 8, 128], BF16)
    c_tri = din("c_tri", [128, 128], F32)
    out = nc.dram_tensor("out", [S, D], F32, kind="ExternalOutput").ap()

    OAT = dscr("OAT", [128, 8, S], BF16)
    OBT = dscr("OBT", [128, 8, S], BF16)
    MT = dscr("MT", [128, 16, S], BF16)
    X2 = dscr("X2", [S, D], F32)
    H2T = dscr("H2T", [128, 16, S], BF16)
    GT = dscr("GT", [128, 128, S], BF16)
    b_MT = [Buf() for _ in range(16)]
    b_X2 = [[Buf() for _ in range(4)] for _ in range(NT)]
    b_H2T = [Buf() for _ in range(NT)]
    b_GT = [Buf() for _ in range(NT)]
    UTD = dscr("UTD", [128, 128, 2048], BF16)
    b_UTD = [Buf() for _ in range(128)]

    w_in_v = w_in.rearrange("(c p) n -> p c n", p=128)
    fin = []

    with ExitStack() as es:
        P = Prog(nc, es)
        A = P.add

        def dump(name, ap, shape, dt, bufs):
            if name not in dbg:
                return
            d_ = nc.dram_tensor(name, list(shape), dt, kind="ExternalOutput").ap()
            A("sp", lambda e: e.dma_start(out=d_, in_=ap), reads=bufs, dma=True)

        identb = P.sb("identb", [128, 128], BF16); b_identb = Buf()
        identf = P.sb("identf", [128, 128], F32); b_identf = Buf()
        epsT = P.sb("epsT", [128, 1], F32); b_eps = Buf()
        A("sp", lambda e: e.dma_start(out=identb[:], in_=c_identb), writes=[b_identb], dma=True)
        A("sp", lambda e: e.dma_start(out=identf[:], in_=c_identf), writes=[b_identf], dma=True)
        A("dve", lambda e: e.memset(epsT[:], EPS), writes=[b_eps])

        def rms_stats(src_ap, b_src, ss, b_ss, rstd, b_rstd, junk, b_junk, width):
            A("act", lambda e: e.activation(out=junk, in_=src_ap, func=AF.Square, scale=float(width) ** -0.5, accum_out=ss),
              reads=(b_src if isinstance(b_src, list) else [b_src]), writes=[b_junk, b_ss])
            A("act", lambda e: e.activation(out=ss, in_=ss, func=AF.Sqrt, bias=epsT[:], scale=1.0),
              reads=[b_ss, b_eps], writes=[b_ss])
            A("dve", lambda e: e.reciprocal(out=rstd, in_=ss), reads=[b_ss], writes=[b_rstd])

        with ExitStack() as sc_h:
            P.cur = sc_h
            hT = P.sb("hT", [128, 16, S], BF16); b_hT = P.bufs(NT)

            with ExitStack() as ph:
                P.cur = ph
                gA = P.sb("gA", [128, D], F32); b_gA = Buf()
                xt = [P.sb(f"xt{i}", [128, D], F32) for i in range(2)]; b_xt = P.bufs(2)
                junk = P.sb("junk1", [128, D], BF16); b_junk = Buf()
                ss = [P.sb(f"ss{i}", [128, 1], F32) for i in range(2)]; b_ss = P.bufs(2)
                rstd = [P.sb(f"rstd{i}", [128, 1], F32) for i in range(2)]; b_rstd = P.bufs(2)
                hb = [P.sb(f"hb{i}", [128, D], BF16) for i in range(2)]; b_hb = P.bufs(2)
                pT = [P.ps(f"pT{i}", [128, D], BF16) for i in range(2)]; b_pT = P.bufs(2)
                A("sp", lambda e: e.dma_start(out=gA[:], in_=attn_norm.partition_broadcast(128)), writes=[b_gA], dma=True)
                for tt in range(NT):
                    b = tt % 2
                    A("sp", lambda e, tt=tt, b=b: e.dma_start(out=xt[b][:], in_=x[tt * 128:(tt + 1) * 128, :]), writes=[b_xt[b]], dma=True)
                    rms_stats(xt[b][:], b_xt[b], ss[b][:], b_ss[b], rstd[b][:], b_rstd[b], junk[:], b_junk, D)
                    A("dve", lambda e, b=b: e.scalar_tensor_tensor(out=hb[b][:], in0=xt[b][:], scalar=rstd[b][:, 0:1], in1=gA[:], op0=ALU.mult, op1=ALU.mult),
                      reads=[b_xt[b], b_rstd[b], b_gA], writes=[b_hb[b]])
                    for c in range(16):
                        A("pe", lambda e, b=b, c=c: e.transpose(out=pT[b][:, c * 128:(c + 1) * 128], in_=hb[b][:, c * 128:(c + 1) * 128], identity=identb[:]),
                          reads=[b_hb[b], b_identb], writes=[b_pT[b]])
                    A("act", lambda e, b=b, tt=tt: e.activation(out=hT[:, :, tt * 128:(tt + 1) * 128], in_=pT[b][:].rearrange("p (c t) -> p c t", c=16), func=AF.Copy),
                      reads=[b_pT[b]], writes=[b_hT[tt]])
            P.barrier()

            with ExitStack() as sc_o:
                P.cur = sc_o
                b_OATh = [Buf() for _ in range(8)]
                b_oaT = [[Buf() for _ in range(NT)] for _ in range(8)]
                b_obT = [[Buf() for _ in range(NT)] for _ in range(4)]

                if stop_after >= 2:
                  with ExitStack() as ph:
                    P.cur = ph
                    oaTa = P.sb("oaTa", [128, 8, S], BF16)
                    wq = [P.sb(f"wq{i}", [128, 16, 128], BF16) for i in range(2)]; b_wq = P.bufs(2)
                    wk = [P.sb(f"wk{i}", [128, 16, 128], BF16) for i in range(2)]; b_wk = P.bufs(2)
                    wv = [P.sb(f"wv{i}", [128, 16, 128], BF16) for i in range(2)]; b_wv = P.bufs(2)
                    qT = [P.sb(f"qT{i}", [128, S], BF16) for i in range(2)]; b_qT = [P.bufs(4) for _ in range(2)]
                    kT = [P.sb(f"kT{i}", [128, S], BF16) for i in range(2)]; b_kT = [P.bufs(4) for _ in range(2)]
                    Va = [P.sb(f"Va{i}", [128, NT, 130], BF16) for i in range(2)]; b_Va = [P.bufs(NT) for _ in range(2)]
                    b_Vone = P.bufs(2)
                    kaug = [P.sb("kaug0", [4, S], BF16)] * 2; b_kaug = [Buf()] * 2
                    qaug = [P.sb("qaug0", [4, S], BF16)] * 2; b_qaug = [Buf()] * 2
                    bdiag = P.sb("bdiag", [128, 8, 128], BF16); b_bdiag = Buf()
                    PT = [P.sb(f"PT{i}", [128, NT * 128], BF16) for i in range(2)]; b_PT = [P.bufs(4) for _ in range(2)]
                    om = [P.sb(f"om{i}", [128, 128], F32) for i in range(2)]; b_om = P.bufs(2)
                    rz = [P.sb(f"rz{i}", [128, 1], F32) for i in range(2)]; b_rz = P.bufs(2)
                    ot = P.sb("ot", [128, 128], F32); b_ot = Buf()
                    oab = [P.sb(f"oab{i}", [128, 128], BF16) for i in range(2)]; b_oab = P.bufs(2)
                    junk2 = P.sb("junk2", [128, 128], BF16); b_junk2 = Buf()
                    ss2 = P.sb("ss2", [128, 1], F32); b_ss2 = Buf()
                    rstd2 = P.sb("rstd2", [128, 1], F32); b_rstd2 = Buf()
                    gsub = P.sb("gsub", [128, 128], F32); b_gsub = Buf()
                    lamt = P.sb("lamt", [128, 256], F32); b_lamt = Buf()
                    lprod = P.sb("lprod", [128, 2, 64], F32); b_lprod = Buf()
                    lsum = P.sb("lsum", [128, 2], F32); b_lsum = Buf()
                    neglam = P.sb("neglam", [128, 1], F32); b_neglam = Buf()
                    pQ = [P.ps(f"pQ{i}", [128, 512]) for i in range(2)]; b_pQ = P.bufs(2)
                    pS = [P.ps(f"pS{i}", [128, 512]) for i in range(2)]; b_pS = P.bufs(2)
                    pO = [P.ps(f"pO{i}", [128, 512]) for i in range(2)]; b_pO = P.bufs(2)
                    pTr = P.ps("pTr", [128, 128], BF16); b_pTr = Buf()

                    A("sp", lambda e: e.dma_start(out=bdiag[:], in_=c_bdiag), writes=[b_bdiag], dma=True)
                    A("sp", lambda e: e.dma_start(out=gsub[:], in_=diff_subln.partition_broadcast(128)), writes=[b_gsub], dma=True)
                    A("dve", lambda e: e.tensor_scalar(out=gsub[:], in0=gsub[:], scalar1=1.0 - LAM_INIT, scalar2=None, op0=ALU.mult),
                      reads=[b_gsub], writes=[b_gsub])
                    A("sp", lambda e: e.dma_start(out=lamt[:], in_=diff_lambda.partition_broadcast(128)), writes=[b_lamt], dma=True)
                    A("dve", lambda e: e.tensor_tensor(out=lprod[:, 0, :], in0=lamt[:, 0:64], in1=lamt[:, 64:128], op=ALU.mult), reads=[b_lamt], writes=[b_lprod])
                    A("dve", lambda e: e.tensor_tensor(out=lprod[:, 1, :], in0=lamt[:, 128:192], in1=lamt[:, 192:256], op=ALU.mult), reads=[b_lamt, b_lprod], writes=[b_lprod])
                    A("dve", lambda e: e.reduce_sum(out=lsum[:], in_=lprod[:], axis=AX.X), reads=[b_lprod], writes=[b_lsum])
                    A("act", lambda e: e.activation(out=lsum[:], in_=lsum[:], func=AF.Exp), reads=[b_lsum], writes=[b_lsum])
                    A("dve", lambda e: e.tensor_tensor(out=neglam[:], in0=lsum[:, 0:1], in1=lsum[:, 1:2], op=ALU.subtract), reads=[b_lsum], writes=[b_neglam])
                    A("dve", lambda e: e.tensor_scalar(out=neglam[:], in0=neglam[:], scalar1=LAM_INIT, scalar2=-1.0, op0=ALU.add, op1=ALU.mult),
                      reads=[b_neglam], writes=[b_neglam])
                    for i in range(2):
                        A("dve", lambda e, i=i: e.memset(Va[i][:, :, 128:130], 1.0), writes=[b_Vone[i]])

                    ev = 0
                    for h in range(8):
                        b = h % 2
                        A("pool", lambda e, b=b, h=h: e.dma_start(out=wq[b][:], in_=w_in_v[:, :, h * 128:(h + 1) * 128]), writes=[b_wq[b]], dma=True)
                        A("pool", lambda e, b=b, h=h: e.dma_start(out=wk[b][:], in_=w_in_v[:, :, 1024 + h * 128:1024 + (h + 1) * 128]), writes=[b_wk[b]], dma=True)
                        A("pool", lambda e, b=b, h=h: e.dma_start(out=wv[b][:], in_=w_in_v[:, :, 2048 + h * 128:2048 + (h + 1) * 128]), writes=[b_wv[b]], dma=True)
                        A("sp", lambda e, b=b, h=h: e.dma_start(out=kaug[b][:], in_=c_kaug[h]), writes=[b_kaug[b]], dma=True)
                        A("sp", lambda e, b=b, h=h: e.dma_start(out=qaug[b][:], in_=c_qaug[h]), writes=[b_qaug[b]], dma=True)
                        for (w_, bw, dst, bdst) in ((wq, b_wq, qT, b_qT), (wk, b_wk, kT, b_kT)):
                            for g in range(4):
                                pb = ev % 2; ev += 1
                                for c in range(16):
                                    A("pe", lambda e, pb=pb, c=c, g=g, w_=w_, b=b: e.matmul(pQ[pb][:, :], lhsT=w_[b][:, c, :], rhs=hT[:, c, g * 512:(g + 1) * 512], start=(c == 0), stop=(c == 15)),
                                      reads=[bw[b]] + b_hT[g * 4:(g + 1) * 4], writes=[b_pQ[pb]])
                                if pb == 0:
                                    A("act", lambda e, pb=pb, g=g, dst=dst, b=b: e.activation(out=dst[b][:, g * 512:(g + 1) * 512], in_=pQ[pb][:, :], func=AF.Copy),
                                      reads=[b_pQ[pb]], writes=[bdst[b][g]])
                                else:
                                    A("dve", lambda e, pb=pb, g=g, dst=dst, b=b: e.tensor_copy(out=dst[b][:, g * 512:(g + 1) * 512], in_=pQ[pb][:, :]),
                                      reads=[b_pQ[pb]], writes=[bdst[b][g]])
                        for tt in range(NT):
                            pb = ev % 2; ev += 1
                            for c in range(16):
                                A("pe", lambda e, pb=pb, c=c, tt=tt, b=b: e.matmul(pQ[pb][:, 0:128], lhsT=hT[:, c, tt * 128:(tt + 1) * 128], rhs=wv[b][:, c, :], start=(c == 0), stop=(c == 15)),
                                  reads=[b_wv[b], b_hT[tt]], writes=[b_pQ[pb]])
                            if pb == 0:
                                A("act", lambda e, pb=pb, tt=tt, b=b: e.activation(out=Va[b][:, tt, 0:128], in_=pQ[pb][:, 0:128], func=AF.Copy),
                                  reads=[b_pQ[pb]], writes=[b_Va[b][tt]])
                            else:
                                A("dve", lambda e, pb=pb, tt=tt, b=b: e.tensor_copy(out=Va[b][:, tt, 0:128], in_=pQ[pb][:, 0:128]),
                                  reads=[b_pQ[pb]], writes=[b_Va[b][tt]])
                        if h == 0:
                            dump("d_qT", qT[b][:], [128, S], BF16, b_qT[b])
                            dump("d_kT", kT[b][:], [128, S], BF16, b_kT[b])
                            dump("d_Va", Va[b][:], [128, NT, 130], BF16, b_Va[b] + [b_Vone[b]])
                            dump("d_neglam", neglam[:], [128, 1], F32, [b_neglam])
                        def stQK(j, m, h=h, b=b):
                            pt = m
                            nblk = j + 1
                            for gi in range((nblk + 3) // 4):
                                sb_ = (j * 2 + m + gi) % 2
                                i0 = gi * 4
                                i1 = min(nblk, i0 + 4)
                                for i in range(i0, i1):
                                    col = (i - i0) * 128
                                    A("pe", lambda e, sb_=sb_, col=col, m=m, i=i, j=j, b=b: e.matmul(
                                        pS[sb_][:, col:col + 128], lhsT=kT[b][m * 64:(m + 1) * 64, i * 128:(i + 1) * 128],
                                        rhs=qT[b][m * 64:(m + 1) * 64, j * 128:(j + 1) * 128], start=True, stop=False),
                                      reads=[b_kT[b][i // 4], b_qT[b][j // 4]], writes=[b_pS[sb_]])
                                    if i < j:
                                        A("pe", lambda e, sb_=sb_, col=col, i=i, j=j, b=b: e.matmul(
                                            pS[sb_][:, col:col + 128], lhsT=kaug[b][0:4, i * 128:(i + 1) * 128],
                                            rhs=qaug[b][0:4, j * 128:(j + 1) * 128], start=False, stop=True),
                                          reads=[b_kaug[b], b_qaug[b]], writes=[b_pS[sb_]])
                                    else:
                                        A("pe", lambda e, sb_=sb_, col=col, h=h: e.matmul(
                                            pS[sb_][:, col:col + 128], lhsT=identb[:], rhs=bdiag[:, h, :], start=False, stop=True),
                                          reads=[b_identb, b_bdiag], writes=[b_pS[sb_]])
                                ncol = (i1 - i0) * 128
                                A("act", lambda e, sb_=sb_, pt=pt, i0=i0, ncol=ncol: e.activation(
                                    out=PT[pt][:, i0 * 128:i0 * 128 + ncol], in_=pS[sb_][:, 0:ncol], func=AF.Exp, scale=0.125),
                                  reads=[b_pS[sb_]], writes=[b_PT[pt][gi]])

                        def stPV(j, m, h=h, b=b):
                            pt = m
                            ob_ = m
                            for i in range(j + 1):
                                A("pe", lambda e, ob_=ob_, pt=pt, i=i, j=j, b=b: e.matmul(
                                    pO[ob_][:, 0:129], lhsT=PT[pt][:, i * 128:(i + 1) * 128], rhs=Va[b][:, i, 0:129], start=(i == 0), stop=(i == j)),
                                  reads=[b_PT[pt][i // 4], b_Va[b][i], b_Vone[b]], writes=[b_pO[ob_]])
                            A("dve", lambda e, ob_=ob_, m=m: e.reciprocal(out=rz[m][:], in_=pO[ob_][:, 128:129]), reads=[b_pO[ob_]], writes=[b_rz[m]])
                            A("dve", lambda e, ob_=ob_, m=m: e.tensor_scalar(out=om[m][:], in0=pO[ob_][:, 0:128], scalar1=rz[m][:, 0:1], scalar2=None, op0=ALU.mult),
                              reads=[b_pO[ob_], b_rz[m]], writes=[b_om[m]])
                            if m == 1:
                                A("dve", lambda e: e.scalar_tensor_tensor(out=ot[:], in0=om[1][:], scalar=neglam[:, 0:1], in1=om[0][:], op0=ALU.mult, op1=ALU.add),
                                  reads=[b_om[0], b_om[1], b_neglam], writes=[b_ot])
                                rms_stats(ot[:], b_ot, ss2[:], b_ss2, rstd2[:], b_rstd2, junk2[:], b_junk2, 128)
                                ab = j % 2
                                A("dve", lambda e, ab=ab: e.scalar_tensor_tensor(out=oab[ab][:], in0=ot[:], scalar=rstd2[:, 0:1], in1=gsub[:], op0=ALU.mult, op1=ALU.mult),
                                  reads=[b_ot, b_rstd2, b_gsub], writes=[b_oab[ab]])
                                def _tr(ab=ab, h=h, j=j):
                                    A("pe", lambda e, ab=ab: e.transpose(out=pTr[:, :], in_=oab[ab][:], identity=identb[:]), reads=[b_oab[ab], b_identb], writes=[b_pTr])
                                    A("act", lambda e, h=h, j=j: e.activation(out=oaTa[:, h, j * 128:(j + 1) * 128], in_=pTr[:, :], func=AF.Copy),
                                      reads=[b_pTr], writes=[b_oaT[h][j]])
                                pend_tr.append(_tr)

                        units = [(j, m) for j in range(NT) for m in range(2)]
                        pend_tr = []
                        stQK(*units[0])
                        for ui, (j, m) in enumerate(units):
                            if ui + 1 < len(units):
                                stQK(*units[ui + 1])
                            if m == 1 and pend_tr:
                                pend_tr.pop(0)()
                            stPV(j, m)
                        while pend_tr:
                            pend_tr.pop(0)()
                    for h in range(8):
                        A("sp", lambda e, h=h: e.dma_start(out=OAT[:, h, :], in_=oaTa[:, h, :]), reads=b_oaT[h], writes=[b_OATh[h]], dma=True)
                  P.barrier()
                P.cur = sc_o
                obT = P.sb("obT", [128, 8, S], BF16)

                if stop_after >= 3:
                  with ExitStack() as ph:
                    P.cur = ph
                    Wz = [P.sb(f"Wz{i}", [128, 16, 256], BF16) for i in range(2)]; b_Wz = P.bufs(2)
                    vg = P.sb("vg", [128, NT, 1024], BF16); b_vg = [P.bufs(4) for _ in range(NT)]
                    gn = P.sb("gn", [128, 1024], F32); b_gn = Buf()
                    wsf = P.sb("wsf", [128, 8, 128], F32); b_wsf = Buf()
                    tri = P.sb("tri", [128, 128], F32); b_tri = Buf()
                    wsT = P.sb("wsT", [128, 8, 128], BF16); b_wsT = Buf()
                    bsT = P.sb("bsT", [128, 8], F32); b_bsT = Buf()
                    ssv = P.sb("ssv", [128, NT, 4], F32); b_ssv = [P.bufs(4) for _ in range(NT)]
                    sv1 = P.sb("sv1", [128, 1], F32); b_sv1 = Buf()
                    rsv = P.sb("rsv", [128, 1], F32); b_rsv = Buf()
                    junk3 = P.sb("junk3", [128, 256], BF16); b_junk3 = Buf()
                    ug = [P.sb(f"ug{i}", [128, 256], F32) for i in range(2)]; b_ug = P.bufs(2)
                    tmp = [P.sb(f"tmp{i}", [128, 256], F32) for i in range(2)]; b_tmp = P.bufs(2)
                    obb = [P.sb(f"obb{i}", [128, 256], BF16) for i in range(2)]; b_obb = P.bufs(2)
                    pZ = [P.ps(f"pZ{i}", [128, 512]) for i in range(2)]; b_pZ = P.bufs(2)
                    pSV = [P.ps(f"pSV{i}", [128, 512]) for i in range(2)]; b_pSV = P.bufs(2)
                    pW = P.ps("pW", [128, 512]); b_pW = Buf()
                    pTb = P.ps("pTb", [128, 256], BF16); b_pTb = Buf()

                    A("sp", lambda e: e.dma_start(out=gn[:], in_=gmlp_norm.partition_broadcast(128)), writes=[b_gn], dma=True)
                    A("sp", lambda e: e.dma_start(out=wsf[:], in_=gmlp_ws.rearrange("g t s -> t g s")), writes=[b_wsf], dma=True)
                    A("sp", lambda e: e.dma_start(out=tri[:], in_=c_tri), writes=[b_tri], dma=True)
                    A("sp", lambda e: e.dma_start(out=bsT[:], in_=gmlp_bs.rearrange("g t -> t g"), allow_slow_non_contiguous=True), writes=[b_bsT], dma=True)
                    A("dve", lambda e: e.tensor_tensor(out=wsf[:], in0=wsf[:], in1=tri[:].unsqueeze(1).broadcast_to([128, 8, 128]), op=ALU.mult),
                      reads=[b_wsf, b_tri], writes=[b_wsf])
                    for g0 in range(0, 8, 4):
                        for gg in range(4):
                            A("pe", lambda e, g0=g0, gg=gg: e.transpose(out=pW[:, gg * 128:(gg + 1) * 128], in_=wsf[:, g0 + gg, :], identity=identf[:]),
                              reads=[b_wsf, b_identf], writes=[b_pW])
                        A("dve", lambda e, g0=g0: e.tensor_copy(out=wsT[:, g0:g0 + 4, :], in_=pW[:].rearrange("p (g t) -> p g t", g=4)), reads=[b_pW], writes=[b_wsT])

                    zc = 0
                    for vb in range(4):
                        wb_ = zc % 2; zc += 1
                        c0 = 3072 + 1024 + vb * 256
                        A("pool", lambda e, wb_=wb_, c0=c0: e.dma_start(out=Wz[wb_][:], in_=w_in_v[:, :, c0:c0 + 256]), writes=[b_Wz[wb_]], dma=True)
                        for tt in range(NT):
                            zb = tt % 2
                            for c in range(16):
                                A("pe", lambda e, zb=zb, c=c, tt=tt, wb_=wb_: e.matmul(pZ[zb][:, 0:256], lhsT=hT[:, c, tt * 128:(tt + 1) * 128], rhs=Wz[wb_][:, c, :], start=(c == 0), stop=(c == 15)),
                                  reads=[b_Wz[wb_], b_hT[tt]], writes=[b_pZ[zb]])
                            A("act", lambda e, zb=zb, tt=tt, vb=vb: e.activation(out=vg[:, tt, vb * 256:(vb + 1) * 256], in_=pZ[zb][:, 0:256], func=AF.Gelu),
                              reads=[b_pZ[zb]], writes=[b_vg[tt][vb]])
                            A("act", lambda e, tt=tt, vb=vb: e.activation(out=junk3[:], in_=vg[:, tt, vb * 256:(vb + 1) * 256], func=AF.Square, scale=1.0 / 32.0, accum_out=ssv[:, tt, vb:vb + 1]),
                              reads=[b_vg[tt][vb]], writes=[b_junk3, b_ssv[tt][vb]])
                    for tt in range(NT):
                        A("dve", lambda e, tt=tt: e.reduce_sum(out=sv1[:], in_=ssv[:, tt, :], axis=AX.X), reads=b_ssv[tt], writes=[b_sv1])
                        A("act", lambda e: e.activation(out=sv1[:], in_=sv1[:], func=AF.Sqrt, bias=epsT[:], scale=1.0), reads=[b_sv1, b_eps], writes=[b_sv1])
                        A("dve", lambda e: e.reciprocal(out=rsv[:], in_=sv1[:]), reads=[b_sv1], writes=[b_rsv])
                        A("dve", lambda e, tt=tt: e.scalar_tensor_tensor(out=vg[:, tt, :], in0=vg[:, tt, :], scalar=rsv[:, 0:1], in1=gn[:], op0=ALU.mult, op1=ALU.mult),
                          reads=b_vg[tt] + [b_rsv, b_gn], writes=b_vg[tt])
                    pend3 = []
                    for ub in range(4):
                        wb_ = zc % 2; zc += 1
                        c0 = 3072 + ub * 256
                        A("pool", lambda e, wb_=wb_, c0=c0: e.dma_start(out=Wz[wb_][:], in_=w_in_v[:, :, c0:c0 + 256]), writes=[b_Wz[wb_]], dma=True)
                        for tt in range(NT):
                            zb = tt % 2
                            for c in range(16):
                                A("pe", lambda e, zb=zb, c=c, tt=tt, wb_=wb_: e.matmul(pZ[zb][:, 0:256], lhsT=hT[:, c, tt * 128:(tt + 1) * 128], rhs=Wz[wb_][:, c, :], start=(c == 0), stop=(c == 15)),
                                  reads=[b_Wz[wb_], b_hT[tt]], writes=[b_pZ[zb]])
                            for gg in range(2):
                                g = ub * 2 + gg
                                A("pe", lambda e, zb=zb, gg=gg, g=g, tt=tt: e.matmul(pSV[zb][:, gg * 128:(gg + 1) * 128], lhsT=wsT[:, g, :], rhs=vg[:, tt, g * 128:(g + 1) * 128], start=True, stop=True),
                                  reads=[b_wsT, b_vg[tt][g // 2]], writes=[b_pSV[zb]])
                            while pend3:
                                pend3.pop(0)()
                            A("act", lambda e, zb=zb: e.activation(out=ug[zb][:], in_=pZ[zb][:, 0:256], func=AF.Gelu), reads=[b_pZ[zb]], writes=[b_ug[zb]])
                            A("dve", lambda e, zb=zb, ub=ub: e.tensor_tensor(out=tmp[zb][:].rearrange("p (g c) -> p g c", g=2), in0=pSV[zb][:, 0:256].rearrange("p (g c) -> p g c", g=2),
                                                                             in1=bsT[:, ub * 2:(ub + 1) * 2].unsqueeze(2).broadcast_to([128, 2, 128]), op=ALU.add),
                              reads=[b_pSV[zb], b_bsT], writes=[b_tmp[zb]])
                            A("dve", lambda e, zb=zb: e.tensor_tensor(out=obb[zb][:], in0=tmp[zb][:], in1=ug[zb][:], op=ALU.mult), reads=[b_tmp[zb], b_ug[zb]], writes=[b_obb[zb]])
                            def _tr3(zb=zb, ub=ub, tt=tt):
                                for gg in range(2):
                                    A("pe", lambda e, zb=zb, gg=gg: e.transpose(out=pTb[:, gg * 128:(gg + 1) * 128], in_=obb[zb][:, gg * 128:(gg + 1) * 128], identity=identb[:]),
                                      reads=[b_obb[zb], b_identb], writes=[b_pTb])
                                A("act", lambda e, ub=ub, tt=tt: e.activation(out=obT[:, ub * 2:(ub + 1) * 2, tt * 128:(tt + 1) * 128], in_=pTb[:].rearrange("p (g t) -> p g t", g=2), func=AF.Copy),
                                  reads=[b_pTb], writes=[b_obT[ub][tt]])
                            pend3.append(_tr3)
                    while pend3:
                        pend3.pop(0)()
                    if "OBT" in dbg:
                        for g in range(8):
                            A("sp", lambda e, g=g: e.dma_start(out=OBT[:, g, :], in_=obT[:, g, :]), reads=b_obT[g // 2], dma=True)
                  P.barrier()
                P.cur = sc_o
                oaT = P.sb("oaT2", [128, 8, S], BF16); b_oaT2 = [Buf() for _ in range(8)]

                if stop_after >= 4:
                  with ExitStack() as ph:
                    P.cur = ph
                    Wa = [P.sb(f"Wa{i}", [128, 8, 128], BF16) for i in range(2)]; b_Wa = P.bufs(2)
                    Wb = [P.sb(f"Wb{i}", [128, 8, 128], BF16) for i in range(2)]; b_Wb = P.bufs(2)
                    Wga = [P.sb(f"Wga{i}", [128, 16, 128], BF16) for i in range(2)]; b_Wga = P.bufs(2)
                    Wgb = [P.sb(f"Wgb{i}", [128, 16, 128], BF16) for i in range(2)]; b_Wgb = P.bufs(2)
                    sga = [P.sb(f"sga{i}", [128, 512], F32) for i in range(2)]; b_sga = P.bufs(2)
                    sgb = [P.sb(f"sgb{i}", [128, 512], F32) for i in range(2)]; b_sgb = P.bufs(2)
                    mst = [P.sb(f"mst{i}", [128, S], BF16) for i in range(2)]; b_mst = [P.bufs(4) for _ in range(2)]
                    pA = [P.ps(f"pA{i}", [128, 512]) for i in range(2)]; b_pA = P.bufs(2)
                    pB = [P.ps(f"pB{i}", [128, 512]) for i in range(2)]; b_pB = P.bufs(2)
                    pGA = [P.ps(f"pGA{i}", [128, 512]) for i in range(2)]; b_pGA = P.bufs(2)
                    pGB = [P.ps(f"pGB{i}", [128, 512]) for i in range(2)]; b_pGB = P.bufs(2)
                    wa_v = w_a.rearrange("(c p) n -> p c n", p=128)
                    wb_v = w_b.rearrange("(c p) n -> p c n", p=128)
                    for h in range(8):
                        A("sp", lambda e, h=h: e.dma_start(out=oaT[:, h, :], in_=OAT[:, h, :]), reads=[b_OATh[h]], writes=[b_oaT2[h]], dma=True)
                    it = 0
                    for n in range(16):
                        wb_ = n % 2
                        A("pool", lambda e, wb_=wb_, n=n: e.dma_start(out=Wa[wb_][:], in_=wa_v[:, :, n * 128:(n + 1) * 128]), writes=[b_Wa[wb_]], dma=True)
                        A("pool", lambda e, wb_=wb_, n=n: e.dma_start(out=Wb[wb_][:], in_=wb_v[:, :, n * 128:(n + 1) * 128]), writes=[b_Wb[wb_]], dma=True)
                        A("pool", lambda e, wb_=wb_, n=n: e.dma_start(out=Wga[wb_][:], in_=w_in_v[:, :, 5120 + n * 128:5120 + (n + 1) * 128]), writes=[b_Wga[wb_]], dma=True)
                        A("pool", lambda e, wb_=wb_, n=n: e.dma_start(out=Wgb[wb_][:], in_=w_in_v[:, :, 7168 + n * 128:7168 + (n + 1) * 128]), writes=[b_Wgb[wb_]], dma=True)
                        for g in range(4):
                            pb = it % 2; it += 1
                            gs = slice(g * 512, (g + 1) * 512)
                            for c in range(8):
                                A("pe", lambda e, pb=pb, c=c, gs=gs, wb_=wb_: e.matmul(pA[pb][:, :], lhsT=Wa[wb_][:, c, :], rhs=oaT[:, c, gs], start=(c == 0), stop=(c == 7)),
                                  reads=[b_Wa[wb_], b_oaT2[c]], writes=[b_pA[pb]])
                            for c in range(8):
                                A("pe", lambda e, pb=pb, c=c, gs=gs, wb_=wb_: e.matmul(pB[pb][:, :], lhsT=Wb[wb_][:, c, :], rhs=obT[:, c, gs], start=(c == 0), stop=(c == 7)),
                                  reads=[b_Wb[wb_]], writes=[b_pB[pb]])
                            for c in range(16):
                                A("pe", lambda e, pb=pb, c=c, gs=gs, wb_=wb_: e.matmul(pGA[pb][:, :], lhsT=Wga[wb_][:, c, :], rhs=hT[:, c, gs], start=(c == 0), stop=(c == 15)),
                                  reads=[b_Wga[wb_]], writes=[b_pGA[pb]])
                            for c in range(16):
                                A("pe", lambda e, pb=pb, c=c, gs=gs, wb_=wb_: e.matmul(pGB[pb][:, :], lhsT=Wgb[wb_][:, c, :], rhs=hT[:, c, gs], start=(c == 0), stop=(c == 15)),
                                  reads=[b_Wgb[wb_]], writes=[b_pGB[pb]])
                            A("act", lambda e, pb=pb: e.activation(out=sga[pb][:], in_=pGA[pb][:, :], func=AF.Sigmoid), reads=[b_pGA[pb]], writes=[b_sga[pb]])
                            A("act", lambda e, pb=pb: e.activation(out=sgb[pb][:], in_=pGB[pb][:, :], func=AF.Sigmoid), reads=[b_pGB[pb]], writes=[b_sgb[pb]])
                            A("dve", lambda e, pb=pb: e.tensor_tensor(out=sga[pb][:], in0=pA[pb][:, :], in1=sga[pb][:], op=ALU.mult), reads=[b_pA[pb], b_sga[pb]], writes=[b_sga[pb]])
                            A("dve", lambda e, pb=pb: e.tensor_tensor(out=sgb[pb][:], in0=pB[pb][:, :], in1=sgb[pb][:], op=ALU.mult), reads=[b_pB[pb], b_sgb[pb]], writes=[b_sgb[pb]])
                            A("pool", lambda e, pb=pb, wb_=wb_, gs=gs: e.tensor_tensor(out=mst[wb_][:, gs], in0=sga[pb][:], in1=sgb[pb][:], op=ALU.add),
                              reads=[b_sga[pb], b_sgb[pb]], writes=[b_mst[wb_][g]])
                        A("sp", lambda e, wb_=wb_, n=n: e.dma_start(out=MT[:, n, :], in_=mst[wb_][:]), reads=b_mst[wb_], writes=[b_MT[n]], dma=True)
                  P.barrier()
        P.cur = es
        P.barrier()

        if stop_after >= 5:
          with ExitStack() as ph:
            P.cur = ph
            mT = P.sb("mT", [128, 16, S], BF16); b_mT = P.bufs(16)
            Wo = [P.sb(f"Wo{i}", [128, 16, 512], BF16) for i in range(2)]; b_Wo = P.bufs(2)
            xp = [P.sb(f"xp{i}", [128, 512], F32) for i in range(2)]; b_xp = P.bufs(2)
            x2p = [P.sb(f"x2p{i}", [128, 512], F32) for i in range(2)]; b_x2p = P.bufs(2)
            pX = [P.ps(f"pX{i}", [128, 512]) for i in range(2)]; b_pX = P.bufs(2)
            wo_v = w_out.rearrange("(c p) n -> p c n", p=128)
            for c in range(16):
                A("sp", lambda e, c=c: e.dma_start(out=mT[:, c, :], in_=MT[:, c, :]), reads=[b_MT[c]], writes=[b_mT[c]], dma=True)
            it = 0
            for blk in range(4):
                wb_ = blk % 2
                A("pool", lambda e, wb_=wb_, blk=blk: e.dma_start(out=Wo[wb_][:], in_=wo_v[:, :, blk * 512:(blk + 1) * 512]), writes=[b_Wo[wb_]], dma=True)
                for tt in range(NT):
                    pb = it % 2; it += 1
                    A("sp", lambda e, pb=pb, tt=tt, blk=blk: e.dma_start(out=xp[pb][:], in_=x[tt * 128:(tt + 1) * 128, blk * 512:(blk + 1) * 512]), writes=[b_xp[pb]], dma=True)
                    for c in range(16):
                        A("pe", lambda e, pb=pb, c=c, tt=tt, wb_=wb_: e.matmul(pX[pb][:, :], lhsT=mT[:, c, tt * 128:(tt + 1) * 128], rhs=Wo[wb_][:, c, :], start=(c == 0), stop=(c == 15)),
                          reads=[b_Wo[wb_], b_mT[c]], writes=[b_pX[pb]])
                    A("dve", lambda e, pb=pb: e.tensor_tensor(out=x2p[pb][:], in0=pX[pb][:, :], in1=xp[pb][:], op=ALU.add), reads=[b_pX[pb], b_xp[pb]], writes=[b_x2p[pb]])
                    A("sp", lambda e, pb=pb, tt=tt, blk=blk: e.dma_start(out=X2[tt * 128:(tt + 1) * 128, blk * 512:(blk + 1) * 512], in_=x2p[pb][:]),
                      reads=[b_x2p[pb]], writes=[b_X2[tt][blk]], dma=True)
          P.barrier()
          with ExitStack() as ph:
            P.cur = ph
            gF = P.sb("gF", [128, D], F32); b_gF = Buf()
            xt_5 = [P.sb(f"x2t{i}", [128, D], F32) for i in range(2)]; b_xt = P.bufs(2)
            junk_5 = P.sb("junk5", [128, D], BF16); b_junk = Buf()
            ss_5 = [P.sb(f"ss5{i}", [128, 1], F32) for i in range(2)]; b_ss = P.bufs(2)
            rstd_5 = [P.sb(f"rstd5{i}", [128, 1], F32) for i in range(2)]; b_rstd = P.bufs(2)
            hb_5 = [P.sb(f"hb5{i}", [128, D], BF16) for i in range(2)]; b_hb = P.bufs(2)
            hs = [P.sb(f"hs5{i}", [128, 16, 128], BF16) for i in range(2)]; b_hs = P.bufs(2)
            pT_5 = [P.ps(f"pT5{i}", [128, D], BF16) for i in range(2)]; b_pT = P.bufs(2)
            A("sp", lambda e: e.dma_start(out=gF[:], in_=ffn_norm.partition_broadcast(128)), writes=[b_gF], dma=True)
            for tt in range(NT):
                b = tt % 2
                A("sp", lambda e, tt=tt, b=b: e.dma_start(out=xt_5[b][:], in_=X2[tt * 128:(tt + 1) * 128, :]), reads=b_X2[tt], writes=[b_xt[b]], dma=True)
                rms_stats(xt_5[b][:], b_xt[b], ss_5[b][:], b_ss[b], rstd_5[b][:], b_rstd[b], junk_5[:], b_junk, D)
                A("dve", lambda e, b=b: e.scalar_tensor_tensor(out=hb_5[b][:], in0=xt_5[b][:], scalar=rstd_5[b][:, 0:1], in1=gF[:], op0=ALU.mult, op1=ALU.mult),
                  reads=[b_xt[b], b_rstd[b], b_gF], writes=[b_hb[b]])
                for c in range(16):
                    A("pe", lambda e, b=b, c=c: e.transpose(out=pT_5[b][:, c * 128:(c + 1) * 128], in_=hb_5[b][:, c * 128:(c + 1) * 128], identity=identb[:]),
                      reads=[b_hb[b], b_identb], writes=[b_pT[b]])
                A("act", lambda e, b=b: e.activation(out=hs[b][:], in_=pT_5[b][:].rearrange("p (c t) -> p c t", c=16), func=AF.Copy), reads=[b_pT[b]], writes=[b_hs[b]])
                A("sp", lambda e, b=b, tt=tt: e.dma_start(out=H2T[:, :, tt * 128:(tt + 1) * 128], in_=hs[b][:]), reads=[b_hs[b]], writes=[b_H2T[tt]], dma=True)
          P.barrier()

        if stop_after >= 6:
          with ExitStack() as sc_q:
            P.cur = sc_q
            qpT = P.sb("qpT", [128, 16, S], BF16); b_qpT = [P.bufs(4) for _ in range(16)]
            with ExitStack() as ph:
                P.cur = ph
                h2T = P.sb("h2T", [128, 16, S], BF16); b_h2T = P.bufs(16)
                Wp = [P.sb(f"Wp{i}", [128, 16, 128], BF16) for i in range(2)]; b_Wp = P.bufs(2)
                pQ_6 = [P.ps(f"pQp{i}", [128, 512]) for i in range(2)]; b_pQ = P.bufs(2)
                wp_v = peer_wq.rearrange("(c p) n -> p c n", p=128)
                for c in range(16):
                    A("sp", lambda e, c=c: e.dma_start(out=h2T[:, c, :], in_=H2T[:, c, :]), reads=b_H2T, writes=[b_h2T[c]], dma=True)
                it = 0
                for n in range(16):
                    wb_ = n % 2
                    A("pool", lambda e, wb_=wb_, n=n: e.dma_start(out=Wp[wb_][:], in_=wp_v[:, :, n * 128:(n + 1) * 128]), writes=[b_Wp[wb_]], dma=True)
                    for g in range(4):
                        pb = it % 2; it += 1
                        for c in range(16):
                            A("pe", lambda e, pb=pb, c=c, g=g, wb_=wb_: e.matmul(pQ_6[pb][:, :], lhsT=Wp[wb_][:, c, :], rhs=h2T[:, c, g * 512:(g + 1) * 512], start=(c == 0), stop=(c == 15)),
                              reads=[b_Wp[wb_], b_h2T[c]], writes=[b_pQ[pb]])
                        if pb == 0:
                            A("act", lambda e, pb=pb, n=n, g=g: e.activation(out=qpT[:, n, g * 512:(g + 1) * 512], in_=pQ_6[pb][:, :], func=AF.Copy), reads=[b_pQ[pb]], writes=[b_qpT[n][g]])
                        else:
                            A("dve", lambda e, pb=pb, n=n, g=g: e.tensor_copy(out=qpT[:, n, g * 512:(g + 1) * 512], in_=pQ_6[pb][:, :]), reads=[b_pQ[pb]], writes=[b_qpT[n][g]])
            P.barrier()
            with ExitStack() as ph:
                P.cur = ph
                skf = P.sb("skf", [128, 2, 128], F32); b_skf = Buf()
                skT = P.sb("skT", [128, 2, 128], BF16); b_skT = Buf()
                sc = P.sb("sc", [128, 16, 128], F32); b_sc = P.bufs(16)
                v1 = P.sb("v1", [128, 8, 16], F32); b_v1 = P.bufs(8)
                v2 = P.sb("v2", [128, 8, 16], F32); b_v2 = P.bufs(8)
                wk1 = P.sb("wk1", [128, 128], F32); b_wk1 = Buf()
                wk2 = P.sb("wk2", [128, 128], F32); b_wk2 = Buf()
                cand = P.sb("cand", [128, 8, 256], F32); b_cand = P.bufs(8)
                cw = P.sb("cw", [128, 256], F32); b_cw = Buf()
                cw2 = P.sb("cw2", [128, 256], F32); b_cw2 = Buf()
                t17 = P.sb("t17", [128, 8, 8], F32); b_t17 = P.bufs(8)
                thrm = P.sb("thrm", [128, 8], F32); b_thrm = Buf()
                top = P.sb("top", [128, 8, 16], F32); b_top = P.bufs(8)
                negmx = P.sb("negmx", [128, 8], F32); b_negmx = Buf()
                Zt = P.sb("Zt", [128, 8], F32); b_Zt = P.bufs(8)
                rZ = P.sb("rZ", [128, 8], F32); b_rZ = Buf()
                junk6 = P.sb("junk6", [128, 16], F32); b_junk6 = Buf()
                d1 = P.sb("d1", [128, 8, 16], F32); b_d1 = Buf()
                Q4 = P.sb("Q4", [128, 4, 128], F32); b_Q4 = P.bufs(4)
                sT = [P.sb(f"sT{i}", [128, 4, 128], F32) for i in range(2)]; b_sT = P.bufs(2)
                qrep = [P.sb(f"qrep{i}", [128, 16, 2, 128], BF16) for i in range(2)]; b_qrep = [P.bufs(2) for _ in range(2)]
                Eb = [P.sb(f"Eb{i}", [128, 2, 128], F32) for i in range(2)]; b_Eb = [P.bufs(2) for _ in range(2)]
                lhsG = [P.sb(f"lhsG{i}", [128, 2, 128], BF16) for i in range(3)]; b_lhsG = P.bufs(3)
                rhsG = [P.sb(f"rhsG{i}", [128, 2, 128], BF16) for i in range(3)]; b_rhsG = [P.bufs(2) for _ in range(3)]
                SG = [P.sb(f"SG{i}", [128, 128, 128], BF16) for i in range(2)]; b_SG = [P.bufs(32) for _ in range(2)]
                pSc = [P.ps("pSc0", [128, 512])] * 2; b_pSc = [Buf()] * 2
                pT4 = P.ps("pT4", [128, 512]); b_pT4 = Buf()
                pD = [P.ps(f"pD{i}", [128, 512]) for i in range(2)]; b_pD = P.bufs(2)
                pAa = [P.ps(f"pAa{i}", [128, 512]) for i in range(2)]; b_pAa = P.bufs(2)
                pG = [P.ps(f"pG{i}", [128, 512]) for i in range(2)]; b_pG = P.bufs(2)

                A("sp", lambda e: e.dma_start(out=skf[:], in_=peer_sk.rearrange("p n d -> n p d")), writes=[b_skf], dma=True)
                for p_ in range(2):
                    A("pe", lambda e, p_=p_: e.transpose(out=pT4[:, p_ * 128:(p_ + 1) * 128], in_=skf[:, p_, :], identity=identf[:]), reads=[b_skf, b_identf], writes=[b_pT4])
                A("dve", lambda e: e.tensor_copy(out=skT[:], in_=pT4[:, 0:256].rearrange("d (p n) -> d p n", p=2)), reads=[b_pT4], writes=[b_skT])

                for tt in range(NT):
                    ts_ = slice(tt * 128, (tt + 1) * 128)
                    for r in range(4):
                        pb = r % 2
                        for q_ in range(4):
                            hp = r * 4 + q_
                            A("pe", lambda e, pb=pb, q_=q_, hp=hp, ts_=ts_: e.matmul(pSc[pb][:, q_ * 128:(q_ + 1) * 128], lhsT=qpT[:, hp, ts_], rhs=skT[:, hp % 2, :], start=True, stop=True),
                              reads=[b_qpT[hp][tt // 4], b_skT], writes=[b_pSc[pb]])
                        A("act", lambda e, pb=pb, r=r: e.activation(out=sc[:, r * 4:(r + 1) * 4, :], in_=pSc[pb][:].rearrange("p (a n) -> p a n", a=4), func=AF.Copy),
                          reads=[b_pSc[pb]], writes=b_sc[r * 4:(r + 1) * 4])
                    for h in range(8):
                        for (src, vv, bvv, wkk, bwk) in ((2 * h, v1, b_v1, wk1, b_wk1), (2 * h + 1, v2, b_v2, wk2, b_wk2)):
                            A("dve", lambda e, src=src, vv=vv, h=h: e.max(out=vv[:, h, 0:8], in_=sc[:, src, :]), reads=[b_sc[src]], writes=[bvv[h]])
                            A("dve", lambda e, src=src, vv=vv, h=h, wkk=wkk: e.match_replace(out=wkk[:], in_to_replace=vv[:, h, 0:8], in_values=sc[:, src, :], imm_value=NEG),
                              reads=[b_sc[src], bvv[h]], writes=[bwk])
                            A("dve", lambda e, vv=vv, h=h, wkk=wkk: e.max(out=vv[:, h, 8:16], in_=wkk[:]), reads=[bwk, bvv[h]], writes=[bvv[h]])
                        A("dve", lambda e, h=h: e.tensor_tensor(out=cand[:, h, :].rearrange("p (a b) -> p a b", a=16), in0=v1[:, h, :].unsqueeze(2).broadcast_to([128, 16, 16]),
                                                                in1=v2[:, h, :].unsqueeze(1).broadcast_to([128, 16, 16]), op=ALU.add),
                          reads=[b_v1[h], b_v2[h]], writes=[b_cand[h]])
                        A("dve", lambda e, h=h: e.max(out=top[:, h, 0:8], in_=cand[:, h, :]), reads=[b_cand[h]], writes=[b_top[h]])
                        A("dve", lambda e, h=h: e.match_replace(out=cw[:], in_to_replace=top[:, h, 0:8], in_values=cand[:, h, :], imm_value=NEG),
                          reads=[b_cand[h], b_top[h]], writes=[b_cw])
                        A("dve", lambda e, h=h: e.max(out=top[:, h, 8:16], in_=cw[:]), reads=[b_cw, b_top[h]], writes=[b_top[h]])
                        A("dve", lambda e, h=h: e.match_replace(out=cw2[:], in_to_replace=top[:, h, 8:16], in_values=cw[:], imm_value=NEG),
                          reads=[b_cw, b_top[h]], writes=[b_cw2])
                        A("dve", lambda e, h=h: e.max(out=t17[:, h, :], in_=cw2[:]), reads=[b_cw2], writes=[b_t17[h]])
                    A("dve", lambda e: e.tensor_scalar(out=negmx[:], in0=top[:, :, 0], scalar1=-1.0, scalar2=None, op0=ALU.mult), reads=b_top, writes=[b_negmx])
                    for h in range(8):
                        A("act", lambda e, h=h: e.activation(out=junk6[:], in_=top[:, h, :], func=AF.Exp, bias=negmx[:, h:h + 1], scale=1.0, accum_out=Zt[:, h:h + 1]),
                          reads=[b_top[h], b_negmx], writes=[b_junk6, b_Zt[h]])
                    A("act", lambda e: e.activation(out=rZ[:], in_=Zt[:], func=AF.Ln), reads=b_Zt, writes=[b_rZ])
                    A("dve", lambda e: e.tensor_tensor(out=rZ[:], in0=rZ[:], in1=v2[:, :, 0], op=ALU.add), reads=[b_rZ] + b_v2, writes=[b_rZ])
                    A("dve", lambda e: e.tensor_tensor(out=d1[:], in0=v1[:], in1=v1[:, :, 0:1].broadcast_to([128, 8, 16]), op=ALU.subtract), reads=b_v1, writes=[b_d1])
                    A("dve", lambda e: e.tensor_copy(out=Q4[:, 0, :].rearrange("p (h a) -> p h a", h=8), in_=v1[:]), reads=b_v1, writes=[b_Q4[0]])
                    A("dve", lambda e: e.tensor_tensor(out=Q4[:, 3, :].rearrange("p (h a) -> p h a", h=8), in0=d1[:], in1=rZ[:].unsqueeze(2).broadcast_to([128, 8, 16]), op=ALU.subtract),
                      reads=[b_d1, b_rZ], writes=[b_Q4[3]])
                    A("dve", lambda e: e.tensor_tensor(out=thrm[:], in0=top[:, :, 15], in1=t17[:, :, 0], op=ALU.add), reads=b_top + b_t17, writes=[b_thrm])
                    A("dve", lambda e: e.tensor_scalar(out=thrm[:], in0=thrm[:], scalar1=0.5, scalar2=None, op0=ALU.mult), reads=[b_thrm], writes=[b_thrm])
                    A("dve", lambda e: e.tensor_tensor(out=Q4[:, 2, :].rearrange("p (h a) -> p h a", h=8), in0=thrm[:].unsqueeze(2).broadcast_to([128, 8, 16]), in1=v1[:], op=ALU.subtract),
                      reads=[b_thrm] + b_v1, writes=[b_Q4[2]])
                    A("dve", lambda e: e.tensor_tensor(out=Q4[:, 2, :].rearrange("p (h a) -> p h a", h=8), in0=Q4[:, 2, :].rearrange("p (h a) -> p h a", h=8),
                                                       in1=v2[:, :, 15:16].broadcast_to([128, 8, 16]), op=ALU.max),
                      reads=[b_Q4[2]] + b_v2, writes=[b_Q4[2]])
                    tb = tt % 2
                    for k4 in (0, 2, 3):
                        A("pe", lambda e, k4=k4: e.transpose(out=pT4[:, k4 * 128:(k4 + 1) * 128], in_=Q4[:, k4, :], identity=identf[:]), reads=[b_Q4[k4], b_identf], writes=[b_pT4])
                    A("act", lambda e, tb=tb: e.activation(out=sT[tb][:], in_=pT4[:].rearrange("p (k t) -> p k t", k=4), func=AF.Copy), reads=[b_pT4], writes=[b_sT[tb]])
                    sgb_ = tt % 2

                    def stageA(g, tt=tt, tb=tb):
                        sub, gl = divmod(g, 8)
                        qb = (tt * 8 + sub) % 2
                        if gl == 0:
                            t0 = tt * 128 + sub * 16
                            for p_ in range(2):
                                def rep_cp(e, qb=qb, p_=p_, t0=t0):
                                    src = qpT[:, p_:16:2, t0:t0 + 16].rearrange("d h t -> d t h").unsqueeze(3).broadcast_to([128, 16, 8, 16])
                                    dst = qrep[qb][:, :, p_, :].rearrange("d t (h a) -> d t h a", h=8)
                                    return e.tensor_copy(out=dst, in_=src)
                                A("pool", rep_cp, reads=[b_qpT[hp][tt // 4] for hp in range(p_, 16, 2)], writes=[b_qrep[qb][p_]])
                        gi = tt * 64 + g
                        sl = gi % 2
                        s3 = gi % 3
                        for q in range(2):
                            tl = gl * 2 + q
                            A("pe", lambda e, sl=sl, qb=qb, tl=tl, q=q: e.matmul(pD[sl][:, q * 128:(q + 1) * 128], lhsT=qrep[qb][:, tl, 0, :], rhs=skT[:, 0, :], start=True, stop=True),
                              reads=[b_qrep[qb][0], b_skT], writes=[b_pD[sl]])
                            A("pe", lambda e, sl=sl, qb=qb, tl=tl, q=q: e.matmul(pD[sl][:, 256 + q * 128:256 + (q + 1) * 128], lhsT=qrep[qb][:, tl, 1, :], rhs=skT[:, 1, :], start=True, stop=True),
                              reads=[b_qrep[qb][1], b_skT], writes=[b_pD[sl]])
                            A("pe", lambda e, sl=sl, qb=qb, tl=tl, q=q: e.matmul(pAa[sl][:, q * 128:(q + 1) * 128], lhsT=qrep[qb][:, tl, 1, :], rhs=skT[:, 1, :], start=True, stop=True),
                              reads=[b_qrep[qb][1], b_skT], writes=[b_pAa[sl]])
                        for q in range(2):
                            t = g * 2 + q
                            A("act", lambda e, sl=sl, tb=tb, t=t, q=q: e.activation(out=Eb[sl][:, q, :], in_=pAa[sl][:, q * 128:(q + 1) * 128], func=AF.Exp, bias=sT[tb][:, 3, t:t + 1], scale=1.0),
                              reads=[b_pAa[sl], b_sT[tb]], writes=[b_Eb[sl][q]])
                        A("dve", lambda e, sl=sl, s3=s3, tb=tb, g=g: e.tensor_tensor(out=lhsG[s3][:], in0=pD[sl][:, 0:256].rearrange("p (q i) -> p q i", q=2),
                                                                                    in1=sT[tb][:, 0, g * 2:g * 2 + 2].unsqueeze(2).broadcast_to([128, 2, 128]), op=ALU.is_equal),
                          reads=[b_pD[sl], b_sT[tb]], writes=[b_lhsG[s3]])
                        for q in range(2):
                            t = g * 2 + q
                            A("dve", lambda e, sl=sl, s3=s3, tb=tb, t=t, q=q: e.scalar_tensor_tensor(out=rhsG[s3][:, q, :], in0=pD[sl][:, 256 + q * 128:256 + (q + 1) * 128], scalar=sT[tb][:, 2, t:t + 1], in1=Eb[sl][:, q, :], op0=ALU.is_ge, op1=ALU.mult),
                              reads=[b_pD[sl], b_sT[tb], b_Eb[sl][q]], writes=[b_rhsG[s3][q]])

                    def stageB(g, tt=tt, sgb_=sgb_):
                        gi = tt * 64 + g
                        s3 = gi % 3
                        gq = (gi // 2) % 2
                        for q in range(2):
                            slot = (g * 2 + q) % 4
                            A("pe", lambda e, s3=s3, gq=gq, q=q, slot=slot: e.matmul(pG[gq][:].rearrange("i (j t) -> i t j", t=4)[:, slot, :], lhsT=lhsG[s3][:, q, :], rhs=rhsG[s3][:, q, :], start=True, stop=True),
                              reads=[b_lhsG[s3], b_rhsG[s3][q]], writes=[b_pG[gq]])
                        if g % 2 == 1:
                            g4 = g // 2
                            A("act", lambda e, gq=gq, sgb_=sgb_, g4=g4: e.activation(out=SG[sgb_][:, :, g4 * 4:g4 * 4 + 4], in_=pG[gq][:].rearrange("i (j t) -> i j t", t=4), func=AF.Copy),
                              reads=[b_pG[gq]], writes=[b_SG[sgb_][g4]])

                    for n_ in range(64 + 1):
                        if n_ < 64:
                            stageA(n_)
                        if n_ >= 1:
                            stageB(n_ - 1)
                    for jb in range(8):
                        A("sp", lambda e, jb=jb, sgb_=sgb_, ts_=ts_: e.dma_start(out=GT[jb * 16:(jb + 1) * 16, :, ts_].rearrange("j i t -> i j t"), in_=SG[sgb_][:, jb * 16:(jb + 1) * 16, :]),
                          reads=b_SG[sgb_], writes=[b_GT[tt]], dma=True)
            P.barrier()
          P.cur = es
          P.barrier()

        if stop_after >= 7:
          u_v = peer_u.rearrange("(i j) d -> j i d", j=128)
          v_v = peer_v.rearrange("(i j) d -> j i d", j=128)
          def _half(hf):
            with ExitStack() as sc_a:
                P.cur = sc_a
                TB = hf * 1024
                h2h = P.sb(f"h2h{hf}", [128, 16, 1024], BF16); b_h2h = P.bufs(16)
                acc = P.sb(f"acc{hf}", [128, 8, D], F32); b_acc = [P.bufs(4) for _ in range(8)]
                with ExitStack() as ph:
                    P.cur = ph
                    Ust = [P.sb(f"Ust{hf}{i}", [128, D], BF16) for i in range(2)]; b_Ust = P.bufs(2)
                    UT = [P.sb(f"UT{hf}{i}", [128, 16, 128], BF16) for i in range(2)]; b_UT = P.bufs(2)
                    Vc = [P.sb(f"Vc{hf}{i}", [128, 4, D], BF16) for i in range(2)]; b_Vc = [P.bufs(4) for _ in range(2)]
                    GTc = [P.sb(f"GTc{hf}{i}", [128, 1024], BF16) for i in range(2)]; b_GTc = P.bufs(2)
                    aT = [P.sb(f"aT{hf}{i}", [128, 4, 1024], BF16) for i in range(2)]; b_aT = [[P.bufs(2) for _ in range(4)] for _ in range(2)]
                    ga = [P.sb(f"ga{hf}{i}", [128, 512], F32) for i in range(2)]; b_ga = P.bufs(2)
                    pU = [P.ps(f"pU{hf}{i}", [128, D], BF16) for i in range(2)]; b_pU = P.bufs(2)
                    pA_7 = [P.ps(f"pAe{hf}{i}", [128, 512]) for i in range(2)]; b_pA = P.bufs(2)
                    pO_7 = [P.ps(f"pOe{hf}{i}", [128, 512]) for i in range(2)]; b_pO = P.bufs(2)
                    for c in range(16):
                        A("sp", lambda e, c=c, TB=TB: e.dma_start(out=h2h[:, c, :], in_=H2T[:, c, TB:TB + 1024]), reads=b_H2T, writes=[b_h2h[c]], dma=True)
                    for tt in range(8):
                        A("sp", lambda e, tt=tt, TB=TB: e.dma_start(out=acc[:, tt, :], in_=X2[TB + tt * 128:TB + (tt + 1) * 128, :]), reads=b_X2[(TB // 128) + tt], writes=b_acc[tt], dma=True)
                    cnt = {"ia": 0, "io": 0}

                    def stT(k):
                        s_, jj = divmod(k, 4)
                        sb_ = s_ % 2
                        cb = k % 2
                        A("pool", lambda e, sb_=sb_, jj=jj, k=k: e.dma_start(out=Vc[sb_][:, jj, :], in_=v_v[k]), writes=[b_Vc[sb_][jj]], dma=True)
                        A("sp", lambda e, cb=cb, k=k: e.dma_start(out=GTc[cb][:], in_=GT[k, :, TB:TB + 1024]), reads=b_GT, writes=[b_GTc[cb]], dma=True)
                        if hf == 1:
                            A("sp", lambda e, cb=cb, k=k: e.dma_start(out=UT[cb][:], in_=UTD[k].rearrange("d (c i) -> d c i", c=16)), reads=[b_UTD[k]], writes=[b_UT[cb]], dma=True)
                            return
                        A("pool", lambda e, cb=cb, k=k: e.dma_start(out=Ust[cb][:], in_=u_v[k]), writes=[b_Ust[cb]], dma=True)
                        for c in range(16):
                            A("pe", lambda e, cb=cb, c=c: e.transpose(out=pU[cb][:, c * 128:(c + 1) * 128], in_=Ust[cb][:, c * 128:(c + 1) * 128], identity=identb[:]),
                              reads=[b_Ust[cb], b_identb], writes=[b_pU[cb]])
                        A("act", lambda e, cb=cb: e.activation(out=UT[cb][:], in_=pU[cb][:].rearrange("p (c i) -> p c i", c=16), func=AF.Copy), reads=[b_pU[cb]], writes=[b_UT[cb]])
                        A("sp", lambda e, cb=cb, k=k: e.dma_start(out=UTD[k].rearrange("d (c i) -> d c i", c=16), in_=UT[cb][:]), reads=[b_UT[cb]], writes=[b_UTD[k]], dma=True)

                    def stM(k):
                        s_, jj = divmod(k, 4)
                        sb_ = s_ % 2
                        cb = k % 2
                        for tg in range(2):
                            ab = cnt["ia"] % 2; cnt["ia"] += 1
                            for c in range(16):
                                A("pe", lambda e, ab=ab, cb=cb, c=c, tg=tg: e.matmul(pA_7[ab][:, :], lhsT=UT[cb][:, c, :], rhs=h2h[:, c, tg * 512:(tg + 1) * 512], start=(c == 0), stop=(c == 15)),
                                  reads=[b_UT[cb], b_h2h[c]], writes=[b_pA[ab]])
                            A("act", lambda e, ab=ab: e.activation(out=ga[ab][:], in_=pA_7[ab][:, :], func=AF.Gelu), reads=[b_pA[ab]], writes=[b_ga[ab]])
                            A("dve", lambda e, ab=ab, sb_=sb_, jj=jj, tg=tg, cb=cb: e.tensor_tensor(out=aT[sb_][:, jj, tg * 512:(tg + 1) * 512], in0=ga[ab][:], in1=GTc[cb][:, tg * 512:(tg + 1) * 512], op=ALU.mult),
                              reads=[b_ga[ab], b_GTc[cb]], writes=[b_aT[sb_][jj][tg]])

                    def stV(s_):
                        sb_ = s_ % 2
                        for tt in range(8):
                            for blk in range(4):
                                ob_ = cnt["io"] % 2; cnt["io"] += 1
                                for jj in range(4):
                                    A("pe", lambda e, ob_=ob_, sb_=sb_, jj=jj, tt=tt, blk=blk: e.matmul(pO_7[ob_][:, :], lhsT=aT[sb_][:, jj, tt * 128:(tt + 1) * 128], rhs=Vc[sb_][:, jj, blk * 512:(blk + 1) * 512], start=(jj == 0), stop=(jj == 3)),
                                      reads=[b_aT[sb_][jj][tt // 4], b_Vc[sb_][jj]], writes=[b_pO[ob_]])
                                A("dve", lambda e, ob_=ob_, tt=tt, blk=blk: e.tensor_tensor(out=acc[:, tt, blk * 512:(blk + 1) * 512], in0=pO_7[ob_][:, :], in1=acc[:, tt, blk * 512:(blk + 1) * 512], op=ALU.add),
                                  reads=[b_pO[ob_], b_acc[tt][blk]], writes=[b_acc[tt][blk]])

                    stT(0)
                    for k in range(128):
                        if k + 1 < 128:
                            stT(k + 1)
                        stM(k)
                        if k % 4 == 0 and k > 0:
                            stV(k // 4 - 1)
                    stV(31)
                P.barrier()
                with ExitStack() as ph:
                    P.cur = ph
                    gO = P.sb(f"gO{hf}", [128, D], F32); b_gO = Buf()
                    junk_7 = P.sb(f"junk7{hf}", [128, D], BF16); b_junk = Buf()
                    ss_7 = [P.sb(f"ss7{hf}{i}", [128, 1], F32) for i in range(2)]; b_ss = P.bufs(2)
                    rstd_7 = [P.sb(f"rstd7{hf}{i}", [128, 1], F32) for i in range(2)]; b_rstd = P.bufs(2)
                    A("sp", lambda e: e.dma_start(out=gO[:], in_=final_norm.partition_broadcast(128)), writes=[b_gO], dma=True)
                    for tt in range(8):
                        b = tt % 2
                        rms_stats(acc[:, tt, :], b_acc[tt], ss_7[b][:], b_ss[b], rstd_7[b][:], b_rstd[b], junk_7[:], b_junk, D)
                        A("dve", lambda e, b=b, tt=tt: e.scalar_tensor_tensor(out=acc[:, tt, :], in0=acc[:, tt, :], scalar=rstd_7[b][:, 0:1], in1=gO[:], op0=ALU.mult, op1=ALU.mult),
                          reads=b_acc[tt] + [b_rstd[b], b_gO], writes=b_acc[tt])
                        fin.append(A("sp", lambda e, tt=tt, TB=TB: e.dma_start(out=out[TB + tt * 128:TB + (tt + 1) * 128, :], in_=acc[:, tt, :]), reads=b_acc[tt], dma=True))
                P.barrier()
            P.cur = es
            P.barrier()
          _half(0)
          _half(1)

        P.emit(final_waits=[op for e_ in P.ENGS for op in P.ops[e_] if op.dma])
        build.nops = P.nops
    return nc


def _consts():
    bf = ml_dtypes.bfloat16
    slopes = 2.0 ** (-np.arange(1, 9, dtype=np.float64))
    pos = np.arange(S)
    kaug = np.zeros((8, 4, S), np.float64)
    qaug = np.zeros((8, 4, S), np.float64)
    for h in range(8):
        kaug[h, 0] = 8 * slopes[h] * 128 * (pos // 128)
        kaug[h, 1] = 8 * slopes[h] * (pos % 128)
        kaug[h, 2] = 1
        kaug[h, 3] = 1
        qaug[h, 0] = 1
        qaug[h, 1] = 1
        qaug[h, 2] = -8 * slopes[h] * 128 * (pos // 128)
        qaug[h, 3] = -8 * slopes[h] * (pos % 128)
    kp = np.arange(128)[:, None]
    qp = np.arange(128)[None, :]
    allowed = (kp // 64) <= (qp // 64)
    bdiag = np.zeros((128, 8, 128), np.float64)
    for h in range(8):
        bdiag[:, h, :] = np.where(allowed, -8 * slopes[h] * np.abs(qp - kp), -240000.0)
    tri = (np.arange(128)[None, :] <= np.arange(128)[:, None]).astype(np.float32)
    return {
        "c_identb": np.eye(128, dtype=np.float32).astype(bf),
        "c_identf": np.eye(128, dtype=np.float32),
        "c_kaug": kaug.astype(np.float32).astype(bf),
        "c_qaug": qaug.astype(np.float32).astype(bf),
        "c_bdiag": bdiag.astype(np.float32).astype(bf),
        "c_tri": tri,
    }


def make_in_maps(inputs, cores):
    f = lambda a: np.ascontiguousarray(np.asarray(a, dtype=np.float32))
    shared = {
        "attn_norm": f(inputs["attn_norm"]).reshape(1, D),
        "w_in": f(inputs["w_in"]).reshape(D, IN_COLS),
        "diff_lambda": f(inputs["diff_lambda"]).reshape(1, 256),
        "diff_subln": f(inputs["diff_subln"]).reshape(1, 128),
        "gmlp_norm": f(inputs["gmlp_norm"]).reshape(1, 1024),
        "gmlp_ws": f(inputs["gmlp_ws"]).reshape(8, 128, 128),
        "gmlp_bs": f(inputs["gmlp_bs"]).reshape(8, 128),
        "w_branch_a": f(inputs["w_branch_a"]).reshape(1024, D),
        "w_branch_b": f(inputs["w_branch_b"]).reshape(1024, D),
        "w_out": f(inputs["w_out"]).reshape(D, D),
        "ffn_norm": f(inputs["ffn_norm"]).reshape(1, D),
        "peer_wq": f(inputs["peer_wq"]).reshape(D, 2048),
        "peer_subkeys": f(inputs["peer_subkeys"]).reshape(2, 128, 128),
        "peer_u": f(inputs["peer_u"]).reshape(16384, D),
        "peer_v": f(inputs["peer_v"]).reshape(16384, D),
        "final_norm": f(inputs["final_norm"]).reshape(1, D),
    }
    shared.update(_consts())
    xs = f(inputs["x"])
    return [dict(shared, x=xs[b]) for b in cores]


def kernel(**inputs):
    nc = build()
    in_maps = make_in_maps(inputs, list(range(8)))
    res = run_bass_kernel_spmd(nc, in_maps, core_ids=list(range(8)))
    return np.stack([np.asarray(r["out"], dtype=np.float32) for r in res.results], axis=0)
```

```python
import numpy as np
import ml_dtypes
import concourse.bass as bass
import concourse.mybir as mybir
from concourse.bass_utils import run_bass_kernel_spmd
from contextlib import ExitStack

F32 = mybir.dt.float32
BF16 = mybir.dt.bfloat16
ALU = mybir.AluOpType
AF = mybir.ActivationFunctionType
AX = mybir.AxisListType

S = 2048
D = 2048
NT = S // 128
EPS = 1e-6
IN_COLS = 9216
LAM_INIT = 0.8 - 0.6 * 1.0
NEG = -1.0e30


class Buf:
    __slots__ = ("name", "last_w", "readers")

    def __init__(self, name="b"):
        self.name = name
        self.last_w = None
        self.readers = []


class Op:
    __slots__ = ("eng", "fn", "dma", "deps", "signal", "sem", "semval", "prewait")

    def __init__(self, eng, fn, dma):
        self.eng = eng
        self.fn = fn
        self.dma = dma
        self.deps = []
        self.signal = False
        self.sem = None
        self.semval = None
        self.prewait = None


class Prog:
    ENGS = ("pe", "act", "dve", "pool", "sp")
    NDMA = {"sp": 12, "pool": 12}

    def __init__(self, nc, es):
        self.nc = nc
        self.es = es
        self.cur = es
        self.ops = {e: [] for e in self.ENGS}
        self.nops = 0
        self._bar_idx = {e: 0 for e in self.ENGS}
        self._pending = {e: [] for e in self.ENGS}

    def sb(self, name, shape, dtype):
        self.nops += 0
        self._uid = getattr(self, "_uid", 0) + 1
        return self.cur.enter_context(self.nc.sbuf_tensor(f"{name}_u{self._uid}", list(shape), dtype))

    def ps(self, name, shape, dtype=F32):
        self._uid = getattr(self, "_uid", 0) + 1
        return self.cur.enter_context(self.nc.psum_tensor(f"{name}_u{self._uid}", list(shape), dtype))

    def bufs(self, n, name="b"):
        return [Buf(name) for _ in range(n)]

    def barrier(self):
        deps = []
        for e in self.ENGS:
            ops = self.ops[e]
            for op in reversed(ops):
                if not op.dma:
                    deps.append(op)
                    break
            deps += [op for op in ops[self._bar_idx[e]:] if op.dma]
            self._bar_idx[e] = len(ops)
        for e in self.ENGS:
            self._pending[e] = self._pending[e] + deps

    def add(self, eng, fn, reads=(), writes=(), dma=False):
        op = Op(eng, fn, dma)
        deps = {}
        for b in reads:
            w = b.last_w
            if w is not None:
                deps[id(w)] = (w, 0)
        for b in writes:
            w = b.last_w
            if w is not None and id(w) not in deps:
                deps[id(w)] = (w, 1)
            for r in b.readers:
                if id(r) not in deps:
                    deps[id(r)] = (r, 2)
        for d, kind in deps.values():
            if d is op:
                continue
            if not d.dma and not op.dma and d.eng == eng:
                if eng == "pe" or kind == 2:
                    continue
            op.deps.append(d)
            d.signal = True
        if self._pending[eng]:
            for d in self._pending[eng]:
                op.deps.append(d)
                d.signal = True
            self._pending[eng] = []
        for b in reads:
            b.readers.append(op)
        for b in writes:
            b.last_w = op
            b.readers = []
        self.ops[eng].append(op)
        self.nops += 1
        return op

    def emit(self, final_waits=()):
        nc = self.nc
        es = self.es
        esem = {e: es.enter_context(nc.semaphore(f"s_{e}")) for e in self.ENGS if e != "sp"}
        dsem = {e: [es.enter_context(nc.semaphore(f"d_{e}{i}")) for i in range(n)] for e, n in self.NDMA.items()}
        for e in self.ENGS:
            tick = 0
            k = 0
            cnt = [0] * self.NDMA.get(e, 0)
            for op in self.ops[e]:
                if op.dma:
                    n = self.NDMA[e]
                    j = k % n
                    k += 1
                    op.prewait = (dsem[e][j], 16 * cnt[j]) if cnt[j] > 0 else None
                    cnt[j] += 1
                    op.sem = dsem[e][j]
                    op.semval = 16 * cnt[j]
                elif op.signal:
                    tick += 1
                    op.sem = esem[e]
                    op.semval = tick
        block = es.enter_context(nc.Block())
        engfn = {"pe": block.tensor, "act": block.scalar, "dve": block.vector, "pool": block.gpsimd, "sp": block.sync}

        def make(e):
            ops = self.ops[e]

            def body(eng):
                waited = {}

                def flush(need):
                    for key, (sem, val) in need.items():
                        if waited.get(key, 0) >= val:
                            continue
                        waited[key] = val
                        eng.wait_ge(sem, val)

                for op in ops:
                    need = {}
                    for d in op.deps:
                        key = id(d.sem)
                        if key not in need or need[key][1] < d.semval:
                            need[key] = (d.sem, d.semval)
                    if op.prewait is not None:
                        key = id(op.prewait[0])
                        if key not in need or need[key][1] < op.prewait[1]:
                            need[key] = op.prewait
                    flush(need)
                    ins = op.fn(eng)
                    if op.dma:
                        ins.then_inc(op.sem, 16)
                    elif op.signal:
                        ins.then_inc(op.sem, 1)
                if e == "sp":
                    need = {}
                    for d in final_waits:
                        key = id(d.sem)
                        if key not in need or need[key][1] < d.semval:
                            need[key] = (d.sem, d.semval)
                    flush(need)

            return body

        for e in self.ENGS:
            engfn[e](make(e))


def build(stop_after=99, dbg=()):
    nc = bass.Bass("TRN2", target_bir_lowering=False)

    def din(name, shape, dt=F32):
        return nc.dram_tensor(name, list(shape), dt, kind="ExternalInput").ap()

    def dscr(name, shape, dt):
        kind = "ExternalOutput" if name in dbg else "Internal"
        return nc.dram_tensor(name, list(shape), dt, kind=kind).ap()

    x = din("x", [S, D])
    attn_norm = din("attn_norm", [1, D])
    w_in = din("w_in", [D, IN_COLS])
    diff_lambda = din("diff_lambda", [1, 256])
    diff_subln = din("diff_subln", [1, 128])
    gmlp_norm = din("gmlp_norm", [1, 1024])
    gmlp_ws = din("gmlp_ws", [8, 128, 128])
    gmlp_bs = din("gmlp_bs", [8, 128])
    w_a = din("w_branch_a", [1024, D])
    w_b = din("w_branch_b", [1024, D])
    w_out = din("w_out", [D, D])
    ffn_norm = din("ffn_norm", [1, D])
    peer_wq = din("peer_wq", [D, 2048])
    peer_sk = din("peer_subkeys", [2, 128, 128])
    peer_u = din("peer_u", [16384, D])
    peer_v = din("peer_v", [16384, D])
    final_norm = din("final_norm", [1, D])
    c_identb = din("c_identb", [128, 128], BF16)
    c_identf = din("c_identf", [128, 128], F32)
    c_kaug = din("c_kaug", [8, 4, S], BF16)
    c_qaug = din("c_qaug", [8, 4, S], BF16)
    c_bdiag = din("c_bdiag", [128, 8, 128], BF16)
    c_tri = din("c_tri", [128, 128], F32)
    out = nc.dram_tensor("out", [S, D], F32, kind="ExternalOutput").ap()

    OAT = dscr("OAT", [128, 8, S], BF16)
    OBT = dscr("OBT", [128, 8, S], BF16)
    MT = dscr("MT", [128, 16, S], BF16)
    X2 = dscr("X2", [S, D], F32)
    H2T = dscr("H2T", [128, 16, S], BF16)
    GT = dscr("GT", [128, 128, S], BF16)
    b_MT = [Buf() for _ in range(16)]
    b_X2 = [[Buf() for _ in range(4)] for _ in range(NT)]
    b_H2T = [Buf() for _ in range(NT)]
    b_GT = [Buf() for _ in range(NT)]
    UTD = dscr("UTD", [128, 128, 2048], BF16)
    b_UTD = [Buf() for _ in range(128)]

    w_in_v = w_in.rearrange("(c p) n -> p c n", p=128)
    fin = []

    with ExitStack() as es:
        P = Prog(nc, es)
        A = P.add

        def dump(name, ap, shape, dt, bufs):
            if name not in dbg:
                return
            d_ = nc.dram_tensor(name, list(shape), dt, kind="ExternalOutput").ap()
            A("sp", lambda e: e.dma_start(out=d_, in_=ap), reads=bufs, dma=True)

        identb = P.sb("identb", [128, 128], BF16); b_identb = Buf()
        identf = P.sb("identf", [128, 128], F32); b_identf = Buf()
        epsT = P.sb("epsT", [128, 1], F32); b_eps = Buf()
        A("sp", lambda e: e.dma_start(out=identb[:], in_=c_identb), writes=[b_identb], dma=True)
        A("sp", lambda e: e.dma_start(out=identf[:], in_=c_identf), writes=[b_identf], dma=True)
        A("dve", lambda e: e.memset(epsT[:], EPS), writes=[b_eps])

        def rms_stats(src_ap, b_src, ss, b_ss, rstd, b_rstd, junk, b_junk, width):
            A("act", lambda e: e.activation(out=junk, in_=src_ap, func=AF.Square, scale=float(width) ** -0.5, accum_out=ss),
              reads=(b_src if isinstance(b_src, list) else [b_src]), writes=[b_junk, b_ss])
            A("act", lambda e: e.activation(out=ss, in_=ss, func=AF.Sqrt, bias=epsT[:], scale=1.0),
              reads=[b_ss, b_eps], writes=[b_ss])
            A("dve", lambda e: e.reciprocal(out=rstd, in_=ss), reads=[b_ss], writes=[b_rstd])

        with ExitStack() as sc_h:
            P.cur = sc_h
            hT = P.sb("hT", [128, 16, S], BF16); b_hT = P.bufs(NT)

            with ExitStack() as ph:
                P.cur = ph
                gA = P.sb("gA", [128, D], F32); b_gA = Buf()
                xt = [P.sb(f"xt{i}", [128, D], F32) for i in range(2)]; b_xt = P.bufs(2)
                junk = P.sb("junk1", [128, D], BF16); b_junk = Buf()
                ss = [P.sb(f"ss{i}", [128, 1], F32) for i in range(2)]; b_ss = P.bufs(2)
                rstd = [P.sb(f"rstd{i}", [128, 1], F32) for i in range(2)]; b_rstd = P.bufs(2)
                hb = [P.sb(f"hb{i}", [128, D], BF16) for i in range(2)]; b_hb = P.bufs(2)
                pT = [P.ps(f"pT{i}", [128, D], BF16) for i in range(2)]; b_pT = P.bufs(2)
                A("sp", lambda e: e.dma_start(out=gA[:], in_=attn_norm.partition_broadcast(128)), writes=[b_gA], dma=True)
                for tt in range(NT):
                    b = tt % 2
                    A("sp", lambda e, tt=tt, b=b: e.dma_start(out=xt[b][:], in_=x[tt * 128:(tt + 1) * 128, :]), writes=[b_xt[b]], dma=True)
                    rms_stats(xt[b][:], b_xt[b], ss[b][:], b_ss[b], rstd[b][:], b_rstd[b], junk[:], b_junk, D)
                    A("dve", lambda e, b=b: e.scalar_tensor_tensor(out=hb[b][:], in0=xt[b][:], scalar=rstd[b][:, 0:1], in1=gA[:], op0=ALU.mult, op1=ALU.mult),
                      reads=[b_xt[b], b_rstd[b], b_gA], writes=[b_hb[b]])
                    for c in range(16):
                        A("pe", lambda e, b=b, c=c: e.transpose(out=pT[b][:, c * 128:(c + 1) * 128], in_=hb[b][:, c * 128:(c + 1) * 128], identity=identb[:]),
                          reads=[b_hb[b], b_identb], writes=[b_pT[b]])
                    A("act", lambda e, b=b, tt=tt: e.activation(out=hT[:, :, tt * 128:(tt + 1) * 128], in_=pT[b][:].rearrange("p (c t) -> p c t", c=16), func=AF.Copy),
                      reads=[b_pT[b]], writes=[b_hT[tt]])
            P.barrier()

            with ExitStack() as sc_o:
                P.cur = sc_o
                b_OATh = [Buf() for _ in range(8)]
                b_oaT = [[Buf() for _ in range(NT)] for _ in range(8)]
                b_obT = [[Buf() for _ in range(NT)] for _ in range(4)]

                if stop_after >= 2:
                  with ExitStack() as ph:
                    P.cur = ph
                    oaTa = P.sb("oaTa", [128, 8, S], BF16)
                    wq = [P.sb(f"wq{i}", [128, 16, 128], BF16) for i in range(2)]; b_wq = P.bufs(2)
                    wk = [P.sb(f"wk{i}", [128, 16, 128], BF16) for i in range(2)]; b_wk = P.bufs(2)
                    wv = [P.sb(f"wv{i}", [128, 16, 128], BF16) for i in range(2)]; b_wv = P.bufs(2)
                    qT = [P.sb(f"qT{i}", [128, S], BF16) for i in range(2)]; b_qT = [P.bufs(4) for _ in range(2)]
                    kT = [P.sb(f"kT{i}", [128, S], BF16) for i in range(2)]; b_kT = [P.bufs(4) for _ in range(2)]
                    Va = [P.sb(f"Va{i}", [128, NT, 130], BF16) for i in range(2)]; b_Va = [P.bufs(NT) for _ in range(2)]
                    b_Vone = P.bufs(2)
                    kaug = [P.sb("kaug0", [4, S], BF16)] * 2; b_kaug = [Buf()] * 2
                    qaug = [P.sb("qaug0", [4, S], BF16)] * 2; b_qaug = [Buf()] * 2
                    bdiag = P.sb("bdiag", [128, 8, 128], BF16); b_bdiag = Buf()
                    PT = [P.sb(f"PT{i}", [128, NT * 128], BF16) for i in range(2)]; b_PT = [P.bufs(4) for _ in range(2)]
                    om = [P.sb(f"om{i}", [128, 128], F32) for i in range(2)]; b_om = P.bufs(2)
                    rz = [P.sb(f"rz{i}", [128, 1], F32) for i in range(2)]; b_rz = P.bufs(2)
                    ot = P.sb("ot", [128, 128], F32); b_ot = Buf()
                    oab = [P.sb(f"oab{i}", [128, 128], BF16) for i in range(2)]; b_oab = P.bufs(2)
                    junk2 = P.sb("junk2", [128, 128], BF16); b_junk2 = Buf()
                    ss2 = P.sb("ss2", [128, 1], F32); b_ss2 = Buf()
                    rstd2 = P.sb("rstd2", [128, 1], F32); b_rstd2 = Buf()
                    gsub = P.sb("gsub", [128, 128], F32); b_gsub = Buf()
                    lamt = P.sb("lamt", [128, 256], F32); b_lamt = Buf()
                    lprod = P.sb("lprod", [128, 2, 64], F32); b_lprod = Buf()
                    lsum = P.sb("lsum", [128, 2], F32); b_lsum = Buf()
                    neglam = P.sb("neglam", [128, 1], F32); b_neglam = Buf()
                    pQ = [P.ps(f"pQ{i}", [128, 512]) for i in range(2)]; b_pQ = P.bufs(2)
                    pS = [P.ps(f"pS{i}", [128, 512]) for i in range(2)]; b_pS = P.bufs(2)
                    pO = [P.ps(f"pO{i}", [128, 512]) for i in range(2)]; b_pO = P.bufs(2)
                    pTr = P.ps("pTr", [128, 128], BF16); b_pTr = Buf()

                    A("sp", lambda e: e.dma_start(out=bdiag[:], in_=c_bdiag), writes=[b_bdiag], dma=True)
                    A("sp", lambda e: e.dma_start(out=gsub[:], in_=diff_subln.partition_broadcast(128)), writes=[b_gsub], dma=True)
                    A("dve", lambda e: e.tensor_scalar(out=gsub[:], in0=gsub[:], scalar1=1.0 - LAM_INIT, scalar2=None, op0=ALU.mult),
                      reads=[b_gsub], writes=[b_gsub])
                    A("sp", lambda e: e.dma_start(out=lamt[:], in_=diff_lambda.partition_broadcast(128)), writes=[b_lamt], dma=True)
                    A("dve", lambda e: e.tensor_tensor(out=lprod[:, 0, :], in0=lamt[:, 0:64], in1=lamt[:, 64:128], op=ALU.mult), reads=[b_lamt], writes=[b_lprod])
                    A("dve", lambda e: e.tensor_tensor(out=lprod[:, 1, :], in0=lamt[:, 128:192], in1=lamt[:, 192:256], op=ALU.mult), reads=[b_lamt, b_lprod], writes=[b_lprod])
                    A("dve", lambda e: e.reduce_sum(out=lsum[:], in_=lprod[:], axis=AX.X), reads=[b_lprod], writes=[b_lsum])
                    A("act", lambda e: e.activation(out=lsum[:], in_=lsum[:], func=AF.Exp), reads=[b_lsum], writes=[b_lsum])
                    A("dve", lambda e: e.tensor_tensor(out=neglam[:], in0=lsum[:, 0:1], in1=lsum[:, 1:2], op=ALU.subtract), reads=[b_lsum], writes=[b_neglam])
                    A("dve", lambda e: e.tensor_scalar(out=neglam[:], in0=neglam[:], scalar1=LAM_INIT, scalar2=-1.0, op0=ALU.add, op1=ALU.mult),
                      reads=[b_neglam], writes=[b_neglam])
                    for i in range(2):
                        A("dve", lambda e, i=i: e.memset(Va[i][:, :, 128:130], 1.0), writes=[b_Vone[i]])

                    ev = 0
                    for h in range(8):
                        b = h % 2
                        A("pool", lambda e, b=b, h=h: e.dma_start(out=wq[b][:], in_=w_in_v[:, :, h * 128:(h + 1) * 128]), writes=[b_wq[b]], dma=True)
                        A("pool", lambda e, b=b, h=h: e.dma_start(out=wk[b][:], in_=w_in_v[:, :, 1024 + h * 128:1024 + (h + 1) * 128]), writes=[b_wk[b]], dma=True)
                        A("pool", lambda e, b=b, h=h: e.dma_start(out=wv[b][:], in_=w_in_v[:, :, 2048 + h * 128:2048 + (h + 1) * 128]), writes=[b_wv[b]], dma=True)
                        A("sp", lambda e, b=b, h=h: e.dma_start(out=kaug[b][:], in_=c_kaug[h]), writes=[b_kaug[b]], dma=True)
                        A("sp", lambda e, b=b, h=h: e.dma_start(out=qaug[b][:], in_=c_qaug[h]), writes=[b_qaug[b]], dma=True)
                        for (w_, bw, dst, bdst) in ((wq, b_wq, qT, b_qT), (wk, b_wk, kT, b_kT)):
                            for g in range(4):
                                pb = ev % 2; ev += 1
                                for c in range(16):
                                    A("pe", lambda e, pb=pb, c=c, g=g, w_=w_, b=b: e.matmul(pQ[pb][:, :], lhsT=w_[b][:, c, :], rhs=hT[:, c, g * 512:(g + 1) * 512], start=(c == 0), stop=(c == 15)),
                                      reads=[bw[b]] + b_hT[g * 4:(g + 1) * 4], writes=[b_pQ[pb]])
                                if pb == 0:
                                    A("act", lambda e, pb=pb, g=g, dst=dst, b=b: e.activation(out=dst[b][:, g * 512:(g + 1) * 512], in_=pQ[pb][:, :], func=AF.Copy),
                                      reads=[b_pQ[pb]], writes=[bdst[b][g]])
                                else:
                                    A("dve", lambda e, pb=pb, g=g, dst=dst, b=b: e.tensor_copy(out=dst[b][:, g * 512:(g + 1) * 512], in_=pQ[pb][:, :]),
                                      reads=[b_pQ[pb]], writes=[bdst[b][g]])
                        for tt in range(NT):
                            pb = ev % 2; ev += 1
                            for c in range(16):
                                A("pe", lambda e, pb=pb, c=c, tt=tt, b=b: e.matmul(pQ[pb][:, 0:128], lhsT=hT[:, c, tt * 128:(tt + 1) * 128], rhs=wv[b][:, c, :], start=(c == 0), stop=(c == 15)),
                                  reads=[b_wv[b], b_hT[tt]], writes=[b_pQ[pb]])
                            if pb == 0:
                                A("act", lambda e, pb=pb, tt=tt, b=b: e.activation(out=Va[b][:, tt, 0:128], in_=pQ[pb][:, 0:128], func=AF.Copy),
                                  reads=[b_pQ[pb]], writes=[b_Va[b][tt]])
                            else:
                                A("dve", lambda e, pb=pb, tt=tt, b=b: e.tensor_copy(out=Va[b][:, tt, 0:128], in_=pQ[pb][:, 0:128]),
                                  reads=[b_pQ[pb]], writes=[b_Va[b][tt]])
                        if h == 0:
                            dump("d_qT", qT[b][:], [128, S], BF16, b_qT[b])
                            dump("d_kT", kT[b][:], [128, S], BF16, b_kT[b])
                            dump("d_Va", Va[b][:], [128, NT, 130], BF16, b_Va[b] + [b_Vone[b]])
                            dump("d_neglam", neglam[:], [128, 1], F32, [b_neglam])
                        def stQK(j, m, h=h, b=b):
                            pt = m
                            nblk = j + 1
                            for gi in range((nblk + 3) // 4):
                                sb_ = (j * 2 + m + gi) % 2
                                i0 = gi * 4
                                i1 = min(nblk, i0 + 4)
                                for i in range(i0, i1):
                                    col = (i - i0) * 128
                                    A("pe", lambda e, sb_=sb_, col=col, m=m, i=i, j=j, b=b: e.matmul(
                                        pS[sb_][:, col:col + 128], lhsT=kT[b][m * 64:(m + 1) * 64, i * 128:(i + 1) * 128],
                                        rhs=qT[b][m * 64:(m + 1) * 64, j * 128:(j + 1) * 128], start=True, stop=False),
                                      reads=[b_kT[b][i // 4], b_qT[b][j // 4]], writes=[b_pS[sb_]])
                                    if i < j:
                                        A("pe", lambda e, sb_=sb_, col=col, i=i, j=j, b=b: e.matmul(
                                            pS[sb_][:, col:col + 128], lhsT=kaug[b][0:4, i * 128:(i + 1) * 128],
                                            rhs=qaug[b][0:4, j * 128:(j + 1) * 128], start=False, stop=True),
                                          reads=[b_kaug[b], b_qaug[b]], writes=[b_pS[sb_]])
                                    else:
                                        A("pe", lambda e, sb_=sb_, col=col, h=h: e.matmul(
                                            pS[sb_][:, col:col + 128], lhsT=identb[:], rhs=bdiag[:, h, :], start=False, stop=True),
                                          reads=[b_identb, b_bdiag], writes=[b_pS[sb_]])
                                ncol = (i1 - i0) * 128
                                A("act", lambda e, sb_=sb_, pt=pt, i0=i0, ncol=ncol: e.activation(
                                    out=PT[pt][:, i0 * 128:i0 * 128 + ncol], in_=pS[sb_][:, 0:ncol], func=AF.Exp, scale=0.125),
                                  reads=[b_pS[sb_]], writes=[b_PT[pt][gi]])

                        def stPV(j, m, h=h, b=b):
                            pt = m
                            ob_ = m
                            for i in range(j + 1):
                                A("pe", lambda e, ob_=ob_, pt=pt, i=i, j=j, b=b: e.matmul(
                                    pO[ob_][:, 0:129], lhsT=PT[pt][:, i * 128:(i + 1) * 128], rhs=Va[b][:, i, 0:129], start=(i == 0), stop=(i == j)),
                                  reads=[b_PT[pt][i // 4], b_Va[b][i], b_Vone[b]], writes=[b_pO[ob_]])
                            A("dve", lambda e, ob_=ob_, m=m: e.reciprocal(out=rz[m][:], in_=pO[ob_][:, 128:129]), reads=[b_pO[ob_]], writes=[b_rz[m]])
                            A("dve", lambda e, ob_=ob_, m=m: e.tensor_scalar(out=om[m][:], in0=pO[ob_][:, 0:128], scalar1=rz[m][:, 0:1], scalar2=None, op0=ALU.mult),
                              reads=[b_pO[ob_], b_rz[m]], writes=[b_om[m]])
                            if m == 1:
                                A("dve", lambda e: e.scalar_tensor_tensor(out=ot[:], in0=om[1][:], scalar=neglam[:, 0:1], in1=om[0][:], op0=ALU.mult, op1=ALU.add),
                                  reads=[b_om[0], b_om[1], b_neglam], writes=[b_ot])
                                rms_stats(ot[:], b_ot, ss2[:], b_ss2, rstd2[:], b_rstd2, junk2[:], b_junk2, 128)
                                ab = j % 2
                                A("dve", lambda e, ab=ab: e.scalar_tensor_tensor(out=oab[ab][:], in0=ot[:], scalar=rstd2[:, 0:1], in1=gsub[:], op0=ALU.mult, op1=ALU.mult),
                                  reads=[b_ot, b_rstd2, b_gsub], writes=[b_oab[ab]])
                                def _tr(ab=ab, h=h, j=j):
                                    A("pe", lambda e, ab=ab: e.transpose(out=pTr[:, :], in_=oab[ab][:], identity=identb[:]), reads=[b_oab[ab], b_identb], writes=[b_pTr])
                                    A("act", lambda e, h=h, j=j: e.activation(out=oaTa[:, h, j * 128:(j + 1) * 128], in_=pTr[:, :], func=AF.Copy),
                                      reads=[b_pTr], writes=[b_oaT[h][j]])
                                pend_tr.append(_tr)

                        units = [(j, m) for j in range(NT) for m in range(2)]
                        pend_tr = []
                        stQK(*units[0])
                        for ui, (j, m) in enumerate(units):
                            if ui + 1 < len(units):
                                stQK(*units[ui + 1])
                            if m == 1 and pend_tr:
                                pend_tr.pop(0)()
                            stPV(j, m)
                        while pend_tr:
                            pend_tr.pop(0)()
                    for h in range(8):
                        A("sp", lambda e, h=h: e.dma_start(out=OAT[:, h, :], in_=oaTa[:, h, :]), reads=b_oaT[h], writes=[b_OATh[h]], dma=True)
                  P.barrier()
                P.cur = sc_o
                obT = P.sb("obT", [128, 8, S], BF16)

                if stop_after >= 3:
                  with ExitStack() as ph:
                    P.cur = ph
                    Wz = [P.sb(f"Wz{i}", [128, 16, 256], BF16) for i in range(2)]; b_Wz = P.bufs(2)
                    vg = P.sb("vg", [128, NT, 1024], BF16); b_vg = [P.bufs(4) for _ in range(NT)]
                    gn = P.sb("gn", [128, 1024], F32); b_gn = Buf()
                    wsf = P.sb("wsf", [128, 8, 128], F32); b_wsf = Buf()
                    tri = P.sb("tri", [128, 128], F32); b_tri = Buf()
                    wsT = P.sb("wsT", [128, 8, 128], BF16); b_wsT = Buf()
                    bsT = P.sb("bsT", [128, 8], F32); b_bsT = Buf()
                    ssv = P.sb("ssv", [128, NT, 4], F32); b_ssv = [P.bufs(4) for _ in range(NT)]
                    sv1 = P.sb("sv1", [128, 1], F32); b_sv1 = Buf()
                    rsv = P.sb("rsv", [128, 1], F32); b_rsv = Buf()
                    junk3 = P.sb("junk3", [128, 256], BF16); b_junk3 = Buf()
                    ug = [P.sb(f"ug{i}", [128, 256], F32) for i in range(2)]; b_ug = P.bufs(2)
                    tmp = [P.sb(f"tmp{i}", [128, 256], F32) for i in range(2)]; b_tmp = P.bufs(2)
                    obb = [P.sb(f"obb{i}", [128, 256], BF16) for i in range(2)]; b_obb = P.bufs(2)
                    pZ = [P.ps(f"pZ{i}", [128, 512]) for i in range(2)]; b_pZ = P.bufs(2)
                    pSV = [P.ps(f"pSV{i}", [128, 512]) for i in range(2)]; b_pSV = P.bufs(2)
                    pW = P.ps("pW", [128, 512]); b_pW = Buf()
                    pTb = P.ps("pTb", [128, 256], BF16); b_pTb = Buf()

                    A("sp", lambda e: e.dma_start(out=gn[:], in_=gmlp_norm.partition_broadcast(128)), writes=[b_gn], dma=True)
                    A("sp", lambda e: e.dma_start(out=wsf[:], in_=gmlp_ws.rearrange("g t s -> t g s")), writes=[b_wsf], dma=True)
                    A("sp", lambda e: e.dma_start(out=tri[:], in_=c_tri), writes=[b_tri], dma=True)
                    A("sp", lambda e: e.dma_start(out=bsT[:], in_=gmlp_bs.rearrange("g t -> t g"), allow_slow_non_contiguous=True), writes=[b_bsT], dma=True)
                    A("dve", lambda e: e.tensor_tensor(out=wsf[:], in0=wsf[:], in1=tri[:].unsqueeze(1).broadcast_to([128, 8, 128]), op=ALU.mult),
                      reads=[b_wsf, b_tri], writes=[b_wsf])
                    for g0 in range(0, 8, 4):
                        for gg in range(4):
                            A("pe", lambda e, g0=g0, gg=gg: e.transpose(out=pW[:, gg * 128:(gg + 1) * 128], in_=wsf[:, g0 + gg, :], identity=identf[:]),
                              reads=[b_wsf, b_identf], writes=[b_pW])
                        A("dve", lambda e, g0=g0: e.tensor_copy(out=wsT[:, g0:g0 + 4, :], in_=pW[:].rearrange("p (g t) -> p g t", g=4)), reads=[b_pW], writes=[b_wsT])

                    zc = 0
                    for vb in range(4):
                        wb_ = zc % 2; zc += 1
                        c0 = 3072 + 1024 + vb * 256
                        A("pool", lambda e, wb_=wb_, c0=c0: e.dma_start(out=Wz[wb_][:], in_=w_in_v[:, :, c0:c0 + 256]), writes=[b_Wz[wb_]], dma=True)
                        for tt in range(NT):
                            zb = tt % 2
                            for c in range(16):
                                A("pe", lambda e, zb=zb, c=c, tt=tt, wb_=wb_: e.matmul(pZ[zb][:, 0:256], lhsT=hT[:, c, tt * 128:(tt + 1) * 128], rhs=Wz[wb_][:, c, :], start=(c == 0), stop=(c == 15)),
                                  reads=[b_Wz[wb_], b_hT[tt]], writes=[b_pZ[zb]])
                            A("act", lambda e, zb=zb, tt=tt, vb=vb: e.activation(out=vg[:, tt, vb * 256:(vb + 1) * 256], in_=pZ[zb][:, 0:256], func=AF.Gelu),
                              reads=[b_pZ[zb]], writes=[b_vg[tt][vb]])
                            A("act", lambda e, tt=tt, vb=vb: e.activation(out=junk3[:], in_=vg[:, tt, vb * 256:(vb + 1) * 256], func=AF.Square, scale=1.0 / 32.0, accum_out=ssv[:, tt, vb:vb + 1]),
                              reads=[b_vg[tt][vb]], writes=[b_junk3, b_ssv[tt][vb]])
                    for tt in range(NT):
                        A("dve", lambda e, tt=tt: e.reduce_sum(out=sv1[:], in_=ssv[:, tt, :], axis=AX.X), reads=b_ssv[tt], writes=[b_sv1])
                        A("act", lambda e: e.activation(out=sv1[:], in_=sv1[:], func=AF.Sqrt, bias=epsT[:], scale=1.0), reads=[b_sv1, b_eps], writes=[b_sv1])
                        A("dve", lambda e: e.reciprocal(out=rsv[:], in_=sv1[:]), reads=[b_sv1], writes=[b_rsv])
                        A("dve", lambda e, tt=tt: e.scalar_tensor_tensor(out=vg[:, tt, :], in0=vg[:, tt, :], scalar=rsv[:, 0:1], in1=gn[:], op0=ALU.mult, op1=ALU.mult),
                          reads=b_vg[tt] + [b_rsv, b_gn], writes=b_vg[tt])
                    pend3 = []
                    for ub in range(4):
                        wb_ = zc % 2; zc += 1
                        c0 = 3072 + ub * 256
                        A("pool", lambda e, wb_=wb_, c0=c0: e.dma_start(out=Wz[wb_][:], in_=w_in_v[:, :, c0:c0 + 256]), writes=[b_Wz[wb_]], dma=True)
                        for tt in range(NT):
                            zb = tt % 2
                            for c in range(16):
                                A("pe", lambda e, zb=zb, c=c, tt=tt, wb_=wb_: e.matmul(pZ[zb][:, 0:256], lhsT=hT[:, c, tt * 128:(tt + 1) * 128], rhs=Wz[wb_][:, c, :], start=(c == 0), stop=(c == 15)),
                                  reads=[b_Wz[wb_], b_hT[tt]], writes=[b_pZ[zb]])
                            for gg in range(2):
                                g = ub * 2 + gg
                                A("pe", lambda e, zb=zb, gg=gg, g=g, tt=tt: e.matmul(pSV[zb][:, gg * 128:(gg + 1) * 128], lhsT=wsT[:, g, :], rhs=vg[:, tt, g * 128:(g + 1) * 128], start=True, stop=True),
                                  reads=[b_wsT, b_vg[tt][g // 2]], writes=[b_pSV[zb]])
                            while pend3:
                                pend3.pop(0)()
                            A("act", lambda e, zb=zb: e.activation(out=ug[zb][:], in_=pZ[zb][:, 0:256], func=AF.Gelu), reads=[b_pZ[zb]], writes=[b_ug[zb]])
                            A("dve", lambda e, zb=zb, ub=ub: e.tensor_tensor(out=tmp[zb][:].rearrange("p (g c) -> p g c", g=2), in0=pSV[zb][:, 0:256].rearrange("p (g c) -> p g c", g=2),
                                                                             in1=bsT[:, ub * 2:(ub + 1) * 2].unsqueeze(2).broadcast_to([128, 2, 128]), op=ALU.add),
                              reads=[b_pSV[zb], b_bsT], writes=[b_tmp[zb]])
                            A("dve", lambda e, zb=zb: e.tensor_tensor(out=obb[zb][:], in0=tmp[zb][:], in1=ug[zb][:], op=ALU.mult), reads=[b_tmp[zb], b_ug[zb]], writes=[b_obb[zb]])
                            def _tr3(zb=zb, ub=ub, tt=tt):
                                for gg in range(2):
                                    A("pe", lambda e, zb=zb, gg=gg: e.transpose(out=pTb[:, gg * 128:(gg + 1) * 128], in_=obb[zb][:, gg * 128:(gg + 1) * 128], identity=identb[:]),
                                      reads=[b_obb[zb], b_identb], writes=[b_pTb])
                                A("act", lambda e, ub=ub, tt=tt: e.activation(out=obT[:, ub * 2:(ub + 1) * 2, tt * 128:(tt + 1) * 128], in_=pTb[:].rearrange("p (g t) -> p g t", g=2), func=AF.Copy),
                                  reads=[b_pTb], writes=[b_obT[ub][tt]])
                            pend3.append(_tr3)
                    while pend3:
                        pend3.pop(0)()
                    if "OBT" in dbg:
                        for g in range(8):
                            A("sp", lambda e, g=g: e.dma_start(out=OBT[:, g, :], in_=obT[:, g, :]), reads=b_obT[g // 2], dma=True)
                  P.barrier()
                P.cur = sc_o
                oaT = P.sb("oaT2", [128, 8, S], BF16); b_oaT2 = [Buf() for _ in range(8)]

                if stop_after >= 4:
                  with ExitStack() as ph:
                    P.cur = ph
                    Wa = [P.sb(f"Wa{i}", [128, 8, 128], BF16) for i in range(2)]; b_Wa = P.bufs(2)
                    Wb = [P.sb(f"Wb{i}", [128, 8, 128], BF16) for i in range(2)]; b_Wb = P.bufs(2)
                    Wga = [P.sb(f"Wga{i}", [128, 16, 128], BF16) for i in range(2)]; b_Wga = P.bufs(2)
                    Wgb = [P.sb(f"Wgb{i}", [128, 16, 128], BF16) for i in range(2)]; b_Wgb = P.bufs(2)
                    sga = [P.sb(f"sga{i}", [128, 512], F32) for i in range(2)]; b_sga = P.bufs(2)
                    sgb = [P.sb(f"sgb{i}", [128, 512], F32) for i in range(2)]; b_sgb = P.bufs(2)
                    mst = [P.sb(f"mst{i}", [128, S], BF16) for i in range(2)]; b_mst = [P.bufs(4) for _ in range(2)]
                    pA = [P.ps(f"pA{i}", [128, 512]) for i in range(2)]; b_pA = P.bufs(2)
                    pB = [P.ps(f"pB{i}", [128, 512]) for i in range(2)]; b_pB = P.bufs(2)
                    pGA = [P.ps(f"pGA{i}", [128, 512]) for i in range(2)]; b_pGA = P.bufs(2)
                    pGB = [P.ps(f"pGB{i}", [128, 512]) for i in range(2)]; b_pGB = P.bufs(2)
                    wa_v = w_a.rearrange("(c p) n -> p c n", p=128)
                    wb_v = w_b.rearrange("(c p) n -> p c n", p=128)
                    for h in range(8):
                        A("sp", lambda e, h=h: e.dma_start(out=oaT[:, h, :], in_=OAT[:, h, :]), reads=[b_OATh[h]], writes=[b_oaT2[h]], dma=True)
                    it = 0
                    for n in range(16):
                        wb_ = n % 2
                        A("pool", lambda e, wb_=wb_, n=n: e.dma_start(out=Wa[wb_][:], in_=wa_v[:, :, n * 128:(n + 1) * 128]), writes=[b_Wa[wb_]], dma=True)
                        A("pool", lambda e, wb_=wb_, n=n: e.dma_start(out=Wb[wb_][:], in_=wb_v[:, :, n * 128:(n + 1) * 128]), writes=[b_Wb[wb_]], dma=True)
                        A("pool", lambda e, wb_=wb_, n=n: e.dma_start(out=Wga[wb_][:], in_=w_in_v[:, :, 5120 + n * 128:5120 + (n + 1) * 128]), writes=[b_Wga[wb_]], dma=True)
                        A("pool", lambda e, wb_=wb_, n=n: e.dma_start(out=Wgb[wb_][:], in_=w_in_v[:, :, 7168 + n * 128:7168 + (n + 1) * 128]), writes=[b_Wgb[wb_]], dma=True)
                        for g in range(4):
                            pb = it % 2; it += 1
                            gs = slice(g * 512, (g + 1) * 512)
                            for c in range(8):
                                A("pe", lambda e, pb=pb, c=c, gs=gs, wb_=wb_: e.matmul(pA[pb][:, :], lhsT=Wa[wb_][:, c, :], rhs=oaT[:, c, gs], start=(c == 0), stop=(c == 7)),
                                  reads=[b_Wa[wb_], b_oaT2[c]], writes=[b_pA[pb]])
                            for c in range(8):
                                A("pe", lambda e, pb=pb, c=c, gs=gs, wb_=wb_: e.matmul(pB[pb][:, :], lhsT=Wb[wb_][:, c, :], rhs=obT[:, c, gs], start=(c == 0), stop=(c == 7)),
                                  reads=[b_Wb[wb_]], writes=[b_pB[pb]])
                            for c in range(16):
                                A("pe", lambda e, pb=pb, c=c, gs=gs, wb_=wb_: e.matmul(pGA[pb][:, :], lhsT=Wga[wb_][:, c, :], rhs=hT[:, c, gs], start=(c == 0), stop=(c == 15)),
                                  reads=[b_Wga[wb_]], writes=[b_pGA[pb]])
                            for c in range(16):
                                A("pe", lambda e, pb=pb, c=c, gs=gs, wb_=wb_: e.matmul(pGB[pb][:, :], lhsT=Wgb[wb_][:, c, :], rhs=hT[:, c, gs], start=(c == 0), stop=(c == 15)),
                                  reads=[b_Wgb[wb_]], writes=[b_pGB[pb]])
                            A("act", lambda e, pb=pb: e.activation(out=sga[pb][:], in_=pGA[pb][:, :], func=AF.Sigmoid), reads=[b_pGA[pb]], writes=[b_sga[pb]])
                            A("act", lambda e, pb=pb: e.activation(out=sgb[pb][:], in_=pGB[pb][:, :], func=AF.Sigmoid), reads=[b_pGB[pb]], writes=[b_sgb[pb]])
                            A("dve", lambda e, pb=pb: e.tensor_tensor(out=sga[pb][:], in0=pA[pb][:, :], in1=sga[pb][:], op=ALU.mult), reads=[b_pA[pb], b_sga[pb]], writes=[b_sga[pb]])
                            A("dve", lambda e, pb=pb: e.tensor_tensor(out=sgb[pb][:], in0=pB[pb][:, :], in1=sgb[pb][:], op=ALU.mult), reads=[b_pB[pb], b_sgb[pb]], writes=[b_sgb[pb]])
                            A("pool", lambda e, pb=pb, wb_=wb_, gs=gs: e.tensor_tensor(out=mst[wb_][:, gs], in0=sga[pb][:], in1=sgb[pb][:], op=ALU.add),
                              reads=[b_sga[pb], b_sgb[pb]], writes=[b_mst[wb_][g]])
                        A("sp", lambda e, wb_=wb_, n=n: e.dma_start(out=MT[:, n, :], in_=mst[wb_][:]), reads=b_mst[wb_], writes=[b_MT[n]], dma=True)
                  P.barrier()
        P.cur = es
        P.barrier()

        if stop_after >= 5:
          with ExitStack() as ph:
            P.cur = ph
            mT = P.sb("mT", [128, 16, S], BF16); b_mT = P.bufs(16)
            Wo = [P.sb(f"Wo{i}", [128, 16, 512], BF16) for i in range(2)]; b_Wo = P.bufs(2)
            xp = [P.sb(f"xp{i}", [128, 512], F32) for i in range(2)]; b_xp = P.bufs(2)
            x2p = [P.sb(f"x2p{i}", [128, 512], F32) for i in range(2)]; b_x2p = P.bufs(2)
            pX = [P.ps(f"pX{i}", [128, 512]) for i in range(2)]; b_pX = P.bufs(2)
            wo_v = w_out.rearrange("(c p) n -> p c n", p=128)
            for c in range(16):
                A("sp", lambda e, c=c: e.dma_start(out=mT[:, c, :], in_=MT[:, c, :]), reads=[b_MT[c]], writes=[b_mT[c]], dma=True)
            it = 0
            for blk in range(4):
                wb_ = blk % 2
                A("pool", lambda e, wb_=wb_, blk=blk: e.dma_start(out=Wo[wb_][:], in_=wo_v[:, :, blk * 512:(blk + 1) * 512]), writes=[b_Wo[wb_]], dma=True)
                for tt in range(NT):
                    pb = it % 2; it += 1
                    A("sp", lambda e, pb=pb, tt=tt, blk=blk: e.dma_start(out=xp[pb][:], in_=x[tt * 128:(tt + 1) * 128, blk * 512:(blk + 1) * 512]), writes=[b_xp[pb]], dma=True)
                    for c in range(16):
                        A("pe", lambda e, pb=pb, c=c, tt=tt, wb_=wb_: e.matmul(pX[pb][:, :], lhsT=mT[:, c, tt * 128:(tt + 1) * 128], rhs=Wo[wb_][:, c, :], start=(c == 0), stop=(c == 15)),
                          reads=[b_Wo[wb_], b_mT[c]], writes=[b_pX[pb]])
                    A("dve", lambda e, pb=pb: e.tensor_tensor(out=x2p[pb][:], in0=pX[pb][:, :], in1=xp[pb][:], op=ALU.add), reads=[b_pX[pb], b_xp[pb]], writes=[b_x2p[pb]])
                    A("sp", lambda e, pb=pb, tt=tt, blk=blk: e.dma_start(out=X2[tt * 128:(tt + 1) * 128, blk * 512:(blk + 1) * 512], in_=x2p[pb][:]),
                      reads=[b_x2p[pb]], writes=[b_X2[tt][blk]], dma=True)
          P.barrier()
          with ExitStack() as ph:
            P.cur = ph
            gF = P.sb("gF", [128, D], F32); b_gF = Buf()
            xt_5 = [P.sb(f"x2t{i}", [128, D], F32) for i in range(2)]; b_xt = P.bufs(2)
            junk_5 = P.sb("junk5", [128, D], BF16); b_junk = Buf()
            ss_5 = [P.sb(f"ss5{i}", [128, 1], F32) for i in range(2)]; b_ss = P.bufs(2)
            rstd_5 = [P.sb(f"rstd5{i}", [128, 1], F32) for i in range(2)]; b_rstd = P.bufs(2)
            hb_5 = [P.sb(f"hb5{i}", [128, D], BF16) for i in range(2)]; b_hb = P.bufs(2)
            hs = [P.sb(f"hs5{i}", [128, 16, 128], BF16) for i in range(2)]; b_hs = P.bufs(2)
            pT_5 = [P.ps(f"pT5{i}", [128, D], BF16) for i in range(2)]; b_pT = P.bufs(2)
            A("sp", lambda e: e.dma_start(out=gF[:], in_=ffn_norm.partition_broadcast(128)), writes=[b_gF], dma=True)
            for tt in range(NT):
                b = tt % 2
                A("sp", lambda e, tt=tt, b=b: e.dma_start(out=xt_5[b][:], in_=X2[tt * 128:(tt + 1) * 128, :]), reads=b_X2[tt], writes=[b_xt[b]], dma=True)
                rms_stats(xt_5[b][:], b_xt[b], ss_5[b][:], b_ss[b], rstd_5[b][:], b_rstd[b], junk_5[:], b_junk, D)
                A("dve", lambda e, b=b: e.scalar_tensor_tensor(out=hb_5[b][:], in0=xt_5[b][:], scalar=rstd_5[b][:, 0:1], in1=gF[:], op0=ALU.mult, op1=ALU.mult),
                  reads=[b_xt[b], b_rstd[b], b_gF], writes=[b_hb[b]])
                for c in range(16):
                    A("pe", lambda e, b=b, c=c: e.transpose(out=pT_5[b][:, c * 128:(c + 1) * 128], in_=hb_5[b][:, c * 128:(c + 1) * 128], identity=identb[:]),
                      reads=[b_hb[b], b_identb], writes=[b_pT[b]])
                A("act", lambda e, b=b: e.activation(out=hs[b][:], in_=pT_5[b][:].rearrange("p (c t) -> p c t", c=16), func=AF.Copy), reads=[b_pT[b]], writes=[b_hs[b]])
                A("sp", lambda e, b=b, tt=tt: e.dma_start(out=H2T[:, :, tt * 128:(tt + 1) * 128], in_=hs[b][:]), reads=[b_hs[b]], writes=[b_H2T[tt]], dma=True)
          P.barrier()

        if stop_after >= 6:
          with ExitStack() as sc_q:
            P.cur = sc_q
            qpT = P.sb("qpT", [128, 16, S], BF16); b_qpT = [P.bufs(4) for _ in range(16)]
            with ExitStack() as ph:
                P.cur = ph
                h2T = P.sb("h2T", [128, 16, S], BF16); b_h2T = P.bufs(16)
                Wp = [P.sb(f"Wp{i}", [128, 16, 128], BF16) for i in range(2)]; b_Wp = P.bufs(2)
                pQ_6 = [P.ps(f"pQp{i}", [128, 512]) for i in range(2)]; b_pQ = P.bufs(2)
                wp_v = peer_wq.rearrange("(c p) n -> p c n", p=128)
                for c in range(16):
                    A("sp", lambda e, c=c: e.dma_start(out=h2T[:, c, :], in_=H2T[:, c, :]), reads=b_H2T, writes=[b_h2T[c]], dma=True)
                it = 0
                for n in range(16):
                    wb_ = n % 2
                    A("pool", lambda e, wb_=wb_, n=n: e.dma_start(out=Wp[wb_][:], in_=wp_v[:, :, n * 128:(n + 1) * 128]), writes=[b_Wp[wb_]], dma=True)
                    for g in range(4):
                        pb = it % 2; it += 1
                        for c in range(16):
                            A("pe", lambda e, pb=pb, c=c, g=g, wb_=wb_: e.matmul(pQ_6[pb][:, :], lhsT=Wp[wb_][:, c, :], rhs=h2T[:, c, g * 512:(g + 1) * 512], start=(c == 0), stop=(c == 15)),
                              reads=[b_Wp[wb_], b_h2T[c]], writes=[b_pQ[pb]])
                        if pb == 0:
                            A("act", lambda e, pb=pb, n=n, g=g: e.activation(out=qpT[:, n, g * 512:(g + 1) * 512], in_=pQ_6[pb][:, :], func=AF.Copy), reads=[b_pQ[pb]], writes=[b_qpT[n][g]])
                        else:
                            A("dve", lambda e, pb=pb, n=n, g=g: e.tensor_copy(out=qpT[:, n, g * 512:(g + 1) * 512], in_=pQ_6[pb][:, :]), reads=[b_pQ[pb]], writes=[b_qpT[n][g]])
            P.barrier()
            with ExitStack() as ph:
                P.cur = ph
                skf = P.sb("skf", [128, 2, 128], F32); b_skf = Buf()
                skT = P.sb("skT", [128, 2, 128], BF16); b_skT = Buf()
                sc = [P.sb(f"sc{i}", [128, 16, 128], F32) for i in range(2)]; b_sc = [P.bufs(16) for _ in range(2)]
                v1 = P.sb("v1", [128, 8, 16], F32); b_v1 = P.bufs(8)
                v2 = P.sb("v2", [128, 8, 16], F32); b_v2 = P.bufs(8)
                wk1 = P.sb("wk1", [128, 128], F32); b_wk1 = Buf()
                wk2 = P.sb("wk2", [128, 128], F32); b_wk2 = Buf()
                cand = P.sb("cand", [128, 8, 256], F32); b_cand = P.bufs(8)
                cw = P.sb("cw", [128, 256], F32); b_cw = Buf()
                cw2 = P.sb("cw2", [128, 256], F32); b_cw2 = Buf()
                t17 = P.sb("t17", [128, 8, 8], F32); b_t17 = P.bufs(8)
                thrm = P.sb("thrm", [128, 8], F32); b_thrm = Buf()
                top = P.sb("top", [128, 8, 16], F32); b_top = P.bufs(8)
                negmx = P.sb("negmx", [128, 8], F32); b_negmx = Buf()
                Zt = P.sb("Zt", [128, 8], F32); b_Zt = P.bufs(8)
                rZ = P.sb("rZ", [128, 8], F32); b_rZ = Buf()
                junk6 = P.sb("junk6", [128, 16], F32); b_junk6 = Buf()
                d1 = P.sb("d1", [128, 8, 16], F32); b_d1 = Buf()
                Q4 = P.sb("Q4", [128, 4, 128], F32); b_Q4 = P.bufs(4)
                sT = [P.sb(f"sT{i}", [128, 4, 128], F32) for i in range(2)]; b_sT = P.bufs(2)
                qrep = [P.sb(f"qrep{i}", [128, 16, 2, 128], BF16) for i in range(2)]; b_qrep = [P.bufs(2) for _ in range(2)]
                Eb = [P.sb(f"Eb{i}", [128, 2, 128], F32) for i in range(2)]; b_Eb = [P.bufs(2) for _ in range(2)]
                lhsG = [P.sb(f"lhsG{i}", [128, 2, 128], BF16) for i in range(3)]; b_lhsG = P.bufs(3)
                rhsG = [P.sb(f"rhsG{i}", [128, 2, 128], BF16) for i in range(3)]; b_rhsG = [P.bufs(2) for _ in range(3)]
                SG = [P.sb(f"SG{i}", [128, 128, 128], BF16) for i in range(2)]; b_SG = [P.bufs(32) for _ in range(2)]
                pSc = [P.ps("pSc0", [128, 512])] * 2; b_pSc = [Buf()] * 2
                pT4 = P.ps("pT4", [128, 512]); b_pT4 = Buf()
                pD = [P.ps(f"pD{i}", [128, 512]) for i in range(2)]; b_pD = P.bufs(2)
                pAa = [P.ps(f"pAa{i}", [128, 512]) for i in range(2)]; b_pAa = P.bufs(2)
                pG = [P.ps(f"pG{i}", [128, 512]) for i in range(2)]; b_pG = P.bufs(2)

                A("sp", lambda e: e.dma_start(out=skf[:], in_=peer_sk.rearrange("p n d -> n p d")), writes=[b_skf], dma=True)
                for p_ in range(2):
                    A("pe", lambda e, p_=p_: e.transpose(out=pT4[:, p_ * 128:(p_ + 1) * 128], in_=skf[:, p_, :], identity=identf[:]), reads=[b_skf, b_identf], writes=[b_pT4])
                A("dve", lambda e: e.tensor_copy(out=skT[:], in_=pT4[:, 0:256].rearrange("d (p n) -> d p n", p=2)), reads=[b_pT4], writes=[b_skT])

                def front_scores(tt):
                    ts_ = slice(tt * 128, (tt + 1) * 128)
                    for r in range(4):
                        pb = r % 2
                        for q_ in range(4):
                            hp = r * 4 + q_
                            A("pe", lambda e, pb=pb, q_=q_, hp=hp, ts_=ts_: e.matmul(pSc[pb][:, q_ * 128:(q_ + 1) * 128], lhsT=qpT[:, hp, ts_], rhs=skT[:, hp % 2, :], start=True, stop=True),
                              reads=[b_qpT[hp][tt // 4], b_skT], writes=[b_pSc[pb]])
                        A("act", lambda e, pb=pb, r=r: e.activation(out=sc[tt % 2][:, r * 4:(r + 1) * 4, :], in_=pSc[pb][:].rearrange("p (a n) -> p a n", a=4), func=AF.Copy),
                          reads=[b_pSc[pb]], writes=b_sc[tt % 2][r * 4:(r + 1) * 4])

                def front_topk(tt):
                    for h in range(8):
                        for (src, vv, bvv, wkk, bwk) in ((2 * h, v1, b_v1, wk1, b_wk1), (2 * h + 1, v2, b_v2, wk2, b_wk2)):
                            A("dve", lambda e, src=src, vv=vv, h=h: e.max(out=vv[:, h, 0:8], in_=sc[tt % 2][:, src, :]), reads=[b_sc[tt % 2][src]], writes=[bvv[h]])
                            A("dve", lambda e, src=src, vv=vv, h=h, wkk=wkk: e.match_replace(out=wkk[:], in_to_replace=vv[:, h, 0:8], in_values=sc[tt % 2][:, src, :], imm_value=NEG),
                              reads=[b_sc[tt % 2][src], bvv[h]], writes=[bwk])
                            A("dve", lambda e, vv=vv, h=h, wkk=wkk: e.max(out=vv[:, h, 8:16], in_=wkk[:]), reads=[bwk, bvv[h]], writes=[bvv[h]])
                        A("dve", lambda e, h=h: e.tensor_tensor(out=cand[:, h, :].rearrange("p (a b) -> p a b", a=16), in0=v1[:, h, :].unsqueeze(2).broadcast_to([128, 16, 16]),
                                                                in1=v2[:, h, :].unsqueeze(1).broadcast_to([128, 16, 16]), op=ALU.add),
                          reads=[b_v1[h], b_v2[h]], writes=[b_cand[h]])
                        A("dve", lambda e, h=h: e.max(out=top[:, h, 0:8], in_=cand[:, h, :]), reads=[b_cand[h]], writes=[b_top[h]])
                        A("dve", lambda e, h=h: e.match_replace(out=cw[:], in_to_replace=top[:, h, 0:8], in_values=cand[:, h, :], imm_value=NEG),
                          reads=[b_cand[h], b_top[h]], writes=[b_cw])
                        A("dve", lambda e, h=h: e.max(out=top[:, h, 8:16], in_=cw[:]), reads=[b_cw, b_top[h]], writes=[b_top[h]])
                        A("dve", lambda e, h=h: e.match_replace(out=cw2[:], in_to_replace=top[:, h, 8:16], in_values=cw[:], imm_value=NEG),
                          reads=[b_cw, b_top[h]], writes=[b_cw2])
                        A("dve", lambda e, h=h: e.max(out=t17[:, h, :], in_=cw2[:]), reads=[b_cw2], writes=[b_t17[h]])
                    A("dve", lambda e: e.tensor_scalar(out=negmx[:], in0=top[:, :, 0], scalar1=-1.0, scalar2=None, op0=ALU.mult), reads=b_top, writes=[b_negmx])
                    for h in range(8):
                        A("act", lambda e, h=h: e.activation(out=junk6[:], in_=top[:, h, :], func=AF.Exp, bias=negmx[:, h:h + 1], scale=1.0, accum_out=Zt[:, h:h + 1]),
                          reads=[b_top[h], b_negmx], writes=[b_junk6, b_Zt[h]])
                    A("act", lambda e: e.activation(out=rZ[:], in_=Zt[:], func=AF.Ln), reads=b_Zt, writes=[b_rZ])
                    A("dve", lambda e: e.tensor_tensor(out=rZ[:], in0=rZ[:], in1=v2[:, :, 0], op=ALU.add), reads=[b_rZ] + b_v2, writes=[b_rZ])
                    A("dve", lambda e: e.tensor_tensor(out=d1[:], in0=v1[:], in1=v1[:, :, 0:1].broadcast_to([128, 8, 16]), op=ALU.subtract), reads=b_v1, writes=[b_d1])
                    A("dve", lambda e: e.tensor_copy(out=Q4[:, 0, :].rearrange("p (h a) -> p h a", h=8), in_=v1[:]), reads=b_v1, writes=[b_Q4[0]])
                    A("dve", lambda e: e.tensor_tensor(out=Q4[:, 3, :].rearrange("p (h a) -> p h a", h=8), in0=d1[:], in1=rZ[:].unsqueeze(2).broadcast_to([128, 8, 16]), op=ALU.subtract),
                      reads=[b_d1, b_rZ], writes=[b_Q4[3]])
                    A("dve", lambda e: e.tensor_tensor(out=thrm[:], in0=top[:, :, 15], in1=t17[:, :, 0], op=ALU.add), reads=b_top + b_t17, writes=[b_thrm])
                    A("dve", lambda e: e.tensor_scalar(out=thrm[:], in0=thrm[:], scalar1=0.5, scalar2=None, op0=ALU.mult), reads=[b_thrm], writes=[b_thrm])
                    A("dve", lambda e: e.tensor_tensor(out=Q4[:, 2, :].rearrange("p (h a) -> p h a", h=8), in0=thrm[:].unsqueeze(2).broadcast_to([128, 8, 16]), in1=v1[:], op=ALU.subtract),
                      reads=[b_thrm] + b_v1, writes=[b_Q4[2]])
                    A("dve", lambda e: e.tensor_tensor(out=Q4[:, 2, :].rearrange("p (h a) -> p h a", h=8), in0=Q4[:, 2, :].rearrange("p (h a) -> p h a", h=8),
                                                       in1=v2[:, :, 15:16].broadcast_to([128, 8, 16]), op=ALU.max),
                      reads=[b_Q4[2]] + b_v2, writes=[b_Q4[2]])
                    tb = tt % 2
                    for k4 in (0, 2, 3):
                        A("pe", lambda e, k4=k4: e.transpose(out=pT4[:, k4 * 128:(k4 + 1) * 128], in_=Q4[:, k4, :], identity=identf[:]), reads=[b_Q4[k4], b_identf], writes=[b_pT4])
                    A("act", lambda e, tb=tb: e.activation(out=sT[tb][:], in_=pT4[:].rearrange("p (k t) -> p k t", k=4), func=AF.Copy), reads=[b_pT4], writes=[b_sT[tb]])

                def issue_qrep(S_):
                    if S_ >= NT * 8:
                        return
                    qb = S_ % 2
                    t0 = S_ * 16
                    ttq = S_ // 8
                    for p_ in range(2):
                        def rep_cp(e, qb=qb, p_=p_, t0=t0):
                            src = qpT[:, p_:16:2, t0:t0 + 16].rearrange("d h t -> d t h").unsqueeze(3).broadcast_to([128, 16, 8, 16])
                            dst = qrep[qb][:, :, p_, :].rearrange("d t (h a) -> d t h a", h=8)
                            if p_ == 0:
                                return e.tensor_copy(out=dst, in_=src)
                            return e.activation(out=dst, in_=src, func=AF.Copy)
                        A("pool" if p_ == 0 else "act", rep_cp, reads=[b_qpT[hp][ttq // 4] for hp in range(p_, 16, 2)], writes=[b_qrep[qb][p_]])

                def gates(tt):
                    tb = tt % 2
                    sgb_ = tt % 2

                    def stageA(g, tt=tt, tb=tb):
                        sub, gl = divmod(g, 8)
                        qb = (tt * 8 + sub) % 2
                        if gl == 0:
                            issue_qrep(tt * 8 + sub + 1)
                        gi = tt * 64 + g
                        sl = gi % 2
                        s3 = gi % 3
                        for q in range(2):
                            tl = gl * 2 + q
                            A("pe", lambda e, sl=sl, qb=qb, tl=tl, q=q: e.matmul(pD[sl][:, q * 128:(q + 1) * 128], lhsT=qrep[qb][:, tl, 0, :], rhs=skT[:, 0, :], start=True, stop=True),
                              reads=[b_qrep[qb][0], b_skT], writes=[b_pD[sl]])
                            A("pe", lambda e, sl=sl, qb=qb, tl=tl, q=q: e.matmul(pD[sl][:, 256 + q * 128:256 + (q + 1) * 128], lhsT=qrep[qb][:, tl, 1, :], rhs=skT[:, 1, :], start=True, stop=True),
                              reads=[b_qrep[qb][1], b_skT], writes=[b_pD[sl]])
                            A("pe", lambda e, sl=sl, qb=qb, tl=tl, q=q: e.matmul(pAa[sl][:, q * 128:(q + 1) * 128], lhsT=qrep[qb][:, tl, 1, :], rhs=skT[:, 1, :], start=True, stop=True),
                              reads=[b_qrep[qb][1], b_skT], writes=[b_pAa[sl]])
                        for q in range(2):
                            t = g * 2 + q
                            A("act", lambda e, sl=sl, tb=tb, t=t, q=q: e.activation(out=Eb[sl][:, q, :], in_=pAa[sl][:, q * 128:(q + 1) * 128], func=AF.Exp, bias=sT[tb][:, 3, t:t + 1], scale=1.0),
                              reads=[b_pAa[sl], b_sT[tb]], writes=[b_Eb[sl][q]])
                        A("dve", lambda e, sl=sl, s3=s3, tb=tb, g=g: e.tensor_tensor(out=lhsG[s3][:], in0=pD[sl][:, 0:256].rearrange("p (q i) -> p q i", q=2),
                                                                                    in1=sT[tb][:, 0, g * 2:g * 2 + 2].unsqueeze(2).broadcast_to([128, 2, 128]), op=ALU.is_equal),
                          reads=[b_pD[sl], b_sT[tb]], writes=[b_lhsG[s3]])
                        for q in range(2):
                            t = g * 2 + q
                            A("dve", lambda e, sl=sl, s3=s3, tb=tb, t=t, q=q: e.scalar_tensor_tensor(out=rhsG[s3][:, q, :], in0=pD[sl][:, 256 + q * 128:256 + (q + 1) * 128], scalar=sT[tb][:, 2, t:t + 1], in1=Eb[sl][:, q, :], op0=ALU.is_ge, op1=ALU.mult),
                              reads=[b_pD[sl], b_sT[tb], b_Eb[sl][q]], writes=[b_rhsG[s3][q]])

                    def stageB(g, tt=tt, sgb_=sgb_):
                        gi = tt * 64 + g
                        s3 = gi % 3
                        gq = (gi // 2) % 2
                        for q in range(2):
                            slot = (g * 2 + q) % 4
                            A("pe", lambda e, s3=s3, gq=gq, q=q, slot=slot: e.matmul(pG[gq][:].rearrange("i (j t) -> i t j", t=4)[:, slot, :], lhsT=lhsG[s3][:, q, :], rhs=rhsG[s3][:, q, :], start=True, stop=True),
                              reads=[b_lhsG[s3], b_rhsG[s3][q]], writes=[b_pG[gq]])
                        if g % 2 == 1:
                            g4 = g // 2
                            A("act", lambda e, gq=gq, sgb_=sgb_, g4=g4: e.activation(out=SG[sgb_][:, :, g4 * 4:g4 * 4 + 4], in_=pG[gq][:].rearrange("i (j t) -> i j t", t=4), func=AF.Copy),
                              reads=[b_pG[gq]], writes=[b_SG[sgb_][g4]])

                    return stageA, stageB

                issue_qrep(0)
                front_scores(0)
                front_topk(0)
                for tt in range(NT):
                    ts_ = slice(tt * 128, (tt + 1) * 128)
                    sgb_ = tt % 2
                    if tt + 1 < NT:
                        front_scores(tt + 1)
                    stageA, stageB = gates(tt)
                    for n_ in range(64 + 1):
                        if n_ == 33 and tt + 1 < NT:
                            front_topk(tt + 1)
                        if n_ < 64:
                            stageA(n_)
                        if n_ >= 1:
                            stageB(n_ - 1)
                    for jb in range(8):
                        A("sp", lambda e, jb=jb, sgb_=sgb_, ts_=ts_: e.dma_start(out=GT[jb * 16:(jb + 1) * 16, :, ts_].rearrange("j i t -> i j t"), in_=SG[sgb_][:, jb * 16:(jb + 1) * 16, :]),
                          reads=b_SG[sgb_], writes=[b_GT[tt]], dma=True)
            P.barrier()
          P.cur = es
          P.barrier()

        if stop_after >= 7:
          u_v = peer_u.rearrange("(i j) d -> j i d", j=128)
          v_v = peer_v.rearrange("(i j) d -> j i d", j=128)
          def _half(hf):
            with ExitStack() as sc_a:
                P.cur = sc_a
                TB = hf * 1024
                h2h = P.sb(f"h2h{hf}", [128, 16, 1024], BF16); b_h2h = P.bufs(16)
                acc = P.sb(f"acc{hf}", [128, 8, D], F32); b_acc = [P.bufs(4) for _ in range(8)]
                with ExitStack() as ph:
                    P.cur = ph
                    Ust = [P.sb(f"Ust{hf}{i}", [128, D], BF16) for i in range(2)]; b_Ust = P.bufs(2)
                    UT = [P.sb(f"UT{hf}{i}", [128, 16, 128], BF16) for i in range(2)]; b_UT = P.bufs(2)
                    Vc = [P.sb(f"Vc{hf}{i}", [128, 4, D], BF16) for i in range(2)]; b_Vc = [P.bufs(4) for _ in range(2)]
                    GTc = [P.sb(f"GTc{hf}{i}", [128, 1024], BF16) for i in range(2)]; b_GTc = P.bufs(2)
                    aT = [P.sb(f"aT{hf}{i}", [128, 4, 1024], BF16) for i in range(2)]; b_aT = [[P.bufs(2) for _ in range(4)] for _ in range(2)]
                    ga = [P.sb(f"ga{hf}{i}", [128, 512], F32) for i in range(2)]; b_ga = P.bufs(2)
                    pU = [P.ps(f"pU{hf}{i}", [128, D], BF16) for i in range(2)]; b_pU = P.bufs(2)
                    pA_7 = [P.ps(f"pAe{hf}{i}", [128, 512]) for i in range(2)]; b_pA = P.bufs(2)
                    pO_7 = [P.ps(f"pOe{hf}{i}", [128, 512]) for i in range(2)]; b_pO = P.bufs(2)
                    for c in range(16):
                        A("sp", lambda e, c=c, TB=TB: e.dma_start(out=h2h[:, c, :], in_=H2T[:, c, TB:TB + 1024]), reads=b_H2T, writes=[b_h2h[c]], dma=True)
                    for tt in range(8):
                        A("sp", lambda e, tt=tt, TB=TB: e.dma_start(out=acc[:, tt, :], in_=X2[TB + tt * 128:TB + (tt + 1) * 128, :]), reads=b_X2[(TB // 128) + tt], writes=b_acc[tt], dma=True)
                    cnt = {"ia": 0, "io": 0}

                    def stT(k):
                        s_, jj = divmod(k, 4)
                        sb_ = s_ % 2
                        cb = k % 2
                        A("pool", lambda e, sb_=sb_, jj=jj, k=k: e.dma_start(out=Vc[sb_][:, jj, :], in_=v_v[k]), writes=[b_Vc[sb_][jj]], dma=True)
                        A("sp", lambda e, cb=cb, k=k: e.dma_start(out=GTc[cb][:], in_=GT[k, :, TB:TB + 1024]), reads=b_GT, writes=[b_GTc[cb]], dma=True)
                        if hf == 1:
                            A("sp", lambda e, cb=cb, k=k: e.dma_start(out=UT[cb][:], in_=UTD[k].rearrange("d (c i) -> d c i", c=16)), reads=[b_UTD[k]], writes=[b_UT[cb]], dma=True)
                            return
                        A("pool", lambda e, cb=cb, k=k: e.dma_start(out=Ust[cb][:], in_=u_v[k]), writes=[b_Ust[cb]], dma=True)
                        for c in range(16):
                            A("pe", lambda e, cb=cb, c=c: e.transpose(out=pU[cb][:, c * 128:(c + 1) * 128], in_=Ust[cb][:, c * 128:(c + 1) * 128], identity=identb[:]),
                              reads=[b_Ust[cb], b_identb], writes=[b_pU[cb]])
                        A("act", lambda e, cb=cb: e.activation(out=UT[cb][:], in_=pU[cb][:].rearrange("p (c i) -> p c i", c=16), func=AF.Copy), reads=[b_pU[cb]], writes=[b_UT[cb]])
                        A("sp", lambda e, cb=cb, k=k: e.dma_start(out=UTD[k].rearrange("d (c i) -> d c i", c=16), in_=UT[cb][:]), reads=[b_UT[cb]], writes=[b_UTD[k]], dma=True)

                    def stM(k):
                        s_, jj = divmod(k, 4)
                        sb_ = s_ % 2
                        cb = k % 2
                        for tg in range(2):
                            ab = cnt["ia"] % 2; cnt["ia"] += 1
                            for c in range(16):
                                A("pe", lambda e, ab=ab, cb=cb, c=c, tg=tg: e.matmul(pA_7[ab][:, :], lhsT=UT[cb][:, c, :], rhs=h2h[:, c, tg * 512:(tg + 1) * 512], start=(c == 0), stop=(c == 15)),
                                  reads=[b_UT[cb], b_h2h[c]], writes=[b_pA[ab]])
                            A("act", lambda e, ab=ab: e.activation(out=ga[ab][:], in_=pA_7[ab][:, :], func=AF.Gelu), reads=[b_pA[ab]], writes=[b_ga[ab]])
                            A("dve", lambda e, ab=ab, sb_=sb_, jj=jj, tg=tg, cb=cb: e.tensor_tensor(out=aT[sb_][:, jj, tg * 512:(tg + 1) * 512], in0=ga[ab][:], in1=GTc[cb][:, tg * 512:(tg + 1) * 512], op=ALU.mult),
                              reads=[b_ga[ab], b_GTc[cb]], writes=[b_aT[sb_][jj][tg]])

                    def stV(s_):
                        sb_ = s_ % 2
                        for tt in range(8):
                            for blk in range(4):
                                ob_ = cnt["io"] % 2; cnt["io"] += 1
                                for jj in range(4):
                                    A("pe", lambda e, ob_=ob_, sb_=sb_, jj=jj, tt=tt, blk=blk: e.matmul(pO_7[ob_][:, :], lhsT=aT[sb_][:, jj, tt * 128:(tt + 1) * 128], rhs=Vc[sb_][:, jj, blk * 512:(blk + 1) * 512], start=(jj == 0), stop=(jj == 3)),
                                      reads=[b_aT[sb_][jj][tt // 4], b_Vc[sb_][jj]], writes=[b_pO[ob_]])
                                A("dve", lambda e, ob_=ob_, tt=tt, blk=blk: e.tensor_tensor(out=acc[:, tt, blk * 512:(blk + 1) * 512], in0=pO_7[ob_][:, :], in1=acc[:, tt, blk * 512:(blk + 1) * 512], op=ALU.add),
                                  reads=[b_pO[ob_], b_acc[tt][blk]], writes=[b_acc[tt][blk]])

                    stT(0)
                    for k in range(128):
                        if k + 1 < 128:
                            stT(k + 1)
                        stM(k)
                        if k % 4 == 0 and k > 0:
                            stV(k // 4 - 1)
                    stV(31)
                P.barrier()
                with ExitStack() as ph:
                    P.cur = ph
                    gO = P.sb(f"gO{hf}", [128, D], F32); b_gO = Buf()
                    junk_7 = P.sb(f"junk7{hf}", [128, D], BF16); b_junk = Buf()
                    ss_7 = [P.sb(f"ss7{hf}{i}", [128, 1], F32) for i in range(2)]; b_ss = P.bufs(2)
                    rstd_7 = [P.sb(f"rstd7{hf}{i}", [128, 1], F32) for i in range(2)]; b_rstd = P.bufs(2)
                    A("sp", lambda e: e.dma_start(out=gO[:], in_=final_norm.partition_broadcast(128)), writes=[b_gO], dma=True)
                    for tt in range(8):
                        b = tt % 2
                        rms_stats(acc[:, tt, :], b_acc[tt], ss_7[b][:], b_ss[b], rstd_7[b][:], b_rstd[b], junk_7[:], b_junk, D)
                        A("dve", lambda e, b=b, tt=tt: e.scalar_tensor_tensor(out=acc[:, tt, :], in0=acc[:, tt, :], scalar=rstd_7[b][:, 0:1], in1=gO[:], op0=ALU.mult, op1=ALU.mult),
                          reads=b_acc[tt] + [b_rstd[b], b_gO], writes=b_acc[tt])
                        fin.append(A("sp", lambda e, tt=tt, TB=TB: e.dma_start(out=out[TB + tt * 128:TB + (tt + 1) * 128, :], in_=acc[:, tt, :]), reads=b_acc[tt], dma=True))
                P.barrier()
            P.cur = es
            P.barrier()
          _half(0)
          _half(1)

        P.emit(final_waits=[op for e_ in P.ENGS for op in P.ops[e_] if op.dma])
        build.nops = P.nops
    return nc


def _consts():
    bf = ml_dtypes.bfloat16
    slopes = 2.0 ** (-np.arange(1, 9, dtype=np.float64))
    pos = np.arange(S)
    kaug = np.zeros((8, 4, S), np.float64)
    qaug = np.zeros((8, 4, S), np.float64)
    for h in range(8):
        kaug[h, 0] = 8 * slopes[h] * 128 * (pos // 128)
        kaug[h, 1] = 8 * slopes[h] * (pos % 128)
        kaug[h, 2] = 1
        kaug[h, 3] = 1
        qaug[h, 0] = 1
        qaug[h, 1] = 1
        qaug[h, 2] = -8 * slopes[h] * 128 * (pos // 128)
        qaug[h, 3] = -8 * slopes[h] * (pos % 128)
    kp = np.arange(128)[:, None]
    qp = np.arange(128)[None, :]
    allowed = (kp // 64) <= (qp // 64)
    bdiag = np.zeros((128, 8, 128), np.float64)
    for h in range(8):
        bdiag[:, h, :] = np.where(allowed, -8 * slopes[h] * np.abs(qp - kp), -240000.0)
    tri = (np.arange(128)[None, :] <= np.arange(128)[:, None]).astype(np.float32)
    return {
        "c_identb": np.eye(128, dtype=np.float32).astype(bf),
        "c_identf": np.eye(128, dtype=np.float32),
        "c_kaug": kaug.astype(np.float32).astype(bf),
        "c_qaug": qaug.astype(np.float32).astype(bf),
        "c_bdiag": bdiag.astype(np.float32).astype(bf),
        "c_tri": tri,
    }


def make_in_maps(inputs, cores):
    f = lambda a: np.ascontiguousarray(np.asarray(a, dtype=np.float32))
    shared = {
        "attn_norm": f(inputs["attn_norm"]).reshape(1, D),
        "w_in": f(inputs["w_in"]).reshape(D, IN_COLS),
        "diff_lambda": f(inputs["diff_lambda"]).reshape(1, 256),
        "diff_subln": f(inputs["diff_subln"]).reshape(1, 128),
        "gmlp_norm": f(inputs["gmlp_norm"]).reshape(1, 1024),
        "gmlp_ws": f(inputs["gmlp_ws"]).reshape(8, 128, 128),
        "gmlp_bs": f(inputs["gmlp_bs"]).reshape(8, 128),
        "w_branch_a": f(inputs["w_branch_a"]).reshape(1024, D),
        "w_branch_b": f(inputs["w_branch_b"]).reshape(1024, D),
        "w_out": f(inputs["w_out"]).reshape(D, D),
        "ffn_norm": f(inputs["ffn_norm"]).reshape(1, D),
        "peer_wq": f(inputs["peer_wq"]).reshape(D, 2048),
        "peer_subkeys": f(inputs["peer_subkeys"]).reshape(2, 128, 128),
        "peer_u": f(inputs["peer_u"]).reshape(16384, D),
        "peer_v": f(inputs["peer_v"]).reshape(16384, D),
        "final_norm": f(inputs["final_norm"]).reshape(1, D),
    }
    shared.update(_consts())
    xs = f(inputs["x"])
    return [dict(shared, x=xs[b]) for b in cores]


def kernel(**inputs):
    nc = build()
    in_maps = make_in_maps(inputs, list(range(8)))
    res = run_bass_kernel_spmd(nc, in_maps, core_ids=list(range(8)))
    return np.stack([np.asarray(r["out"], dtype=np.float32) for r in res.results], axis=0)
```

```python
import numpy as np
import ml_dtypes
import concourse.bass as bass
import concourse.mybir as mybir
from concourse.bass_utils import run_bass_kernel_spmd
from contextlib import ExitStack

F32 = mybir.dt.float32
BF16 = mybir.dt.bfloat16
ALU = mybir.AluOpType
AF = mybir.ActivationFunctionType
AX = mybir.AxisListType

S = 2048
D = 2048
NT = S // 128
EPS = 1e-6
IN_COLS = 9216
LAM_INIT = 0.8 - 0.6 * 1.0
NEG = -1.0e30


class Buf:
    __slots__ = ("name", "last_w", "readers")

    def __init__(self, name="b"):
        self.name = name
        self.last_w = None
        self.readers = []


class Op:
    __slots__ = ("eng", "fn", "dma", "deps", "signal", "sem", "semval", "prewait")

    def __init__(self, eng, fn, dma):
        self.eng = eng
        self.fn = fn
        self.dma = dma
        self.deps = []
        self.signal = False
        self.sem = None
        self.semval = None
        self.prewait = None


class Prog:
    ENGS = ("pe", "act", "dve", "pool", "sp")
    NDMA = {"sp": 12, "pool": 12}

    def __init__(self, nc, es):
        self.nc = nc
        self.es = es
        self.cur = es
        self.ops = {e: [] for e in self.ENGS}
        self.nops = 0
        self._bar_idx = {e: 0 for e in self.ENGS}
        self._pending = {e: [] for e in self.ENGS}

    def sb(self, name, shape, dtype):
        self.nops += 0
        self._uid = getattr(self, "_uid", 0) + 1
        return self.cur.enter_context(self.nc.sbuf_tensor(f"{name}_u{self._uid}", list(shape), dtype))

    def ps(self, name, shape, dtype=F32):
        self._uid = getattr(self, "_uid", 0) + 1
        return self.cur.enter_context(self.nc.psum_tensor(f"{name}_u{self._uid}", list(shape), dtype))

    def bufs(self, n, name="b"):
        return [Buf(name) for _ in range(n)]

    def barrier(self):
        deps = []
        for e in self.ENGS:
            ops = self.ops[e]
            for op in reversed(ops):
                if not op.dma:
                    deps.append(op)
                    break
            deps += [op for op in ops[self._bar_idx[e]:] if op.dma]
            self._bar_idx[e] = len(ops)
        for e in self.ENGS:
            self._pending[e] = self._pending[e] + deps

    def add(self, eng, fn, reads=(), writes=(), dma=False):
        op = Op(eng, fn, dma)
        deps = {}
        for b in reads:
            w = b.last_w
            if w is not None:
                deps[id(w)] = (w, 0)
        for b in writes:
            w = b.last_w
            if w is not None and id(w) not in deps:
                deps[id(w)] = (w, 1)
            for r in b.readers:
                if id(r) not in deps:
                    deps[id(r)] = (r, 2)
        for d, kind in deps.values():
            if d is op:
                continue
            if not d.dma and not op.dma and d.eng == eng:
                if eng == "pe" or kind == 2:
                    continue
            op.deps.append(d)
            d.signal = True
        if self._pending[eng]:
            for d in self._pending[eng]:
                op.deps.append(d)
                d.signal = True
            self._pending[eng] = []
        for b in reads:
            b.readers.append(op)
        for b in writes:
            b.last_w = op
            b.readers = []
        self.ops[eng].append(op)
        self.nops += 1
        return op

    def emit(self, final_waits=()):
        nc = self.nc
        es = self.es
        esem = {e: es.enter_context(nc.semaphore(f"s_{e}")) for e in self.ENGS if e != "sp"}
        dsem = {e: [es.enter_context(nc.semaphore(f"d_{e}{i}")) for i in range(n)] for e, n in self.NDMA.items()}
        for e in self.ENGS:
            tick = 0
            k = 0
            cnt = [0] * self.NDMA.get(e, 0)
            for op in self.ops[e]:
                if op.dma:
                    n = self.NDMA[e]
                    j = k % n
                    k += 1
                    op.prewait = (dsem[e][j], 16 * cnt[j]) if cnt[j] > 0 else None
                    cnt[j] += 1
                    op.sem = dsem[e][j]
                    op.semval = 16 * cnt[j]
                elif op.signal:
                    tick += 1
                    op.sem = esem[e]
                    op.semval = tick
        block = es.enter_context(nc.Block())
        engfn = {"pe": block.tensor, "act": block.scalar, "dve": block.vector, "pool": block.gpsimd, "sp": block.sync}

        def make(e):
            ops = self.ops[e]

            def body(eng):
                waited = {}

                def flush(need):
                    for key, (sem, val) in need.items():
                        if waited.get(key, 0) >= val:
                            continue
                        waited[key] = val
                        eng.wait_ge(sem, val)

                for op in ops:
                    need = {}
                    for d in op.deps:
                        key = id(d.sem)
                        if key not in need or need[key][1] < d.semval:
                            need[key] = (d.sem, d.semval)
                    if op.prewait is not None:
                        key = id(op.prewait[0])
                        if key not in need or need[key][1] < op.prewait[1]:
                            need[key] = op.prewait
                    flush(need)
                    ins = op.fn(eng)
                    if op.dma:
                        ins.then_inc(op.sem, 16)
                    elif op.signal:
                        ins.then_inc(op.sem, 1)
                if e == "sp":
                    need = {}
                    for d in final_waits:
                        key = id(d.sem)
                        if key not in need or need[key][1] < d.semval:
                            need[key] = (d.sem, d.semval)
                    flush(need)

            return body

        for e in self.ENGS:
            engfn[e](make(e))


def build(stop_after=99, dbg=()):
    nc = bass.Bass("TRN2", target_bir_lowering=False)

    def din(name, shape, dt=F32):
        return nc.dram_tensor(name, list(shape), dt, kind="ExternalInput").ap()

    def dscr(name, shape, dt):
        kind = "ExternalOutput" if name in dbg else "Internal"
        return nc.dram_tensor(name, list(shape), dt, kind=kind).ap()

    x = din("x", [S, D])
    attn_norm = din("attn_norm", [1, D])
    w_in = din("w_in", [D, IN_COLS])
    diff_lambda = din("diff_lambda", [1, 256])
    diff_subln = din("diff_subln", [1, 128])
    gmlp_norm = din("gmlp_norm", [1, 1024])
    gmlp_ws = din("gmlp_ws", [8, 128, 128])
    gmlp_bs = din("gmlp_bs", [8, 128])
    w_a = din("w_branch_a", [1024, D])
    w_b = din("w_branch_b", [1024, D])
    w_out = din("w_out", [D, D])
    ffn_norm = din("ffn_norm", [1, D])
    peer_wq = din("peer_wq", [D, 2048])
    peer_sk = din("peer_subkeys", [2, 128, 128])
    peer_u = din("peer_u", [16384, D])
    peer_v = din("peer_v", [16384, D])
    final_norm = din("final_norm", [1, D])
    c_identb = din("c_identb", [128, 128], BF16)
    c_identf = din("c_identf", [128, 128], F32)
    c_kaug = din("c_kaug", [8, 4, S], BF16)
    c_qaug = din("c_qaug", [8, 4, S], BF16)
    c_bdiag = din("c_bdiag", [128, 8, 128], BF16)
    c_tri = din("c_tri", [128, 128], F32)
    out = nc.dram_tensor("out", [S, D], F32, kind="ExternalOutput").ap()

    OAT = dscr("OAT", [128, 8, S], BF16)
    OBT = dscr("OBT", [128, 8, S], BF16)
    MT = dscr("MT", [128, 16, S], BF16)
    X2 = dscr("X2", [S, D], F32)
    H2T = dscr("H2T", [128, 16, S], BF16)
    GT = dscr("GT", [128, 128, S], BF16)
    b_MT = [Buf() for _ in range(16)]
    b_X2 = [[Buf() for _ in range(4)] for _ in range(NT)]
    b_H2T = [Buf() for _ in range(NT)]
    b_GT = [Buf() for _ in range(NT)]
    UTD = dscr("UTD", [128, 128, 2048], BF16)
    b_UTD = [Buf() for _ in range(128)]

    w_in_v = w_in.rearrange("(c p) n -> p c n", p=128)
    fin = []

    with ExitStack() as es:
        P = Prog(nc, es)
        A = P.add

        def dump(name, ap, shape, dt, bufs):
            if name not in dbg:
                return
            d_ = nc.dram_tensor(name, list(shape), dt, kind="ExternalOutput").ap()
            A("sp", lambda e: e.dma_start(out=d_, in_=ap), reads=bufs, dma=True)

        identb = P.sb("identb", [128, 128], BF16); b_identb = Buf()
        identf = P.sb("identf", [128, 128], F32); b_identf = Buf()
        epsT = P.sb("epsT", [128, 1], F32); b_eps = Buf()
        A("sp", lambda e: e.dma_start(out=identb[:], in_=c_identb), writes=[b_identb], dma=True)
        A("sp", lambda e: e.dma_start(out=identf[:], in_=c_identf), writes=[b_identf], dma=True)
        A("dve", lambda e: e.memset(epsT[:], EPS), writes=[b_eps])

        def rms_stats(src_ap, b_src, ss, b_ss, rstd, b_rstd, junk, b_junk, width):
            A("act", lambda e: e.activation(out=junk, in_=src_ap, func=AF.Square, scale=float(width) ** -0.5, accum_out=ss),
              reads=(b_src if isinstance(b_src, list) else [b_src]), writes=[b_junk, b_ss])
            A("act", lambda e: e.activation(out=ss, in_=ss, func=AF.Ln, bias=epsT[:], scale=1.0),
              reads=[b_ss, b_eps], writes=[b_ss])
            A("act", lambda e: e.activation(out=rstd, in_=ss, func=AF.Exp, scale=-0.5), reads=[b_ss], writes=[b_rstd])

        with ExitStack() as sc_h:
            P.cur = sc_h
            hT = P.sb("hT", [128, 16, S], BF16); b_hT = P.bufs(NT)

            with ExitStack() as ph:
                P.cur = ph
                gA = P.sb("gA", [128, D], F32); b_gA = Buf()
                xt = [P.sb(f"xt{i}", [128, D], F32) for i in range(2)]; b_xt = P.bufs(2)
                junk = P.sb("junk1", [128, D], BF16); b_junk = Buf()
                ss = [P.sb(f"ss{i}", [128, 1], F32) for i in range(2)]; b_ss = P.bufs(2)
                rstd = [P.sb(f"rstd{i}", [128, 1], F32) for i in range(2)]; b_rstd = P.bufs(2)
                hb = [P.sb(f"hb{i}", [128, D], BF16) for i in range(2)]; b_hb = P.bufs(2)
                pT = [P.ps(f"pT{i}", [128, D], BF16) for i in range(2)]; b_pT = P.bufs(2)
                A("sp", lambda e: e.dma_start(out=gA[:], in_=attn_norm.partition_broadcast(128)), writes=[b_gA], dma=True)
                for tt in range(NT):
                    b = tt % 2
                    A("sp", lambda e, tt=tt, b=b: e.dma_start(out=xt[b][:], in_=x[tt * 128:(tt + 1) * 128, :]), writes=[b_xt[b]], dma=True)
                    rms_stats(xt[b][:], b_xt[b], ss[b][:], b_ss[b], rstd[b][:], b_rstd[b], junk[:], b_junk, D)
                    A("dve", lambda e, b=b: e.scalar_tensor_tensor(out=hb[b][:], in0=xt[b][:], scalar=rstd[b][:, 0:1], in1=gA[:], op0=ALU.mult, op1=ALU.mult),
                      reads=[b_xt[b], b_rstd[b], b_gA], writes=[b_hb[b]])
                    for c in range(16):
                        A("pe", lambda e, b=b, c=c: e.transpose(out=pT[b][:, c * 128:(c + 1) * 128], in_=hb[b][:, c * 128:(c + 1) * 128], identity=identb[:]),
                          reads=[b_hb[b], b_identb], writes=[b_pT[b]])
                    A("act", lambda e, b=b, tt=tt: e.activation(out=hT[:, :, tt * 128:(tt + 1) * 128], in_=pT[b][:].rearrange("p (c t) -> p c t", c=16), func=AF.Copy),
                      reads=[b_pT[b]], writes=[b_hT[tt]])
            P.barrier()

            with ExitStack() as sc_o:
                P.cur = sc_o
                b_OATh = [Buf() for _ in range(8)]
                _boa = [[Buf() for _ in range(NT)] for _ in range(2)]
                b_oaT = [_boa[h_ % 2] for h_ in range(8)]
                b_obT = [[Buf() for _ in range(NT)] for _ in range(4)]

                if stop_after >= 2:
                  with ExitStack() as ph:
                    P.cur = ph
                    oaTa = [P.sb(f"oaTa{i}", [128, S], BF16) for i in range(2)]
                    wq = [P.sb(f"wq{i}", [128, 16, 128], BF16) for i in range(2)]; b_wq = P.bufs(2)
                    wk = [P.sb(f"wk{i}", [128, 16, 128], BF16) for i in range(2)]; b_wk = P.bufs(2)
                    wv4 = P.sb("wv4", [128, 16, 512], BF16); b_wv4 = Buf()
                    qT = [P.sb(f"qT{i}", [128, S], BF16) for i in range(2)]; b_qT = [P.bufs(4) for _ in range(2)]
                    kT = [P.sb(f"kT{i}", [128, S], BF16) for i in range(2)]; b_kT = [P.bufs(4) for _ in range(2)]
                    Va4 = P.sb("Va4", [128, NT, 4, 130], BF16); b_Va4 = P.bufs(NT)
                    b_Vone = Buf()
                    kaug = [P.sb("kaug0", [4, S], BF16)] * 2; b_kaug = [Buf()] * 2
                    qaug = [P.sb("qaug0", [4, S], BF16)] * 2; b_qaug = [Buf()] * 2
                    bdiag = P.sb("bdiag", [128, 8, 128], BF16); b_bdiag = Buf()
                    PT = [P.sb(f"PT{i}", [128, NT * 128], BF16) for i in range(2)]; b_PT = [P.bufs(4) for _ in range(2)]
                    om = [P.sb(f"om{i}", [128, 128], F32) for i in range(2)]; b_om = P.bufs(2)
                    rz = [P.sb(f"rz{i}", [128, 1], F32) for i in range(2)]; b_rz = P.bufs(2)
                    ot = P.sb("ot", [128, 128], F32); b_ot = Buf()
                    oab = [P.sb(f"oab{i}", [128, 128], BF16) for i in range(2)]; b_oab = P.bufs(2)
                    junk2 = P.sb("junk2", [128, 128], BF16); b_junk2 = Buf()
                    ss2 = P.sb("ss2", [128, 1], F32); b_ss2 = Buf()
                    rstd2 = P.sb("rstd2", [128, 1], F32); b_rstd2 = Buf()
                    gsub = P.sb("gsub", [128, 128], F32); b_gsub = Buf()
                    lamt = P.sb("lamt", [128, 256], F32); b_lamt = Buf()
                    lprod = P.sb("lprod", [128, 2, 64], F32); b_lprod = Buf()
                    lsum = P.sb("lsum", [128, 2], F32); b_lsum = Buf()
                    neglam = P.sb("neglam", [128, 1], F32); b_neglam = Buf()
                    pQ = [P.ps(f"pQ{i}", [128, 512]) for i in range(2)]; b_pQ = P.bufs(2)
                    pS = [P.ps(f"pS{i}", [128, 512]) for i in range(2)]; b_pS = P.bufs(2)
                    pO = [P.ps(f"pO{i}", [128, 512]) for i in range(2)]; b_pO = P.bufs(2)
                    pTr = P.ps("pTr", [128, 128], BF16); b_pTr = Buf()

                    A("sp", lambda e: e.dma_start(out=bdiag[:], in_=c_bdiag), writes=[b_bdiag], dma=True)
                    A("sp", lambda e: e.dma_start(out=gsub[:], in_=diff_subln.partition_broadcast(128)), writes=[b_gsub], dma=True)
                    A("dve", lambda e: e.tensor_scalar(out=gsub[:], in0=gsub[:], scalar1=1.0 - LAM_INIT, scalar2=None, op0=ALU.mult),
                      reads=[b_gsub], writes=[b_gsub])
                    A("sp", lambda e: e.dma_start(out=lamt[:], in_=diff_lambda.partition_broadcast(128)), writes=[b_lamt], dma=True)
                    A("dve", lambda e: e.tensor_tensor(out=lprod[:, 0, :], in0=lamt[:, 0:64], in1=lamt[:, 64:128], op=ALU.mult), reads=[b_lamt], writes=[b_lprod])
                    A("dve", lambda e: e.tensor_tensor(out=lprod[:, 1, :], in0=lamt[:, 128:192], in1=lamt[:, 192:256], op=ALU.mult), reads=[b_lamt, b_lprod], writes=[b_lprod])
                    A("dve", lambda e: e.reduce_sum(out=lsum[:], in_=lprod[:], axis=AX.X), reads=[b_lprod], writes=[b_lsum])
                    A("act", lambda e: e.activation(out=lsum[:], in_=lsum[:], func=AF.Exp), reads=[b_lsum], writes=[b_lsum])
                    A("dve", lambda e: e.tensor_tensor(out=neglam[:], in0=lsum[:, 0:1], in1=lsum[:, 1:2], op=ALU.subtract), reads=[b_lsum], writes=[b_neglam])
                    A("dve", lambda e: e.tensor_scalar(out=neglam[:], in0=neglam[:], scalar1=LAM_INIT, scalar2=-1.0, op0=ALU.add, op1=ALU.mult),
                      reads=[b_neglam], writes=[b_neglam])
                    A("dve", lambda e: e.memset(Va4[:, :, :, 128:130], 1.0), writes=[b_Vone])

                    ev = 0
                    for h in range(8):
                        b = h % 2
                        A("pool", lambda e, b=b, h=h: e.dma_start(out=wq[b][:], in_=w_in_v[:, :, h * 128:(h + 1) * 128]), writes=[b_wq[b]], dma=True)
                        A("pool", lambda e, b=b, h=h: e.dma_start(out=wk[b][:], in_=w_in_v[:, :, 1024 + h * 128:1024 + (h + 1) * 128]), writes=[b_wk[b]], dma=True)
                        if h % 4 == 0:
                            A("pool", lambda e, h=h: e.dma_start(out=wv4[:], in_=w_in_v[:, :, 2048 + h * 128:2048 + (h + 4) * 128]), writes=[b_wv4], dma=True)
                        A("sp", lambda e, b=b, h=h: e.dma_start(out=kaug[b][:], in_=c_kaug[h]), writes=[b_kaug[b]], dma=True)
                        A("sp", lambda e, b=b, h=h: e.dma_start(out=qaug[b][:], in_=c_qaug[h]), writes=[b_qaug[b]], dma=True)
                        for (w_, bw, dst, bdst) in ((wq, b_wq, qT, b_qT), (wk, b_wk, kT, b_kT)):
                            for g in range(4):
                                pb = ev % 2; ev += 1
                                for c in range(16):
                                    A("pe", lambda e, pb=pb, c=c, g=g, w_=w_, b=b: e.matmul(pQ[pb][:, :], lhsT=w_[b][:, c, :], rhs=hT[:, c, g * 512:(g + 1) * 512], start=(c == 0), stop=(c == 15)),
                                      reads=[bw[b]] + b_hT[g * 4:(g + 1) * 4], writes=[b_pQ[pb]])
                                if pb == 0:
                                    A("act", lambda e, pb=pb, g=g, dst=dst, b=b: e.activation(out=dst[b][:, g * 512:(g + 1) * 512], in_=pQ[pb][:, :], func=AF.Copy),
                                      reads=[b_pQ[pb]], writes=[bdst[b][g]])
                                else:
                                    A("dve", lambda e, pb=pb, g=g, dst=dst, b=b: e.tensor_copy(out=dst[b][:, g * 512:(g + 1) * 512], in_=pQ[pb][:, :]),
                                      reads=[b_pQ[pb]], writes=[bdst[b][g]])
                        if h % 4 == 0:
                            for tt in range(NT):
                                pb = ev % 2; ev += 1
                                for c in range(16):
                                    A("pe", lambda e, pb=pb, c=c, tt=tt: e.matmul(pQ[pb][:, :], lhsT=hT[:, c, tt * 128:(tt + 1) * 128], rhs=wv4[:, c, :], start=(c == 0), stop=(c == 15)),
                                      reads=[b_wv4, b_hT[tt]], writes=[b_pQ[pb]])
                                if pb == 0:
                                    A("act", lambda e, pb=pb, tt=tt: e.activation(out=Va4[:, tt, :, 0:128], in_=pQ[pb][:].rearrange("p (a n) -> p a n", a=4), func=AF.Copy),
                                      reads=[b_pQ[pb]], writes=[b_Va4[tt]])
                                else:
                                    A("dve", lambda e, pb=pb, tt=tt: e.tensor_copy(out=Va4[:, tt, :, 0:128], in_=pQ[pb][:].rearrange("p (a n) -> p a n", a=4)),
                                      reads=[b_pQ[pb]], writes=[b_Va4[tt]])
                        if h == 0:
                            dump("d_qT", qT[b][:], [128, S], BF16, b_qT[b])
                            dump("d_kT", kT[b][:], [128, S], BF16, b_kT[b])
                            dump("d_neglam", neglam[:], [128, 1], F32, [b_neglam])
                        def stQK(j, m, h=h, b=b):
                            pt = m
                            nblk = j + 1
                            for gi in range((nblk + 3) // 4):
                                sb_ = (j * 2 + m + gi) % 2
                                i0 = gi * 4
                                i1 = min(nblk, i0 + 4)
                                for i in range(i0, i1):
                                    col = (i - i0) * 128
                                    A("pe", lambda e, sb_=sb_, col=col, m=m, i=i, j=j, b=b: e.matmul(
                                        pS[sb_][:, col:col + 128], lhsT=kT[b][m * 64:(m + 1) * 64, i * 128:(i + 1) * 128],
                                        rhs=qT[b][m * 64:(m + 1) * 64, j * 128:(j + 1) * 128], start=True, stop=False),
                                      reads=[b_kT[b][i // 4], b_qT[b][j // 4]], writes=[b_pS[sb_]])
                                    if i < j:
                                        A("pe", lambda e, sb_=sb_, col=col, i=i, j=j, b=b: e.matmul(
                                            pS[sb_][:, col:col + 128], lhsT=kaug[b][0:4, i * 128:(i + 1) * 128],
                                            rhs=qaug[b][0:4, j * 128:(j + 1) * 128], start=False, stop=True),
                                          reads=[b_kaug[b], b_qaug[b]], writes=[b_pS[sb_]])
                                    else:
                                        A("pe", lambda e, sb_=sb_, col=col, h=h: e.matmul(
                                            pS[sb_][:, col:col + 128], lhsT=identb[:], rhs=bdiag[:, h, :], start=False, stop=True),
                                          reads=[b_identb, b_bdiag], writes=[b_pS[sb_]])
                                ncol = (i1 - i0) * 128
                                A("act", lambda e, sb_=sb_, pt=pt, i0=i0, ncol=ncol: e.activation(
                                    out=PT[pt][:, i0 * 128:i0 * 128 + ncol], in_=pS[sb_][:, 0:ncol], func=AF.Exp, scale=0.125),
                                  reads=[b_pS[sb_]], writes=[b_PT[pt][gi]])

                        def stPV(j, m, h=h, b=b):
                            pt = m
                            ob_ = m
                            for i in range(j + 1):
                                A("pe", lambda e, ob_=ob_, pt=pt, i=i, j=j, b=b: e.matmul(
                                    pO[ob_][:, 0:129], lhsT=PT[pt][:, i * 128:(i + 1) * 128], rhs=Va4[:, i, h % 4, 0:129], start=(i == 0), stop=(i == j)),
                                  reads=[b_PT[pt][i // 4], b_Va4[i], b_Vone], writes=[b_pO[ob_]])
                            A("dve", lambda e, ob_=ob_, m=m: e.reciprocal(out=rz[m][:], in_=pO[ob_][:, 128:129]), reads=[b_pO[ob_]], writes=[b_rz[m]])
                            A("dve", lambda e, ob_=ob_, m=m: e.tensor_scalar(out=om[m][:], in0=pO[ob_][:, 0:128], scalar1=rz[m][:, 0:1], scalar2=None, op0=ALU.mult),
                              reads=[b_pO[ob_], b_rz[m]], writes=[b_om[m]])
                            if m == 1:
                                A("dve", lambda e: e.scalar_tensor_tensor(out=ot[:], in0=om[1][:], scalar=neglam[:, 0:1], in1=om[0][:], op0=ALU.mult, op1=ALU.add),
                                  reads=[b_om[0], b_om[1], b_neglam], writes=[b_ot])
                                rms_stats(ot[:], b_ot, ss2[:], b_ss2, rstd2[:], b_rstd2, junk2[:], b_junk2, 128)
                                ab = j % 2
                                A("dve", lambda e, ab=ab: e.scalar_tensor_tensor(out=oab[ab][:], in0=ot[:], scalar=rstd2[:, 0:1], in1=gsub[:], op0=ALU.mult, op1=ALU.mult),
                                  reads=[b_ot, b_rstd2, b_gsub], writes=[b_oab[ab]])
                                def _tr(ab=ab, h=h, j=j):
                                    A("pe", lambda e, ab=ab: e.transpose(out=pTr[:, :], in_=oab[ab][:], identity=identb[:]), reads=[b_oab[ab], b_identb], writes=[b_pTr])
                                    A("act", lambda e, h=h, j=j: e.activation(out=oaTa[h % 2][:, j * 128:(j + 1) * 128], in_=pTr[:, :], func=AF.Copy),
                                      reads=[b_pTr], writes=[b_oaT[h][j]])
                                pend_tr.append(_tr)

                        units = [(j, m) for j in range(NT) for m in range(2)]
                        pend_tr = []
                        stQK(*units[0])
                        for ui, (j, m) in enumerate(units):
                            if ui + 1 < len(units):
                                stQK(*units[ui + 1])
                            if m == 1 and pend_tr:
                                pend_tr.pop(0)()
                            stPV(j, m)
                        while pend_tr:
                            pend_tr.pop(0)()
                        A("sp", lambda e, h=h: e.dma_start(out=OAT[:, h, :], in_=oaTa[h % 2][:]), reads=b_oaT[h], writes=[b_OATh[h]], dma=True)
                  P.barrier()
                P.cur = sc_o
                obT = P.sb("obT", [128, 8, S], BF16)

                if stop_after >= 3:
                  with ExitStack() as ph:
                    P.cur = ph
                    Wz = [P.sb(f"Wz{i}", [128, 16, 256], BF16) for i in range(2)]; b_Wz = P.bufs(2)
                    vg = P.sb("vg", [128, NT, 1024], BF16); b_vg = [P.bufs(4) for _ in range(NT)]
                    gn = P.sb("gn", [128, 1024], F32); b_gn = Buf()
                    wsf = P.sb("wsf", [128, 8, 128], F32); b_wsf = Buf()
                    tri = P.sb("tri", [128, 128], F32); b_tri = Buf()
                    wsT = P.sb("wsT", [128, 8, 128], BF16); b_wsT = Buf()
                    bsT = P.sb("bsT", [128, 8], F32); b_bsT = Buf()
                    ssv = P.sb("ssv", [128, NT, 4], F32); b_ssv = [P.bufs(4) for _ in range(NT)]
                    sv1 = P.sb("sv1", [128, 1], F32); b_sv1 = Buf()
                    rsv = P.sb("rsv", [128, 1], F32); b_rsv = Buf()
                    junk3 = P.sb("junk3", [128, 256], BF16); b_junk3 = Buf()
                    ug = [P.sb(f"ug{i}", [128, 256], F32) for i in range(2)]; b_ug = P.bufs(2)
                    tmp = [P.sb(f"tmp{i}", [128, 256], F32) for i in range(2)]; b_tmp = P.bufs(2)
                    obb = [P.sb(f"obb{i}", [128, 256], BF16) for i in range(2)]; b_obb = P.bufs(2)
                    pZ = [P.ps(f"pZ{i}", [128, 512]) for i in range(2)]; b_pZ = P.bufs(2)
                    pSV = [P.ps(f"pSV{i}", [128, 512]) for i in range(2)]; b_pSV = P.bufs(2)
                    pW = P.ps("pW", [128, 512]); b_pW = Buf()
                    pTb = P.ps("pTb", [128, 256], BF16); b_pTb = Buf()

                    A("sp", lambda e: e.dma_start(out=gn[:], in_=gmlp_norm.partition_broadcast(128)), writes=[b_gn], dma=True)
                    A("sp", lambda e: e.dma_start(out=wsf[:], in_=gmlp_ws.rearrange("g t s -> t g s")), writes=[b_wsf], dma=True)
                    A("sp", lambda e: e.dma_start(out=tri[:], in_=c_tri), writes=[b_tri], dma=True)
                    A("sp", lambda e: e.dma_start(out=bsT[:], in_=gmlp_bs.rearrange("g t -> t g"), allow_slow_non_contiguous=True), writes=[b_bsT], dma=True)
                    A("dve", lambda e: e.tensor_tensor(out=wsf[:], in0=wsf[:], in1=tri[:].unsqueeze(1).broadcast_to([128, 8, 128]), op=ALU.mult),
                      reads=[b_wsf, b_tri], writes=[b_wsf])
                    for g0 in range(0, 8, 4):
                        for gg in range(4):
                            A("pe", lambda e, g0=g0, gg=gg: e.transpose(out=pW[:, gg * 128:(gg + 1) * 128], in_=wsf[:, g0 + gg, :], identity=identf[:]),
                              reads=[b_wsf, b_identf], writes=[b_pW])
                        A("dve", lambda e, g0=g0: e.tensor_copy(out=wsT[:, g0:g0 + 4, :], in_=pW[:].rearrange("p (g t) -> p g t", g=4)), reads=[b_pW], writes=[b_wsT])

                    zc = 0
                    for vb in range(4):
                        wb_ = zc % 2; zc += 1
                        c0 = 3072 + 1024 + vb * 256
                        A("pool", lambda e, wb_=wb_, c0=c0: e.dma_start(out=Wz[wb_][:], in_=w_in_v[:, :, c0:c0 + 256]), writes=[b_Wz[wb_]], dma=True)
                        for tt in range(NT):
                            zb = tt % 2
                            for c in range(16):
                                A("pe", lambda e, zb=zb, c=c, tt=tt, wb_=wb_: e.matmul(pZ[zb][:, 0:256], lhsT=hT[:, c, tt * 128:(tt + 1) * 128], rhs=Wz[wb_][:, c, :], start=(c == 0), stop=(c == 15)),
                                  reads=[b_Wz[wb_], b_hT[tt]], writes=[b_pZ[zb]])
                            A("act", lambda e, zb=zb, tt=tt, vb=vb: e.activation(out=vg[:, tt, vb * 256:(vb + 1) * 256], in_=pZ[zb][:, 0:256], func=AF.Gelu),
                              reads=[b_pZ[zb]], writes=[b_vg[tt][vb]])
                            A("act", lambda e, tt=tt, vb=vb: e.activation(out=junk3[:], in_=vg[:, tt, vb * 256:(vb + 1) * 256], func=AF.Square, scale=1.0 / 32.0, accum_out=ssv[:, tt, vb:vb + 1]),
                              reads=[b_vg[tt][vb]], writes=[b_junk3, b_ssv[tt][vb]])
                    for tt in range(NT):
                        A("dve", lambda e, tt=tt: e.reduce_sum(out=sv1[:], in_=ssv[:, tt, :], axis=AX.X), reads=b_ssv[tt], writes=[b_sv1])
                        A("act", lambda e: e.activation(out=sv1[:], in_=sv1[:], func=AF.Sqrt, bias=epsT[:], scale=1.0), reads=[b_sv1, b_eps], writes=[b_sv1])
                        A("dve", lambda e: e.reciprocal(out=rsv[:], in_=sv1[:]), reads=[b_sv1], writes=[b_rsv])
                        A("dve", lambda e, tt=tt: e.scalar_tensor_tensor(out=vg[:, tt, :], in0=vg[:, tt, :], scalar=rsv[:, 0:1], in1=gn[:], op0=ALU.mult, op1=ALU.mult),
                          reads=b_vg[tt] + [b_rsv, b_gn], writes=b_vg[tt])
                    pend3 = []
                    for ub in range(4):
                        wb_ = zc % 2; zc += 1
                        c0 = 3072 + ub * 256
                        A("pool", lambda e, wb_=wb_, c0=c0: e.dma_start(out=Wz[wb_][:], in_=w_in_v[:, :, c0:c0 + 256]), writes=[b_Wz[wb_]], dma=True)
                        for tt in range(NT):
                            zb = tt % 2
                            for c in range(16):
                                A("pe", lambda e, zb=zb, c=c, tt=tt, wb_=wb_: e.matmul(pZ[zb][:, 0:256], lhsT=hT[:, c, tt * 128:(tt + 1) * 128], rhs=Wz[wb_][:, c, :], start=(c == 0), stop=(c == 15)),
                                  reads=[b_Wz[wb_], b_hT[tt]], writes=[b_pZ[zb]])
                            for gg in range(2):
                                g = ub * 2 + gg
                                A("pe", lambda e, zb=zb, gg=gg, g=g, tt=tt: e.matmul(pSV[zb][:, gg * 128:(gg + 1) * 128], lhsT=wsT[:, g, :], rhs=vg[:, tt, g * 128:(g + 1) * 128], start=True, stop=True),
                                  reads=[b_wsT, b_vg[tt][g // 2]], writes=[b_pSV[zb]])
                            while pend3:
                                pend3.pop(0)()
                            A("act", lambda e, zb=zb: e.activation(out=ug[zb][:], in_=pZ[zb][:, 0:256], func=AF.Gelu), reads=[b_pZ[zb]], writes=[b_ug[zb]])
                            A("dve", lambda e, zb=zb, ub=ub: e.tensor_tensor(out=tmp[zb][:].rearrange("p (g c) -> p g c", g=2), in0=pSV[zb][:, 0:256].rearrange("p (g c) -> p g c", g=2),
                                                                             in1=bsT[:, ub * 2:(ub + 1) * 2].unsqueeze(2).broadcast_to([128, 2, 128]), op=ALU.add),
                              reads=[b_pSV[zb], b_bsT], writes=[b_tmp[zb]])
                            A("dve", lambda e, zb=zb: e.tensor_tensor(out=obb[zb][:], in0=tmp[zb][:], in1=ug[zb][:], op=ALU.mult), reads=[b_tmp[zb], b_ug[zb]], writes=[b_obb[zb]])
                            def _tr3(zb=zb, ub=ub, tt=tt):
                                for gg in range(2):
                                    A("pe", lambda e, zb=zb, gg=gg: e.transpose(out=pTb[:, gg * 128:(gg + 1) * 128], in_=obb[zb][:, gg * 128:(gg + 1) * 128], identity=identb[:]),
                                      reads=[b_obb[zb], b_identb], writes=[b_pTb])
                                A("act", lambda e, ub=ub, tt=tt: e.activation(out=obT[:, ub * 2:(ub + 1) * 2, tt * 128:(tt + 1) * 128], in_=pTb[:].rearrange("p (g t) -> p g t", g=2), func=AF.Copy),
                                  reads=[b_pTb], writes=[b_obT[ub][tt]])
                            pend3.append(_tr3)
                    while pend3:
                        pend3.pop(0)()
                    if "OBT" in dbg:
                        for g in range(8):
                            A("sp", lambda e, g=g: e.dma_start(out=OBT[:, g, :], in_=obT[:, g, :]), reads=b_obT[g // 2], dma=True)
                  P.barrier()
                P.cur = sc_o
                oaT = P.sb("oaT2", [128, 8, S], BF16); b_oaT2 = [Buf() for _ in range(8)]

                if stop_after >= 4:
                  with ExitStack() as ph:
                    P.cur = ph
                    Wa = [P.sb(f"Wa{i}", [128, 8, 128], BF16) for i in range(2)]; b_Wa = P.bufs(2)
                    Wb = [P.sb(f"Wb{i}", [128, 8, 128], BF16) for i in range(2)]; b_Wb = P.bufs(2)
                    Wga = [P.sb(f"Wga{i}", [128, 16, 128], BF16) for i in range(2)]; b_Wga = P.bufs(2)
                    Wgb = [P.sb(f"Wgb{i}", [128, 16, 128], BF16) for i in range(2)]; b_Wgb = P.bufs(2)
                    sga = [P.sb(f"sga{i}", [128, 512], F32) for i in range(2)]; b_sga = P.bufs(2)
                    sgb = [P.sb(f"sgb{i}", [128, 512], F32) for i in range(2)]; b_sgb = P.bufs(2)
                    mst = [P.sb(f"mst{i}", [128, S], BF16) for i in range(2)]; b_mst = [P.bufs(4) for _ in range(2)]
                    pA = [P.ps(f"pA{i}", [128, 512]) for i in range(2)]; b_pA = P.bufs(2)
                    pB = [P.ps(f"pB{i}", [128, 512]) for i in range(2)]; b_pB = P.bufs(2)
                    pGA = [P.ps(f"pGA{i}", [128, 512]) for i in range(2)]; b_pGA = P.bufs(2)
                    pGB = [P.ps(f"pGB{i}", [128, 512]) for i in range(2)]; b_pGB = P.bufs(2)
                    wa_v = w_a.rearrange("(c p) n -> p c n", p=128)
                    wb_v = w_b.rearrange("(c p) n -> p c n", p=128)
                    for h in range(8):
                        A("sp", lambda e, h=h: e.dma_start(out=oaT[:, h, :], in_=OAT[:, h, :]), reads=[b_OATh[h]], writes=[b_oaT2[h]], dma=True)
                    it = 0
                    for n in range(16):
                        wb_ = n % 2
                        A("pool", lambda e, wb_=wb_, n=n: e.dma_start(out=Wa[wb_][:], in_=wa_v[:, :, n * 128:(n + 1) * 128]), writes=[b_Wa[wb_]], dma=True)
                        A("pool", lambda e, wb_=wb_, n=n: e.dma_start(out=Wb[wb_][:], in_=wb_v[:, :, n * 128:(n + 1) * 128]), writes=[b_Wb[wb_]], dma=True)
                        A("pool", lambda e, wb_=wb_, n=n: e.dma_start(out=Wga[wb_][:], in_=w_in_v[:, :, 5120 + n * 128:5120 + (n + 1) * 128]), writes=[b_Wga[wb_]], dma=True)
                        A("pool", lambda e, wb_=wb_, n=n: e.dma_start(out=Wgb[wb_][:], in_=w_in_v[:, :, 7168 + n * 128:7168 + (n + 1) * 128]), writes=[b_Wgb[wb_]], dma=True)
                        for g in range(4):
                            pb = it % 2; it += 1
                            gs = slice(g * 512, (g + 1) * 512)
                            for c in range(8):
                                A("pe", lambda e, pb=pb, c=c, gs=gs, wb_=wb_: e.matmul(pA[pb][:, :], lhsT=Wa[wb_][:, c, :], rhs=oaT[:, c, gs], start=(c == 0), stop=(c == 7)),
                                  reads=[b_Wa[wb_], b_oaT2[c]], writes=[b_pA[pb]])
                            for c in range(8):
                                A("pe", lambda e, pb=pb, c=c, gs=gs, wb_=wb_: e.matmul(pB[pb][:, :], lhsT=Wb[wb_][:, c, :], rhs=obT[:, c, gs], start=(c == 0), stop=(c == 7)),
                                  reads=[b_Wb[wb_]], writes=[b_pB[pb]])
                            for c in range(16):
                                A("pe", lambda e, pb=pb, c=c, gs=gs, wb_=wb_: e.matmul(pGA[pb][:, :], lhsT=Wga[wb_][:, c, :], rhs=hT[:, c, gs], start=(c == 0), stop=(c == 15)),
                                  reads=[b_Wga[wb_]], writes=[b_pGA[pb]])
                            for c in range(16):
                                A("pe", lambda e, pb=pb, c=c, gs=gs, wb_=wb_: e.matmul(pGB[pb][:, :], lhsT=Wgb[wb_][:, c, :], rhs=hT[:, c, gs], start=(c == 0), stop=(c == 15)),
                                  reads=[b_Wgb[wb_]], writes=[b_pGB[pb]])
                            A("act", lambda e, pb=pb: e.activation(out=sga[pb][:], in_=pGA[pb][:, :], func=AF.Sigmoid), reads=[b_pGA[pb]], writes=[b_sga[pb]])
                            A("act", lambda e, pb=pb: e.activation(out=sgb[pb][:], in_=pGB[pb][:, :], func=AF.Sigmoid), reads=[b_pGB[pb]], writes=[b_sgb[pb]])
                            A("dve", lambda e, pb=pb: e.tensor_tensor(out=sga[pb][:], in0=pA[pb][:, :], in1=sga[pb][:], op=ALU.mult), reads=[b_pA[pb], b_sga[pb]], writes=[b_sga[pb]])
                            A("dve", lambda e, pb=pb: e.tensor_tensor(out=sgb[pb][:], in0=pB[pb][:, :], in1=sgb[pb][:], op=ALU.mult), reads=[b_pB[pb], b_sgb[pb]], writes=[b_sgb[pb]])
                            A("pool", lambda e, pb=pb, wb_=wb_, gs=gs: e.tensor_tensor(out=mst[wb_][:, gs], in0=sga[pb][:], in1=sgb[pb][:], op=ALU.add),
                              reads=[b_sga[pb], b_sgb[pb]], writes=[b_mst[wb_][g]])
                        A("sp", lambda e, wb_=wb_, n=n: e.dma_start(out=MT[:, n, :], in_=mst[wb_][:]), reads=b_mst[wb_], writes=[b_MT[n]], dma=True)
                  P.barrier()
        P.cur = es
        P.barrier()

        if stop_after >= 5:
          with ExitStack() as ph:
            P.cur = ph
            mT = P.sb("mT", [128, 16, S], BF16); b_mT = P.bufs(16)
            Wo = [P.sb(f"Wo{i}", [128, 16, 512], BF16) for i in range(2)]; b_Wo = P.bufs(2)
            xp = [P.sb(f"xp{i}", [128, 512], F32) for i in range(2)]; b_xp = P.bufs(2)
            x2p = [P.sb(f"x2p{i}", [128, 512], F32) for i in range(2)]; b_x2p = P.bufs(2)
            pX = [P.ps(f"pX{i}", [128, 512]) for i in range(2)]; b_pX = P.bufs(2)
            wo_v = w_out.rearrange("(c p) n -> p c n", p=128)
            for c in range(16):
                A("sp", lambda e, c=c: e.dma_start(out=mT[:, c, :], in_=MT[:, c, :]), reads=[b_MT[c]], writes=[b_mT[c]], dma=True)
            it = 0
            for blk in range(4):
                wb_ = blk % 2
                A("pool", lambda e, wb_=wb_, blk=blk: e.dma_start(out=Wo[wb_][:], in_=wo_v[:, :, blk * 512:(blk + 1) * 512]), writes=[b_Wo[wb_]], dma=True)
                for tt in range(NT):
                    pb = it % 2; it += 1
                    A("sp", lambda e, pb=pb, tt=tt, blk=blk: e.dma_start(out=xp[pb][:], in_=x[tt * 128:(tt + 1) * 128, blk * 512:(blk + 1) * 512]), writes=[b_xp[pb]], dma=True)
                    for c in range(16):
                        A("pe", lambda e, pb=pb, c=c, tt=tt, wb_=wb_: e.matmul(pX[pb][:, :], lhsT=mT[:, c, tt * 128:(tt + 1) * 128], rhs=Wo[wb_][:, c, :], start=(c == 0), stop=(c == 15)),
                          reads=[b_Wo[wb_], b_mT[c]], writes=[b_pX[pb]])
                    A("dve", lambda e, pb=pb: e.tensor_tensor(out=x2p[pb][:], in0=pX[pb][:, :], in1=xp[pb][:], op=ALU.add), reads=[b_pX[pb], b_xp[pb]], writes=[b_x2p[pb]])
                    A("sp", lambda e, pb=pb, tt=tt, blk=blk: e.dma_start(out=X2[tt * 128:(tt + 1) * 128, blk * 512:(blk + 1) * 512], in_=x2p[pb][:]),
                      reads=[b_x2p[pb]], writes=[b_X2[tt][blk]], dma=True)
          P.barrier()
          with ExitStack() as ph:
            P.cur = ph
            gF = P.sb("gF", [128, D], F32); b_gF = Buf()
            xt_5 = [P.sb(f"x2t{i}", [128, D], F32) for i in range(2)]; b_xt = P.bufs(2)
            junk_5 = P.sb("junk5", [128, D], BF16); b_junk = Buf()
            ss_5 = [P.sb(f"ss5{i}", [128, 1], F32) for i in range(2)]; b_ss = P.bufs(2)
            rstd_5 = [P.sb(f"rstd5{i}", [128, 1], F32) for i in range(2)]; b_rstd = P.bufs(2)
            hb_5 = [P.sb(f"hb5{i}", [128, D], BF16) for i in range(2)]; b_hb = P.bufs(2)
            hs = [P.sb(f"hs5{i}", [128, 16, 128], BF16) for i in range(2)]; b_hs = P.bufs(2)
            pT_5 = [P.ps(f"pT5{i}", [128, D], BF16) for i in range(2)]; b_pT = P.bufs(2)
            A("sp", lambda e: e.dma_start(out=gF[:], in_=ffn_norm.partition_broadcast(128)), writes=[b_gF], dma=True)
            for tt in range(NT):
                b = tt % 2
                A("sp", lambda e, tt=tt, b=b: e.dma_start(out=xt_5[b][:], in_=X2[tt * 128:(tt + 1) * 128, :]), reads=b_X2[tt], writes=[b_xt[b]], dma=True)
                rms_stats(xt_5[b][:], b_xt[b], ss_5[b][:], b_ss[b], rstd_5[b][:], b_rstd[b], junk_5[:], b_junk, D)
                A("dve", lambda e, b=b: e.scalar_tensor_tensor(out=hb_5[b][:], in0=xt_5[b][:], scalar=rstd_5[b][:, 0:1], in1=gF[:], op0=ALU.mult, op1=ALU.mult),
                  reads=[b_xt[b], b_rstd[b], b_gF], writes=[b_hb[b]])
                for c in range(16):
                    A("pe", lambda e, b=b, c=c: e.transpose(out=pT_5[b][:, c * 128:(c + 1) * 128], in_=hb_5[b][:, c * 128:(c + 1) * 128], identity=identb[:]),
                      reads=[b_hb[b], b_identb], writes=[b_pT[b]])
                A("act", lambda e, b=b: e.activation(out=hs[b][:], in_=pT_5[b][:].rearrange("p (c t) -> p c t", c=16), func=AF.Copy), reads=[b_pT[b]], writes=[b_hs[b]])
                A("sp", lambda e, b=b, tt=tt: e.dma_start(out=H2T[:, :, tt * 128:(tt + 1) * 128], in_=hs[b][:]), reads=[b_hs[b]], writes=[b_H2T[tt]], dma=True)
          P.barrier()

        if stop_after >= 6:
          with ExitStack() as sc_q:
            P.cur = sc_q
            qpT = P.sb("qpT", [128, 16, S], BF16); b_qpT = [P.bufs(4) for _ in range(16)]
            with ExitStack() as ph:
                P.cur = ph
                h2T = P.sb("h2T", [128, 16, S], BF16); b_h2T = P.bufs(16)
                Wp = [P.sb(f"Wp{i}", [128, 16, 128], BF16) for i in range(2)]; b_Wp = P.bufs(2)
                pQ_6 = [P.ps(f"pQp{i}", [128, 512]) for i in range(2)]; b_pQ = P.bufs(2)
                wp_v = peer_wq.rearrange("(c p) n -> p c n", p=128)
                for c in range(16):
                    A("sp", lambda e, c=c: e.dma_start(out=h2T[:, c, :], in_=H2T[:, c, :]), reads=b_H2T, writes=[b_h2T[c]], dma=True)
                it = 0
                for n in range(16):
                    wb_ = n % 2
                    A("pool", lambda e, wb_=wb_, n=n: e.dma_start(out=Wp[wb_][:], in_=wp_v[:, :, n * 128:(n + 1) * 128]), writes=[b_Wp[wb_]], dma=True)
                    for g in range(4):
                        pb = it % 2; it += 1
                        for c in range(16):
                            A("pe", lambda e, pb=pb, c=c, g=g, wb_=wb_: e.matmul(pQ_6[pb][:, :], lhsT=Wp[wb_][:, c, :], rhs=h2T[:, c, g * 512:(g + 1) * 512], start=(c == 0), stop=(c == 15)),
                              reads=[b_Wp[wb_], b_h2T[c]], writes=[b_pQ[pb]])
                        if pb == 0:
                            A("act", lambda e, pb=pb, n=n, g=g: e.activation(out=qpT[:, n, g * 512:(g + 1) * 512], in_=pQ_6[pb][:, :], func=AF.Copy), reads=[b_pQ[pb]], writes=[b_qpT[n][g]])
                        else:
                            A("dve", lambda e, pb=pb, n=n, g=g: e.tensor_copy(out=qpT[:, n, g * 512:(g + 1) * 512], in_=pQ_6[pb][:, :]), reads=[b_pQ[pb]], writes=[b_qpT[n][g]])
            P.barrier()
            with ExitStack() as ph:
                P.cur = ph
                skf = P.sb("skf", [128, 2, 128], F32); b_skf = Buf()
                skT = P.sb("skT", [128, 2, 128], BF16); b_skT = Buf()
                sc = [P.sb(f"sc{i}", [128, 16, 128], F32) for i in range(2)]; b_sc = [P.bufs(16) for _ in range(2)]
                v1 = P.sb("v1", [128, 8, 16], F32); b_v1 = P.bufs(8)
                v2 = P.sb("v2", [128, 8, 16], F32); b_v2 = P.bufs(8)
                wk1 = P.sb("wk1", [128, 128], F32); b_wk1 = Buf()
                wk2 = P.sb("wk2", [128, 128], F32); b_wk2 = Buf()
                cand = P.sb("cand", [128, 8, 256], F32); b_cand = P.bufs(8)
                cw = P.sb("cw", [128, 256], F32); b_cw = Buf()
                cw2 = P.sb("cw2", [128, 256], F32); b_cw2 = Buf()
                t17 = P.sb("t17", [128, 8, 8], F32); b_t17 = P.bufs(8)
                thrm = P.sb("thrm", [128, 8], F32); b_thrm = Buf()
                top = P.sb("top", [128, 8, 16], F32); b_top = P.bufs(8)
                negmx = P.sb("negmx", [128, 8], F32); b_negmx = Buf()
                Zt = P.sb("Zt", [128, 8], F32); b_Zt = P.bufs(8)
                rZ = P.sb("rZ", [128, 8], F32); b_rZ = Buf()
                junk6 = P.sb("junk6", [128, 16], F32); b_junk6 = Buf()
                d1 = P.sb("d1", [128, 8, 16], F32); b_d1 = Buf()
                Q4 = P.sb("Q4", [128, 4, 128], F32); b_Q4 = P.bufs(4)
                sT = [P.sb(f"sT{i}", [128, 4, 128], F32) for i in range(2)]; b_sT = P.bufs(2)
                qrep = [P.sb(f"qrep{i}", [128, 16, 2, 128], BF16) for i in range(2)]; b_qrep = [P.bufs(2) for _ in range(2)]
                Eb = [P.sb(f"Eb{i}", [128, 2, 128], F32) for i in range(2)]; b_Eb = [P.bufs(2) for _ in range(2)]
                lhsG = [P.sb(f"lhsG{i}", [128, 2, 128], BF16) for i in range(3)]; b_lhsG = P.bufs(3)
                rhsG = [P.sb(f"rhsG{i}", [128, 2, 128], BF16) for i in range(3)]; b_rhsG = [P.bufs(2) for _ in range(3)]
                SG = [P.sb(f"SG{i}", [128, 128, 128], BF16) for i in range(2)]; b_SG = [P.bufs(32) for _ in range(2)]
                pSc = [P.ps("pSc0", [128, 512])] * 2; b_pSc = [Buf()] * 2
                pT4 = P.ps("pT4", [128, 512]); b_pT4 = Buf()
                pD = [P.ps(f"pD{i}", [128, 512]) for i in range(2)]; b_pD = P.bufs(2)
                pAa = [P.ps(f"pAa{i}", [128, 512]) for i in range(2)]; b_pAa = P.bufs(2)
                pG = [P.ps(f"pG{i}", [128, 512]) for i in range(2)]; b_pG = P.bufs(2)

                A("sp", lambda e: e.dma_start(out=skf[:], in_=peer_sk.rearrange("p n d -> n p d")), writes=[b_skf], dma=True)
                for p_ in range(2):
                    A("pe", lambda e, p_=p_: e.transpose(out=pT4[:, p_ * 128:(p_ + 1) * 128], in_=skf[:, p_, :], identity=identf[:]), reads=[b_skf, b_identf], writes=[b_pT4])
                A("dve", lambda e: e.tensor_copy(out=skT[:], in_=pT4[:, 0:256].rearrange("d (p n) -> d p n", p=2)), reads=[b_pT4], writes=[b_skT])

                def front_scores(tt):
                    ts_ = slice(tt * 128, (tt + 1) * 128)
                    for r in range(4):
                        pb = r % 2
                        for q_ in range(4):
                            hp = r * 4 + q_
                            A("pe", lambda e, pb=pb, q_=q_, hp=hp, ts_=ts_: e.matmul(pSc[pb][:, q_ * 128:(q_ + 1) * 128], lhsT=qpT[:, hp, ts_], rhs=skT[:, hp % 2, :], start=True, stop=True),
                              reads=[b_qpT[hp][tt // 4], b_skT], writes=[b_pSc[pb]])
                        A("act", lambda e, pb=pb, r=r: e.activation(out=sc[tt % 2][:, r * 4:(r + 1) * 4, :], in_=pSc[pb][:].rearrange("p (a n) -> p a n", a=4), func=AF.Copy),
                          reads=[b_pSc[pb]], writes=b_sc[tt % 2][r * 4:(r + 1) * 4])

                def front_topk(tt):
                    for h in range(8):
                        for (src, vv, bvv, wkk, bwk) in ((2 * h, v1, b_v1, wk1, b_wk1), (2 * h + 1, v2, b_v2, wk2, b_wk2)):
                            A("dve", lambda e, src=src, vv=vv, h=h: e.max(out=vv[:, h, 0:8], in_=sc[tt % 2][:, src, :]), reads=[b_sc[tt % 2][src]], writes=[bvv[h]])
                            A("dve", lambda e, src=src, vv=vv, h=h, wkk=wkk: e.match_replace(out=wkk[:], in_to_replace=vv[:, h, 0:8], in_values=sc[tt % 2][:, src, :], imm_value=NEG),
                              reads=[b_sc[tt % 2][src], bvv[h]], writes=[bwk])
                            A("dve", lambda e, vv=vv, h=h, wkk=wkk: e.max(out=vv[:, h, 8:16], in_=wkk[:]), reads=[bwk, bvv[h]], writes=[bvv[h]])
                        A("dve", lambda e, h=h: e.tensor_tensor(out=cand[:, h, :].rearrange("p (a b) -> p a b", a=16), in0=v1[:, h, :].unsqueeze(2).broadcast_to([128, 16, 16]),
                                                                in1=v2[:, h, :].unsqueeze(1).broadcast_to([128, 16, 16]), op=ALU.add),
                          reads=[b_v1[h], b_v2[h]], writes=[b_cand[h]])
                        A("dve", lambda e, h=h: e.max(out=top[:, h, 0:8], in_=cand[:, h, :]), reads=[b_cand[h]], writes=[b_top[h]])
                        A("dve", lambda e, h=h: e.match_replace(out=cw[:], in_to_replace=top[:, h, 0:8], in_values=cand[:, h, :], imm_value=NEG),
                          reads=[b_cand[h], b_top[h]], writes=[b_cw])
                        A("dve", lambda e, h=h: e.max(out=top[:, h, 8:16], in_=cw[:]), reads=[b_cw, b_top[h]], writes=[b_top[h]])
                        A("dve", lambda e, h=h: e.match_replace(out=cw2[:], in_to_replace=top[:, h, 8:16], in_values=cw[:], imm_value=NEG),
                          reads=[b_cw, b_top[h]], writes=[b_cw2])
                        A("dve", lambda e, h=h: e.max(out=t17[:, h, :], in_=cw2[:]), reads=[b_cw2], writes=[b_t17[h]])
                    A("dve", lambda e: e.tensor_scalar(out=negmx[:], in0=top[:, :, 0], scalar1=-1.0, scalar2=None, op0=ALU.mult), reads=b_top, writes=[b_negmx])
                    for h in range(8):
                        A("act", lambda e, h=h: e.activation(out=junk6[:], in_=top[:, h, :], func=AF.Exp, bias=negmx[:, h:h + 1], scale=1.0, accum_out=Zt[:, h:h + 1]),
                          reads=[b_top[h], b_negmx], writes=[b_junk6, b_Zt[h]])
                    A("act", lambda e: e.activation(out=rZ[:], in_=Zt[:], func=AF.Ln), reads=b_Zt, writes=[b_rZ])
                    A("dve", lambda e: e.tensor_tensor(out=rZ[:], in0=rZ[:], in1=v2[:, :, 0], op=ALU.add), reads=[b_rZ] + b_v2, writes=[b_rZ])
                    A("dve", lambda e: e.tensor_tensor(out=d1[:], in0=v1[:], in1=v1[:, :, 0:1].broadcast_to([128, 8, 16]), op=ALU.subtract), reads=b_v1, writes=[b_d1])
                    A("dve", lambda e: e.tensor_copy(out=Q4[:, 0, :].rearrange("p (h a) -> p h a", h=8), in_=v1[:]), reads=b_v1, writes=[b_Q4[0]])
                    A("dve", lambda e: e.tensor_tensor(out=Q4[:, 3, :].rearrange("p (h a) -> p h a", h=8), in0=d1[:], in1=rZ[:].unsqueeze(2).broadcast_to([128, 8, 16]), op=ALU.subtract),
                      reads=[b_d1, b_rZ], writes=[b_Q4[3]])
                    A("dve", lambda e: e.tensor_tensor(out=thrm[:], in0=top[:, :, 15], in1=t17[:, :, 0], op=ALU.add), reads=b_top + b_t17, writes=[b_thrm])
                    A("dve", lambda e: e.tensor_scalar(out=thrm[:], in0=thrm[:], scalar1=0.5, scalar2=None, op0=ALU.mult), reads=[b_thrm], writes=[b_thrm])
                    A("dve", lambda e: e.tensor_tensor(out=Q4[:, 2, :].rearrange("p (h a) -> p h a", h=8), in0=thrm[:].unsqueeze(2).broadcast_to([128, 8, 16]), in1=v1[:], op=ALU.subtract),
                      reads=[b_thrm] + b_v1, writes=[b_Q4[2]])
                    A("dve", lambda e: e.tensor_tensor(out=Q4[:, 2, :].rearrange("p (h a) -> p h a", h=8), in0=Q4[:, 2, :].rearrange("p (h a) -> p h a", h=8),
                                                       in1=v2[:, :, 15:16].broadcast_to([128, 8, 16]), op=ALU.max),
                      reads=[b_Q4[2]] + b_v2, writes=[b_Q4[2]])
                    tb = tt % 2
                    for k4 in (0, 2, 3):
                        A("pe", lambda e, k4=k4: e.transpose(out=pT4[:, k4 * 128:(k4 + 1) * 128], in_=Q4[:, k4, :], identity=identf[:]), reads=[b_Q4[k4], b_identf], writes=[b_pT4])
                    A("act", lambda e, tb=tb: e.activation(out=sT[tb][:], in_=pT4[:].rearrange("p (k t) -> p k t", k=4), func=AF.Copy), reads=[b_pT4], writes=[b_sT[tb]])

                def issue_qrep(S_):
                    if S_ >= NT * 8:
                        return
                    qb = S_ % 2
                    t0 = S_ * 16
                    ttq = S_ // 8
                    for p_ in range(2):
                        def rep_cp(e, qb=qb, p_=p_, t0=t0):
                            src = qpT[:, p_:16:2, t0:t0 + 16].rearrange("d h t -> d t h").unsqueeze(3).broadcast_to([128, 16, 8, 16])
                            dst = qrep[qb][:, :, p_, :].rearrange("d t (h a) -> d t h a", h=8)
                            if p_ == 0:
                                return e.tensor_copy(out=dst, in_=src)
                            return e.activation(out=dst, in_=src, func=AF.Copy)
                        A("pool" if p_ == 0 else "act", rep_cp, reads=[b_qpT[hp][ttq // 4] for hp in range(p_, 16, 2)], writes=[b_qrep[qb][p_]])

                def gates(tt):
                    tb = tt % 2
                    sgb_ = tt % 2

                    def stageA(g, tt=tt, tb=tb):
                        sub, gl = divmod(g, 8)
                        qb = (tt * 8 + sub) % 2
                        if gl == 0:
                            issue_qrep(tt * 8 + sub + 1)
                        gi = tt * 64 + g
                        sl = gi % 2
                        s3 = gi % 3
                        for q in range(2):
                            tl = gl * 2 + q
                            A("pe", lambda e, sl=sl, qb=qb, tl=tl, q=q: e.matmul(pD[sl][:, q * 128:(q + 1) * 128], lhsT=qrep[qb][:, tl, 0, :], rhs=skT[:, 0, :], start=True, stop=True),
                              reads=[b_qrep[qb][0], b_skT], writes=[b_pD[sl]])
                            A("pe", lambda e, sl=sl, qb=qb, tl=tl, q=q: e.matmul(pD[sl][:, 256 + q * 128:256 + (q + 1) * 128], lhsT=qrep[qb][:, tl, 1, :], rhs=skT[:, 1, :], start=True, stop=True),
                              reads=[b_qrep[qb][1], b_skT], writes=[b_pD[sl]])
                            A("pe", lambda e, sl=sl, qb=qb, tl=tl, q=q: e.matmul(pAa[sl][:, q * 128:(q + 1) * 128], lhsT=qrep[qb][:, tl, 1, :], rhs=skT[:, 1, :], start=True, stop=True),
                              reads=[b_qrep[qb][1], b_skT], writes=[b_pAa[sl]])
                        for q in range(2):
                            t = g * 2 + q
                            A("act", lambda e, sl=sl, tb=tb, t=t, q=q: e.activation(out=Eb[sl][:, q, :], in_=pAa[sl][:, q * 128:(q + 1) * 128], func=AF.Exp, bias=sT[tb][:, 3, t:t + 1], scale=1.0),
                              reads=[b_pAa[sl], b_sT[tb]], writes=[b_Eb[sl][q]])
                        A("dve", lambda e, sl=sl, s3=s3, tb=tb, g=g: e.tensor_tensor(out=lhsG[s3][:], in0=pD[sl][:, 0:256].rearrange("p (q i) -> p q i", q=2),
                                                                                    in1=sT[tb][:, 0, g * 2:g * 2 + 2].unsqueeze(2).broadcast_to([128, 2, 128]), op=ALU.is_equal),
                          reads=[b_pD[sl], b_sT[tb]], writes=[b_lhsG[s3]])
                        for q in range(2):
                            t = g * 2 + q
                            A("dve", lambda e, sl=sl, s3=s3, tb=tb, t=t, q=q: e.scalar_tensor_tensor(out=rhsG[s3][:, q, :], in0=pD[sl][:, 256 + q * 128:256 + (q + 1) * 128], scalar=sT[tb][:, 2, t:t + 1], in1=Eb[sl][:, q, :], op0=ALU.is_ge, op1=ALU.mult),
                              reads=[b_pD[sl], b_sT[tb], b_Eb[sl][q]], writes=[b_rhsG[s3][q]])

                    def stageB(g, tt=tt, sgb_=sgb_):
                        gi = tt * 64 + g
                        s3 = gi % 3
                        gq = (gi // 2) % 2
                        for q in range(2):
                            slot = (g * 2 + q) % 4
                            A("pe", lambda e, s3=s3, gq=gq, q=q, slot=slot: e.matmul(pG[gq][:].rearrange("i (j t) -> i t j", t=4)[:, slot, :], lhsT=lhsG[s3][:, q, :], rhs=rhsG[s3][:, q, :], start=True, stop=True),
                              reads=[b_lhsG[s3], b_rhsG[s3][q]], writes=[b_pG[gq]])
                        if g % 2 == 1:
                            g4 = g // 2
                            A("act", lambda e, gq=gq, sgb_=sgb_, g4=g4: e.activation(out=SG[sgb_][:, :, g4 * 4:g4 * 4 + 4], in_=pG[gq][:].rearrange("i (j t) -> i j t", t=4), func=AF.Copy),
                              reads=[b_pG[gq]], writes=[b_SG[sgb_][g4]])

                    return stageA, stageB

                issue_qrep(0)
                front_scores(0)
                front_topk(0)
                for tt in range(NT):
                    ts_ = slice(tt * 128, (tt + 1) * 128)
                    sgb_ = tt % 2
                    if tt + 1 < NT:
                        front_scores(tt + 1)
                    stageA, stageB = gates(tt)
                    for n_ in range(64 + 1):
                        if n_ == 33 and tt + 1 < NT:
                            front_topk(tt + 1)
                        if n_ < 64:
                            stageA(n_)
                        if n_ >= 1:
                            stageB(n_ - 1)
                    for jb in range(8):
                        A("sp", lambda e, jb=jb, sgb_=sgb_, ts_=ts_: e.dma_start(out=GT[jb * 16:(jb + 1) * 16, :, ts_].rearrange("j i t -> i j t"), in_=SG[sgb_][:, jb * 16:(jb + 1) * 16, :]),
                          reads=b_SG[sgb_], writes=[b_GT[tt]], dma=True)
            P.barrier()
          P.cur = es
          P.barrier()

        if stop_after >= 7:
          u_v = peer_u.rearrange("(i j) d -> j i d", j=128)
          v_v = peer_v.rearrange("(i j) d -> j i d", j=128)
          def _half(hf):
            with ExitStack() as sc_a:
                P.cur = sc_a
                TB = hf * 1024
                h2h = P.sb(f"h2h{hf}", [128, 16, 1024], BF16); b_h2h = P.bufs(16)
                acc = P.sb(f"acc{hf}", [128, 8, D], F32); b_acc = [P.bufs(4) for _ in range(8)]
                with ExitStack() as ph:
                    P.cur = ph
                    Ust = [P.sb(f"Ust{hf}{i}", [128, D], BF16) for i in range(2)]; b_Ust = P.bufs(2)
                    UT = [P.sb(f"UT{hf}{i}", [128, 16, 128], BF16) for i in range(2)]; b_UT = P.bufs(2)
                    Vc = [P.sb(f"Vc{hf}{i}", [128, 4, D], BF16) for i in range(2)]; b_Vc = [P.bufs(4) for _ in range(2)]
                    GTc = [P.sb(f"GTc{hf}{i}", [128, 1024], BF16) for i in range(2)]; b_GTc = P.bufs(2)
                    aT = [P.sb(f"aT{hf}{i}", [128, 4, 1024], BF16) for i in range(2)]; b_aT = [[P.bufs(2) for _ in range(4)] for _ in range(2)]
                    ga = [P.sb(f"ga{hf}{i}", [128, 512], F32) for i in range(2)]; b_ga = P.bufs(2)
                    pU = [P.ps(f"pU{hf}{i}", [128, D], BF16) for i in range(2)]; b_pU = P.bufs(2)
                    pA_7 = [P.ps(f"pAe{hf}{i}", [128, 512]) for i in range(2)]; b_pA = P.bufs(2)
                    pO_7 = [P.ps(f"pOe{hf}{i}", [128, 512]) for i in range(2)]; b_pO = P.bufs(2)
                    for c in range(16):
                        A("sp", lambda e, c=c, TB=TB: e.dma_start(out=h2h[:, c, :], in_=H2T[:, c, TB:TB + 1024]), reads=b_H2T, writes=[b_h2h[c]], dma=True)
                    for tt in range(8):
                        A("sp", lambda e, tt=tt, TB=TB: e.dma_start(out=acc[:, tt, :], in_=X2[TB + tt * 128:TB + (tt + 1) * 128, :]), reads=b_X2[(TB // 128) + tt], writes=b_acc[tt], dma=True)
                    cnt = {"ia": 0, "io": 0}

                    def stT(k):
                        s_, jj = divmod(k, 4)
                        sb_ = s_ % 2
                        cb = k % 2
                        A("pool", lambda e, sb_=sb_, jj=jj, k=k: e.dma_start(out=Vc[sb_][:, jj, :], in_=v_v[k]), writes=[b_Vc[sb_][jj]], dma=True)
                        A("sp", lambda e, cb=cb, k=k: e.dma_start(out=GTc[cb][:], in_=GT[k, :, TB:TB + 1024]), reads=b_GT, writes=[b_GTc[cb]], dma=True)
                        if hf == 1:
                            A("sp", lambda e, cb=cb, k=k: e.dma_start(out=UT[cb][:], in_=UTD[k].rearrange("d (c i) -> d c i", c=16)), reads=[b_UTD[k]], writes=[b_UT[cb]], dma=True)
                            return
                        A("pool", lambda e, cb=cb, k=k: e.dma_start(out=Ust[cb][:], in_=u_v[k]), writes=[b_Ust[cb]], dma=True)
                        for c in range(16):
                            A("pe", lambda e, cb=cb, c=c: e.transpose(out=pU[cb][:, c * 128:(c + 1) * 128], in_=Ust[cb][:, c * 128:(c + 1) * 128], identity=identb[:]),
                              reads=[b_Ust[cb], b_identb], writes=[b_pU[cb]])
                        A("act", lambda e, cb=cb: e.activation(out=UT[cb][:], in_=pU[cb][:].rearrange("p (c i) -> p c i", c=16), func=AF.Copy), reads=[b_pU[cb]], writes=[b_UT[cb]])
                        A("sp", lambda e, cb=cb, k=k: e.dma_start(out=UTD[k].rearrange("d (c i) -> d c i", c=16), in_=UT[cb][:]), reads=[b_UT[cb]], writes=[b_UTD[k]], dma=True)

                    def stM(k):
                        s_, jj = divmod(k, 4)
                        sb_ = s_ % 2
                        cb = k % 2
                        for tg in range(2):
                            ab = cnt["ia"] % 2; cnt["ia"] += 1
                            for c in range(16):
                                A("pe", lambda e, ab=ab, cb=cb, c=c, tg=tg: e.matmul(pA_7[ab][:, :], lhsT=UT[cb][:, c, :], rhs=h2h[:, c, tg * 512:(tg + 1) * 512], start=(c == 0), stop=(c == 15)),
                                  reads=[b_UT[cb], b_h2h[c]], writes=[b_pA[ab]])
                            A("act", lambda e, ab=ab: e.activation(out=ga[ab][:], in_=pA_7[ab][:, :], func=AF.Gelu), reads=[b_pA[ab]], writes=[b_ga[ab]])
                            A("dve", lambda e, ab=ab, sb_=sb_, jj=jj, tg=tg, cb=cb: e.tensor_tensor(out=aT[sb_][:, jj, tg * 512:(tg + 1) * 512], in0=ga[ab][:], in1=GTc[cb][:, tg * 512:(tg + 1) * 512], op=ALU.mult),
                              reads=[b_ga[ab], b_GTc[cb]], writes=[b_aT[sb_][jj][tg]])

                    def stV(s_):
                        sb_ = s_ % 2
                        for tt in range(8):
                            for blk in range(4):
                                ob_ = cnt["io"] % 2; cnt["io"] += 1
                                for jj in range(4):
                                    A("pe", lambda e, ob_=ob_, sb_=sb_, jj=jj, tt=tt, blk=blk: e.matmul(pO_7[ob_][:, :], lhsT=aT[sb_][:, jj, tt * 128:(tt + 1) * 128], rhs=Vc[sb_][:, jj, blk * 512:(blk + 1) * 512], start=(jj == 0), stop=(jj == 3)),
                                      reads=[b_aT[sb_][jj][tt // 4], b_Vc[sb_][jj]], writes=[b_pO[ob_]])
                                A("dve", lambda e, ob_=ob_, tt=tt, blk=blk: e.tensor_tensor(out=acc[:, tt, blk * 512:(blk + 1) * 512], in0=pO_7[ob_][:, :], in1=acc[:, tt, blk * 512:(blk + 1) * 512], op=ALU.add),
                                  reads=[b_pO[ob_], b_acc[tt][blk]], writes=[b_acc[tt][blk]])

                    stT(0)
                    for k in range(128):
                        if k + 1 < 128:
                            stT(k + 1)
                        stM(k)
                        if k % 4 == 0 and k > 0:
                            stV(k // 4 - 1)
                    stV(31)
                P.barrier()
                with ExitStack() as ph:
                    P.cur = ph
                    gO = P.sb(f"gO{hf}", [128, D], F32); b_gO = Buf()
                    junk_7 = P.sb(f"junk7{hf}", [128, D], BF16); b_junk = Buf()
                    ss_7 = [P.sb(f"ss7{hf}{i}", [128, 1], F32) for i in range(2)]; b_ss = P.bufs(2)
                    rstd_7 = [P.sb(f"rstd7{hf}{i}", [128, 1], F32) for i in range(2)]; b_rstd = P.bufs(2)
                    A("sp", lambda e: e.dma_start(out=gO[:], in_=final_norm.partition_broadcast(128)), writes=[b_gO], dma=True)
                    for tt in range(8):
                        b = tt % 2
                        rms_stats(acc[:, tt, :], b_acc[tt], ss_7[b][:], b_ss[b], rstd_7[b][:], b_rstd[b], junk_7[:], b_junk, D)
                        A("dve", lambda e, b=b, tt=tt: e.scalar_tensor_tensor(out=acc[:, tt, :], in0=acc[:, tt, :], scalar=rstd_7[b][:, 0:1], in1=gO[:], op0=ALU.mult, op1=ALU.mult),
                          reads=b_acc[tt] + [b_rstd[b], b_gO], writes=b_acc[tt])
                        fin.append(A("sp", lambda e, tt=tt, TB=TB: e.dma_start(out=out[TB + tt * 128:TB + (tt + 1) * 128, :], in_=acc[:, tt, :]), reads=b_acc[tt], dma=True))
                P.barrier()
            P.cur = es
            P.barrier()
          _half(0)
          _half(1)

        P.emit(final_waits=[op for e_ in P.ENGS for op in P.ops[e_] if op.dma])
        build.nops = P.nops
    return nc


def _consts():
    bf = ml_dtypes.bfloat16
    slopes = 2.0 ** (-np.arange(1, 9, dtype=np.float64))
    pos = np.arange(S)
    kaug = np.zeros((8, 4, S), np.float64)
    qaug = np.zeros((8, 4, S), np.float64)
    for h in range(8):
        kaug[h, 0] = 8 * slopes[h] * 128 * (pos // 128)
        kaug[h, 1] = 8 * slopes[h] * (pos % 128)
        kaug[h, 2] = 1
        kaug[h, 3] = 1
        qaug[h, 0] = 1
        qaug[h, 1] = 1
        qaug[h, 2] = -8 * slopes[h] * 128 * (pos // 128)
        qaug[h, 3] = -8 * slopes[h] * (pos % 128)
    kp = np.arange(128)[:, None]
    qp = np.arange(128)[None, :]
    allowed = (kp // 64) <= (qp // 64)
    bdiag = np.zeros((128, 8, 128), np.float64)
    for h in range(8):
        bdiag[:, h, :] = np.where(allowed, -8 * slopes[h] * np.abs(qp - kp), -240000.0)
    tri = (np.arange(128)[None, :] <= np.arange(128)[:, None]).astype(np.float32)
    return {
        "c_identb": np.eye(128, dtype=np.float32).astype(bf),
        "c_identf": np.eye(128, dtype=np.float32),
        "c_kaug": kaug.astype(np.float32).astype(bf),
        "c_qaug": qaug.astype(np.float32).astype(bf),
        "c_bdiag": bdiag.astype(np.float32).astype(bf),
        "c_tri": tri,
    }


def make_in_maps(inputs, cores):
    f = lambda a: np.ascontiguousarray(np.asarray(a, dtype=np.float32))
    shared = {
        "attn_norm": f(inputs["attn_norm"]).reshape(1, D),
        "w_in": f(inputs["w_in"]).reshape(D, IN_COLS),
        "diff_lambda": f(inputs["diff_lambda"]).reshape(1, 256),
        "diff_subln": f(inputs["diff_subln"]).reshape(1, 128),
        "gmlp_norm": f(inputs["gmlp_norm"]).reshape(1, 1024),
        "gmlp_ws": f(inputs["gmlp_ws"]).reshape(8, 128, 128),
        "gmlp_bs": f(inputs["gmlp_bs"]).reshape(8, 128),
        "w_branch_a": f(inputs["w_branch_a"]).reshape(1024, D),
        "w_branch_b": f(inputs["w_branch_b"]).reshape(1024, D),
        "w_out": f(inputs["w_out"]).reshape(D, D),
        "ffn_norm": f(inputs["ffn_norm"]).reshape(1, D),
        "peer_wq": f(inputs["peer_wq"]).reshape(D, 2048),
        "peer_subkeys": f(inputs["peer_subkeys"]).reshape(2, 128, 128),
        "peer_u": f(inputs["peer_u"]).reshape(16384, D),
        "peer_v": f(inputs["peer_v"]).reshape(16384, D),
        "final_norm": f(inputs["final_norm"]).reshape(1, D),
    }
    shared.update(_consts())
    xs = f(inputs["x"])
    return [dict(shared, x=xs[b]) for b in cores]


def kernel(**inputs):
    nc = build()
    in_maps = make_in_maps(inputs, list(range(8)))
    res = run_bass_kernel_spmd(nc, in_maps, core_ids=list(range(8)))
    return np.stack([np.asarray(r["out"], dtype=np.float32) for r in res.results], axis=0)
```

```python
import numpy as np
import ml_dtypes
import concourse.bass as bass
import concourse.mybir as mybir
from concourse.bass_utils import run_bass_kernel_spmd
from contextlib import ExitStack

F32 = mybir.dt.float32
BF16 = mybir.dt.bfloat16
ALU = mybir.AluOpType
AF = mybir.ActivationFunctionType
AX = mybir.AxisListType

S = 2048
D = 2048
NT = S // 128
EPS = 1e-6
IN_COLS = 9216
LAM_INIT = 0.8 - 0.6 * 1.0
NEG = -1.0e30


class Buf:
    __slots__ = ("name", "last_w", "readers")

    def __init__(self, name="b"):
        self.name = name
        self.last_w = None
        self.readers = []


class Op:
    __slots__ = ("eng", "fn", "dma", "deps", "signal", "sem", "semval", "prewait")

    def __init__(self, eng, fn, dma):
        self.eng = eng
        self.fn = fn
        self.dma = dma
        self.deps = []
        self.signal = False
        self.sem = None
        self.semval = None
        self.prewait = None


class Prog:
    ENGS = ("pe", "act", "dve", "pool", "sp")
    NDMA = {"sp": 12, "pool": 12}

    def __init__(self, nc, es):
        self.nc = nc
        self.es = es
        self.cur = es
        self.ops = {e: [] for e in self.ENGS}
        self.nops = 0
        self._bar_idx = {e: 0 for e in self.ENGS}
        self._pending = {e: [] for e in self.ENGS}

    def sb(self, name, shape, dtype):
        self.nops += 0
        self._uid = getattr(self, "_uid", 0) + 1
        return self.cur.enter_context(self.nc.sbuf_tensor(f"{name}_u{self._uid}", list(shape), dtype))

    def ps(self, name, shape, dtype=F32):
        self._uid = getattr(self, "_uid", 0) + 1
        return self.cur.enter_context(self.nc.psum_tensor(f"{name}_u{self._uid}", list(shape), dtype))

    def bufs(self, n, name="b"):
        return [Buf(name) for _ in range(n)]

    def barrier(self):
        deps = []
        for e in self.ENGS:
            ops = self.ops[e]
            for op in reversed(ops):
                if not op.dma:
                    deps.append(op)
                    break
            deps += [op for op in ops[self._bar_idx[e]:] if op.dma]
            self._bar_idx[e] = len(ops)
        for e in self.ENGS:
            self._pending[e] = self._pending[e] + deps

    def add(self, eng, fn, reads=(), writes=(), dma=False):
        op = Op(eng, fn, dma)
        deps = {}
        for b in reads:
            w = b.last_w
            if w is not None:
                deps[id(w)] = (w, 0)
        for b in writes:
            w = b.last_w
            if w is not None and id(w) not in deps:
                deps[id(w)] = (w, 1)
            for r in b.readers:
                if id(r) not in deps:
                    deps[id(r)] = (r, 2)
        for d, kind in deps.values():
            if d is op:
                continue
            if not d.dma and not op.dma and d.eng == eng:
                if eng == "pe" or kind == 2:
                    continue
            op.deps.append(d)
            d.signal = True
        if self._pending[eng]:
            for d in self._pending[eng]:
                op.deps.append(d)
                d.signal = True
            self._pending[eng] = []
        for b in reads:
            b.readers.append(op)
        for b in writes:
            b.last_w = op
            b.readers = []
        self.ops[eng].append(op)
        self.nops += 1
        return op

    def emit(self, final_waits=()):
        nc = self.nc
        es = self.es
        esem = {e: es.enter_context(nc.semaphore(f"s_{e}")) for e in self.ENGS if e != "sp"}
        dsem = {e: [es.enter_context(nc.semaphore(f"d_{e}{i}")) for i in range(n)] for e, n in self.NDMA.items()}
        for e in self.ENGS:
            tick = 0
            k = 0
            cnt = [0] * self.NDMA.get(e, 0)
            for op in self.ops[e]:
                if op.dma:
                    n = self.NDMA[e]
                    j = k % n
                    k += 1
                    op.prewait = (dsem[e][j], 16 * cnt[j]) if cnt[j] > 0 else None
                    cnt[j] += 1
                    op.sem = dsem[e][j]
                    op.semval = 16 * cnt[j]
                elif op.signal:
                    tick += 1
                    op.sem = esem[e]
                    op.semval = tick
        block = es.enter_context(nc.Block())
        engfn = {"pe": block.tensor, "act": block.scalar, "dve": block.vector, "pool": block.gpsimd, "sp": block.sync}

        def make(e):
            ops = self.ops[e]

            def body(eng):
                waited = {}

                def flush(need):
                    for key, (sem, val) in need.items():
                        if waited.get(key, 0) >= val:
                            continue
                        waited[key] = val
                        eng.wait_ge(sem, val)

                for op in ops:
                    need = {}
                    for d in op.deps:
                        key = id(d.sem)
                        if key not in need or need[key][1] < d.semval:
                            need[key] = (d.sem, d.semval)
                    if op.prewait is not None:
                        key = id(op.prewait[0])
                        if key not in need or need[key][1] < op.prewait[1]:
                            need[key] = op.prewait
                    flush(need)
                    ins = op.fn(eng)
                    if op.dma:
                        ins.then_inc(op.sem, 16)
                    elif op.signal:
                        ins.then_inc(op.sem, 1)
                if e == "sp":
                    need = {}
                    for d in final_waits:
                        key = id(d.sem)
                        if key not in need or need[key][1] < d.semval:
                            need[key] = (d.sem, d.semval)
                    flush(need)

            return body

        for e in self.ENGS:
            engfn[e](make(e))


def build(stop_after=99, dbg=()):
    nc = bass.Bass("TRN2", target_bir_lowering=False)

    def din(name, shape, dt=F32):
        return nc.dram_tensor(name, list(shape), dt, kind="ExternalInput").ap()

    def dscr(name, shape, dt):
        kind = "ExternalOutput" if name in dbg else "Internal"
        return nc.dram_tensor(name, list(shape), dt, kind=kind).ap()

    x = din("x", [S, D])
    attn_norm = din("attn_norm", [1, D])
    w_in = din("w_in", [D, IN_COLS])
    diff_lambda = din("diff_lambda", [1, 256])
    diff_subln = din("diff_subln", [1, 128])
    gmlp_norm = din("gmlp_norm", [1, 1024])
    gmlp_ws = din("gmlp_ws", [8, 128, 128])
    gmlp_bs = din("gmlp_bs", [8, 128])
    w_a = din("w_branch_a", [1024, D])
    w_b = din("w_branch_b", [1024, D])
    w_out = din("w_out", [D, D])
    ffn_norm = din("ffn_norm", [1, D])
    peer_wq = din("peer_wq", [D, 2048])
    peer_sk = din("peer_subkeys", [2, 128, 128])
    peer_u = din("peer_u", [16384, D])
    peer_v = din("peer_v", [16384, D])
    final_norm = din("final_norm", [1, D])
    c_identb = din("c_identb", [128, 128], BF16)
    c_identf = din("c_identf", [128, 128], F32)
    c_kaug = din("c_kaug", [8, 4, S], BF16)
    c_qaug = din("c_qaug", [8, 4, S], BF16)
    c_bdiag = din("c_bdiag", [128, 8, 128], BF16)
    c_tri = din("c_tri", [128, 128], F32)
    out = nc.dram_tensor("out", [S, D], F32, kind="ExternalOutput").ap()

    OAT = dscr("OAT", [128, 8, S], BF16)
    OBT = dscr("OBT", [128, 8, S], BF16)
    MT = dscr("MT", [128, 16, S], BF16)
    X2 = dscr("X2", [S, D], F32)
    H2T = dscr("H2T", [128, 16, S], BF16)
    GT = dscr("GT", [128, 128, S], BF16)
    b_MT = [Buf() for _ in range(16)]
    b_X2 = [[Buf() for _ in range(4)] for _ in range(NT)]
    b_H2T = [Buf() for _ in range(NT)]
    b_GT = [Buf() for _ in range(NT)]
    UTD = dscr("UTD", [128, 128, 2048], BF16)
    b_UTD = [Buf() for _ in range(128)]

    w_in_v = w_in.rearrange("(c p) n -> p c n", p=128)
    fin = []

    with ExitStack() as es:
        P = Prog(nc, es)
        A = P.add

        def dump(name, ap, shape, dt, bufs):
            if name not in dbg:
                return
            d_ = nc.dram_tensor(name, list(shape), dt, kind="ExternalOutput").ap()
            A("sp", lambda e: e.dma_start(out=d_, in_=ap), reads=bufs, dma=True)

        identb = P.sb("identb", [128, 128], BF16); b_identb = Buf()
        identf = P.sb("identf", [128, 128], F32); b_identf = Buf()
        epsT = P.sb("epsT", [128, 1], F32); b_eps = Buf()
        A("sp", lambda e: e.dma_start(out=identb[:], in_=c_identb), writes=[b_identb], dma=True)
        A("sp", lambda e: e.dma_start(out=identf[:], in_=c_identf), writes=[b_identf], dma=True)
        A("dve", lambda e: e.memset(epsT[:], EPS), writes=[b_eps])

        def rms_stats(src_ap, b_src, ss, b_ss, rstd, b_rstd, junk, b_junk, width):
            A("act", lambda e: e.activation(out=junk, in_=src_ap, func=AF.Square, scale=float(width) ** -0.5, accum_out=ss),
              reads=(b_src if isinstance(b_src, list) else [b_src]), writes=[b_junk, b_ss])
            A("act", lambda e: e.activation(out=ss, in_=ss, func=AF.Ln, bias=epsT[:], scale=1.0),
              reads=[b_ss, b_eps], writes=[b_ss])
            A("act", lambda e: e.activation(out=rstd, in_=ss, func=AF.Exp, scale=-0.5), reads=[b_ss], writes=[b_rstd])

        with ExitStack() as sc_h:
            P.cur = sc_h
            hT = P.sb("hT", [128, 16, S], BF16); b_hT = P.bufs(NT)

            with ExitStack() as ph:
                P.cur = ph
                gA = P.sb("gA", [128, D], F32); b_gA = Buf()
                xt = [P.sb(f"xt{i}", [128, D], F32) for i in range(2)]; b_xt = P.bufs(2)
                junk = P.sb("junk1", [128, D], BF16); b_junk = Buf()
                ss = [P.sb(f"ss{i}", [128, 1], F32) for i in range(2)]; b_ss = P.bufs(2)
                rstd = [P.sb(f"rstd{i}", [128, 1], F32) for i in range(2)]; b_rstd = P.bufs(2)
                hb = [P.sb(f"hb{i}", [128, D], BF16) for i in range(2)]; b_hb = P.bufs(2)
                pT = [P.ps(f"pT{i}", [128, D], BF16) for i in range(2)]; b_pT = P.bufs(2)
                A("sp", lambda e: e.dma_start(out=gA[:], in_=attn_norm.partition_broadcast(128)), writes=[b_gA], dma=True)
                for tt in range(NT):
                    b = tt % 2
                    A("sp", lambda e, tt=tt, b=b: e.dma_start(out=xt[b][:], in_=x[tt * 128:(tt + 1) * 128, :]), writes=[b_xt[b]], dma=True)
                    rms_stats(xt[b][:], b_xt[b], ss[b][:], b_ss[b], rstd[b][:], b_rstd[b], junk[:], b_junk, D)
                    A("dve", lambda e, b=b: e.scalar_tensor_tensor(out=hb[b][:], in0=xt[b][:], scalar=rstd[b][:, 0:1], in1=gA[:], op0=ALU.mult, op1=ALU.mult),
                      reads=[b_xt[b], b_rstd[b], b_gA], writes=[b_hb[b]])
                    for c in range(16):
                        A("pe", lambda e, b=b, c=c: e.transpose(out=pT[b][:, c * 128:(c + 1) * 128], in_=hb[b][:, c * 128:(c + 1) * 128], identity=identb[:]),
                          reads=[b_hb[b], b_identb], writes=[b_pT[b]])
                    A("act", lambda e, b=b, tt=tt: e.activation(out=hT[:, :, tt * 128:(tt + 1) * 128], in_=pT[b][:].rearrange("p (c t) -> p c t", c=16), func=AF.Copy),
                      reads=[b_pT[b]], writes=[b_hT[tt]])
            P.barrier()

            with ExitStack() as sc_o:
                P.cur = sc_o
                b_OATh = [Buf() for _ in range(8)]
                _boa = [[Buf() for _ in range(NT)] for _ in range(2)]
                b_oaT = [_boa[h_ % 2] for h_ in range(8)]
                b_obT = [[Buf() for _ in range(NT)] for _ in range(4)]

                if stop_after >= 2:
                  with ExitStack() as ph:
                    P.cur = ph
                    oaTa = [P.sb(f"oaTa{i}", [128, S], BF16) for i in range(2)]
                    wq = [P.sb(f"wq{i}", [128, 16, 128], BF16) for i in range(2)]; b_wq = P.bufs(2)
                    wk = [P.sb(f"wk{i}", [128, 16, 128], BF16) for i in range(2)]; b_wk = P.bufs(2)
                    wv4 = P.sb("wv4", [128, 16, 512], BF16); b_wv4 = Buf()
                    qT = [P.sb(f"qT{i}", [128, S], BF16) for i in range(2)]; b_qT = [P.bufs(4) for _ in range(2)]
                    kT = [P.sb(f"kT{i}", [128, S], BF16) for i in range(2)]; b_kT = [P.bufs(4) for _ in range(2)]
                    Va4 = P.sb("Va4", [128, NT, 4, 130], BF16); b_Va4 = P.bufs(NT)
                    b_Vone = Buf()
                    q1a = [P.sb(f"q1a{i}", [128, S], BF16) for i in range(2)]; b_q1f = P.bufs(2); b_q1g = P.bufs(2)
                    k1a = [P.sb(f"k1a{i}", [128, S], BF16) for i in range(2)]; b_k1f = P.bufs(2); b_k1g = P.bufs(2)
                    b_qTg = P.bufs(2); b_kTg = P.bufs(2)
                    bdiag = P.sb("bdiag", [128, 8, 128], BF16); b_bdiag = Buf()
                    PT = [P.sb(f"PT{i}", [128, NT * 128], BF16) for i in range(2)]; b_PT = [P.bufs(4) for _ in range(2)]
                    om = [P.sb(f"om{i}", [128, 128], F32) for i in range(2)]; b_om = P.bufs(2)
                    rz = [P.sb(f"rz{i}", [128, 1], F32) for i in range(2)]; b_rz = P.bufs(2)
                    ot = P.sb("ot", [128, 128], F32); b_ot = Buf()
                    oab = [P.sb(f"oab{i}", [128, 128], BF16) for i in range(2)]; b_oab = P.bufs(2)
                    junk2 = P.sb("junk2", [128, 128], BF16); b_junk2 = Buf()
                    ss2 = P.sb("ss2", [128, 1], F32); b_ss2 = Buf()
                    rstd2 = P.sb("rstd2", [128, 1], F32); b_rstd2 = Buf()
                    gsub = P.sb("gsub", [128, 128], F32); b_gsub = Buf()
                    lamt = P.sb("lamt", [128, 256], F32); b_lamt = Buf()
                    lprod = P.sb("lprod", [128, 2, 64], F32); b_lprod = Buf()
                    lsum = P.sb("lsum", [128, 2], F32); b_lsum = Buf()
                    neglam = P.sb("neglam", [128, 1], F32); b_neglam = Buf()
                    pQ = [P.ps(f"pQ{i}", [128, 512]) for i in range(2)]; b_pQ = P.bufs(2)
                    pS = [P.ps(f"pS{i}", [128, 512]) for i in range(2)]; b_pS = P.bufs(2)
                    pO = [P.ps(f"pO{i}", [128, 512]) for i in range(2)]; b_pO = P.bufs(2)
                    pTr = P.ps("pTr", [128, 128], BF16); b_pTr = Buf()

                    A("sp", lambda e: e.dma_start(out=bdiag[:], in_=c_bdiag), writes=[b_bdiag], dma=True)
                    A("sp", lambda e: e.dma_start(out=gsub[:], in_=diff_subln.partition_broadcast(128)), writes=[b_gsub], dma=True)
                    A("dve", lambda e: e.tensor_scalar(out=gsub[:], in0=gsub[:], scalar1=1.0 - LAM_INIT, scalar2=None, op0=ALU.mult),
                      reads=[b_gsub], writes=[b_gsub])
                    A("sp", lambda e: e.dma_start(out=lamt[:], in_=diff_lambda.partition_broadcast(128)), writes=[b_lamt], dma=True)
                    A("dve", lambda e: e.tensor_tensor(out=lprod[:, 0, :], in0=lamt[:, 0:64], in1=lamt[:, 64:128], op=ALU.mult), reads=[b_lamt], writes=[b_lprod])
                    A("dve", lambda e: e.tensor_tensor(out=lprod[:, 1, :], in0=lamt[:, 128:192], in1=lamt[:, 192:256], op=ALU.mult), reads=[b_lamt, b_lprod], writes=[b_lprod])
                    A("dve", lambda e: e.reduce_sum(out=lsum[:], in_=lprod[:], axis=AX.X), reads=[b_lprod], writes=[b_lsum])
                    A("act", lambda e: e.activation(out=lsum[:], in_=lsum[:], func=AF.Exp), reads=[b_lsum], writes=[b_lsum])
                    A("dve", lambda e: e.tensor_tensor(out=neglam[:], in0=lsum[:, 0:1], in1=lsum[:, 1:2], op=ALU.subtract), reads=[b_lsum], writes=[b_neglam])
                    A("dve", lambda e: e.tensor_scalar(out=neglam[:], in0=neglam[:], scalar1=LAM_INIT, scalar2=-1.0, op0=ALU.add, op1=ALU.mult),
                      reads=[b_neglam], writes=[b_neglam])
                    A("dve", lambda e: e.memset(Va4[:, :, :, 128:130], 1.0), writes=[b_Vone])

                    ev = 0
                    for h in range(8):
                        b = h % 2
                        A("pool", lambda e, b=b, h=h: e.dma_start(out=wq[b][:], in_=w_in_v[:, :, h * 128:(h + 1) * 128]), writes=[b_wq[b]], dma=True)
                        A("pool", lambda e, b=b, h=h: e.dma_start(out=wk[b][:], in_=w_in_v[:, :, 1024 + h * 128:1024 + (h + 1) * 128]), writes=[b_wk[b]], dma=True)
                        if h % 4 == 0:
                            A("pool", lambda e, h=h: e.dma_start(out=wv4[:], in_=w_in_v[:, :, 2048 + h * 128:2048 + (h + 4) * 128]), writes=[b_wv4], dma=True)
                        for (w_, bw, dst, bdst) in ((wq, b_wq, qT, b_qT), (wk, b_wk, kT, b_kT)):
                            for g in range(4):
                                pb = ev % 2; ev += 1
                                for c in range(16):
                                    A("pe", lambda e, pb=pb, c=c, g=g, w_=w_, b=b: e.matmul(pQ[pb][:, :], lhsT=w_[b][:, c, :], rhs=hT[:, c, g * 512:(g + 1) * 512], start=(c == 0), stop=(c == 15)),
                                      reads=[bw[b]] + b_hT[g * 4:(g + 1) * 4], writes=[b_pQ[pb]])
                                bg_ = b_qTg[b] if dst is qT else b_kTg[b]
                                if pb == 0:
                                    A("act", lambda e, pb=pb, g=g, dst=dst, b=b: e.activation(out=dst[b][:, g * 512:(g + 1) * 512], in_=pQ[pb][:, :], func=AF.Copy),
                                      reads=[b_pQ[pb]], writes=[bdst[b][g], bg_])
                                else:
                                    A("dve", lambda e, pb=pb, g=g, dst=dst, b=b: e.tensor_copy(out=dst[b][:, g * 512:(g + 1) * 512], in_=pQ[pb][:, :]),
                                      reads=[b_pQ[pb]], writes=[bdst[b][g], bg_])
                        A("sp", lambda e, b=b: e.dma_start(out=q1a[b][0:64, :], in_=qT[b][64:128, :]), reads=b_qT[b] + [b_qTg[b]], writes=[b_q1f[b]], dma=True)
                        A("sp", lambda e, b=b: e.dma_start(out=k1a[b][0:64, :], in_=kT[b][64:128, :]), reads=b_kT[b] + [b_kTg[b]], writes=[b_k1f[b]], dma=True)
                        A("sp", lambda e, b=b, h=h: e.dma_start(out=q1a[b][64:68, :], in_=c_qaug[h]), writes=[b_q1g[b]], dma=True)
                        A("sp", lambda e, b=b, h=h: e.dma_start(out=k1a[b][64:68, :], in_=c_kaug[h]), writes=[b_k1g[b]], dma=True)
                        A("sp", lambda e, b=b, h=h: e.dma_start(out=qT[b][64:68, :], in_=c_qaug[h]), writes=[b_qTg[b]], dma=True)
                        A("sp", lambda e, b=b, h=h: e.dma_start(out=kT[b][64:68, :], in_=c_kaug[h]), writes=[b_kTg[b]], dma=True)
                        if h % 4 == 0:
                            for tt in range(NT):
                                pb = ev % 2; ev += 1
                                for c in range(16):
                                    A("pe", lambda e, pb=pb, c=c, tt=tt: e.matmul(pQ[pb][:, :], lhsT=hT[:, c, tt * 128:(tt + 1) * 128], rhs=wv4[:, c, :], start=(c == 0), stop=(c == 15)),
                                      reads=[b_wv4, b_hT[tt]], writes=[b_pQ[pb]])
                                if pb == 0:
                                    A("act", lambda e, pb=pb, tt=tt: e.activation(out=Va4[:, tt, :, 0:128], in_=pQ[pb][:].rearrange("p (a n) -> p a n", a=4), func=AF.Copy),
                                      reads=[b_pQ[pb]], writes=[b_Va4[tt]])
                                else:
                                    A("dve", lambda e, pb=pb, tt=tt: e.tensor_copy(out=Va4[:, tt, :, 0:128], in_=pQ[pb][:].rearrange("p (a n) -> p a n", a=4)),
                                      reads=[b_pQ[pb]], writes=[b_Va4[tt]])
                        if h == 0:
                            dump("d_qT", qT[b][:], [128, S], BF16, b_qT[b])
                            dump("d_kT", kT[b][:], [128, S], BF16, b_kT[b])
                            dump("d_neglam", neglam[:], [128, 1], F32, [b_neglam])
                        def stQK(j, m, h=h, b=b):
                            pt = m
                            nblk = j + 1
                            for gi in range((nblk + 3) // 4):
                                sb_ = (j * 2 + m + gi) % 2
                                i0 = gi * 4
                                i1 = min(nblk, i0 + 4)
                                for i in range(i0, i1):
                                    col = (i - i0) * 128
                                    kop = kT[b] if m == 0 else k1a[b]
                                    qop = qT[b] if m == 0 else q1a[b]
                                    rds = ([b_kT[b][i // 4], b_qT[b][j // 4], b_kTg[b], b_qTg[b]] if m == 0 else [b_k1f[b], b_q1f[b], b_k1g[b], b_q1g[b]])
                                    kk = 68 if i < j else 64
                                    A("pe", lambda e, sb_=sb_, col=col, i=i, j=j, kop=kop, qop=qop, kk=kk: e.matmul(
                                        pS[sb_][:, col:col + 128], lhsT=kop[0:kk, i * 128:(i + 1) * 128],
                                        rhs=qop[0:kk, j * 128:(j + 1) * 128], start=True, stop=(i < j)),
                                      reads=rds, writes=[b_pS[sb_]])
                                    if i == j:
                                        A("pe", lambda e, sb_=sb_, col=col, h=h: e.matmul(
                                            pS[sb_][:, col:col + 128], lhsT=identb[:], rhs=bdiag[:, h, :], start=False, stop=True),
                                          reads=[b_identb, b_bdiag], writes=[b_pS[sb_]])
                                ncol = (i1 - i0) * 128
                                A("act", lambda e, sb_=sb_, pt=pt, i0=i0, ncol=ncol: e.activation(
                                    out=PT[pt][:, i0 * 128:i0 * 128 + ncol], in_=pS[sb_][:, 0:ncol], func=AF.Exp, scale=0.125),
                                  reads=[b_pS[sb_]], writes=[b_PT[pt][gi]])

                        def stPV(j, m, h=h, b=b):
                            pt = m
                            ob_ = m
                            for i in range(j + 1):
                                A("pe", lambda e, ob_=ob_, pt=pt, i=i, j=j, b=b: e.matmul(
                                    pO[ob_][:, 0:129], lhsT=PT[pt][:, i * 128:(i + 1) * 128], rhs=Va4[:, i, h % 4, 0:129], start=(i == 0), stop=(i == j)),
                                  reads=[b_PT[pt][i // 4], b_Va4[i], b_Vone], writes=[b_pO[ob_]])
                            A("dve", lambda e, ob_=ob_, m=m: e.reciprocal(out=rz[m][:], in_=pO[ob_][:, 128:129]), reads=[b_pO[ob_]], writes=[b_rz[m]])
                            A("dve", lambda e, ob_=ob_, m=m: e.tensor_scalar(out=om[m][:], in0=pO[ob_][:, 0:128], scalar1=rz[m][:, 0:1], scalar2=None, op0=ALU.mult),
                              reads=[b_pO[ob_], b_rz[m]], writes=[b_om[m]])
                            if m == 1:
                                A("dve", lambda e: e.scalar_tensor_tensor(out=ot[:], in0=om[1][:], scalar=neglam[:, 0:1], in1=om[0][:], op0=ALU.mult, op1=ALU.add),
                                  reads=[b_om[0], b_om[1], b_neglam], writes=[b_ot])
                                rms_stats(ot[:], b_ot, ss2[:], b_ss2, rstd2[:], b_rstd2, junk2[:], b_junk2, 128)
                                ab = j % 2
                                A("dve", lambda e, ab=ab: e.scalar_tensor_tensor(out=oab[ab][:], in0=ot[:], scalar=rstd2[:, 0:1], in1=gsub[:], op0=ALU.mult, op1=ALU.mult),
                                  reads=[b_ot, b_rstd2, b_gsub], writes=[b_oab[ab]])
                                def _tr(ab=ab, h=h, j=j):
                                    A("pe", lambda e, ab=ab: e.transpose(out=pTr[:, :], in_=oab[ab][:], identity=identb[:]), reads=[b_oab[ab], b_identb], writes=[b_pTr])
                                    A("act", lambda e, h=h, j=j: e.activation(out=oaTa[h % 2][:, j * 128:(j + 1) * 128], in_=pTr[:, :], func=AF.Copy),
                                      reads=[b_pTr], writes=[b_oaT[h][j]])
                                pend_tr.append(_tr)

                        units = [(j, m) for j in range(NT) for m in range(2)]
                        pend_tr = []
                        stQK(*units[0])
                        for ui, (j, m) in enumerate(units):
                            if ui + 1 < len(units):
                                stQK(*units[ui + 1])
                            if m == 1 and pend_tr:
                                pend_tr.pop(0)()
                            stPV(j, m)
                        while pend_tr:
                            pend_tr.pop(0)()
                        A("sp", lambda e, h=h: e.dma_start(out=OAT[:, h, :], in_=oaTa[h % 2][:]), reads=b_oaT[h], writes=[b_OATh[h]], dma=True)
                  P.barrier()
                P.cur = sc_o
                obT = P.sb("obT", [128, 8, S], BF16)

                if stop_after >= 3:
                  with ExitStack() as ph:
                    P.cur = ph
                    Wz = [P.sb(f"Wz{i}", [128, 16, 256], BF16) for i in range(2)]; b_Wz = P.bufs(2)
                    vg = P.sb("vg", [128, NT, 1024], BF16); b_vg = [P.bufs(4) for _ in range(NT)]
                    gn = P.sb("gn", [128, 1024], F32); b_gn = Buf()
                    wsf = P.sb("wsf", [128, 8, 128], F32); b_wsf = Buf()
                    tri = P.sb("tri", [128, 128], F32); b_tri = Buf()
                    wsT = P.sb("wsT", [128, 8, 128], BF16); b_wsT = Buf()
                    bsT = P.sb("bsT", [128, 8], F32); b_bsT = Buf()
                    ssv = P.sb("ssv", [128, NT, 4], F32); b_ssv = [P.bufs(4) for _ in range(NT)]
                    sv1 = P.sb("sv1", [128, 1], F32); b_sv1 = Buf()
                    rsv = P.sb("rsv", [128, 1], F32); b_rsv = Buf()
                    junk3 = P.sb("junk3", [128, 256], BF16); b_junk3 = Buf()
                    ug = [P.sb(f"ug{i}", [128, 256], F32) for i in range(2)]; b_ug = P.bufs(2)
                    tmp = [P.sb(f"tmp{i}", [128, 256], F32) for i in range(2)]; b_tmp = P.bufs(2)
                    obb = [P.sb(f"obb{i}", [128, 256], BF16) for i in range(2)]; b_obb = P.bufs(2)
                    pZ = [P.ps(f"pZ{i}", [128, 512]) for i in range(2)]; b_pZ = P.bufs(2)
                    pSV = [P.ps(f"pSV{i}", [128, 512]) for i in range(2)]; b_pSV = P.bufs(2)
                    pW = P.ps("pW", [128, 512]); b_pW = Buf()
                    pTb = P.ps("pTb", [128, 256], BF16); b_pTb = Buf()

                    A("sp", lambda e: e.dma_start(out=gn[:], in_=gmlp_norm.partition_broadcast(128)), writes=[b_gn], dma=True)
                    A("sp", lambda e: e.dma_start(out=wsf[:], in_=gmlp_ws.rearrange("g t s -> t g s")), writes=[b_wsf], dma=True)
                    A("sp", lambda e: e.dma_start(out=tri[:], in_=c_tri), writes=[b_tri], dma=True)
                    A("sp", lambda e: e.dma_start(out=bsT[:], in_=gmlp_bs.rearrange("g t -> t g"), allow_slow_non_contiguous=True), writes=[b_bsT], dma=True)
                    A("dve", lambda e: e.tensor_tensor(out=wsf[:], in0=wsf[:], in1=tri[:].unsqueeze(1).broadcast_to([128, 8, 128]), op=ALU.mult),
                      reads=[b_wsf, b_tri], writes=[b_wsf])
                    for g0 in range(0, 8, 4):
                        for gg in range(4):
                            A("pe", lambda e, g0=g0, gg=gg: e.transpose(out=pW[:, gg * 128:(gg + 1) * 128], in_=wsf[:, g0 + gg, :], identity=identf[:]),
                              reads=[b_wsf, b_identf], writes=[b_pW])
                        A("dve", lambda e, g0=g0: e.tensor_copy(out=wsT[:, g0:g0 + 4, :], in_=pW[:].rearrange("p (g t) -> p g t", g=4)), reads=[b_pW], writes=[b_wsT])

                    zc = 0
                    for vb in range(4):
                        wb_ = zc % 2; zc += 1
                        c0 = 3072 + 1024 + vb * 256
                        A("pool", lambda e, wb_=wb_, c0=c0: e.dma_start(out=Wz[wb_][:], in_=w_in_v[:, :, c0:c0 + 256]), writes=[b_Wz[wb_]], dma=True)
                        for tt in range(NT):
                            zb = tt % 2
                            for c in range(16):
                                A("pe", lambda e, zb=zb, c=c, tt=tt, wb_=wb_: e.matmul(pZ[zb][:, 0:256], lhsT=hT[:, c, tt * 128:(tt + 1) * 128], rhs=Wz[wb_][:, c, :], start=(c == 0), stop=(c == 15)),
                                  reads=[b_Wz[wb_], b_hT[tt]], writes=[b_pZ[zb]])
                            A("act", lambda e, zb=zb, tt=tt, vb=vb: e.activation(out=vg[:, tt, vb * 256:(vb + 1) * 256], in_=pZ[zb][:, 0:256], func=AF.Gelu),
                              reads=[b_pZ[zb]], writes=[b_vg[tt][vb]])
                            A("act", lambda e, tt=tt, vb=vb: e.activation(out=junk3[:], in_=vg[:, tt, vb * 256:(vb + 1) * 256], func=AF.Square, scale=1.0 / 32.0, accum_out=ssv[:, tt, vb:vb + 1]),
                              reads=[b_vg[tt][vb]], writes=[b_junk3, b_ssv[tt][vb]])
                    for tt in range(NT):
                        A("dve", lambda e, tt=tt: e.reduce_sum(out=sv1[:], in_=ssv[:, tt, :], axis=AX.X), reads=b_ssv[tt], writes=[b_sv1])
                        A("act", lambda e: e.activation(out=sv1[:], in_=sv1[:], func=AF.Sqrt, bias=epsT[:], scale=1.0), reads=[b_sv1, b_eps], writes=[b_sv1])
                        A("dve", lambda e: e.reciprocal(out=rsv[:], in_=sv1[:]), reads=[b_sv1], writes=[b_rsv])
                        A("dve", lambda e, tt=tt: e.scalar_tensor_tensor(out=vg[:, tt, :], in0=vg[:, tt, :], scalar=rsv[:, 0:1], in1=gn[:], op0=ALU.mult, op1=ALU.mult),
                          reads=b_vg[tt] + [b_rsv, b_gn], writes=b_vg[tt])
                    pend3 = []
                    for ub in range(4):
                        wb_ = zc % 2; zc += 1
                        c0 = 3072 + ub * 256
                        A("pool", lambda e, wb_=wb_, c0=c0: e.dma_start(out=Wz[wb_][:], in_=w_in_v[:, :, c0:c0 + 256]), writes=[b_Wz[wb_]], dma=True)
                        for tt in range(NT):
                            zb = tt % 2
                            for c in range(16):
                                A("pe", lambda e, zb=zb, c=c, tt=tt, wb_=wb_: e.matmul(pZ[zb][:, 0:256], lhsT=hT[:, c, tt * 128:(tt + 1) * 128], rhs=Wz[wb_][:, c, :], start=(c == 0), stop=(c == 15)),
                                  reads=[b_Wz[wb_], b_hT[tt]], writes=[b_pZ[zb]])
                            for gg in range(2):
                                g = ub * 2 + gg
                                A("pe", lambda e, zb=zb, gg=gg, g=g, tt=tt: e.matmul(pSV[zb][:, gg * 128:(gg + 1) * 128], lhsT=wsT[:, g, :], rhs=vg[:, tt, g * 128:(g + 1) * 128], start=True, stop=True),
                                  reads=[b_wsT, b_vg[tt][g // 2]], writes=[b_pSV[zb]])
                            while pend3:
                                pend3.pop(0)()
                            A("act", lambda e, zb=zb: e.activation(out=ug[zb][:], in_=pZ[zb][:, 0:256], func=AF.Gelu), reads=[b_pZ[zb]], writes=[b_ug[zb]])
                            A("dve", lambda e, zb=zb, ub=ub: e.tensor_tensor(out=tmp[zb][:].rearrange("p (g c) -> p g c", g=2), in0=pSV[zb][:, 0:256].rearrange("p (g c) -> p g c", g=2),
                                                                             in1=bsT[:, ub * 2:(ub + 1) * 2].unsqueeze(2).broadcast_to([128, 2, 128]), op=ALU.add),
                              reads=[b_pSV[zb], b_bsT], writes=[b_tmp[zb]])
                            A("dve", lambda e, zb=zb: e.tensor_tensor(out=obb[zb][:], in0=tmp[zb][:], in1=ug[zb][:], op=ALU.mult), reads=[b_tmp[zb], b_ug[zb]], writes=[b_obb[zb]])
                            def _tr3(zb=zb, ub=ub, tt=tt):
                                for gg in range(2):
                                    A("pe", lambda e, zb=zb, gg=gg: e.transpose(out=pTb[:, gg * 128:(gg + 1) * 128], in_=obb[zb][:, gg * 128:(gg + 1) * 128], identity=identb[:]),
                                      reads=[b_obb[zb], b_identb], writes=[b_pTb])
                                A("act", lambda e, ub=ub, tt=tt: e.activation(out=obT[:, ub * 2:(ub + 1) * 2, tt * 128:(tt + 1) * 128], in_=pTb[:].rearrange("p (g t) -> p g t", g=2), func=AF.Copy),
                                  reads=[b_pTb], writes=[b_obT[ub][tt]])
                            pend3.append(_tr3)
                    while pend3:
                        pend3.pop(0)()
                    if "OBT" in dbg:
                        for g in range(8):
                            A("sp", lambda e, g=g: e.dma_start(out=OBT[:, g, :], in_=obT[:, g, :]), reads=b_obT[g // 2], dma=True)
                  P.barrier()
                P.cur = sc_o
                oaT = P.sb("oaT2", [128, 8, S], BF16); b_oaT2 = [Buf() for _ in range(8)]

                if stop_after >= 4:
                  with ExitStack() as ph:
                    P.cur = ph
                    Wa = [P.sb(f"Wa{i}", [128, 8, 128], BF16) for i in range(2)]; b_Wa = P.bufs(2)
                    Wb = [P.sb(f"Wb{i}", [128, 8, 128], BF16) for i in range(2)]; b_Wb = P.bufs(2)
                    Wga = [P.sb(f"Wga{i}", [128, 16, 128], BF16) for i in range(2)]; b_Wga = P.bufs(2)
                    Wgb = [P.sb(f"Wgb{i}", [128, 16, 128], BF16) for i in range(2)]; b_Wgb = P.bufs(2)
                    sga = [P.sb(f"sga{i}", [128, 512], F32) for i in range(2)]; b_sga = P.bufs(2)
                    sgb = [P.sb(f"sgb{i}", [128, 512], F32) for i in range(2)]; b_sgb = P.bufs(2)
                    mst = [P.sb(f"mst{i}", [128, S], BF16) for i in range(2)]; b_mst = [P.bufs(4) for _ in range(2)]
                    pA = [P.ps(f"pA{i}", [128, 512]) for i in range(2)]; b_pA = P.bufs(2)
                    pB = [P.ps(f"pB{i}", [128, 512]) for i in range(2)]; b_pB = P.bufs(2)
                    pGA = [P.ps(f"pGA{i}", [128, 512]) for i in range(2)]; b_pGA = P.bufs(2)
                    pGB = [P.ps(f"pGB{i}", [128, 512]) for i in range(2)]; b_pGB = P.bufs(2)
                    wa_v = w_a.rearrange("(c p) n -> p c n", p=128)
                    wb_v = w_b.rearrange("(c p) n -> p c n", p=128)
                    for h in range(8):
                        A("sp", lambda e, h=h: e.dma_start(out=oaT[:, h, :], in_=OAT[:, h, :]), reads=[b_OATh[h]], writes=[b_oaT2[h]], dma=True)
                    it = 0
                    for n in range(16):
                        wb_ = n % 2
                        A("pool", lambda e, wb_=wb_, n=n: e.dma_start(out=Wa[wb_][:], in_=wa_v[:, :, n * 128:(n + 1) * 128]), writes=[b_Wa[wb_]], dma=True)
                        A("pool", lambda e, wb_=wb_, n=n: e.dma_start(out=Wb[wb_][:], in_=wb_v[:, :, n * 128:(n + 1) * 128]), writes=[b_Wb[wb_]], dma=True)
                        A("pool", lambda e, wb_=wb_, n=n: e.dma_start(out=Wga[wb_][:], in_=w_in_v[:, :, 5120 + n * 128:5120 + (n + 1) * 128]), writes=[b_Wga[wb_]], dma=True)
                        A("pool", lambda e, wb_=wb_, n=n: e.dma_start(out=Wgb[wb_][:], in_=w_in_v[:, :, 7168 + n * 128:7168 + (n + 1) * 128]), writes=[b_Wgb[wb_]], dma=True)
                        for g in range(4):
                            pb = it % 2; it += 1
                            gs = slice(g * 512, (g + 1) * 512)
                            for c in range(8):
                                A("pe", lambda e, pb=pb, c=c, gs=gs, wb_=wb_: e.matmul(pA[pb][:, :], lhsT=Wa[wb_][:, c, :], rhs=oaT[:, c, gs], start=(c == 0), stop=(c == 7)),
                                  reads=[b_Wa[wb_], b_oaT2[c]], writes=[b_pA[pb]])
                            for c in range(8):
                                A("pe", lambda e, pb=pb, c=c, gs=gs, wb_=wb_: e.matmul(pB[pb][:, :], lhsT=Wb[wb_][:, c, :], rhs=obT[:, c, gs], start=(c == 0), stop=(c == 7)),
                                  reads=[b_Wb[wb_]], writes=[b_pB[pb]])
                            for c in range(16):
                                A("pe", lambda e, pb=pb, c=c, gs=gs, wb_=wb_: e.matmul(pGA[pb][:, :], lhsT=Wga[wb_][:, c, :], rhs=hT[:, c, gs], start=(c == 0), stop=(c == 15)),
                                  reads=[b_Wga[wb_]], writes=[b_pGA[pb]])
                            for c in range(16):
                                A("pe", lambda e, pb=pb, c=c, gs=gs, wb_=wb_: e.matmul(pGB[pb][:, :], lhsT=Wgb[wb_][:, c, :], rhs=hT[:, c, gs], start=(c == 0), stop=(c == 15)),
                                  reads=[b_Wgb[wb_]], writes=[b_pGB[pb]])
                            A("act", lambda e, pb=pb: e.activation(out=sga[pb][:], in_=pGA[pb][:, :], func=AF.Sigmoid), reads=[b_pGA[pb]], writes=[b_sga[pb]])
                            A("act", lambda e, pb=pb: e.activation(out=sgb[pb][:], in_=pGB[pb][:, :], func=AF.Sigmoid), reads=[b_pGB[pb]], writes=[b_sgb[pb]])
                            A("dve", lambda e, pb=pb: e.tensor_tensor(out=sga[pb][:], in0=pA[pb][:, :], in1=sga[pb][:], op=ALU.mult), reads=[b_pA[pb], b_sga[pb]], writes=[b_sga[pb]])
                            A("dve", lambda e, pb=pb: e.tensor_tensor(out=sgb[pb][:], in0=pB[pb][:, :], in1=sgb[pb][:], op=ALU.mult), reads=[b_pB[pb], b_sgb[pb]], writes=[b_sgb[pb]])
                            A("pool", lambda e, pb=pb, wb_=wb_, gs=gs: e.tensor_tensor(out=mst[wb_][:, gs], in0=sga[pb][:], in1=sgb[pb][:], op=ALU.add),
                              reads=[b_sga[pb], b_sgb[pb]], writes=[b_mst[wb_][g]])
                        A("sp", lambda e, wb_=wb_, n=n: e.dma_start(out=MT[:, n, :], in_=mst[wb_][:]), reads=b_mst[wb_], writes=[b_MT[n]], dma=True)
                  P.barrier()
        P.cur = es
        P.barrier()

        if stop_after >= 5:
          with ExitStack() as ph:
            P.cur = ph
            mT = P.sb("mT", [128, 16, S], BF16); b_mT = P.bufs(16)
            Wo = [P.sb(f"Wo{i}", [128, 16, 512], BF16) for i in range(2)]; b_Wo = P.bufs(2)
            xp = [P.sb(f"xp{i}", [128, 512], F32) for i in range(2)]; b_xp = P.bufs(2)
            x2p = [P.sb(f"x2p{i}", [128, 512], F32) for i in range(2)]; b_x2p = P.bufs(2)
            pX = [P.ps(f"pX{i}", [128, 512]) for i in range(2)]; b_pX = P.bufs(2)
            wo_v = w_out.rearrange("(c p) n -> p c n", p=128)
            for c in range(16):
                A("sp", lambda e, c=c: e.dma_start(out=mT[:, c, :], in_=MT[:, c, :]), reads=[b_MT[c]], writes=[b_mT[c]], dma=True)
            it = 0
            for blk in range(4):
                wb_ = blk % 2
                A("pool", lambda e, wb_=wb_, blk=blk: e.dma_start(out=Wo[wb_][:], in_=wo_v[:, :, blk * 512:(blk + 1) * 512]), writes=[b_Wo[wb_]], dma=True)
                for tt in range(NT):
                    pb = it % 2; it += 1
                    A("sp", lambda e, pb=pb, tt=tt, blk=blk: e.dma_start(out=xp[pb][:], in_=x[tt * 128:(tt + 1) * 128, blk * 512:(blk + 1) * 512]), writes=[b_xp[pb]], dma=True)
                    for c in range(16):
                        A("pe", lambda e, pb=pb, c=c, tt=tt, wb_=wb_: e.matmul(pX[pb][:, :], lhsT=mT[:, c, tt * 128:(tt + 1) * 128], rhs=Wo[wb_][:, c, :], start=(c == 0), stop=(c == 15)),
                          reads=[b_Wo[wb_], b_mT[c]], writes=[b_pX[pb]])
                    A("dve", lambda e, pb=pb: e.tensor_tensor(out=x2p[pb][:], in0=pX[pb][:, :], in1=xp[pb][:], op=ALU.add), reads=[b_pX[pb], b_xp[pb]], writes=[b_x2p[pb]])
                    A("sp", lambda e, pb=pb, tt=tt, blk=blk: e.dma_start(out=X2[tt * 128:(tt + 1) * 128, blk * 512:(blk + 1) * 512], in_=x2p[pb][:]),
                      reads=[b_x2p[pb]], writes=[b_X2[tt][blk]], dma=True)
          P.barrier()
          with ExitStack() as ph:
            P.cur = ph
            gF = P.sb("gF", [128, D], F32); b_gF = Buf()
            xt_5 = [P.sb(f"x2t{i}", [128, D], F32) for i in range(2)]; b_xt = P.bufs(2)
            junk_5 = P.sb("junk5", [128, D], BF16); b_junk = Buf()
            ss_5 = [P.sb(f"ss5{i}", [128, 1], F32) for i in range(2)]; b_ss = P.bufs(2)
            rstd_5 = [P.sb(f"rstd5{i}", [128, 1], F32) for i in range(2)]; b_rstd = P.bufs(2)
            hb_5 = [P.sb(f"hb5{i}", [128, D], BF16) for i in range(2)]; b_hb = P.bufs(2)
            hs = [P.sb(f"hs5{i}", [128, 16, 128], BF16) for i in range(2)]; b_hs = P.bufs(2)
            pT_5 = [P.ps(f"pT5{i}", [128, D], BF16) for i in range(2)]; b_pT = P.bufs(2)
            A("sp", lambda e: e.dma_start(out=gF[:], in_=ffn_norm.partition_broadcast(128)), writes=[b_gF], dma=True)
            for tt in range(NT):
                b = tt % 2
                A("sp", lambda e, tt=tt, b=b: e.dma_start(out=xt_5[b][:], in_=X2[tt * 128:(tt + 1) * 128, :]), reads=b_X2[tt], writes=[b_xt[b]], dma=True)
                rms_stats(xt_5[b][:], b_xt[b], ss_5[b][:], b_ss[b], rstd_5[b][:], b_rstd[b], junk_5[:], b_junk, D)
                A("dve", lambda e, b=b: e.scalar_tensor_tensor(out=hb_5[b][:], in0=xt_5[b][:], scalar=rstd_5[b][:, 0:1], in1=gF[:], op0=ALU.mult, op1=ALU.mult),
                  reads=[b_xt[b], b_rstd[b], b_gF], writes=[b_hb[b]])
                for c in range(16):
                    A("pe", lambda e, b=b, c=c: e.transpose(out=pT_5[b][:, c * 128:(c + 1) * 128], in_=hb_5[b][:, c * 128:(c + 1) * 128], identity=identb[:]),
                      reads=[b_hb[b], b_identb], writes=[b_pT[b]])
                A("act", lambda e, b=b: e.activation(out=hs[b][:], in_=pT_5[b][:].rearrange("p (c t) -> p c t", c=16), func=AF.Copy), reads=[b_pT[b]], writes=[b_hs[b]])
                A("sp", lambda e, b=b, tt=tt: e.dma_start(out=H2T[:, :, tt * 128:(tt + 1) * 128], in_=hs[b][:]), reads=[b_hs[b]], writes=[b_H2T[tt]], dma=True)
          P.barrier()

        if stop_after >= 6:
          with ExitStack() as sc_q:
            P.cur = sc_q
            qpT = P.sb("qpT", [128, 16, S], BF16); b_qpT = [P.bufs(4) for _ in range(16)]
            with ExitStack() as ph:
                P.cur = ph
                h2T = P.sb("h2T", [128, 16, S], BF16); b_h2T = P.bufs(16)
                Wp = [P.sb(f"Wp{i}", [128, 16, 128], BF16) for i in range(2)]; b_Wp = P.bufs(2)
                pQ_6 = [P.ps(f"pQp{i}", [128, 512]) for i in range(2)]; b_pQ = P.bufs(2)
                wp_v = peer_wq.rearrange("(c p) n -> p c n", p=128)
                for c in range(16):
                    A("sp", lambda e, c=c: e.dma_start(out=h2T[:, c, :], in_=H2T[:, c, :]), reads=b_H2T, writes=[b_h2T[c]], dma=True)
                it = 0
                for n in range(16):
                    wb_ = n % 2
                    A("pool", lambda e, wb_=wb_, n=n: e.dma_start(out=Wp[wb_][:], in_=wp_v[:, :, n * 128:(n + 1) * 128]), writes=[b_Wp[wb_]], dma=True)
                    for g in range(4):
                        pb = it % 2; it += 1
                        for c in range(16):
                            A("pe", lambda e, pb=pb, c=c, g=g, wb_=wb_: e.matmul(pQ_6[pb][:, :], lhsT=Wp[wb_][:, c, :], rhs=h2T[:, c, g * 512:(g + 1) * 512], start=(c == 0), stop=(c == 15)),
                              reads=[b_Wp[wb_], b_h2T[c]], writes=[b_pQ[pb]])
                        if pb == 0:
                            A("act", lambda e, pb=pb, n=n, g=g: e.activation(out=qpT[:, n, g * 512:(g + 1) * 512], in_=pQ_6[pb][:, :], func=AF.Copy), reads=[b_pQ[pb]], writes=[b_qpT[n][g]])
                        else:
                            A("dve", lambda e, pb=pb, n=n, g=g: e.tensor_copy(out=qpT[:, n, g * 512:(g + 1) * 512], in_=pQ_6[pb][:, :]), reads=[b_pQ[pb]], writes=[b_qpT[n][g]])
            P.barrier()
            with ExitStack() as ph:
                P.cur = ph
                skf = P.sb("skf", [128, 2, 128], F32); b_skf = Buf()
                skT = P.sb("skT", [128, 2, 128], BF16); b_skT = Buf()
                sc = [P.sb(f"sc{i}", [128, 16, 128], F32) for i in range(2)]; b_sc = [P.bufs(16) for _ in range(2)]
                v1 = P.sb("v1", [128, 8, 16], F32); b_v1 = P.bufs(8)
                v2 = P.sb("v2", [128, 8, 16], F32); b_v2 = P.bufs(8)
                wk1 = P.sb("wk1", [128, 128], F32); b_wk1 = Buf()
                wk2 = P.sb("wk2", [128, 128], F32); b_wk2 = Buf()
                cand = P.sb("cand", [128, 8, 256], F32); b_cand = P.bufs(8)
                cw = P.sb("cw", [128, 256], F32); b_cw = Buf()
                cw2 = P.sb("cw2", [128, 256], F32); b_cw2 = Buf()
                t17 = P.sb("t17", [128, 8, 8], F32); b_t17 = P.bufs(8)
                thrm = P.sb("thrm", [128, 8], F32); b_thrm = Buf()
                top = P.sb("top", [128, 8, 16], F32); b_top = P.bufs(8)
                negmx = P.sb("negmx", [128, 8], F32); b_negmx = Buf()
                Zt = P.sb("Zt", [128, 8], F32); b_Zt = P.bufs(8)
                rZ = P.sb("rZ", [128, 8], F32); b_rZ = Buf()
                junk6 = P.sb("junk6", [128, 16], F32); b_junk6 = Buf()
                d1 = P.sb("d1", [128, 8, 16], F32); b_d1 = Buf()
                Q4 = P.sb("Q4", [128, 4, 128], F32); b_Q4 = P.bufs(4)
                sT = [P.sb(f"sT{i}", [128, 4, 128], F32) for i in range(2)]; b_sT = P.bufs(2)
                qrep = [P.sb(f"qrep{i}", [128, 16, 2, 128], BF16) for i in range(2)]; b_qrep = [P.bufs(2) for _ in range(2)]
                Eb = [P.sb(f"Eb{i}", [128, 2, 128], F32) for i in range(2)]; b_Eb = [P.bufs(2) for _ in range(2)]
                lhsG = [P.sb(f"lhsG{i}", [128, 2, 128], BF16) for i in range(3)]; b_lhsG = P.bufs(3)
                rhsG = [P.sb(f"rhsG{i}", [128, 2, 128], BF16) for i in range(3)]; b_rhsG = [P.bufs(2) for _ in range(3)]
                SG = [P.sb(f"SG{i}", [128, 128, 128], BF16) for i in range(2)]; b_SG = [P.bufs(32) for _ in range(2)]
                pSc = [P.ps("pSc0", [128, 512])] * 2; b_pSc = [Buf()] * 2
                pT4 = P.ps("pT4", [128, 512]); b_pT4 = Buf()
                pD = [P.ps(f"pD{i}", [128, 512]) for i in range(2)]; b_pD = P.bufs(2)
                pAa = [P.ps(f"pAa{i}", [128, 512]) for i in range(2)]; b_pAa = P.bufs(2)
                pG = [P.ps(f"pG{i}", [128, 512]) for i in range(2)]; b_pG = P.bufs(2)

                A("sp", lambda e: e.dma_start(out=skf[:], in_=peer_sk.rearrange("p n d -> n p d")), writes=[b_skf], dma=True)
                for p_ in range(2):
                    A("pe", lambda e, p_=p_: e.transpose(out=pT4[:, p_ * 128:(p_ + 1) * 128], in_=skf[:, p_, :], identity=identf[:]), reads=[b_skf, b_identf], writes=[b_pT4])
                A("dve", lambda e: e.tensor_copy(out=skT[:], in_=pT4[:, 0:256].rearrange("d (p n) -> d p n", p=2)), reads=[b_pT4], writes=[b_skT])

                def front_scores(tt):
                    ts_ = slice(tt * 128, (tt + 1) * 128)
                    for r in range(4):
                        pb = r % 2
                        for q_ in range(4):
                            hp = r * 4 + q_
                            A("pe", lambda e, pb=pb, q_=q_, hp=hp, ts_=ts_: e.matmul(pSc[pb][:, q_ * 128:(q_ + 1) * 128], lhsT=qpT[:, hp, ts_], rhs=skT[:, hp % 2, :], start=True, stop=True),
                              reads=[b_qpT[hp][tt // 4], b_skT], writes=[b_pSc[pb]])
                        A("act", lambda e, pb=pb, r=r: e.activation(out=sc[tt % 2][:, r * 4:(r + 1) * 4, :], in_=pSc[pb][:].rearrange("p (a n) -> p a n", a=4), func=AF.Copy),
                          reads=[b_pSc[pb]], writes=b_sc[tt % 2][r * 4:(r + 1) * 4])

                def front_topk(tt):
                    for h in range(8):
                        for (src, vv, bvv, wkk, bwk) in ((2 * h, v1, b_v1, wk1, b_wk1), (2 * h + 1, v2, b_v2, wk2, b_wk2)):
                            A("dve", lambda e, src=src, vv=vv, h=h: e.max(out=vv[:, h, 0:8], in_=sc[tt % 2][:, src, :]), reads=[b_sc[tt % 2][src]], writes=[bvv[h]])
                            A("dve", lambda e, src=src, vv=vv, h=h, wkk=wkk: e.match_replace(out=wkk[:], in_to_replace=vv[:, h, 0:8], in_values=sc[tt % 2][:, src, :], imm_value=NEG),
                              reads=[b_sc[tt % 2][src], bvv[h]], writes=[bwk])
                            A("dve", lambda e, vv=vv, h=h, wkk=wkk: e.max(out=vv[:, h, 8:16], in_=wkk[:]), reads=[bwk, bvv[h]], writes=[bvv[h]])
                        A("pool", lambda e, h=h: e.tensor_tensor(out=cand[:, h, :].rearrange("p (a b) -> p a b", a=16), in0=v1[:, h, :].unsqueeze(2).broadcast_to([128, 16, 16]),
                                                                 in1=v2[:, h, :].unsqueeze(1).broadcast_to([128, 16, 16]), op=ALU.add),
                          reads=[b_v1[h], b_v2[h]], writes=[b_cand[h]])
                    for h in range(8):
                        A("dve", lambda e, h=h: e.max(out=top[:, h, 0:8], in_=cand[:, h, :]), reads=[b_cand[h]], writes=[b_top[h]])
                        A("dve", lambda e, h=h: e.match_replace(out=cw[:], in_to_replace=top[:, h, 0:8], in_values=cand[:, h, :], imm_value=NEG),
                          reads=[b_cand[h], b_top[h]], writes=[b_cw])
                        A("dve", lambda e, h=h: e.max(out=top[:, h, 8:16], in_=cw[:]), reads=[b_cw, b_top[h]], writes=[b_top[h]])
                    A("dve", lambda e: e.tensor_scalar(out=negmx[:], in0=top[:, :, 0], scalar1=-1.0, scalar2=None, op0=ALU.mult), reads=b_top, writes=[b_negmx])
                    for h in range(8):
                        A("act", lambda e, h=h: e.activation(out=junk6[:], in_=top[:, h, :], func=AF.Exp, bias=negmx[:, h:h + 1], scale=1.0, accum_out=Zt[:, h:h + 1]),
                          reads=[b_top[h], b_negmx], writes=[b_junk6, b_Zt[h]])
                    A("act", lambda e: e.activation(out=rZ[:], in_=Zt[:], func=AF.Ln), reads=b_Zt, writes=[b_rZ])
                    A("dve", lambda e: e.tensor_tensor(out=rZ[:], in0=rZ[:], in1=v2[:, :, 0], op=ALU.add), reads=[b_rZ] + b_v2, writes=[b_rZ])
                    A("dve", lambda e: e.tensor_tensor(out=d1[:], in0=v1[:], in1=v1[:, :, 0:1].broadcast_to([128, 8, 16]), op=ALU.subtract), reads=b_v1, writes=[b_d1])
                    A("dve", lambda e: e.tensor_copy(out=Q4[:, 0, :].rearrange("p (h a) -> p h a", h=8), in_=v1[:]), reads=b_v1, writes=[b_Q4[0]])
                    A("dve", lambda e: e.tensor_tensor(out=Q4[:, 3, :].rearrange("p (h a) -> p h a", h=8), in0=d1[:], in1=rZ[:].unsqueeze(2).broadcast_to([128, 8, 16]), op=ALU.subtract),
                      reads=[b_d1, b_rZ], writes=[b_Q4[3]])
                    A("dve", lambda e: e.tensor_scalar(out=thrm[:], in0=top[:, :, 15], scalar1=-1.0e-5, scalar2=None, op0=ALU.add), reads=b_top, writes=[b_thrm])
                    A("dve", lambda e: e.tensor_tensor(out=Q4[:, 2, :].rearrange("p (h a) -> p h a", h=8), in0=thrm[:].unsqueeze(2).broadcast_to([128, 8, 16]), in1=v1[:], op=ALU.subtract),
                      reads=[b_thrm] + b_v1, writes=[b_Q4[2]])
                    A("dve", lambda e: e.tensor_tensor(out=Q4[:, 2, :].rearrange("p (h a) -> p h a", h=8), in0=Q4[:, 2, :].rearrange("p (h a) -> p h a", h=8),
                                                       in1=v2[:, :, 15:16].broadcast_to([128, 8, 16]), op=ALU.max),
                      reads=[b_Q4[2]] + b_v2, writes=[b_Q4[2]])
                    tb = tt % 2
                    for k4 in (0, 2, 3):
                        A("pe", lambda e, k4=k4: e.transpose(out=pT4[:, k4 * 128:(k4 + 1) * 128], in_=Q4[:, k4, :], identity=identf[:]), reads=[b_Q4[k4], b_identf], writes=[b_pT4])
                    A("act", lambda e, tb=tb: e.activation(out=sT[tb][:], in_=pT4[:].rearrange("p (k t) -> p k t", k=4), func=AF.Copy), reads=[b_pT4], writes=[b_sT[tb]])

                def issue_qrep(S_):
                    if S_ >= NT * 8:
                        return
                    qb = S_ % 2
                    t0 = S_ * 16
                    ttq = S_ // 8
                    for p_ in range(2):
                        def rep_cp(e, qb=qb, p_=p_, t0=t0):
                            src = qpT[:, p_:16:2, t0:t0 + 16].rearrange("d h t -> d t h").unsqueeze(3).broadcast_to([128, 16, 8, 16])
                            dst = qrep[qb][:, :, p_, :].rearrange("d t (h a) -> d t h a", h=8)
                            if p_ == 0:
                                return e.tensor_copy(out=dst, in_=src)
                            return e.activation(out=dst, in_=src, func=AF.Copy)
                        A("pool" if p_ == 0 else "act", rep_cp, reads=[b_qpT[hp][ttq // 4] for hp in range(p_, 16, 2)], writes=[b_qrep[qb][p_]])

                def gates(tt):
                    tb = tt % 2
                    sgb_ = tt % 2

                    def stageA(g, tt=tt, tb=tb):
                        sub, gl = divmod(g, 8)
                        qb = (tt * 8 + sub) % 2
                        if gl == 0:
                            issue_qrep(tt * 8 + sub + 1)
                        gi = tt * 64 + g
                        sl = gi % 2
                        s3 = gi % 3
                        for q in range(2):
                            tl = gl * 2 + q
                            A("pe", lambda e, sl=sl, qb=qb, tl=tl, q=q: e.matmul(pD[sl][:, q * 128:(q + 1) * 128], lhsT=qrep[qb][:, tl, 0, :], rhs=skT[:, 0, :], start=True, stop=True),
                              reads=[b_qrep[qb][0], b_skT], writes=[b_pD[sl]])
                            A("pe", lambda e, sl=sl, qb=qb, tl=tl, q=q: e.matmul(pD[sl][:, 256 + q * 128:256 + (q + 1) * 128], lhsT=qrep[qb][:, tl, 1, :], rhs=skT[:, 1, :], start=True, stop=True),
                              reads=[b_qrep[qb][1], b_skT], writes=[b_pD[sl]])
                            A("pe", lambda e, sl=sl, qb=qb, tl=tl, q=q: e.matmul(pAa[sl][:, q * 128:(q + 1) * 128], lhsT=qrep[qb][:, tl, 1, :], rhs=skT[:, 1, :], start=True, stop=True),
                              reads=[b_qrep[qb][1], b_skT], writes=[b_pAa[sl]])
                        for q in range(2):
                            t = g * 2 + q
                            A("act", lambda e, sl=sl, tb=tb, t=t, q=q: e.activation(out=Eb[sl][:, q, :], in_=pAa[sl][:, q * 128:(q + 1) * 128], func=AF.Exp, bias=sT[tb][:, 3, t:t + 1], scale=1.0),
                              reads=[b_pAa[sl], b_sT[tb]], writes=[b_Eb[sl][q]])
                        A("dve", lambda e, sl=sl, s3=s3, tb=tb, g=g: e.tensor_tensor(out=lhsG[s3][:], in0=pD[sl][:, 0:256].rearrange("p (q i) -> p q i", q=2),
                                                                                    in1=sT[tb][:, 0, g * 2:g * 2 + 2].unsqueeze(2).broadcast_to([128, 2, 128]), op=ALU.is_equal),
                          reads=[b_pD[sl], b_sT[tb]], writes=[b_lhsG[s3]])
                        for q in range(2):
                            t = g * 2 + q
                            A("dve", lambda e, sl=sl, s3=s3, tb=tb, t=t, q=q: e.scalar_tensor_tensor(out=rhsG[s3][:, q, :], in0=pD[sl][:, 256 + q * 128:256 + (q + 1) * 128], scalar=sT[tb][:, 2, t:t + 1], in1=Eb[sl][:, q, :], op0=ALU.is_ge, op1=ALU.mult),
                              reads=[b_pD[sl], b_sT[tb], b_Eb[sl][q]], writes=[b_rhsG[s3][q]])

                    def stageB(g, tt=tt, sgb_=sgb_):
                        gi = tt * 64 + g
                        s3 = gi % 3
                        gq = (gi // 2) % 2
                        for q in range(2):
                            slot = (g * 2 + q) % 4
                            A("pe", lambda e, s3=s3, gq=gq, q=q, slot=slot: e.matmul(pG[gq][:].rearrange("i (j t) -> i t j", t=4)[:, slot, :], lhsT=lhsG[s3][:, q, :], rhs=rhsG[s3][:, q, :], start=True, stop=True),
                              reads=[b_lhsG[s3], b_rhsG[s3][q]], writes=[b_pG[gq]])
                        if g % 2 == 1:
                            g4 = g // 2
                            A("act", lambda e, gq=gq, sgb_=sgb_, g4=g4: e.activation(out=SG[sgb_][:, :, g4 * 4:g4 * 4 + 4], in_=pG[gq][:].rearrange("i (j t) -> i j t", t=4), func=AF.Copy),
                              reads=[b_pG[gq]], writes=[b_SG[sgb_][g4]])

                    return stageA, stageB

                issue_qrep(0)
                front_scores(0)
                front_topk(0)
                for tt in range(NT):
                    ts_ = slice(tt * 128, (tt + 1) * 128)
                    sgb_ = tt % 2
                    if tt + 1 < NT:
                        front_scores(tt + 1)
                    stageA, stageB = gates(tt)
                    for n_ in range(64 + 1):
                        if n_ == 33 and tt + 1 < NT:
                            front_topk(tt + 1)
                        if n_ < 64:
                            stageA(n_)
                        if n_ >= 1:
                            stageB(n_ - 1)
                    for jb in range(8):
                        A("sp", lambda e, jb=jb, sgb_=sgb_, ts_=ts_: e.dma_start(out=GT[jb * 16:(jb + 1) * 16, :, ts_].rearrange("j i t -> i j t"), in_=SG[sgb_][:, jb * 16:(jb + 1) * 16, :]),
                          reads=b_SG[sgb_], writes=[b_GT[tt]], dma=True)
            P.barrier()
          P.cur = es
          P.barrier()

        if stop_after >= 7:
          u_v = peer_u.rearrange("(i j) d -> j i d", j=128)
          v_v = peer_v.rearrange("(i j) d -> j i d", j=128)
          def _half(hf):
            with ExitStack() as sc_a:
                P.cur = sc_a
                TB = hf * 1024
                h2h = P.sb(f"h2h{hf}", [128, 16, 1024], BF16); b_h2h = P.bufs(16)
                acc = P.sb(f"acc{hf}", [128, 8, D], F32); b_acc = [P.bufs(4) for _ in range(8)]
                with ExitStack() as ph:
                    P.cur = ph
                    Ust = [P.sb(f"Ust{hf}{i}", [128, D], BF16) for i in range(2)]; b_Ust = P.bufs(2)
                    UT = [P.sb(f"UT{hf}{i}", [128, 16, 128], BF16) for i in range(2)]; b_UT = P.bufs(2)
                    Vc = [P.sb(f"Vc{hf}{i}", [128, 4, D], BF16) for i in range(2)]; b_Vc = [P.bufs(4) for _ in range(2)]
                    GTc = [P.sb(f"GTc{hf}{i}", [128, 1024], BF16) for i in range(2)]; b_GTc = P.bufs(2)
                    aT = [P.sb(f"aT{hf}{i}", [128, 4, 1024], BF16) for i in range(2)]; b_aT = [[P.bufs(2) for _ in range(4)] for _ in range(2)]
                    ga = [P.sb(f"ga{hf}{i}", [128, 512], F32) for i in range(2)]; b_ga = P.bufs(2)
                    pU = [P.ps(f"pU{hf}{i}", [128, D], BF16) for i in range(2)]; b_pU = P.bufs(2)
                    pA_7 = [P.ps(f"pAe{hf}{i}", [128, 512]) for i in range(2)]; b_pA = P.bufs(2)
                    pO_7 = [P.ps(f"pOe{hf}{i}", [128, 512]) for i in range(2)]; b_pO = P.bufs(2)
                    for c in range(16):
                        A("sp", lambda e, c=c, TB=TB: e.dma_start(out=h2h[:, c, :], in_=H2T[:, c, TB:TB + 1024]), reads=b_H2T, writes=[b_h2h[c]], dma=True)
                    for tt in range(8):
                        A("sp", lambda e, tt=tt, TB=TB: e.dma_start(out=acc[:, tt, :], in_=X2[TB + tt * 128:TB + (tt + 1) * 128, :]), reads=b_X2[(TB // 128) + tt], writes=b_acc[tt], dma=True)
                    cnt = {"ia": 0, "io": 0}

                    def stT(k):
                        s_, jj = divmod(k, 4)
                        sb_ = s_ % 2
                        cb = k % 2
                        A("pool", lambda e, sb_=sb_, jj=jj, k=k: e.dma_start(out=Vc[sb_][:, jj, :], in_=v_v[k]), writes=[b_Vc[sb_][jj]], dma=True)
                        A("sp", lambda e, cb=cb, k=k: e.dma_start(out=GTc[cb][:], in_=GT[k, :, TB:TB + 1024]), reads=b_GT, writes=[b_GTc[cb]], dma=True)
                        if hf == 1:
                            A("sp", lambda e, cb=cb, k=k: e.dma_start(out=UT[cb][:], in_=UTD[k].rearrange("d (c i) -> d c i", c=16)), reads=[b_UTD[k]], writes=[b_UT[cb]], dma=True)
                            return
                        A("pool", lambda e, cb=cb, k=k: e.dma_start(out=Ust[cb][:], in_=u_v[k]), writes=[b_Ust[cb]], dma=True)
                        for c in range(16):
                            A("pe", lambda e, cb=cb, c=c: e.transpose(out=pU[cb][:, c * 128:(c + 1) * 128], in_=Ust[cb][:, c * 128:(c + 1) * 128], identity=identb[:]),
                              reads=[b_Ust[cb], b_identb], writes=[b_pU[cb]])
                        A("act", lambda e, cb=cb: e.activation(out=UT[cb][:], in_=pU[cb][:].rearrange("p (c i) -> p c i", c=16), func=AF.Copy), reads=[b_pU[cb]], writes=[b_UT[cb]])
                        A("sp", lambda e, cb=cb, k=k: e.dma_start(out=UTD[k].rearrange("d (c i) -> d c i", c=16), in_=UT[cb][:]), reads=[b_UT[cb]], writes=[b_UTD[k]], dma=True)

                    def stM(k):
                        s_, jj = divmod(k, 4)
                        sb_ = s_ % 2
                        cb = k % 2
                        for tg in range(2):
                            ab = cnt["ia"] % 2; cnt["ia"] += 1
                            for c in range(16):
                                A("pe", lambda e, ab=ab, cb=cb, c=c, tg=tg: e.matmul(pA_7[ab][:, :], lhsT=UT[cb][:, c, :], rhs=h2h[:, c, tg * 512:(tg + 1) * 512], start=(c == 0), stop=(c == 15)),
                                  reads=[b_UT[cb], b_h2h[c]], writes=[b_pA[ab]])
                            A("act", lambda e, ab=ab: e.activation(out=ga[ab][:], in_=pA_7[ab][:, :], func=AF.Gelu), reads=[b_pA[ab]], writes=[b_ga[ab]])
                            A("dve", lambda e, ab=ab, sb_=sb_, jj=jj, tg=tg, cb=cb: e.tensor_tensor(out=aT[sb_][:, jj, tg * 512:(tg + 1) * 512], in0=ga[ab][:], in1=GTc[cb][:, tg * 512:(tg + 1) * 512], op=ALU.mult),
                              reads=[b_ga[ab], b_GTc[cb]], writes=[b_aT[sb_][jj][tg]])

                    def stV(s_):
                        sb_ = s_ % 2
                        for tt in range(8):
                            for blk in range(4):
                                ob_ = cnt["io"] % 2; cnt["io"] += 1
                                for jj in range(4):
                                    A("pe", lambda e, ob_=ob_, sb_=sb_, jj=jj, tt=tt, blk=blk: e.matmul(pO_7[ob_][:, :], lhsT=aT[sb_][:, jj, tt * 128:(tt + 1) * 128], rhs=Vc[sb_][:, jj, blk * 512:(blk + 1) * 512], start=(jj == 0), stop=(jj == 3)),
                                      reads=[b_aT[sb_][jj][tt // 4], b_Vc[sb_][jj]], writes=[b_pO[ob_]])
                                A("dve", lambda e, ob_=ob_, tt=tt, blk=blk: e.tensor_tensor(out=acc[:, tt, blk * 512:(blk + 1) * 512], in0=pO_7[ob_][:, :], in1=acc[:, tt, blk * 512:(blk + 1) * 512], op=ALU.add),
                                  reads=[b_pO[ob_], b_acc[tt][blk]], writes=[b_acc[tt][blk]])

                    stT(0)
                    for k in range(128):
                        if k + 1 < 128:
                            stT(k + 1)
                        stM(k)
                        if k % 4 == 0 and k > 0:
                            stV(k // 4 - 1)
                    stV(31)
                P.barrier()
                with ExitStack() as ph:
                    P.cur = ph
                    gO = P.sb(f"gO{hf}", [128, D], F32); b_gO = Buf()
                    junk_7 = P.sb(f"junk7{hf}", [128, D], BF16); b_junk = Buf()
                    ss_7 = [P.sb(f"ss7{hf}{i}", [128, 1], F32) for i in range(2)]; b_ss = P.bufs(2)
                    rstd_7 = [P.sb(f"rstd7{hf}{i}", [128, 1], F32) for i in range(2)]; b_rstd = P.bufs(2)
                    A("sp", lambda e: e.dma_start(out=gO[:], in_=final_norm.partition_broadcast(128)), writes=[b_gO], dma=True)
                    for tt in range(8):
                        b = tt % 2
                        rms_stats(acc[:, tt, :], b_acc[tt], ss_7[b][:], b_ss[b], rstd_7[b][:], b_rstd[b], junk_7[:], b_junk, D)
                        A("dve", lambda e, b=b, tt=tt: e.scalar_tensor_tensor(out=acc[:, tt, :], in0=acc[:, tt, :], scalar=rstd_7[b][:, 0:1], in1=gO[:], op0=ALU.mult, op1=ALU.mult),
                          reads=b_acc[tt] + [b_rstd[b], b_gO], writes=b_acc[tt])
                        fin.append(A("sp", lambda e, tt=tt, TB=TB: e.dma_start(out=out[TB + tt * 128:TB + (tt + 1) * 128, :], in_=acc[:, tt, :]), reads=b_acc[tt], dma=True))
                P.barrier()
            P.cur = es
            P.barrier()
          _half(0)
          _half(1)

        P.emit(final_waits=[op for e_ in P.ENGS for op in P.ops[e_] if op.dma])
        build.nops = P.nops
    return nc


def _consts():
    bf = ml_dtypes.bfloat16
    slopes = 2.0 ** (-np.arange(1, 9, dtype=np.float64))
    pos = np.arange(S)
    kaug = np.zeros((8, 4, S), np.float64)
    qaug = np.zeros((8, 4, S), np.float64)
    for h in range(8):
        kaug[h, 0] = 8 * slopes[h] * 128 * (pos // 128)
        kaug[h, 1] = 8 * slopes[h] * (pos % 128)
        kaug[h, 2] = 1
        kaug[h, 3] = 1
        qaug[h, 0] = 1
        qaug[h, 1] = 1
        qaug[h, 2] = -8 * slopes[h] * 128 * (pos // 128)
        qaug[h, 3] = -8 * slopes[h] * (pos % 128)
    kp = np.arange(128)[:, None]
    qp = np.arange(128)[None, :]
    allowed = (kp // 64) <= (qp // 64)
    bdiag = np.zeros((128, 8, 128), np.float64)
    for h in range(8):
        bdiag[:, h, :] = np.where(allowed, -8 * slopes[h] * np.abs(qp - kp), -240000.0)
    tri = (np.arange(128)[None, :] <= np.arange(128)[:, None]).astype(np.float32)
    return {
        "c_identb": np.eye(128, dtype=np.float32).astype(bf),
        "c_identf": np.eye(128, dtype=np.float32),
        "c_kaug": kaug.astype(np.float32).astype(bf),
        "c_qaug": qaug.astype(np.float32).astype(bf),
        "c_bdiag": bdiag.astype(np.float32).astype(bf),
        "c_tri": tri,
    }


def make_in_maps(inputs, cores):
    f = lambda a: np.ascontiguousarray(np.asarray(a, dtype=np.float32))
    shared = {
        "attn_norm": f(inputs["attn_norm"]).reshape(1, D),
        "w_in": f(inputs["w_in"]).reshape(D, IN_COLS),
        "diff_lambda": f(inputs["diff_lambda"]).reshape(1, 256),
        "diff_subln": f(inputs["diff_subln"]).reshape(1, 128),
        "gmlp_norm": f(inputs["gmlp_norm"]).reshape(1, 1024),
        "gmlp_ws": f(inputs["gmlp_ws"]).reshape(8, 128, 128),
        "gmlp_bs": f(inputs["gmlp_bs"]).reshape(8, 128),
        "w_branch_a": f(inputs["w_branch_a"]).reshape(1024, D),
        "w_branch_b": f(inputs["w_branch_b"]).reshape(1024, D),
        "w_out": f(inputs["w_out"]).reshape(D, D),
        "ffn_norm": f(inputs["ffn_norm"]).reshape(1, D),
        "peer_wq": f(inputs["peer_wq"]).reshape(D, 2048),
        "peer_subkeys": f(inputs["peer_subkeys"]).reshape(2, 128, 128),
        "peer_u": f(inputs["peer_u"]).reshape(16384, D),
        "peer_v": f(inputs["peer_v"]).reshape(16384, D),
        "final_norm": f(inputs["final_norm"]).reshape(1, D),
    }
    shared.update(_consts())
    xs = f(inputs["x"])
    return [dict(shared, x=xs[b]) for b in cores]


def kernel(**inputs):
    nc = build()
    in_maps = make_in_maps(inputs, list(range(8)))
    res = run_bass_kernel_spmd(nc, in_maps, core_ids=list(range(8)))
    return np.stack([np.asarray(r["out"], dtype=np.float32) for r in res.results], axis=0)
```

```python
import numpy as np
import ml_dtypes
import concourse.bass as bass
import concourse.mybir as mybir
from concourse.bass_utils import run_bass_kernel_spmd
from contextlib import ExitStack

F32 = mybir.dt.float32
BF16 = mybir.dt.bfloat16
ALU = mybir.AluOpType
AF = mybir.ActivationFunctionType
AX = mybir.AxisListType

S = 2048
D = 2048
NT = S // 128
EPS = 1e-6
IN_COLS = 9216
LAM_INIT = 0.8 - 0.6 * 1.0
NEG = -1.0e30


class Buf:
    __slots__ = ("name", "last_w", "readers")

    def __init__(self, name="b"):
        self.name = name
        self.last_w = None
        self.readers = []


class Op:
    __slots__ = ("eng", "fn", "dma", "deps", "signal", "sem", "semval", "prewait")

    def __init__(self, eng, fn, dma):
        self.eng = eng
        self.fn = fn
        self.dma = dma
        self.deps = []
        self.signal = False
        self.sem = None
        self.semval = None
        self.prewait = None


class Prog:
    ENGS = ("pe", "act", "dve", "pool", "sp")
    NDMA = {"sp": 12, "pool": 12}

    def __init__(self, nc, es):
        self.nc = nc
        self.es = es
        self.cur = es
        self.ops = {e: [] for e in self.ENGS}
        self.nops = 0
        self._bar_idx = {e: 0 for e in self.ENGS}
        self._pending = {e: [] for e in self.ENGS}

    def sb(self, name, shape, dtype):
        self.nops += 0
        self._uid = getattr(self, "_uid", 0) + 1
        return self.cur.enter_context(self.nc.sbuf_tensor(f"{name}_u{self._uid}", list(shape), dtype))

    def ps(self, name, shape, dtype=F32):
        self._uid = getattr(self, "_uid", 0) + 1
        return self.cur.enter_context(self.nc.psum_tensor(f"{name}_u{self._uid}", list(shape), dtype))

    def bufs(self, n, name="b"):
        return [Buf(name) for _ in range(n)]

    def barrier(self):
        deps = []
        for e in self.ENGS:
            ops = self.ops[e]
            for op in reversed(ops):
                if not op.dma:
                    deps.append(op)
                    break
            deps += [op for op in ops[self._bar_idx[e]:] if op.dma]
            self._bar_idx[e] = len(ops)
        for e in self.ENGS:
            self._pending[e] = self._pending[e] + deps

    def add(self, eng, fn, reads=(), writes=(), dma=False):
        op = Op(eng, fn, dma)
        deps = {}
        for b in reads:
            w = b.last_w
            if w is not None:
                deps[id(w)] = (w, 0)
        for b in writes:
            w = b.last_w
            if w is not None and id(w) not in deps:
                deps[id(w)] = (w, 1)
            for r in b.readers:
                if id(r) not in deps:
                    deps[id(r)] = (r, 2)
        for d, kind in deps.values():
            if d is op:
                continue
            if not d.dma and not op.dma and d.eng == eng:
                if eng == "pe" or kind == 2:
                    continue
            op.deps.append(d)
            d.signal = True
        if self._pending[eng]:
            for d in self._pending[eng]:
                op.deps.append(d)
                d.signal = True
            self._pending[eng] = []
        for b in reads:
            b.readers.append(op)
        for b in writes:
            b.last_w = op
            b.readers = []
        self.ops[eng].append(op)
        self.nops += 1
        return op

    def emit(self, final_waits=()):
        nc = self.nc
        es = self.es
        esem = {e: es.enter_context(nc.semaphore(f"s_{e}")) for e in self.ENGS if e != "sp"}
        dsem = {e: [es.enter_context(nc.semaphore(f"d_{e}{i}")) for i in range(n)] for e, n in self.NDMA.items()}
        for e in self.ENGS:
            tick = 0
            k = 0
            cnt = [0] * self.NDMA.get(e, 0)
            for op in self.ops[e]:
                if op.dma:
                    n = self.NDMA[e]
                    j = k % n
                    k += 1
                    op.prewait = (dsem[e][j], 16 * cnt[j]) if cnt[j] > 0 else None
                    cnt[j] += 1
                    op.sem = dsem[e][j]
                    op.semval = 16 * cnt[j]
                elif op.signal:
                    tick += 1
                    op.sem = esem[e]
                    op.semval = tick
        block = es.enter_context(nc.Block())
        engfn = {"pe": block.tensor, "act": block.scalar, "dve": block.vector, "pool": block.gpsimd, "sp": block.sync}

        def make(e):
            ops = self.ops[e]

            def body(eng):
                waited = {}

                def flush(need):
                    for key, (sem, val) in need.items():
                        if waited.get(key, 0) >= val:
                            continue
                        waited[key] = val
                        eng.wait_ge(sem, val)

                for op in ops:
                    need = {}
                    for d in op.deps:
                        key = id(d.sem)
                        if key not in need or need[key][1] < d.semval:
                            need[key] = (d.sem, d.semval)
                    if op.prewait is not None:
                        key = id(op.prewait[0])
                        if key not in need or need[key][1] < op.prewait[1]:
                            need[key] = op.prewait
                    flush(need)
                    ins = op.fn(eng)
                    if op.dma:
                        ins.then_inc(op.sem, 16)
                    elif op.signal:
                        ins.then_inc(op.sem, 1)
                if e == "sp":
                    need = {}
                    for d in final_waits:
                        key = id(d.sem)
                        if key not in need or need[key][1] < d.semval:
                            need[key] = (d.sem, d.semval)
                    flush(need)

            return body

        for e in self.ENGS:
            engfn[e](make(e))


def build(stop_after=99, dbg=()):
    nc = bass.Bass("TRN2", target_bir_lowering=False)

    def din(name, shape, dt=F32):
        return nc.dram_tensor(name, list(shape), dt, kind="ExternalInput").ap()

    def dscr(name, shape, dt):
        kind = "ExternalOutput" if name in dbg else "Internal"
        return nc.dram_tensor(name, list(shape), dt, kind=kind).ap()

    x = din("x", [S, D])
    attn_norm = din("attn_norm", [1, D])
    w_in = din("w_in", [D, IN_COLS])
    diff_lambda = din("diff_lambda", [1, 256])
    diff_subln = din("diff_subln", [1, 128])
    gmlp_norm = din("gmlp_norm", [1, 1024])
    gmlp_ws = din("gmlp_ws", [8, 128, 128])
    gmlp_bs = din("gmlp_bs", [8, 128])
    w_a = din("w_branch_a", [1024, D])
    w_b = din("w_branch_b", [1024, D])
    w_out = din("w_out", [D, D])
    ffn_norm = din("ffn_norm", [1, D])
    peer_wq = din("peer_wq", [D, 2048])
    peer_sk = din("peer_subkeys", [2, 128, 128])
    peer_u = din("peer_u", [16384, D])
    peer_v = din("peer_v", [16384, D])
    final_norm = din("final_norm", [1, D])
    c_identb = din("c_identb", [128, 128], BF16)
    c_identf = din("c_identf", [128, 128], F32)
    c_kaug = din("c_kaug", [8, 4, S], BF16)
    c_qaug = din("c_qaug", [8, 4, S], BF16)
    c_bdiag = din("c_bdiag", [128, 8, 128], BF16)
    c_tri = din("c_tri", [128, 128], F32)
    out = nc.dram_tensor("out", [S, D], F32, kind="ExternalOutput").ap()

    OAT = dscr("OAT", [128, 8, S], BF16)
    OBT = dscr("OBT", [128, 8, S], BF16)
    MT = dscr("MT", [128, 16, S], BF16)
    X2 = dscr("X2", [S, D], F32)
    H2T = dscr("H2T", [128, 16, S], BF16)
    GT = dscr("GT", [128, 128, S], BF16)
    b_MT = [Buf() for _ in range(16)]
    b_X2 = [[Buf() for _ in range(4)] for _ in range(NT)]
    b_H2T = [Buf() for _ in range(NT)]
    b_GT = [Buf() for _ in range(NT)]
    UTD = dscr("UTD", [128, 128, 2048], BF16)
    b_UTD = [Buf() for _ in range(128)]

    w_in_v = w_in.rearrange("(c p) n -> p c n", p=128)
    fin = []

    with ExitStack() as es:
        P = Prog(nc, es)
        A = P.add

        def dump(name, ap, shape, dt, bufs):
            if name not in dbg:
                return
            d_ = nc.dram_tensor(name, list(shape), dt, kind="ExternalOutput").ap()
            A("sp", lambda e: e.dma_start(out=d_, in_=ap), reads=bufs, dma=True)

        identb = P.sb("identb", [128, 128], BF16); b_identb = Buf()
        identf = P.sb("identf", [128, 128], F32); b_identf = Buf()
        epsT = P.sb("epsT", [128, 1], F32); b_eps = Buf()
        A("sp", lambda e: e.dma_start(out=identb[:], in_=c_identb), writes=[b_identb], dma=True)
        A("sp", lambda e: e.dma_start(out=identf[:], in_=c_identf), writes=[b_identf], dma=True)
        A("dve", lambda e: e.memset(epsT[:], EPS), writes=[b_eps])

        def rms_stats(src_ap, b_src, ss, b_ss, rstd, b_rstd, junk, b_junk, width):
            A("act", lambda e: e.activation(out=junk, in_=src_ap, func=AF.Square, scale=float(width) ** -0.5, accum_out=ss),
              reads=(b_src if isinstance(b_src, list) else [b_src]), writes=[b_junk, b_ss])
            A("act", lambda e: e.activation(out=ss, in_=ss, func=AF.Ln, bias=epsT[:], scale=1.0),
              reads=[b_ss, b_eps], writes=[b_ss])
            A("act", lambda e: e.activation(out=rstd, in_=ss, func=AF.Exp, scale=-0.5), reads=[b_ss], writes=[b_rstd])

        with ExitStack() as sc_h:
            P.cur = sc_h
            hT = P.sb("hT", [128, 16, S], BF16); b_hT = P.bufs(NT)

            with ExitStack() as ph:
                P.cur = ph
                gA = P.sb("gA", [128, D], F32); b_gA = Buf()
                xt = [P.sb(f"xt{i}", [128, D], F32) for i in range(3)]; b_xt = P.bufs(3)
                junk = P.sb("junk1", [128, D], BF16); b_junk = Buf()
                ss = [P.sb(f"ss{i}", [128, 1], F32) for i in range(3)]; b_ss = P.bufs(3)
                rstd = [P.sb(f"rstd{i}", [128, 1], F32) for i in range(3)]; b_rstd = P.bufs(3)
                hb = [P.sb(f"hb{i}", [128, D], BF16) for i in range(3)]; b_hb = P.bufs(3)
                pT = [P.ps(f"pT{i}", [128, D], BF16) for i in range(3)]; b_pT = P.bufs(3)
                A("sp", lambda e: e.dma_start(out=gA[:], in_=attn_norm.partition_broadcast(128)), writes=[b_gA], dma=True)
                pend1 = []
                for tt in range(NT):
                    b = tt % 3
                    A("sp", lambda e, tt=tt, b=b: e.dma_start(out=xt[b][:], in_=x[tt * 128:(tt + 1) * 128, :]), writes=[b_xt[b]], dma=True)
                    rms_stats(xt[b][:], b_xt[b], ss[b][:], b_ss[b], rstd[b][:], b_rstd[b], junk[:], b_junk, D)
                    while pend1:
                        pend1.pop(0)()
                    A("dve", lambda e, b=b: e.scalar_tensor_tensor(out=hb[b][:], in0=xt[b][:], scalar=rstd[b][:, 0:1], in1=gA[:], op0=ALU.mult, op1=ALU.mult),
                      reads=[b_xt[b], b_rstd[b], b_gA], writes=[b_hb[b]])
                    for c in range(16):
                        A("pe", lambda e, b=b, c=c: e.transpose(out=pT[b][:, c * 128:(c + 1) * 128], in_=hb[b][:, c * 128:(c + 1) * 128], identity=identb[:]),
                          reads=[b_hb[b], b_identb], writes=[b_pT[b]])
                    def _cp1(b=b, tt=tt):
                        A("act", lambda e, b=b, tt=tt: e.activation(out=hT[:, :, tt * 128:(tt + 1) * 128], in_=pT[b][:].rearrange("p (c t) -> p c t", c=16), func=AF.Copy),
                          reads=[b_pT[b]], writes=[b_hT[tt]])
                    pend1.append(_cp1)
                while pend1:
                    pend1.pop(0)()
            P.barrier()

            with ExitStack() as sc_o:
                P.cur = sc_o
                b_OATh = [Buf() for _ in range(8)]
                _boa = [[Buf() for _ in range(NT)] for _ in range(2)]
                b_oaT = [_boa[h_ % 2] for h_ in range(8)]
                b_obT = [[Buf() for _ in range(NT)] for _ in range(4)]

                if stop_after >= 2:
                  with ExitStack() as ph:
                    P.cur = ph
                    oaTa = [P.sb(f"oaTa{i}", [128, S], BF16) for i in range(2)]
                    wq = [P.sb(f"wq{i}", [128, 16, 128], BF16) for i in range(2)]; b_wq = P.bufs(2)
                    wk = [P.sb(f"wk{i}", [128, 16, 128], BF16) for i in range(2)]; b_wk = P.bufs(2)
                    wv4 = P.sb("wv4", [128, 16, 512], BF16); b_wv4 = Buf()
                    qT = [P.sb(f"qT{i}", [128, S], BF16) for i in range(2)]; b_qT = [P.bufs(4) for _ in range(2)]
                    kT = [P.sb(f"kT{i}", [128, S], BF16) for i in range(2)]; b_kT = [P.bufs(4) for _ in range(2)]
                    Va4 = P.sb("Va4", [128, NT, 4, 130], BF16); b_Va4 = P.bufs(NT)
                    b_Vone = Buf()
                    q1a = [P.sb(f"q1a{i}", [128, S], BF16) for i in range(2)]; b_q1f = P.bufs(2); b_q1g = P.bufs(2)
                    k1a = [P.sb(f"k1a{i}", [128, S], BF16) for i in range(2)]; b_k1f = P.bufs(2); b_k1g = P.bufs(2)
                    b_qTg = P.bufs(2); b_kTg = P.bufs(2)
                    bdiag = P.sb("bdiag", [128, 8, 128], BF16); b_bdiag = Buf()
                    PT = [P.sb(f"PT{i}", [128, NT * 128], BF16) for i in range(2)]; b_PT = [P.bufs(4) for _ in range(2)]
                    om = [P.sb(f"om{i}", [128, 128], F32) for i in range(2)]; b_om = P.bufs(2)
                    rz = [P.sb(f"rz{i}", [128, 1], F32) for i in range(2)]; b_rz = P.bufs(2)
                    ot = P.sb("ot", [128, 128], F32); b_ot = Buf()
                    oab = [P.sb(f"oab{i}", [128, 128], BF16) for i in range(2)]; b_oab = P.bufs(2)
                    junk2 = P.sb("junk2", [128, 128], BF16); b_junk2 = Buf()
                    ss2 = P.sb("ss2", [128, 1], F32); b_ss2 = Buf()
                    rstd2 = P.sb("rstd2", [128, 1], F32); b_rstd2 = Buf()
                    gsub = P.sb("gsub", [128, 128], F32); b_gsub = Buf()
                    lamt = P.sb("lamt", [128, 256], F32); b_lamt = Buf()
                    lprod = P.sb("lprod", [128, 2, 64], F32); b_lprod = Buf()
                    lsum = P.sb("lsum", [128, 2], F32); b_lsum = Buf()
                    neglam = P.sb("neglam", [128, 1], F32); b_neglam = Buf()
                    pQ = [P.ps(f"pQ{i}", [128, 512]) for i in range(2)]; b_pQ = P.bufs(2)
                    pS = [P.ps(f"pS{i}", [128, 512]) for i in range(2)]; b_pS = P.bufs(2)
                    pO = [P.ps(f"pO{i}", [128, 512]) for i in range(2)]; b_pO = P.bufs(2)
                    pTr = P.ps("pTr", [128, 128], BF16); b_pTr = Buf()

                    A("sp", lambda e: e.dma_start(out=bdiag[:], in_=c_bdiag), writes=[b_bdiag], dma=True)
                    A("sp", lambda e: e.dma_start(out=gsub[:], in_=diff_subln.partition_broadcast(128)), writes=[b_gsub], dma=True)
                    A("dve", lambda e: e.tensor_scalar(out=gsub[:], in0=gsub[:], scalar1=1.0 - LAM_INIT, scalar2=None, op0=ALU.mult),
                      reads=[b_gsub], writes=[b_gsub])
                    A("sp", lambda e: e.dma_start(out=lamt[:], in_=diff_lambda.partition_broadcast(128)), writes=[b_lamt], dma=True)
                    A("dve", lambda e: e.tensor_tensor(out=lprod[:, 0, :], in0=lamt[:, 0:64], in1=lamt[:, 64:128], op=ALU.mult), reads=[b_lamt], writes=[b_lprod])
                    A("dve", lambda e: e.tensor_tensor(out=lprod[:, 1, :], in0=lamt[:, 128:192], in1=lamt[:, 192:256], op=ALU.mult), reads=[b_lamt, b_lprod], writes=[b_lprod])
                    A("dve", lambda e: e.reduce_sum(out=lsum[:], in_=lprod[:], axis=AX.X), reads=[b_lprod], writes=[b_lsum])
                    A("act", lambda e: e.activation(out=lsum[:], in_=lsum[:], func=AF.Exp), reads=[b_lsum], writes=[b_lsum])
                    A("dve", lambda e: e.tensor_tensor(out=neglam[:], in0=lsum[:, 0:1], in1=lsum[:, 1:2], op=ALU.subtract), reads=[b_lsum], writes=[b_neglam])
                    A("dve", lambda e: e.tensor_scalar(out=neglam[:], in0=neglam[:], scalar1=LAM_INIT, scalar2=-1.0, op0=ALU.add, op1=ALU.mult),
                      reads=[b_neglam], writes=[b_neglam])
                    A("dve", lambda e: e.memset(Va4[:, :, :, 128:130], 1.0), writes=[b_Vone])

                    ev = 0
                    for h in range(8):
                        b = h % 2
                        A("pool", lambda e, b=b, h=h: e.dma_start(out=wq[b][:], in_=w_in_v[:, :, h * 128:(h + 1) * 128]), writes=[b_wq[b]], dma=True)
                        A("pool", lambda e, b=b, h=h: e.dma_start(out=wk[b][:], in_=w_in_v[:, :, 1024 + h * 128:1024 + (h + 1) * 128]), writes=[b_wk[b]], dma=True)
                        if h % 4 == 0:
                            A("pool", lambda e, h=h: e.dma_start(out=wv4[:], in_=w_in_v[:, :, 2048 + h * 128:2048 + (h + 4) * 128]), writes=[b_wv4], dma=True)
                        for (w_, bw, dst, bdst) in ((wq, b_wq, qT, b_qT), (wk, b_wk, kT, b_kT)):
                            for g in range(4):
                                pb = ev % 2; ev += 1
                                for c in range(16):
                                    A("pe", lambda e, pb=pb, c=c, g=g, w_=w_, b=b: e.matmul(pQ[pb][:, :], lhsT=w_[b][:, c, :], rhs=hT[:, c, g * 512:(g + 1) * 512], start=(c == 0), stop=(c == 15)),
                                      reads=[bw[b]] + b_hT[g * 4:(g + 1) * 4], writes=[b_pQ[pb]])
                                bg_ = b_qTg[b] if dst is qT else b_kTg[b]
                                if pb == 0:
                                    A("act", lambda e, pb=pb, g=g, dst=dst, b=b: e.activation(out=dst[b][:, g * 512:(g + 1) * 512], in_=pQ[pb][:, :], func=AF.Copy),
                                      reads=[b_pQ[pb]], writes=[bdst[b][g], bg_])
                                else:
                                    A("dve", lambda e, pb=pb, g=g, dst=dst, b=b: e.tensor_copy(out=dst[b][:, g * 512:(g + 1) * 512], in_=pQ[pb][:, :]),
                                      reads=[b_pQ[pb]], writes=[bdst[b][g], bg_])
                        A("sp", lambda e, b=b: e.dma_start(out=q1a[b][0:64, :], in_=qT[b][64:128, :]), reads=b_qT[b] + [b_qTg[b]], writes=[b_q1f[b]], dma=True)
                        A("sp", lambda e, b=b: e.dma_start(out=k1a[b][0:64, :], in_=kT[b][64:128, :]), reads=b_kT[b] + [b_kTg[b]], writes=[b_k1f[b]], dma=True)
                        A("sp", lambda e, b=b, h=h: e.dma_start(out=q1a[b][64:68, :], in_=c_qaug[h]), writes=[b_q1g[b]], dma=True)
                        A("sp", lambda e, b=b, h=h: e.dma_start(out=k1a[b][64:68, :], in_=c_kaug[h]), writes=[b_k1g[b]], dma=True)
                        A("sp", lambda e, b=b, h=h: e.dma_start(out=qT[b][64:68, :], in_=c_qaug[h]), writes=[b_qTg[b]], dma=True)
                        A("sp", lambda e, b=b, h=h: e.dma_start(out=kT[b][64:68, :], in_=c_kaug[h]), writes=[b_kTg[b]], dma=True)
                        if h % 4 == 0:
                            for tt in range(NT):
                                pb = ev % 2; ev += 1
                                for c in range(16):
                                    A("pe", lambda e, pb=pb, c=c, tt=tt: e.matmul(pQ[pb][:, :], lhsT=hT[:, c, tt * 128:(tt + 1) * 128], rhs=wv4[:, c, :], start=(c == 0), stop=(c == 15)),
                                      reads=[b_wv4, b_hT[tt]], writes=[b_pQ[pb]])
                                if pb == 0:
                                    A("act", lambda e, pb=pb, tt=tt: e.activation(out=Va4[:, tt, :, 0:128], in_=pQ[pb][:].rearrange("p (a n) -> p a n", a=4), func=AF.Copy),
                                      reads=[b_pQ[pb]], writes=[b_Va4[tt]])
                                else:
                                    A("dve", lambda e, pb=pb, tt=tt: e.tensor_copy(out=Va4[:, tt, :, 0:128], in_=pQ[pb][:].rearrange("p (a n) -> p a n", a=4)),
                                      reads=[b_pQ[pb]], writes=[b_Va4[tt]])
                        if h == 0:
                            dump("d_qT", qT[b][:], [128, S], BF16, b_qT[b])
                            dump("d_kT", kT[b][:], [128, S], BF16, b_kT[b])
                            dump("d_neglam", neglam[:], [128, 1], F32, [b_neglam])
                        def stQK(j, m, h=h, b=b):
                            pt = m
                            nblk = j + 1
                            for gi in range((nblk + 3) // 4):
                                sb_ = (j * 2 + m + gi) % 2
                                i0 = gi * 4
                                i1 = min(nblk, i0 + 4)
                                for i in range(i0, i1):
                                    col = (i - i0) * 128
                                    kop = kT[b] if m == 0 else k1a[b]
                                    qop = qT[b] if m == 0 else q1a[b]
                                    rds = ([b_kT[b][i // 4], b_qT[b][j // 4], b_kTg[b], b_qTg[b]] if m == 0 else [b_k1f[b], b_q1f[b], b_k1g[b], b_q1g[b]])
                                    kk = 68 if i < j else 64
                                    A("pe", lambda e, sb_=sb_, col=col, i=i, j=j, kop=kop, qop=qop, kk=kk: e.matmul(
                                        pS[sb_][:, col:col + 128], lhsT=kop[0:kk, i * 128:(i + 1) * 128],
                                        rhs=qop[0:kk, j * 128:(j + 1) * 128], start=True, stop=(i < j)),
                                      reads=rds, writes=[b_pS[sb_]])
                                    if i == j:
                                        A("pe", lambda e, sb_=sb_, col=col, h=h: e.matmul(
                                            pS[sb_][:, col:col + 128], lhsT=identb[:], rhs=bdiag[:, h, :], start=False, stop=True),
                                          reads=[b_identb, b_bdiag], writes=[b_pS[sb_]])
                                ncol = (i1 - i0) * 128
                                A("act", lambda e, sb_=sb_, pt=pt, i0=i0, ncol=ncol: e.activation(
                                    out=PT[pt][:, i0 * 128:i0 * 128 + ncol], in_=pS[sb_][:, 0:ncol], func=AF.Exp, scale=0.125),
                                  reads=[b_pS[sb_]], writes=[b_PT[pt][gi]])

                        def stPV(j, m, h=h, b=b):
                            pt = m
                            ob_ = m
                            for i in range(j + 1):
                                A("pe", lambda e, ob_=ob_, pt=pt, i=i, j=j, b=b: e.matmul(
                                    pO[ob_][:, 0:129], lhsT=PT[pt][:, i * 128:(i + 1) * 128], rhs=Va4[:, i, h % 4, 0:129], start=(i == 0), stop=(i == j)),
                                  reads=[b_PT[pt][i // 4], b_Va4[i], b_Vone], writes=[b_pO[ob_]])
                            A("dve", lambda e, ob_=ob_, m=m: e.reciprocal(out=rz[m][:], in_=pO[ob_][:, 128:129]), reads=[b_pO[ob_]], writes=[b_rz[m]])
                            A("dve", lambda e, ob_=ob_, m=m: e.tensor_scalar(out=om[m][:], in0=pO[ob_][:, 0:128], scalar1=rz[m][:, 0:1], scalar2=None, op0=ALU.mult),
                              reads=[b_pO[ob_], b_rz[m]], writes=[b_om[m]])
                            if m == 1:
                                A("dve", lambda e: e.scalar_tensor_tensor(out=ot[:], in0=om[1][:], scalar=neglam[:, 0:1], in1=om[0][:], op0=ALU.mult, op1=ALU.add),
                                  reads=[b_om[0], b_om[1], b_neglam], writes=[b_ot])
                                rms_stats(ot[:], b_ot, ss2[:], b_ss2, rstd2[:], b_rstd2, junk2[:], b_junk2, 128)
                                ab = j % 2
                                A("dve", lambda e, ab=ab: e.scalar_tensor_tensor(out=oab[ab][:], in0=ot[:], scalar=rstd2[:, 0:1], in1=gsub[:], op0=ALU.mult, op1=ALU.mult),
                                  reads=[b_ot, b_rstd2, b_gsub], writes=[b_oab[ab]])
                                def _tr(ab=ab, h=h, j=j):
                                    A("pe", lambda e, ab=ab: e.transpose(out=pTr[:, :], in_=oab[ab][:], identity=identb[:]), reads=[b_oab[ab], b_identb], writes=[b_pTr])
                                    A("act", lambda e, h=h, j=j: e.activation(out=oaTa[h % 2][:, j * 128:(j + 1) * 128], in_=pTr[:, :], func=AF.Copy),
                                      reads=[b_pTr], writes=[b_oaT[h][j]])
                                pend_tr.append(_tr)

                        units = [(j, m) for j in range(NT) for m in range(2)]
                        pend_tr = []
                        stQK(*units[0])
                        for ui, (j, m) in enumerate(units):
                            if ui + 1 < len(units):
                                stQK(*units[ui + 1])
                            if m == 1 and pend_tr:
                                pend_tr.pop(0)()
                            stPV(j, m)
                        while pend_tr:
                            pend_tr.pop(0)()
                        A("sp", lambda e, h=h: e.dma_start(out=OAT[:, h, :], in_=oaTa[h % 2][:]), reads=b_oaT[h], writes=[b_OATh[h]], dma=True)
                  P.barrier()
                P.cur = sc_o
                obT = P.sb("obT", [128, 8, S], BF16)

                if stop_after >= 3:
                  with ExitStack() as ph:
                    P.cur = ph
                    Wz = [P.sb(f"Wz{i}", [128, 16, 256], BF16) for i in range(2)]; b_Wz = P.bufs(2)
                    vg = P.sb("vg", [128, NT, 1024], BF16); b_vg = [P.bufs(4) for _ in range(NT)]
                    gn = P.sb("gn", [128, 1024], F32); b_gn = Buf()
                    wsf = P.sb("wsf", [128, 8, 128], F32); b_wsf = Buf()
                    tri = P.sb("tri", [128, 128], F32); b_tri = Buf()
                    wsT = P.sb("wsT", [128, 8, 128], BF16); b_wsT = Buf()
                    bsT = P.sb("bsT", [128, 8], F32); b_bsT = Buf()
                    ssv = P.sb("ssv", [128, NT, 4], F32); b_ssv = [P.bufs(4) for _ in range(NT)]
                    sv1 = P.sb("sv1", [128, 1], F32); b_sv1 = Buf()
                    rsv = P.sb("rsv", [128, 1], F32); b_rsv = Buf()
                    junk3 = P.sb("junk3", [128, 256], BF16); b_junk3 = Buf()
                    ug = [P.sb(f"ug{i}", [128, 256], F32) for i in range(2)]; b_ug = P.bufs(2)
                    tmp = [P.sb(f"tmp{i}", [128, 256], F32) for i in range(2)]; b_tmp = P.bufs(2)
                    obb = [P.sb(f"obb{i}", [128, 256], BF16) for i in range(2)]; b_obb = P.bufs(2)
                    pZ = [P.ps(f"pZ{i}", [128, 512]) for i in range(2)]; b_pZ = P.bufs(2)
                    pSV = [P.ps(f"pSV{i}", [128, 512]) for i in range(2)]; b_pSV = P.bufs(2)
                    pW = P.ps("pW", [128, 512]); b_pW = Buf()
                    pTb = P.ps("pTb", [128, 256], BF16); b_pTb = Buf()

                    A("sp", lambda e: e.dma_start(out=gn[:], in_=gmlp_norm.partition_broadcast(128)), writes=[b_gn], dma=True)
                    A("sp", lambda e: e.dma_start(out=wsf[:], in_=gmlp_ws.rearrange("g t s -> t g s")), writes=[b_wsf], dma=True)
                    A("sp", lambda e: e.dma_start(out=tri[:], in_=c_tri), writes=[b_tri], dma=True)
                    A("sp", lambda e: e.dma_start(out=bsT[:], in_=gmlp_bs.rearrange("g t -> t g"), allow_slow_non_contiguous=True), writes=[b_bsT], dma=True)
                    A("dve", lambda e: e.tensor_tensor(out=wsf[:], in0=wsf[:], in1=tri[:].unsqueeze(1).broadcast_to([128, 8, 128]), op=ALU.mult),
                      reads=[b_wsf, b_tri], writes=[b_wsf])
                    for g0 in range(0, 8, 4):
                        for gg in range(4):
                            A("pe", lambda e, g0=g0, gg=gg: e.transpose(out=pW[:, gg * 128:(gg + 1) * 128], in_=wsf[:, g0 + gg, :], identity=identf[:]),
                              reads=[b_wsf, b_identf], writes=[b_pW])
                        A("dve", lambda e, g0=g0: e.tensor_copy(out=wsT[:, g0:g0 + 4, :], in_=pW[:].rearrange("p (g t) -> p g t", g=4)), reads=[b_pW], writes=[b_wsT])

                    zc = 0
                    for vb in range(4):
                        wb_ = zc % 2; zc += 1
                        c0 = 3072 + 1024 + vb * 256
                        A("pool", lambda e, wb_=wb_, c0=c0: e.dma_start(out=Wz[wb_][:], in_=w_in_v[:, :, c0:c0 + 256]), writes=[b_Wz[wb_]], dma=True)
                        for tt in range(NT):
                            zb = tt % 2
                            for c in range(16):
                                A("pe", lambda e, zb=zb, c=c, tt=tt, wb_=wb_: e.matmul(pZ[zb][:, 0:256], lhsT=hT[:, c, tt * 128:(tt + 1) * 128], rhs=Wz[wb_][:, c, :], start=(c == 0), stop=(c == 15)),
                                  reads=[b_Wz[wb_], b_hT[tt]], writes=[b_pZ[zb]])
                            A("act", lambda e, zb=zb, tt=tt, vb=vb: e.activation(out=vg[:, tt, vb * 256:(vb + 1) * 256], in_=pZ[zb][:, 0:256], func=AF.Gelu),
                              reads=[b_pZ[zb]], writes=[b_vg[tt][vb]])
                            A("act", lambda e, tt=tt, vb=vb: e.activation(out=junk3[:], in_=vg[:, tt, vb * 256:(vb + 1) * 256], func=AF.Square, scale=1.0 / 32.0, accum_out=ssv[:, tt, vb:vb + 1]),
                              reads=[b_vg[tt][vb]], writes=[b_junk3, b_ssv[tt][vb]])
                    for tt in range(NT):
                        A("dve", lambda e, tt=tt: e.reduce_sum(out=sv1[:], in_=ssv[:, tt, :], axis=AX.X), reads=b_ssv[tt], writes=[b_sv1])
                        A("act", lambda e: e.activation(out=sv1[:], in_=sv1[:], func=AF.Sqrt, bias=epsT[:], scale=1.0), reads=[b_sv1, b_eps], writes=[b_sv1])
                        A("dve", lambda e: e.reciprocal(out=rsv[:], in_=sv1[:]), reads=[b_sv1], writes=[b_rsv])
                        A("dve", lambda e, tt=tt: e.scalar_tensor_tensor(out=vg[:, tt, :], in0=vg[:, tt, :], scalar=rsv[:, 0:1], in1=gn[:], op0=ALU.mult, op1=ALU.mult),
                          reads=b_vg[tt] + [b_rsv, b_gn], writes=b_vg[tt])
                    pend3 = []
                    for ub in range(4):
                        wb_ = zc % 2; zc += 1
                        c0 = 3072 + ub * 256
                        A("pool", lambda e, wb_=wb_, c0=c0: e.dma_start(out=Wz[wb_][:], in_=w_in_v[:, :, c0:c0 + 256]), writes=[b_Wz[wb_]], dma=True)
                        for tt in range(NT):
                            zb = tt % 2
                            for c in range(16):
                                A("pe", lambda e, zb=zb, c=c, tt=tt, wb_=wb_: e.matmul(pZ[zb][:, 0:256], lhsT=hT[:, c, tt * 128:(tt + 1) * 128], rhs=Wz[wb_][:, c, :], start=(c == 0), stop=(c == 15)),
                                  reads=[b_Wz[wb_], b_hT[tt]], writes=[b_pZ[zb]])
                            for gg in range(2):
                                g = ub * 2 + gg
                                A("pe", lambda e, zb=zb, gg=gg, g=g, tt=tt: e.matmul(pSV[zb][:, gg * 128:(gg + 1) * 128], lhsT=wsT[:, g, :], rhs=vg[:, tt, g * 128:(g + 1) * 128], start=True, stop=True),
                                  reads=[b_wsT, b_vg[tt][g // 2]], writes=[b_pSV[zb]])
                            while pend3:
                                pend3.pop(0)()
                            A("act", lambda e, zb=zb: e.activation(out=ug[zb][:], in_=pZ[zb][:, 0:256], func=AF.Gelu), reads=[b_pZ[zb]], writes=[b_ug[zb]])
                            A("dve", lambda e, zb=zb, ub=ub: e.tensor_tensor(out=tmp[zb][:].rearrange("p (g c) -> p g c", g=2), in0=pSV[zb][:, 0:256].rearrange("p (g c) -> p g c", g=2),
                                                                             in1=bsT[:, ub * 2:(ub + 1) * 2].unsqueeze(2).broadcast_to([128, 2, 128]), op=ALU.add),
                              reads=[b_pSV[zb], b_bsT], writes=[b_tmp[zb]])
                            A("dve", lambda e, zb=zb: e.tensor_tensor(out=obb[zb][:], in0=tmp[zb][:], in1=ug[zb][:], op=ALU.mult), reads=[b_tmp[zb], b_ug[zb]], writes=[b_obb[zb]])
                            def _tr3(zb=zb, ub=ub, tt=tt):
                                for gg in range(2):
                                    A("pe", lambda e, zb=zb, gg=gg: e.transpose(out=pTb[:, gg * 128:(gg + 1) * 128], in_=obb[zb][:, gg * 128:(gg + 1) * 128], identity=identb[:]),
                                      reads=[b_obb[zb], b_identb], writes=[b_pTb])
                                A("act", lambda e, ub=ub, tt=tt: e.activation(out=obT[:, ub * 2:(ub + 1) * 2, tt * 128:(tt + 1) * 128], in_=pTb[:].rearrange("p (g t) -> p g t", g=2), func=AF.Copy),
                                  reads=[b_pTb], writes=[b_obT[ub][tt]])
                            pend3.append(_tr3)
                    while pend3:
                        pend3.pop(0)()
                    if "OBT" in dbg:
                        for g in range(8):
                            A("sp", lambda e, g=g: e.dma_start(out=OBT[:, g, :], in_=obT[:, g, :]), reads=b_obT[g // 2], dma=True)
                  P.barrier()
                P.cur = sc_o
                oaT = P.sb("oaT2", [128, 8, S], BF16); b_oaT2 = [Buf() for _ in range(8)]

                if stop_after >= 4:
                  with ExitStack() as ph:
                    P.cur = ph
                    Wa = [P.sb(f"Wa{i}", [128, 8, 128], BF16) for i in range(2)]; b_Wa = P.bufs(2)
                    Wb = [P.sb(f"Wb{i}", [128, 8, 128], BF16) for i in range(2)]; b_Wb = P.bufs(2)
                    Wga = [P.sb(f"Wga{i}", [128, 16, 128], BF16) for i in range(2)]; b_Wga = P.bufs(2)
                    Wgb = [P.sb(f"Wgb{i}", [128, 16, 128], BF16) for i in range(2)]; b_Wgb = P.bufs(2)
                    sga = [P.sb(f"sga{i}", [128, 512], F32) for i in range(2)]; b_sga = P.bufs(2)
                    sgb = [P.sb(f"sgb{i}", [128, 512], F32) for i in range(2)]; b_sgb = P.bufs(2)
                    mst = [P.sb(f"mst{i}", [128, S], BF16) for i in range(2)]; b_mst = [P.bufs(4) for _ in range(2)]
                    pA = [P.ps(f"pA{i}", [128, 512]) for i in range(2)]; b_pA = P.bufs(2)
                    pB = [P.ps(f"pB{i}", [128, 512]) for i in range(2)]; b_pB = P.bufs(2)
                    pGA = [P.ps(f"pGA{i}", [128, 512]) for i in range(2)]; b_pGA = P.bufs(2)
                    pGB = [P.ps(f"pGB{i}", [128, 512]) for i in range(2)]; b_pGB = P.bufs(2)
                    wa_v = w_a.rearrange("(c p) n -> p c n", p=128)
                    wb_v = w_b.rearrange("(c p) n -> p c n", p=128)
                    for h in range(8):
                        A("sp", lambda e, h=h: e.dma_start(out=oaT[:, h, :], in_=OAT[:, h, :]), reads=[b_OATh[h]], writes=[b_oaT2[h]], dma=True)
                    it = 0
                    for n in range(16):
                        wb_ = n % 2
                        A("pool", lambda e, wb_=wb_, n=n: e.dma_start(out=Wa[wb_][:], in_=wa_v[:, :, n * 128:(n + 1) * 128]), writes=[b_Wa[wb_]], dma=True)
                        A("pool", lambda e, wb_=wb_, n=n: e.dma_start(out=Wb[wb_][:], in_=wb_v[:, :, n * 128:(n + 1) * 128]), writes=[b_Wb[wb_]], dma=True)
                        A("pool", lambda e, wb_=wb_, n=n: e.dma_start(out=Wga[wb_][:], in_=w_in_v[:, :, 5120 + n * 128:5120 + (n + 1) * 128]), writes=[b_Wga[wb_]], dma=True)
                        A("pool", lambda e, wb_=wb_, n=n: e.dma_start(out=Wgb[wb_][:], in_=w_in_v[:, :, 7168 + n * 128:7168 + (n + 1) * 128]), writes=[b_Wgb[wb_]], dma=True)
                        for g in range(4):
                            pb = it % 2; it += 1
                            gs = slice(g * 512, (g + 1) * 512)
                            for c in range(8):
                                A("pe", lambda e, pb=pb, c=c, gs=gs, wb_=wb_: e.matmul(pA[pb][:, :], lhsT=Wa[wb_][:, c, :], rhs=oaT[:, c, gs], start=(c == 0), stop=(c == 7)),
                                  reads=[b_Wa[wb_], b_oaT2[c]], writes=[b_pA[pb]])
                            for c in range(8):
                                A("pe", lambda e, pb=pb, c=c, gs=gs, wb_=wb_: e.matmul(pB[pb][:, :], lhsT=Wb[wb_][:, c, :], rhs=obT[:, c, gs], start=(c == 0), stop=(c == 7)),
                                  reads=[b_Wb[wb_]], writes=[b_pB[pb]])
                            for c in range(16):
                                A("pe", lambda e, pb=pb, c=c, gs=gs, wb_=wb_: e.matmul(pGA[pb][:, :], lhsT=Wga[wb_][:, c, :], rhs=hT[:, c, gs], start=(c == 0), stop=(c == 15)),
                                  reads=[b_Wga[wb_]], writes=[b_pGA[pb]])
                            for c in range(16):
                                A("pe", lambda e, pb=pb, c=c, gs=gs, wb_=wb_: e.matmul(pGB[pb][:, :], lhsT=Wgb[wb_][:, c, :], rhs=hT[:, c, gs], start=(c == 0), stop=(c == 15)),
                                  reads=[b_Wgb[wb_]], writes=[b_pGB[pb]])
                            A("act", lambda e, pb=pb: e.activation(out=sga[pb][:], in_=pGA[pb][:, :], func=AF.Sigmoid), reads=[b_pGA[pb]], writes=[b_sga[pb]])
                            A("act", lambda e, pb=pb: e.activation(out=sgb[pb][:], in_=pGB[pb][:, :], func=AF.Sigmoid), reads=[b_pGB[pb]], writes=[b_sgb[pb]])
                            A("dve", lambda e, pb=pb: e.tensor_tensor(out=sga[pb][:], in0=pA[pb][:, :], in1=sga[pb][:], op=ALU.mult), reads=[b_pA[pb], b_sga[pb]], writes=[b_sga[pb]])
                            A("dve", lambda e, pb=pb: e.tensor_tensor(out=sgb[pb][:], in0=pB[pb][:, :], in1=sgb[pb][:], op=ALU.mult), reads=[b_pB[pb], b_sgb[pb]], writes=[b_sgb[pb]])
                            A("pool", lambda e, pb=pb, wb_=wb_, gs=gs: e.tensor_tensor(out=mst[wb_][:, gs], in0=sga[pb][:], in1=sgb[pb][:], op=ALU.add),
                              reads=[b_sga[pb], b_sgb[pb]], writes=[b_mst[wb_][g]])
                        A("sp", lambda e, wb_=wb_, n=n: e.dma_start(out=MT[:, n, :], in_=mst[wb_][:]), reads=b_mst[wb_], writes=[b_MT[n]], dma=True)
                  P.barrier()
        P.cur = es
        P.barrier()

        if stop_after >= 5:
          with ExitStack() as ph:
            P.cur = ph
            mT = P.sb("mT", [128, 16, S], BF16); b_mT = P.bufs(16)
            Wo = [P.sb(f"Wo{i}", [128, 16, 512], BF16) for i in range(2)]; b_Wo = P.bufs(2)
            xp = [P.sb(f"xp{i}", [128, 512], F32) for i in range(2)]; b_xp = P.bufs(2)
            x2p = [P.sb(f"x2p{i}", [128, 512], F32) for i in range(2)]; b_x2p = P.bufs(2)
            pX = [P.ps(f"pX{i}", [128, 512]) for i in range(2)]; b_pX = P.bufs(2)
            wo_v = w_out.rearrange("(c p) n -> p c n", p=128)
            for c in range(16):
                A("sp", lambda e, c=c: e.dma_start(out=mT[:, c, :], in_=MT[:, c, :]), reads=[b_MT[c]], writes=[b_mT[c]], dma=True)
            it = 0
            for blk in range(4):
                wb_ = blk % 2
                A("pool", lambda e, wb_=wb_, blk=blk: e.dma_start(out=Wo[wb_][:], in_=wo_v[:, :, blk * 512:(blk + 1) * 512]), writes=[b_Wo[wb_]], dma=True)
                for tt in range(NT):
                    pb = it % 2; it += 1
                    A("sp", lambda e, pb=pb, tt=tt, blk=blk: e.dma_start(out=xp[pb][:], in_=x[tt * 128:(tt + 1) * 128, blk * 512:(blk + 1) * 512]), writes=[b_xp[pb]], dma=True)
                    for c in range(16):
                        A("pe", lambda e, pb=pb, c=c, tt=tt, wb_=wb_: e.matmul(pX[pb][:, :], lhsT=mT[:, c, tt * 128:(tt + 1) * 128], rhs=Wo[wb_][:, c, :], start=(c == 0), stop=(c == 15)),
                          reads=[b_Wo[wb_], b_mT[c]], writes=[b_pX[pb]])
                    A("dve", lambda e, pb=pb: e.tensor_tensor(out=x2p[pb][:], in0=pX[pb][:, :], in1=xp[pb][:], op=ALU.add), reads=[b_pX[pb], b_xp[pb]], writes=[b_x2p[pb]])
                    A("sp", lambda e, pb=pb, tt=tt, blk=blk: e.dma_start(out=X2[tt * 128:(tt + 1) * 128, blk * 512:(blk + 1) * 512], in_=x2p[pb][:]),
                      reads=[b_x2p[pb]], writes=[b_X2[tt][blk]], dma=True)
          P.barrier()
          with ExitStack() as ph:
            P.cur = ph
            gF = P.sb("gF", [128, D], F32); b_gF = Buf()
            xt_5 = [P.sb(f"x2t{i}", [128, D], F32) for i in range(3)]; b_xt = P.bufs(3)
            junk_5 = P.sb("junk5", [128, D], BF16); b_junk = Buf()
            ss_5 = [P.sb(f"ss5{i}", [128, 1], F32) for i in range(3)]; b_ss = P.bufs(3)
            rstd_5 = [P.sb(f"rstd5{i}", [128, 1], F32) for i in range(3)]; b_rstd = P.bufs(3)
            hb_5 = [P.sb(f"hb5{i}", [128, D], BF16) for i in range(3)]; b_hb = P.bufs(3)
            hs = [P.sb(f"hs5{i}", [128, 16, 128], BF16) for i in range(3)]; b_hs = P.bufs(3)
            pT_5 = [P.ps(f"pT5{i}", [128, D], BF16) for i in range(3)]; b_pT = P.bufs(3)
            A("sp", lambda e: e.dma_start(out=gF[:], in_=ffn_norm.partition_broadcast(128)), writes=[b_gF], dma=True)
            pend5 = []
            for tt in range(NT):
                b = tt % 3
                A("sp", lambda e, tt=tt, b=b: e.dma_start(out=xt_5[b][:], in_=X2[tt * 128:(tt + 1) * 128, :]), reads=b_X2[tt], writes=[b_xt[b]], dma=True)
                rms_stats(xt_5[b][:], b_xt[b], ss_5[b][:], b_ss[b], rstd_5[b][:], b_rstd[b], junk_5[:], b_junk, D)
                while pend5:
                    pend5.pop(0)()
                A("dve", lambda e, b=b: e.scalar_tensor_tensor(out=hb_5[b][:], in0=xt_5[b][:], scalar=rstd_5[b][:, 0:1], in1=gF[:], op0=ALU.mult, op1=ALU.mult),
                  reads=[b_xt[b], b_rstd[b], b_gF], writes=[b_hb[b]])
                for c in range(16):
                    A("pe", lambda e, b=b, c=c: e.transpose(out=pT_5[b][:, c * 128:(c + 1) * 128], in_=hb_5[b][:, c * 128:(c + 1) * 128], identity=identb[:]),
                      reads=[b_hb[b], b_identb], writes=[b_pT[b]])
                def _cp5(b=b, tt=tt):
                    A("act", lambda e, b=b: e.activation(out=hs[b][:], in_=pT_5[b][:].rearrange("p (c t) -> p c t", c=16), func=AF.Copy), reads=[b_pT[b]], writes=[b_hs[b]])
                    A("sp", lambda e, b=b, tt=tt: e.dma_start(out=H2T[:, :, tt * 128:(tt + 1) * 128], in_=hs[b][:]), reads=[b_hs[b]], writes=[b_H2T[tt]], dma=True)
                pend5.append(_cp5)
            while pend5:
                pend5.pop(0)()
          P.barrier()

        if stop_after >= 6:
          with ExitStack() as sc_q:
            P.cur = sc_q
            qpT = P.sb("qpT", [128, 16, S], BF16); b_qpT = [P.bufs(4) for _ in range(16)]
            with ExitStack() as ph:
                P.cur = ph
                h2T = P.sb("h2T", [128, 16, S], BF16); b_h2T = P.bufs(16)
                Wp = [P.sb(f"Wp{i}", [128, 16, 128], BF16) for i in range(2)]; b_Wp = P.bufs(2)
                pQ_6 = [P.ps(f"pQp{i}", [128, 512]) for i in range(2)]; b_pQ = P.bufs(2)
                wp_v = peer_wq.rearrange("(c p) n -> p c n", p=128)
                for c in range(16):
                    A("sp", lambda e, c=c: e.dma_start(out=h2T[:, c, :], in_=H2T[:, c, :]), reads=b_H2T, writes=[b_h2T[c]], dma=True)
                it = 0
                for n in range(16):
                    wb_ = n % 2
                    A("pool", lambda e, wb_=wb_, n=n: e.dma_start(out=Wp[wb_][:], in_=wp_v[:, :, n * 128:(n + 1) * 128]), writes=[b_Wp[wb_]], dma=True)
                    for g in range(4):
                        pb = it % 2; it += 1
                        for c in range(16):
                            A("pe", lambda e, pb=pb, c=c, g=g, wb_=wb_: e.matmul(pQ_6[pb][:, :], lhsT=Wp[wb_][:, c, :], rhs=h2T[:, c, g * 512:(g + 1) * 512], start=(c == 0), stop=(c == 15)),
                              reads=[b_Wp[wb_], b_h2T[c]], writes=[b_pQ[pb]])
                        if pb == 0:
                            A("act", lambda e, pb=pb, n=n, g=g: e.activation(out=qpT[:, n, g * 512:(g + 1) * 512], in_=pQ_6[pb][:, :], func=AF.Copy), reads=[b_pQ[pb]], writes=[b_qpT[n][g]])
                        else:
                            A("dve", lambda e, pb=pb, n=n, g=g: e.tensor_copy(out=qpT[:, n, g * 512:(g + 1) * 512], in_=pQ_6[pb][:, :]), reads=[b_pQ[pb]], writes=[b_qpT[n][g]])
            P.barrier()
            with ExitStack() as ph:
                P.cur = ph
                skf = P.sb("skf", [128, 2, 128], F32); b_skf = Buf()
                skT = P.sb("skT", [128, 2, 128], BF16); b_skT = Buf()
                sc = [P.sb(f"sc{i}", [128, 16, 128], F32) for i in range(2)]; b_sc = [P.bufs(16) for _ in range(2)]
                v1 = P.sb("v1", [128, 8, 16], F32); b_v1 = P.bufs(8)
                v2 = P.sb("v2", [128, 8, 16], F32); b_v2 = P.bufs(8)
                wk1 = P.sb("wk1", [128, 128], F32); b_wk1 = Buf()
                wk2 = P.sb("wk2", [128, 128], F32); b_wk2 = Buf()
                cand = P.sb("cand", [128, 8, 256], F32); b_cand = P.bufs(8)
                cw = P.sb("cw", [128, 256], F32); b_cw = Buf()
                cw2 = P.sb("cw2", [128, 256], F32); b_cw2 = Buf()
                t17 = P.sb("t17", [128, 8, 8], F32); b_t17 = P.bufs(8)
                thrm = P.sb("thrm", [128, 8], F32); b_thrm = Buf()
                top = P.sb("top", [128, 8, 16], F32); b_top = P.bufs(8)
                negmx = P.sb("negmx", [128, 8], F32); b_negmx = Buf()
                Zt = P.sb("Zt", [128, 8], F32); b_Zt = P.bufs(8)
                rZ = P.sb("rZ", [128, 8], F32); b_rZ = Buf()
                junk6 = P.sb("junk6", [128, 16], F32); b_junk6 = Buf()
                d1 = P.sb("d1", [128, 8, 16], F32); b_d1 = Buf()
                Q4 = P.sb("Q4", [128, 4, 128], F32); b_Q4 = P.bufs(4)
                sT = [P.sb(f"sT{i}", [128, 4, 128], F32) for i in range(2)]; b_sT = P.bufs(2)
                qrep = [P.sb(f"qrep{i}", [128, 16, 2, 128], BF16) for i in range(2)]; b_qrep = [P.bufs(2) for _ in range(2)]
                Eb = [P.sb(f"Eb{i}", [128, 2, 128], F32) for i in range(2)]; b_Eb = [P.bufs(2) for _ in range(2)]
                lhsG = [P.sb(f"lhsG{i}", [128, 2, 128], BF16) for i in range(3)]; b_lhsG = P.bufs(3)
                rhsG = [P.sb(f"rhsG{i}", [128, 2, 128], BF16) for i in range(3)]; b_rhsG = [P.bufs(2) for _ in range(3)]
                SG = [P.sb(f"SG{i}", [128, 128, 128], BF16) for i in range(2)]; b_SG = [P.bufs(32) for _ in range(2)]
                pSc = [P.ps("pSc0", [128, 512])] * 2; b_pSc = [Buf()] * 2
                pT4 = P.ps("pT4", [128, 512]); b_pT4 = Buf()
                pD = [P.ps(f"pD{i}", [128, 512]) for i in range(2)]; b_pD = P.bufs(2)
                pAa = [P.ps(f"pAa{i}", [128, 512]) for i in range(2)]; b_pAa = P.bufs(2)
                pG = [P.ps(f"pG{i}", [128, 512]) for i in range(2)]; b_pG = P.bufs(2)

                A("sp", lambda e: e.dma_start(out=skf[:], in_=peer_sk.rearrange("p n d -> n p d")), writes=[b_skf], dma=True)
                for p_ in range(2):
                    A("pe", lambda e, p_=p_: e.transpose(out=pT4[:, p_ * 128:(p_ + 1) * 128], in_=skf[:, p_, :], identity=identf[:]), reads=[b_skf, b_identf], writes=[b_pT4])
                A("dve", lambda e: e.tensor_copy(out=skT[:], in_=pT4[:, 0:256].rearrange("d (p n) -> d p n", p=2)), reads=[b_pT4], writes=[b_skT])

                def front_scores(tt):
                    ts_ = slice(tt * 128, (tt + 1) * 128)
                    for r in range(4):
                        pb = r % 2
                        for q_ in range(4):
                            hp = r * 4 + q_
                            A("pe", lambda e, pb=pb, q_=q_, hp=hp, ts_=ts_: e.matmul(pSc[pb][:, q_ * 128:(q_ + 1) * 128], lhsT=qpT[:, hp, ts_], rhs=skT[:, hp % 2, :], start=True, stop=True),
                              reads=[b_qpT[hp][tt // 4], b_skT], writes=[b_pSc[pb]])
                        A("act", lambda e, pb=pb, r=r: e.activation(out=sc[tt % 2][:, r * 4:(r + 1) * 4, :], in_=pSc[pb][:].rearrange("p (a n) -> p a n", a=4), func=AF.Copy),
                          reads=[b_pSc[pb]], writes=b_sc[tt % 2][r * 4:(r + 1) * 4])

                def front_topk(tt):
                    for h in range(8):
                        for (src, vv, bvv, wkk, bwk) in ((2 * h, v1, b_v1, wk1, b_wk1), (2 * h + 1, v2, b_v2, wk2, b_wk2)):
                            A("dve", lambda e, src=src, vv=vv, h=h: e.max(out=vv[:, h, 0:8], in_=sc[tt % 2][:, src, :]), reads=[b_sc[tt % 2][src]], writes=[bvv[h]])
                            A("dve", lambda e, src=src, vv=vv, h=h, wkk=wkk: e.match_replace(out=wkk[:], in_to_replace=vv[:, h, 0:8], in_values=sc[tt % 2][:, src, :], imm_value=NEG),
                              reads=[b_sc[tt % 2][src], bvv[h]], writes=[bwk])
                            A("dve", lambda e, vv=vv, h=h, wkk=wkk: e.max(out=vv[:, h, 8:16], in_=wkk[:]), reads=[bwk, bvv[h]], writes=[bvv[h]])
                        A("pool", lambda e, h=h: e.tensor_tensor(out=cand[:, h, :].rearrange("p (a b) -> p a b", a=16), in0=v1[:, h, :].unsqueeze(2).broadcast_to([128, 16, 16]),
                                                                 in1=v2[:, h, :].unsqueeze(1).broadcast_to([128, 16, 16]), op=ALU.add),
                          reads=[b_v1[h], b_v2[h]], writes=[b_cand[h]])
                    for h in range(8):
                        A("dve", lambda e, h=h: e.max(out=top[:, h, 0:8], in_=cand[:, h, :]), reads=[b_cand[h]], writes=[b_top[h]])
                        A("dve", lambda e, h=h: e.match_replace(out=cw[:], in_to_replace=top[:, h, 0:8], in_values=cand[:, h, :], imm_value=NEG),
                          reads=[b_cand[h], b_top[h]], writes=[b_cw])
                        A("dve", lambda e, h=h: e.max(out=top[:, h, 8:16], in_=cw[:]), reads=[b_cw, b_top[h]], writes=[b_top[h]])
                    A("dve", lambda e: e.tensor_scalar(out=negmx[:], in0=top[:, :, 0], scalar1=-1.0, scalar2=None, op0=ALU.mult), reads=b_top, writes=[b_negmx])
                    for h in range(8):
                        A("act", lambda e, h=h: e.activation(out=junk6[:], in_=top[:, h, :], func=AF.Exp, bias=negmx[:, h:h + 1], scale=1.0, accum_out=Zt[:, h:h + 1]),
                          reads=[b_top[h], b_negmx], writes=[b_junk6, b_Zt[h]])
                    A("act", lambda e: e.activation(out=rZ[:], in_=Zt[:], func=AF.Ln), reads=b_Zt, writes=[b_rZ])
                    A("dve", lambda e: e.tensor_tensor(out=rZ[:], in0=rZ[:], in1=v2[:, :, 0], op=ALU.add), reads=[b_rZ] + b_v2, writes=[b_rZ])
                    A("dve", lambda e: e.tensor_tensor(out=d1[:], in0=v1[:], in1=v1[:, :, 0:1].broadcast_to([128, 8, 16]), op=ALU.subtract), reads=b_v1, writes=[b_d1])
                    A("dve", lambda e: e.tensor_copy(out=Q4[:, 0, :].rearrange("p (h a) -> p h a", h=8), in_=v1[:]), reads=b_v1, writes=[b_Q4[0]])
                    A("dve", lambda e: e.tensor_tensor(out=Q4[:, 3, :].rearrange("p (h a) -> p h a", h=8), in0=d1[:], in1=rZ[:].unsqueeze(2).broadcast_to([128, 8, 16]), op=ALU.subtract),
                      reads=[b_d1, b_rZ], writes=[b_Q4[3]])
                    A("dve", lambda e: e.tensor_scalar(out=thrm[:], in0=top[:, :, 15], scalar1=-1.0e-5, scalar2=None, op0=ALU.add), reads=b_top, writes=[b_thrm])
                    A("dve", lambda e: e.tensor_tensor(out=Q4[:, 2, :].rearrange("p (h a) -> p h a", h=8), in0=thrm[:].unsqueeze(2).broadcast_to([128, 8, 16]), in1=v1[:], op=ALU.subtract),
                      reads=[b_thrm] + b_v1, writes=[b_Q4[2]])
                    A("dve", lambda e: e.tensor_tensor(out=Q4[:, 2, :].rearrange("p (h a) -> p h a", h=8), in0=Q4[:, 2, :].rearrange("p (h a) -> p h a", h=8),
                                                       in1=v2[:, :, 15:16].broadcast_to([128, 8, 16]), op=ALU.max),
                      reads=[b_Q4[2]] + b_v2, writes=[b_Q4[2]])
                    tb = tt % 2
                    for k4 in (0, 2, 3):
                        A("pe", lambda e, k4=k4: e.transpose(out=pT4[:, k4 * 128:(k4 + 1) * 128], in_=Q4[:, k4, :], identity=identf[:]), reads=[b_Q4[k4], b_identf], writes=[b_pT4])
                    A("act", lambda e, tb=tb: e.activation(out=sT[tb][:], in_=pT4[:].rearrange("p (k t) -> p k t", k=4), func=AF.Copy), reads=[b_pT4], writes=[b_sT[tb]])

                def issue_qrep(S_):
                    if S_ >= NT * 8:
                        return
                    qb = S_ % 2
                    t0 = S_ * 16
                    ttq = S_ // 8
                    for p_ in range(2):
                        def rep_cp(e, qb=qb, p_=p_, t0=t0):
                            src = qpT[:, p_:16:2, t0:t0 + 16].rearrange("d h t -> d t h").unsqueeze(3).broadcast_to([128, 16, 8, 16])
                            dst = qrep[qb][:, :, p_, :].rearrange("d t (h a) -> d t h a", h=8)
                            if p_ == 0:
                                return e.tensor_copy(out=dst, in_=src)
                            return e.activation(out=dst, in_=src, func=AF.Copy)
                        A("pool" if p_ == 0 else "act", rep_cp, reads=[b_qpT[hp][ttq // 4] for hp in range(p_, 16, 2)], writes=[b_qrep[qb][p_]])

                def gates(tt):
                    tb = tt % 2
                    sgb_ = tt % 2

                    def stageA(g, tt=tt, tb=tb):
                        sub, gl = divmod(g, 8)
                        qb = (tt * 8 + sub) % 2
                        if gl == 0:
                            issue_qrep(tt * 8 + sub + 1)
                        gi = tt * 64 + g
                        sl = gi % 2
                        s3 = gi % 3
                        for q in range(2):
                            tl = gl * 2 + q
                            A("pe", lambda e, sl=sl, qb=qb, tl=tl, q=q: e.matmul(pD[sl][:, q * 128:(q + 1) * 128], lhsT=qrep[qb][:, tl, 0, :], rhs=skT[:, 0, :], start=True, stop=True),
                              reads=[b_qrep[qb][0], b_skT], writes=[b_pD[sl]])
                            A("pe", lambda e, sl=sl, qb=qb, tl=tl, q=q: e.matmul(pD[sl][:, 256 + q * 128:256 + (q + 1) * 128], lhsT=qrep[qb][:, tl, 1, :], rhs=skT[:, 1, :], start=True, stop=True),
                              reads=[b_qrep[qb][1], b_skT], writes=[b_pD[sl]])
                            A("pe", lambda e, sl=sl, qb=qb, tl=tl, q=q: e.matmul(pAa[sl][:, q * 128:(q + 1) * 128], lhsT=qrep[qb][:, tl, 1, :], rhs=skT[:, 1, :], start=True, stop=True),
                              reads=[b_qrep[qb][1], b_skT], writes=[b_pAa[sl]])
                        for q in range(2):
                            t = g * 2 + q
                            A("act", lambda e, sl=sl, tb=tb, t=t, q=q: e.activation(out=Eb[sl][:, q, :], in_=pAa[sl][:, q * 128:(q + 1) * 128], func=AF.Exp, bias=sT[tb][:, 3, t:t + 1], scale=1.0),
                              reads=[b_pAa[sl], b_sT[tb]], writes=[b_Eb[sl][q]])
                        A("dve", lambda e, sl=sl, s3=s3, tb=tb, g=g: e.tensor_tensor(out=lhsG[s3][:], in0=pD[sl][:, 0:256].rearrange("p (q i) -> p q i", q=2),
                                                                                    in1=sT[tb][:, 0, g * 2:g * 2 + 2].unsqueeze(2).broadcast_to([128, 2, 128]), op=ALU.is_equal),
                          reads=[b_pD[sl], b_sT[tb]], writes=[b_lhsG[s3]])
                        for q in range(2):
                            t = g * 2 + q
                            A("dve", lambda e, sl=sl, s3=s3, tb=tb, t=t, q=q: e.scalar_tensor_tensor(out=rhsG[s3][:, q, :], in0=pD[sl][:, 256 + q * 128:256 + (q + 1) * 128], scalar=sT[tb][:, 2, t:t + 1], in1=Eb[sl][:, q, :], op0=ALU.is_ge, op1=ALU.mult),
                              reads=[b_pD[sl], b_sT[tb], b_Eb[sl][q]], writes=[b_rhsG[s3][q]])

                    def stageB(g, tt=tt, sgb_=sgb_):
                        gi = tt * 64 + g
                        s3 = gi % 3
                        gq = (gi // 2) % 2
                        for q in range(2):
                            slot = (g * 2 + q) % 4
                            A("pe", lambda e, s3=s3, gq=gq, q=q, slot=slot: e.matmul(pG[gq][:].rearrange("i (j t) -> i t j", t=4)[:, slot, :], lhsT=lhsG[s3][:, q, :], rhs=rhsG[s3][:, q, :], start=True, stop=True),
                              reads=[b_lhsG[s3], b_rhsG[s3][q]], writes=[b_pG[gq]])
                        if g % 2 == 1:
                            g4 = g // 2
                            A("act", lambda e, gq=gq, sgb_=sgb_, g4=g4: e.activation(out=SG[sgb_][:, :, g4 * 4:g4 * 4 + 4], in_=pG[gq][:].rearrange("i (j t) -> i j t", t=4), func=AF.Copy),
                              reads=[b_pG[gq]], writes=[b_SG[sgb_][g4]])

                    return stageA, stageB

                issue_qrep(0)
                front_scores(0)
                front_topk(0)
                for tt in range(NT):
                    ts_ = slice(tt * 128, (tt + 1) * 128)
                    sgb_ = tt % 2
                    if tt + 1 < NT:
                        front_scores(tt + 1)
                    stageA, stageB = gates(tt)
                    for n_ in range(64 + 1):
                        if n_ == 33 and tt + 1 < NT:
                            front_topk(tt + 1)
                        if n_ < 64:
                            stageA(n_)
                        if n_ >= 1:
                            stageB(n_ - 1)
                    for jb in range(8):
                        A("sp", lambda e, jb=jb, sgb_=sgb_, ts_=ts_: e.dma_start(out=GT[jb * 16:(jb + 1) * 16, :, ts_].rearrange("j i t -> i j t"), in_=SG[sgb_][:, jb * 16:(jb + 1) * 16, :]),
                          reads=b_SG[sgb_], writes=[b_GT[tt]], dma=True)
            P.barrier()
          P.cur = es
          P.barrier()

        if stop_after >= 7:
          u_v = peer_u.rearrange("(i j) d -> j i d", j=128)
          v_v = peer_v.rearrange("(i j) d -> j i d", j=128)
          def _half(hf):
            with ExitStack() as sc_a:
                P.cur = sc_a
                TB = hf * 1024
                h2h = P.sb(f"h2h{hf}", [128, 16, 1024], BF16); b_h2h = P.bufs(16)
                acc = P.sb(f"acc{hf}", [128, 8, D], F32); b_acc = [P.bufs(4) for _ in range(8)]
                with ExitStack() as ph:
                    P.cur = ph
                    Ust = [P.sb(f"Ust{hf}{i}", [128, D], BF16) for i in range(2)]; b_Ust = P.bufs(2)
                    UT = [P.sb(f"UT{hf}{i}", [128, 16, 128], BF16) for i in range(2)]; b_UT = P.bufs(2)
                    Vc = [P.sb(f"Vc{hf}{i}", [128, 4, D], BF16) for i in range(2)]; b_Vc = [P.bufs(4) for _ in range(2)]
                    GTc = [P.sb(f"GTc{hf}{i}", [128, 1024], BF16) for i in range(2)]; b_GTc = P.bufs(2)
                    aT = [P.sb(f"aT{hf}{i}", [128, 4, 1024], BF16) for i in range(2)]; b_aT = [[P.bufs(2) for _ in range(4)] for _ in range(2)]
                    ga = [P.sb(f"ga{hf}{i}", [128, 512], F32) for i in range(2)]; b_ga = P.bufs(2)
                    pU = [P.ps(f"pU{hf}{i}", [128, D], BF16) for i in range(2)]; b_pU = P.bufs(2)
                    pA_7 = [P.ps(f"pAe{hf}{i}", [128, 512]) for i in range(2)]; b_pA = P.bufs(2)
                    pO_7 = [P.ps(f"pOe{hf}{i}", [128, 512]) for i in range(2)]; b_pO = P.bufs(2)
                    for c in range(16):
                        A("sp", lambda e, c=c, TB=TB: e.dma_start(out=h2h[:, c, :], in_=H2T[:, c, TB:TB + 1024]), reads=b_H2T, writes=[b_h2h[c]], dma=True)
                    for tt in range(8):
                        A("sp", lambda e, tt=tt, TB=TB: e.dma_start(out=acc[:, tt, :], in_=X2[TB + tt * 128:TB + (tt + 1) * 128, :]), reads=b_X2[(TB // 128) + tt], writes=b_acc[tt], dma=True)
                    cnt = {"ia": 0, "io": 0}

                    def stT(k):
                        s_, jj = divmod(k, 4)
                        sb_ = s_ % 2
                        cb = k % 2
                        A("pool", lambda e, sb_=sb_, jj=jj, k=k: e.dma_start(out=Vc[sb_][:, jj, :], in_=v_v[k]), writes=[b_Vc[sb_][jj]], dma=True)
                        A("sp", lambda e, cb=cb, k=k: e.dma_start(out=GTc[cb][:], in_=GT[k, :, TB:TB + 1024]), reads=b_GT, writes=[b_GTc[cb]], dma=True)
                        if hf == 1:
                            A("sp", lambda e, cb=cb, k=k: e.dma_start(out=UT[cb][:], in_=UTD[k].rearrange("d (c i) -> d c i", c=16)), reads=[b_UTD[k]], writes=[b_UT[cb]], dma=True)
                            return
                        A("pool", lambda e, cb=cb, k=k: e.dma_start(out=Ust[cb][:], in_=u_v[k]), writes=[b_Ust[cb]], dma=True)
                        for c in range(16):
                            A("pe", lambda e, cb=cb, c=c: e.transpose(out=pU[cb][:, c * 128:(c + 1) * 128], in_=Ust[cb][:, c * 128:(c + 1) * 128], identity=identb[:]),
                              reads=[b_Ust[cb], b_identb], writes=[b_pU[cb]])
                        A("act", lambda e, cb=cb: e.activation(out=UT[cb][:], in_=pU[cb][:].rearrange("p (c i) -> p c i", c=16), func=AF.Copy), reads=[b_pU[cb]], writes=[b_UT[cb]])
                        A("sp", lambda e, cb=cb, k=k: e.dma_start(out=UTD[k].rearrange("d (c i) -> d c i", c=16), in_=UT[cb][:]), reads=[b_UT[cb]], writes=[b_UTD[k]], dma=True)

                    def stM(k):
                        s_, jj = divmod(k, 4)
                        sb_ = s_ % 2
                        cb = k % 2
                        for tg in range(2):
                            ab = cnt["ia"] % 2; cnt["ia"] += 1
                            for c in range(16):
                                A("pe", lambda e, ab=ab, cb=cb, c=c, tg=tg: e.matmul(pA_7[ab][:, :], lhsT=UT[cb][:, c, :], rhs=h2h[:, c, tg * 512:(tg + 1) * 512], start=(c == 0), stop=(c == 15)),
                                  reads=[b_UT[cb], b_h2h[c]], writes=[b_pA[ab]])
                            A("act", lambda e, ab=ab: e.activation(out=ga[ab][:], in_=pA_7[ab][:, :], func=AF.Gelu), reads=[b_pA[ab]], writes=[b_ga[ab]])
                            A("dve", lambda e, ab=ab, sb_=sb_, jj=jj, tg=tg, cb=cb: e.tensor_tensor(out=aT[sb_][:, jj, tg * 512:(tg + 1) * 512], in0=ga[ab][:], in1=GTc[cb][:, tg * 512:(tg + 1) * 512], op=ALU.mult),
                              reads=[b_ga[ab], b_GTc[cb]], writes=[b_aT[sb_][jj][tg]])

                    def stV(s_):
                        sb_ = s_ % 2
                        for tt in range(8):
                            for blk in range(4):
                                ob_ = cnt["io"] % 2; cnt["io"] += 1
                                for jj in range(4):
                                    A("pe", lambda e, ob_=ob_, sb_=sb_, jj=jj, tt=tt, blk=blk: e.matmul(pO_7[ob_][:, :], lhsT=aT[sb_][:, jj, tt * 128:(tt + 1) * 128], rhs=Vc[sb_][:, jj, blk * 512:(blk + 1) * 512], start=(jj == 0), stop=(jj == 3)),
                                      reads=[b_aT[sb_][jj][tt // 4], b_Vc[sb_][jj]], writes=[b_pO[ob_]])
                                A("dve", lambda e, ob_=ob_, tt=tt, blk=blk: e.tensor_tensor(out=acc[:, tt, blk * 512:(blk + 1) * 512], in0=pO_7[ob_][:, :], in1=acc[:, tt, blk * 512:(blk + 1) * 512], op=ALU.add),
                                  reads=[b_pO[ob_], b_acc[tt][blk]], writes=[b_acc[tt][blk]])

                    stT(0)
                    for k in range(128):
                        if k + 1 < 128:
                            stT(k + 1)
                        stM(k)
                        if k % 4 == 0 and k > 0:
                            stV(k // 4 - 1)
                    stV(31)
                P.barrier()
                with ExitStack() as ph:
                    P.cur = ph
                    gO = P.sb(f"gO{hf}", [128, D], F32); b_gO = Buf()
                    junk_7 = P.sb(f"junk7{hf}", [128, D], BF16); b_junk = Buf()
                    ss_7 = [P.sb(f"ss7{hf}{i}", [128, 1], F32) for i in range(2)]; b_ss = P.bufs(2)
                    rstd_7 = [P.sb(f"rstd7{hf}{i}", [128, 1], F32) for i in range(2)]; b_rstd = P.bufs(2)
                    A("sp", lambda e: e.dma_start(out=gO[:], in_=final_norm.partition_broadcast(128)), writes=[b_gO], dma=True)
                    for tt in range(8):
                        b = tt % 2
                        rms_stats(acc[:, tt, :], b_acc[tt], ss_7[b][:], b_ss[b], rstd_7[b][:], b_rstd[b], junk_7[:], b_junk, D)
                        A("dve", lambda e, b=b, tt=tt: e.scalar_tensor_tensor(out=acc[:, tt, :], in0=acc[:, tt, :], scalar=rstd_7[b][:, 0:1], in1=gO[:], op0=ALU.mult, op1=ALU.mult),
                          reads=b_acc[tt] + [b_rstd[b], b_gO], writes=b_acc[tt])
                        fin.append(A("sp", lambda e, tt=tt, TB=TB: e.dma_start(out=out[TB + tt * 128:TB + (tt + 1) * 128, :], in_=acc[:, tt, :]), reads=b_acc[tt], dma=True))
                P.barrier()
            P.cur = es
            P.barrier()
          _half(0)
          _half(1)

        P.emit(final_waits=[op for e_ in P.ENGS for op in P.ops[e_] if op.dma])
        build.nops = P.nops
    return nc


def _consts():
    bf = ml_dtypes.bfloat16
    slopes = 2.0 ** (-np.arange(1, 9, dtype=np.float64))
    pos = np.arange(S)
    kaug = np.zeros((8, 4, S), np.float64)
    qaug = np.zeros((8, 4, S), np.float64)
    for h in range(8):
        kaug[h, 0] = 8 * slopes[h] * 128 * (pos // 128)
        kaug[h, 1] = 8 * slopes[h] * (pos % 128)
        kaug[h, 2] = 1
        kaug[h, 3] = 1
        qaug[h, 0] = 1
        qaug[h, 1] = 1
        qaug[h, 2] = -8 * slopes[h] * 128 * (pos // 128)
        qaug[h, 3] = -8 * slopes[h] * (pos % 128)
    kp = np.arange(128)[:, None]
    qp = np.arange(128)[None, :]
    allowed = (kp // 64) <= (qp // 64)
    bdiag = np.zeros((128, 8, 128), np.float64)
    for h in range(8):
        bdiag[:, h, :] = np.where(allowed, -8 * slopes[h] * np.abs(qp - kp), -240000.0)
    tri = (np.arange(128)[None, :] <= np.arange(128)[:, None]).astype(np.float32)
    return {
        "c_identb": np.eye(128, dtype=np.float32).astype(bf),
        "c_identf": np.eye(128, dtype=np.float32),
        "c_kaug": kaug.astype(np.float32).astype(bf),
        "c_qaug": qaug.astype(np.float32).astype(bf),
        "c_bdiag": bdiag.astype(np.float32).astype(bf),
        "c_tri": tri,
    }


def make_in_maps(inputs, cores):
    f = lambda a: np.ascontiguousarray(np.asarray(a, dtype=np.float32))
    shared = {
        "attn_norm": f(inputs["attn_norm"]).reshape(1, D),
        "w_in": f(inputs["w_in"]).reshape(D, IN_COLS),
        "diff_lambda": f(inputs["diff_lambda"]).reshape(1, 256),
        "diff_subln": f(inputs["diff_subln"]).reshape(1, 128),
        "gmlp_norm": f(inputs["gmlp_norm"]).reshape(1, 1024),
        "gmlp_ws": f(inputs["gmlp_ws"]).reshape(8, 128, 128),
        "gmlp_bs": f(inputs["gmlp_bs"]).reshape(8, 128),
        "w_branch_a": f(inputs["w_branch_a"]).reshape(1024, D),
        "w_branch_b": f(inputs["w_branch_b"]).reshape(1024, D),
        "w_out": f(inputs["w_out"]).reshape(D, D),
        "ffn_norm": f(inputs["ffn_norm"]).reshape(1, D),
        "peer_wq": f(inputs["peer_wq"]).reshape(D, 2048),
        "peer_subkeys": f(inputs["peer_subkeys"]).reshape(2, 128, 128),
        "peer_u": f(inputs["peer_u"]).reshape(16384, D),
        "peer_v": f(inputs["peer_v"]).reshape(16384, D),
        "final_norm": f(inputs["final_norm"]).reshape(1, D),
    }
    shared.update(_consts())
    xs = f(inputs["x"])
    return [dict(shared, x=xs[b]) for b in cores]


def kernel(**inputs):
    nc = build()
    in_maps = make_in_maps(inputs, list(range(8)))
    res = run_bass_kernel_spmd(nc, in_maps, core_ids=list(range(8)))
    return np.stack([np.asarray(r["out"], dtype=np.float32) for r in res.results], axis=0)
```

```python
import numpy as np
import ml_dtypes
import concourse.bass as bass
import concourse.mybir as mybir
from concourse.bass_utils import run_bass_kernel_spmd
from contextlib import ExitStack

F32 = mybir.dt.float32
BF16 = mybir.dt.bfloat16
ALU = mybir.AluOpType
AF = mybir.ActivationFunctionType
AX = mybir.AxisListType

S = 2048
D = 2048
NT = S // 128
EPS = 1e-6
IN_COLS = 9216
LAM_INIT = 0.8 - 0.6 * 1.0
NEG = -1.0e30


class Buf:
    __slots__ = ("name", "last_w", "readers")

    def __init__(self, name="b"):
        self.name = name
        self.last_w = None
        self.readers = []


class Op:
    __slots__ = ("eng", "fn", "dma", "deps", "signal", "sem", "semval", "prewait")

    def __init__(self, eng, fn, dma):
        self.eng = eng
        self.fn = fn
        self.dma = dma
        self.deps = []
        self.signal = False
        self.sem = None
        self.semval = None
        self.prewait = None


class Prog:
    ENGS = ("pe", "act", "dve", "pool", "sp")
    NDMA = {"sp": 12, "pool": 12}

    def __init__(self, nc, es):
        self.nc = nc
        self.es = es
        self.cur = es
        self.ops = {e: [] for e in self.ENGS}
        self.nops = 0
        self._bar_idx = {e: 0 for e in self.ENGS}
        self._pending = {e: [] for e in self.ENGS}

    def sb(self, name, shape, dtype):
        self.nops += 0
        self._uid = getattr(self, "_uid", 0) + 1
        return self.cur.enter_context(self.nc.sbuf_tensor(f"{name}_u{self._uid}", list(shape), dtype))

    def ps(self, name, shape, dtype=F32):
        self._uid = getattr(self, "_uid", 0) + 1
        return self.cur.enter_context(self.nc.psum_tensor(f"{name}_u{self._uid}", list(shape), dtype))

    def bufs(self, n, name="b"):
        return [Buf(name) for _ in range(n)]

    def barrier(self):
        deps = []
        for e in self.ENGS:
            ops = self.ops[e]
            for op in reversed(ops):
                if not op.dma:
                    deps.append(op)
                    break
            deps += [op for op in ops[self._bar_idx[e]:] if op.dma]
            self._bar_idx[e] = len(ops)
        for e in self.ENGS:
            self._pending[e] = self._pending[e] + deps

    def add(self, eng, fn, reads=(), writes=(), dma=False):
        op = Op(eng, fn, dma)
        deps = {}
        for b in reads:
            w = b.last_w
            if w is not None:
                deps[id(w)] = (w, 0)
        for b in writes:
            w = b.last_w
            if w is not None and id(w) not in deps:
                deps[id(w)] = (w, 1)
            for r in b.readers:
                if id(r) not in deps:
                    deps[id(r)] = (r, 2)
        for d, kind in deps.values():
            if d is op:
                continue
            if not d.dma and not op.dma and d.eng == eng:
                if eng == "pe" or kind == 2:
                    continue
            op.deps.append(d)
            d.signal = True
        if self._pending[eng]:
            for d in self._pending[eng]:
                op.deps.append(d)
                d.signal = True
            self._pending[eng] = []
        for b in reads:
            b.readers.append(op)
        for b in writes:
            b.last_w = op
            b.readers = []
        self.ops[eng].append(op)
        self.nops += 1
        return op

    def emit(self, final_waits=()):
        nc = self.nc
        es = self.es
        esem = {e: es.enter_context(nc.semaphore(f"s_{e}")) for e in self.ENGS if e != "sp"}
        dsem = {e: [es.enter_context(nc.semaphore(f"d_{e}{i}")) for i in range(n)] for e, n in self.NDMA.items()}
        for e in self.ENGS:
            tick = 0
            k = 0
            cnt = [0] * self.NDMA.get(e, 0)
            for op in self.ops[e]:
                if op.dma:
                    n = self.NDMA[e]
                    j = k % n
                    k += 1
                    op.prewait = (dsem[e][j], 16 * cnt[j]) if cnt[j] > 0 else None
                    cnt[j] += 1
                    op.sem = dsem[e][j]
                    op.semval = 16 * cnt[j]
                elif op.signal:
                    tick += 1
                    op.sem = esem[e]
                    op.semval = tick
        block = es.enter_context(nc.Block())
        engfn = {"pe": block.tensor, "act": block.scalar, "dve": block.vector, "pool": block.gpsimd, "sp": block.sync}

        def make(e):
            ops = self.ops[e]

            def body(eng):
                waited = {}

                def flush(need):
                    for key, (sem, val) in need.items():
                        if waited.get(key, 0) >= val:
                            continue
                        waited[key] = val
                        eng.wait_ge(sem, val)

                for op in ops:
                    need = {}
                    for d in op.deps:
                        key = id(d.sem)
                        if key not in need or need[key][1] < d.semval:
                            need[key] = (d.sem, d.semval)
                    if op.prewait is not None:
                        key = id(op.prewait[0])
                        if key not in need or need[key][1] < op.prewait[1]:
                            need[key] = op.prewait
                    flush(need)
                    ins = op.fn(eng)
                    if op.dma:
                        ins.then_inc(op.sem, 16)
                    elif op.signal:
                        ins.then_inc(op.sem, 1)
                if e == "sp":
                    need = {}
                    for d in final_waits:
                        key = id(d.sem)
                        if key not in need or need[key][1] < d.semval:
                            need[key] = (d.sem, d.semval)
                    flush(need)

            return body

        for e in self.ENGS:
            engfn[e](make(e))


def build(stop_after=99, dbg=()):
    nc = bass.Bass("TRN2", target_bir_lowering=False)

    def din(name, shape, dt=F32):
        return nc.dram_tensor(name, list(shape), dt, kind="ExternalInput").ap()

    def dscr(name, shape, dt):
        kind = "ExternalOutput" if name in dbg else "Internal"
        return nc.dram_tensor(name, list(shape), dt, kind=kind).ap()

    x = din("x", [S, D])
    attn_norm = din("attn_norm", [1, D])
    w_in = din("w_in", [D, IN_COLS])
    diff_lambda = din("diff_lambda", [1, 256])
    diff_subln = din("diff_subln", [1, 128])
    gmlp_norm = din("gmlp_norm", [1, 1024])
    gmlp_ws = din("gmlp_ws", [8, 128, 128])
    gmlp_bs = din("gmlp_bs", [8, 128])
    w_a = din("w_branch_a", [1024, D])
    w_b = din("w_branch_b", [1024, D])
    w_out = din("w_out", [D, D])
    ffn_norm = din("ffn_norm", [1, D])
    peer_wq = din("peer_wq", [D, 2048])
    peer_sk = din("peer_subkeys", [2, 128, 128])
    peer_u = din("peer_u", [16384, D])
    peer_v = din("peer_v", [16384, D])
    final_norm = din("final_norm", [1, D])
    c_identb = din("c_identb", [128, 128], BF16)
    c_identf = din("c_identf", [128, 128], F32)
    c_kaug = din("c_kaug", [8, 4, S], BF16)
    c_qaug = din("c_qaug", [8, 4, S], BF16)
    c_bdiag = din("c_bdiag", [128, 8, 128], BF16)
    c_tri = din("c_tri", [128, 128], F32)
    out = nc.dram_tensor("out", [S, D], F32, kind="ExternalOutput").ap()

    OAT = dscr("OAT", [128, 8, S], BF16)
    OBT = dscr("OBT", [128, 8, S], BF16)
    MT = dscr("MT", [128, 16, S], BF16)
    X2 = dscr("X2", [S, D], F32)
    H2T = dscr("H2T", [128, 16, S], BF16)
    GT = dscr("GT", [128, 128, S], BF16)
    b_MT = [Buf() for _ in range(16)]
    b_X2 = [[Buf() for _ in range(4)] for _ in range(NT)]
    b_H2T = [Buf() for _ in range(NT)]
    b_GT = [Buf() for _ in range(NT)]
    UTD = dscr("UTD", [128, 128, 2048], BF16)
    b_UTD = [Buf() for _ in range(128)]

    w_in_v = w_in.rearrange("(c p) n -> p c n", p=128)
    fin = []

    with ExitStack() as es:
        P = Prog(nc, es)
        A = P.add

        def dump(name, ap, shape, dt, bufs):
            if name not in dbg:
                return
            d_ = nc.dram_tensor(name, list(shape), dt, kind="ExternalOutput").ap()
            A("sp", lambda e: e.dma_start(out=d_, in_=ap), reads=bufs, dma=True)

        identb = P.sb("identb", [128, 128], BF16); b_identb = Buf()
        identf = P.sb("identf", [128, 128], F32); b_identf = Buf()
        epsT = P.sb("epsT", [128, 1], F32); b_eps = Buf()
        A("sp", lambda e: e.dma_start(out=identb[:], in_=c_identb), writes=[b_identb], dma=True)
        A("sp", lambda e: e.dma_start(out=identf[:], in_=c_identf), writes=[b_identf], dma=True)
        A("dve", lambda e: e.memset(epsT[:], EPS), writes=[b_eps])

        def rms_stats(src_ap, b_src, ss, b_ss, rstd, b_rstd, junk, b_junk, width):
            A("act", lambda e: e.activation(out=junk, in_=src_ap, func=AF.Square, scale=float(width) ** -0.5, accum_out=ss),
              reads=(b_src if isinstance(b_src, list) else [b_src]), writes=[b_junk, b_ss])
            A("act", lambda e: e.activation(out=ss, in_=ss, func=AF.Ln, bias=epsT[:], scale=1.0),
              reads=[b_ss, b_eps], writes=[b_ss])
            A("act", lambda e: e.activation(out=rstd, in_=ss, func=AF.Exp, scale=-0.5), reads=[b_ss], writes=[b_rstd])

        with ExitStack() as sc_h:
            P.cur = sc_h
            hT = P.sb("hT", [128, 16, S], BF16); b_hT = P.bufs(NT)

            with ExitStack() as ph:
                P.cur = ph
                gA = P.sb("gA", [128, D], F32); b_gA = Buf()
                xt = [P.sb(f"xt{i}", [128, D], F32) for i in range(3)]; b_xt = P.bufs(3)
                junk = P.sb("junk1", [128, D], BF16); b_junk = Buf()
                ss = [P.sb(f"ss{i}", [128, 1], F32) for i in range(3)]; b_ss = P.bufs(3)
                rstd = [P.sb(f"rstd{i}", [128, 1], F32) for i in range(3)]; b_rstd = P.bufs(3)
                hb = [P.sb(f"hb{i}", [128, D], BF16) for i in range(3)]; b_hb = P.bufs(3)
                pT = [P.ps(f"pT{i}", [128, D], BF16) for i in range(3)]; b_pT = P.bufs(3)
                A("sp", lambda e: e.dma_start(out=gA[:], in_=attn_norm.partition_broadcast(128)), writes=[b_gA], dma=True)
                pend1 = []
                for tt in range(NT):
                    b = tt % 3
                    A("sp", lambda e, tt=tt, b=b: e.dma_start(out=xt[b][:], in_=x[tt * 128:(tt + 1) * 128, :]), writes=[b_xt[b]], dma=True)
                    rms_stats(xt[b][:], b_xt[b], ss[b][:], b_ss[b], rstd[b][:], b_rstd[b], junk[:], b_junk, D)
                    while pend1:
                        pend1.pop(0)()
                    A("dve", lambda e, b=b: e.scalar_tensor_tensor(out=hb[b][:], in0=xt[b][:], scalar=rstd[b][:, 0:1], in1=gA[:], op0=ALU.mult, op1=ALU.mult),
                      reads=[b_xt[b], b_rstd[b], b_gA], writes=[b_hb[b]])
                    for c in range(16):
                        A("pe", lambda e, b=b, c=c: e.transpose(out=pT[b][:, c * 128:(c + 1) * 128], in_=hb[b][:, c * 128:(c + 1) * 128], identity=identb[:]),
                          reads=[b_hb[b], b_identb], writes=[b_pT[b]])
                    def _cp1(b=b, tt=tt):
                        A("act", lambda e, b=b, tt=tt: e.activation(out=hT[:, :, tt * 128:(tt + 1) * 128], in_=pT[b][:].rearrange("p (c t) -> p c t", c=16), func=AF.Copy),
                          reads=[b_pT[b]], writes=[b_hT[tt]])
                    pend1.append(_cp1)
                while pend1:
                    pend1.pop(0)()
            P.barrier()

            with ExitStack() as sc_o:
                P.cur = sc_o
                b_OATh = [Buf() for _ in range(8)]
                _boa = [[Buf() for _ in range(NT)] for _ in range(2)]
                b_oaT = [_boa[h_ % 2] for h_ in range(8)]
                b_obT = [[Buf() for _ in range(NT)] for _ in range(4)]

                if stop_after >= 2:
                  with ExitStack() as ph:
                    P.cur = ph
                    oaTa = [P.sb(f"oaTa{i}", [128, S], BF16) for i in range(2)]
                    wq = [P.sb(f"wq{i}", [128, 16, 128], BF16) for i in range(2)]; b_wq = P.bufs(2)
                    wk = [P.sb(f"wk{i}", [128, 16, 128], BF16) for i in range(2)]; b_wk = P.bufs(2)
                    wv4 = P.sb("wv4", [128, 16, 512], BF16); b_wv4 = Buf()
                    qT = [P.sb(f"qT{i}", [128, S], BF16) for i in range(2)]; b_qT = [P.bufs(4) for _ in range(2)]
                    kT = [P.sb(f"kT{i}", [128, S], BF16) for i in range(2)]; b_kT = [P.bufs(4) for _ in range(2)]
                    Va4 = P.sb("Va4", [128, NT, 4, 130], BF16); b_Va4 = P.bufs(NT)
                    b_Vone = Buf()
                    q1a = [P.sb(f"q1a{i}", [128, S], BF16) for i in range(2)]; b_q1f = P.bufs(2); b_q1g = P.bufs(2)
                    k1a = [P.sb(f"k1a{i}", [128, S], BF16) for i in range(2)]; b_k1f = P.bufs(2); b_k1g = P.bufs(2)
                    b_qTg = P.bufs(2); b_kTg = P.bufs(2)
                    bdiag = P.sb("bdiag", [128, 8, 128], BF16); b_bdiag = Buf()
                    PT = [P.sb(f"PT{i}", [128, NT * 128], BF16) for i in range(2)]; b_PT = [P.bufs(4) for _ in range(2)]
                    om = [P.sb(f"om{i}", [128, 128], F32) for i in range(2)]; b_om = P.bufs(2)
                    rz = [P.sb(f"rz{i}", [128, 1], F32) for i in range(2)]; b_rz = P.bufs(2)
                    ot = P.sb("ot", [128, 128], F32); b_ot = Buf()
                    oab = [P.sb(f"oab{i}", [128, 128], BF16) for i in range(2)]; b_oab = P.bufs(2)
                    junk2 = P.sb("junk2", [128, 128], BF16); b_junk2 = Buf()
                    ss2 = P.sb("ss2", [128, 1], F32); b_ss2 = Buf()
                    rstd2 = P.sb("rstd2", [128, 1], F32); b_rstd2 = Buf()
                    gsub = P.sb("gsub", [128, 128], F32); b_gsub = Buf()
                    lamt = P.sb("lamt", [128, 256], F32); b_lamt = Buf()
                    lprod = P.sb("lprod", [128, 2, 64], F32); b_lprod = Buf()
                    lsum = P.sb("lsum", [128, 2], F32); b_lsum = Buf()
                    neglam = P.sb("neglam", [128, 1], F32); b_neglam = Buf()
                    pQ = [P.ps(f"pQ{i}", [128, 512]) for i in range(2)]; b_pQ = P.bufs(2)
                    pS = [P.ps(f"pS{i}", [128, 512]) for i in range(2)]; b_pS = P.bufs(2)
                    pO = [P.ps(f"pO{i}", [128, 512]) for i in range(2)]; b_pO = P.bufs(2)
                    pTr = P.ps("pTr", [128, 128], BF16); b_pTr = Buf()

                    A("sp", lambda e: e.dma_start(out=bdiag[:], in_=c_bdiag), writes=[b_bdiag], dma=True)
                    A("sp", lambda e: e.dma_start(out=gsub[:], in_=diff_subln.partition_broadcast(128)), writes=[b_gsub], dma=True)
                    A("dve", lambda e: e.tensor_scalar(out=gsub[:], in0=gsub[:], scalar1=1.0 - LAM_INIT, scalar2=None, op0=ALU.mult),
                      reads=[b_gsub], writes=[b_gsub])
                    A("sp", lambda e: e.dma_start(out=lamt[:], in_=diff_lambda.partition_broadcast(128)), writes=[b_lamt], dma=True)
                    A("dve", lambda e: e.tensor_tensor(out=lprod[:, 0, :], in0=lamt[:, 0:64], in1=lamt[:, 64:128], op=ALU.mult), reads=[b_lamt], writes=[b_lprod])
                    A("dve", lambda e: e.tensor_tensor(out=lprod[:, 1, :], in0=lamt[:, 128:192], in1=lamt[:, 192:256], op=ALU.mult), reads=[b_lamt, b_lprod], writes=[b_lprod])
                    A("dve", lambda e: e.reduce_sum(out=lsum[:], in_=lprod[:], axis=AX.X), reads=[b_lprod], writes=[b_lsum])
                    A("act", lambda e: e.activation(out=lsum[:], in_=lsum[:], func=AF.Exp), reads=[b_lsum], writes=[b_lsum])
                    A("dve", lambda e: e.tensor_tensor(out=neglam[:], in0=lsum[:, 0:1], in1=lsum[:, 1:2], op=ALU.subtract), reads=[b_lsum], writes=[b_neglam])
                    A("dve", lambda e: e.tensor_scalar(out=neglam[:], in0=neglam[:], scalar1=LAM_INIT, scalar2=-1.0, op0=ALU.add, op1=ALU.mult),
                      reads=[b_neglam], writes=[b_neglam])
                    A("dve", lambda e: e.memset(Va4[:, :, :, 128:130], 1.0), writes=[b_Vone])

                    ev = 0
                    for h in range(8):
                        b = h % 2
                        A("pool", lambda e, b=b, h=h: e.dma_start(out=wq[b][:], in_=w_in_v[:, :, h * 128:(h + 1) * 128]), writes=[b_wq[b]], dma=True)
                        A("pool", lambda e, b=b, h=h: e.dma_start(out=wk[b][:], in_=w_in_v[:, :, 1024 + h * 128:1024 + (h + 1) * 128]), writes=[b_wk[b]], dma=True)
                        if h % 4 == 0:
                            A("pool", lambda e, h=h: e.dma_start(out=wv4[:], in_=w_in_v[:, :, 2048 + h * 128:2048 + (h + 4) * 128]), writes=[b_wv4], dma=True)
                        for (w_, bw, dst, bdst) in ((wq, b_wq, qT, b_qT), (wk, b_wk, kT, b_kT)):
                            for g in range(4):
                                pb = ev % 2; ev += 1
                                for c in range(16):
                                    A("pe", lambda e, pb=pb, c=c, g=g, w_=w_, b=b: e.matmul(pQ[pb][:, :], lhsT=w_[b][:, c, :], rhs=hT[:, c, g * 512:(g + 1) * 512], start=(c == 0), stop=(c == 15)),
                                      reads=[bw[b]] + b_hT[g * 4:(g + 1) * 4], writes=[b_pQ[pb]])
                                bg_ = b_qTg[b] if dst is qT else b_kTg[b]
                                if pb == 0:
                                    A("act", lambda e, pb=pb, g=g, dst=dst, b=b: e.activation(out=dst[b][:, g * 512:(g + 1) * 512], in_=pQ[pb][:, :], func=AF.Copy),
                                      reads=[b_pQ[pb]], writes=[bdst[b][g], bg_])
                                else:
                                    A("dve", lambda e, pb=pb, g=g, dst=dst, b=b: e.tensor_copy(out=dst[b][:, g * 512:(g + 1) * 512], in_=pQ[pb][:, :]),
                                      reads=[b_pQ[pb]], writes=[bdst[b][g], bg_])
                        A("sp", lambda e, b=b: e.dma_start(out=q1a[b][0:64, :], in_=qT[b][64:128, :]), reads=b_qT[b] + [b_qTg[b]], writes=[b_q1f[b]], dma=True)
                        A("sp", lambda e, b=b: e.dma_start(out=k1a[b][0:64, :], in_=kT[b][64:128, :]), reads=b_kT[b] + [b_kTg[b]], writes=[b_k1f[b]], dma=True)
                        A("sp", lambda e, b=b, h=h: e.dma_start(out=q1a[b][64:68, :], in_=c_qaug[h]), writes=[b_q1g[b]], dma=True)
                        A("sp", lambda e, b=b, h=h: e.dma_start(out=k1a[b][64:68, :], in_=c_kaug[h]), writes=[b_k1g[b]], dma=True)
                        A("sp", lambda e, b=b, h=h: e.dma_start(out=qT[b][64:68, :], in_=c_qaug[h]), writes=[b_qTg[b]], dma=True)
                        A("sp", lambda e, b=b, h=h: e.dma_start(out=kT[b][64:68, :], in_=c_kaug[h]), writes=[b_kTg[b]], dma=True)
                        if h % 4 == 0:
                            for tt in range(NT):
                                pb = ev % 2; ev += 1
                                for c in range(16):
                                    A("pe", lambda e, pb=pb, c=c, tt=tt: e.matmul(pQ[pb][:, :], lhsT=hT[:, c, tt * 128:(tt + 1) * 128], rhs=wv4[:, c, :], start=(c == 0), stop=(c == 15)),
                                      reads=[b_wv4, b_hT[tt]], writes=[b_pQ[pb]])
                                if pb == 0:
                                    A("act", lambda e, pb=pb, tt=tt: e.activation(out=Va4[:, tt, :, 0:128], in_=pQ[pb][:].rearrange("p (a n) -> p a n", a=4), func=AF.Copy),
                                      reads=[b_pQ[pb]], writes=[b_Va4[tt]])
                                else:
                                    A("dve", lambda e, pb=pb, tt=tt: e.tensor_copy(out=Va4[:, tt, :, 0:128], in_=pQ[pb][:].rearrange("p (a n) -> p a n", a=4)),
                                      reads=[b_pQ[pb]], writes=[b_Va4[tt]])
                        if h == 0:
                            dump("d_qT", qT[b][:], [128, S], BF16, b_qT[b])
                            dump("d_kT", kT[b][:], [128, S], BF16, b_kT[b])
                            dump("d_neglam", neglam[:], [128, 1], F32, [b_neglam])
                        def stQK(j, m, h=h, b=b):
                            pt = m
                            nblk = j + 1
                            for gi in range((nblk + 3) // 4):
                                sb_ = (j * 2 + m + gi) % 2
                                i0 = gi * 4
                                i1 = min(nblk, i0 + 4)
                                for i in range(i0, i1):
                                    col = (i - i0) * 128
                                    kop = kT[b] if m == 0 else k1a[b]
                                    qop = qT[b] if m == 0 else q1a[b]
                                    rds = ([b_kT[b][i // 4], b_qT[b][j // 4], b_kTg[b], b_qTg[b]] if m == 0 else [b_k1f[b], b_q1f[b], b_k1g[b], b_q1g[b]])
                                    kk = 68 if i < j else 64
                                    A("pe", lambda e, sb_=sb_, col=col, i=i, j=j, kop=kop, qop=qop, kk=kk: e.matmul(
                                        pS[sb_][:, col:col + 128], lhsT=kop[0:kk, i * 128:(i + 1) * 128],
                                        rhs=qop[0:kk, j * 128:(j + 1) * 128], start=True, stop=(i < j)),
                                      reads=rds, writes=[b_pS[sb_]])
                                    if i == j:
                                        A("pe", lambda e, sb_=sb_, col=col, h=h: e.matmul(
                                            pS[sb_][:, col:col + 128], lhsT=identb[:], rhs=bdiag[:, h, :], start=False, stop=True),
                                          reads=[b_identb, b_bdiag], writes=[b_pS[sb_]])
                                ncol = (i1 - i0) * 128
                                A("act", lambda e, sb_=sb_, pt=pt, i0=i0, ncol=ncol: e.activation(
                                    out=PT[pt][:, i0 * 128:i0 * 128 + ncol], in_=pS[sb_][:, 0:ncol], func=AF.Exp, scale=0.125),
                                  reads=[b_pS[sb_]], writes=[b_PT[pt][gi]])

                        def stPV(j, m, h=h, b=b):
                            pt = m
                            ob_ = m
                            for i in range(j + 1):
                                A("pe", lambda e, ob_=ob_, pt=pt, i=i, j=j, b=b: e.matmul(
                                    pO[ob_][:, 0:129], lhsT=PT[pt][:, i * 128:(i + 1) * 128], rhs=Va4[:, i, h % 4, 0:129], start=(i == 0), stop=(i == j)),
                                  reads=[b_PT[pt][i // 4], b_Va4[i], b_Vone], writes=[b_pO[ob_]])
                            A("dve", lambda e, ob_=ob_, m=m: e.reciprocal(out=rz[m][:], in_=pO[ob_][:, 128:129]), reads=[b_pO[ob_]], writes=[b_rz[m]])
                            A("dve", lambda e, ob_=ob_, m=m: e.tensor_scalar(out=om[m][:], in0=pO[ob_][:, 0:128], scalar1=rz[m][:, 0:1], scalar2=None, op0=ALU.mult),
                              reads=[b_pO[ob_], b_rz[m]], writes=[b_om[m]])
                            if m == 1:
                                A("dve", lambda e: e.scalar_tensor_tensor(out=ot[:], in0=om[1][:], scalar=neglam[:, 0:1], in1=om[0][:], op0=ALU.mult, op1=ALU.add),
                                  reads=[b_om[0], b_om[1], b_neglam], writes=[b_ot])
                                rms_stats(ot[:], b_ot, ss2[:], b_ss2, rstd2[:], b_rstd2, junk2[:], b_junk2, 128)
                                ab = j % 2
                                A("dve", lambda e, ab=ab: e.scalar_tensor_tensor(out=oab[ab][:], in0=ot[:], scalar=rstd2[:, 0:1], in1=gsub[:], op0=ALU.mult, op1=ALU.mult),
                                  reads=[b_ot, b_rstd2, b_gsub], writes=[b_oab[ab]])
                                def _tr(ab=ab, h=h, j=j):
                                    A("pe", lambda e, ab=ab: e.transpose(out=pTr[:, :], in_=oab[ab][:], identity=identb[:]), reads=[b_oab[ab], b_identb], writes=[b_pTr])
                                    A("act", lambda e, h=h, j=j: e.activation(out=oaTa[h % 2][:, j * 128:(j + 1) * 128], in_=pTr[:, :], func=AF.Copy),
                                      reads=[b_pTr], writes=[b_oaT[h][j]])
                                pend_tr.append(_tr)

                        units = [(j, m) for j in range(NT) for m in range(2)]
                        pend_tr = []
                        stQK(*units[0])
                        for ui, (j, m) in enumerate(units):
                            if ui + 1 < len(units):
                                stQK(*units[ui + 1])
                            if m == 1 and pend_tr:
                                pend_tr.pop(0)()
                            stPV(j, m)
                        while pend_tr:
                            pend_tr.pop(0)()
                        A("sp", lambda e, h=h: e.dma_start(out=OAT[:, h, :], in_=oaTa[h % 2][:]), reads=b_oaT[h], writes=[b_OATh[h]], dma=True)
                  P.barrier()
                P.cur = sc_o
                obT = P.sb("obT", [128, 8, S], BF16)

                if stop_after >= 3:
                  with ExitStack() as ph:
                    P.cur = ph
                    Wz = [P.sb(f"Wz{i}", [128, 16, 256], BF16) for i in range(2)]; b_Wz = P.bufs(2)
                    vg = P.sb("vg", [128, NT, 1024], BF16); b_vg = [P.bufs(4) for _ in range(NT)]
                    gn = P.sb("gn", [128, 1024], F32); b_gn = Buf()
                    wsf = P.sb("wsf", [128, 8, 128], F32); b_wsf = Buf()
                    tri = P.sb("tri", [128, 128], F32); b_tri = Buf()
                    wsT = P.sb("wsT", [128, 8, 128], BF16); b_wsT = Buf()
                    bsT = P.sb("bsT", [128, 8], F32); b_bsT = Buf()
                    ssv = P.sb("ssv", [128, NT, 4], F32); b_ssv = [P.bufs(4) for _ in range(NT)]
                    sv1 = P.sb("sv1", [128, 1], F32); b_sv1 = Buf()
                    rsv = P.sb("rsv", [128, 1], F32); b_rsv = Buf()
                    junk3 = P.sb("junk3", [128, 256], BF16); b_junk3 = Buf()
                    ug = [P.sb(f"ug{i}", [128, 256], F32) for i in range(2)]; b_ug = P.bufs(2)
                    tmp = [P.sb(f"tmp{i}", [128, 256], F32) for i in range(2)]; b_tmp = P.bufs(2)
                    obb = [P.sb(f"obb{i}", [128, 256], BF16) for i in range(2)]; b_obb = P.bufs(2)
                    pZ = [P.ps(f"pZ{i}", [128, 512]) for i in range(2)]; b_pZ = P.bufs(2)
                    pSV = [P.ps(f"pSV{i}", [128, 512]) for i in range(2)]; b_pSV = P.bufs(2)
                    pW = P.ps("pW", [128, 512]); b_pW = Buf()
                    pTb = P.ps("pTb", [128, 256], BF16); b_pTb = Buf()

                    A("sp", lambda e: e.dma_start(out=gn[:], in_=gmlp_norm.partition_broadcast(128)), writes=[b_gn], dma=True)
                    A("sp", lambda e: e.dma_start(out=wsf[:], in_=gmlp_ws.rearrange("g t s -> t g s")), writes=[b_wsf], dma=True)
                    A("sp", lambda e: e.dma_start(out=tri[:], in_=c_tri), writes=[b_tri], dma=True)
                    A("sp", lambda e: e.dma_start(out=bsT[:], in_=gmlp_bs.rearrange("g t -> t g"), allow_slow_non_contiguous=True), writes=[b_bsT], dma=True)
                    A("dve", lambda e: e.tensor_tensor(out=wsf[:], in0=wsf[:], in1=tri[:].unsqueeze(1).broadcast_to([128, 8, 128]), op=ALU.mult),
                      reads=[b_wsf, b_tri], writes=[b_wsf])
                    for g0 in range(0, 8, 4):
                        for gg in range(4):
                            A("pe", lambda e, g0=g0, gg=gg: e.transpose(out=pW[:, gg * 128:(gg + 1) * 128], in_=wsf[:, g0 + gg, :], identity=identf[:]),
                              reads=[b_wsf, b_identf], writes=[b_pW])
                        A("dve", lambda e, g0=g0: e.tensor_copy(out=wsT[:, g0:g0 + 4, :], in_=pW[:].rearrange("p (g t) -> p g t", g=4)), reads=[b_pW], writes=[b_wsT])

                    zc = 0
                    for vb in range(4):
                        wb_ = zc % 2; zc += 1
                        c0 = 3072 + 1024 + vb * 256
                        A("pool", lambda e, wb_=wb_, c0=c0: e.dma_start(out=Wz[wb_][:], in_=w_in_v[:, :, c0:c0 + 256]), writes=[b_Wz[wb_]], dma=True)
                        for tt in range(NT):
                            zb = tt % 2
                            for c in range(16):
                                A("pe", lambda e, zb=zb, c=c, tt=tt, wb_=wb_: e.matmul(pZ[zb][:, 0:256], lhsT=hT[:, c, tt * 128:(tt + 1) * 128], rhs=Wz[wb_][:, c, :], start=(c == 0), stop=(c == 15)),
                                  reads=[b_Wz[wb_], b_hT[tt]], writes=[b_pZ[zb]])
                            A("act", lambda e, zb=zb, tt=tt, vb=vb: e.activation(out=vg[:, tt, vb * 256:(vb + 1) * 256], in_=pZ[zb][:, 0:256], func=AF.Gelu),
                              reads=[b_pZ[zb]], writes=[b_vg[tt][vb]])
                            A("act", lambda e, tt=tt, vb=vb: e.activation(out=junk3[:], in_=vg[:, tt, vb * 256:(vb + 1) * 256], func=AF.Square, scale=1.0 / 32.0, accum_out=ssv[:, tt, vb:vb + 1]),
                              reads=[b_vg[tt][vb]], writes=[b_junk3, b_ssv[tt][vb]])
                    for tt in range(NT):
                        A("dve", lambda e, tt=tt: e.reduce_sum(out=sv1[:], in_=ssv[:, tt, :], axis=AX.X), reads=b_ssv[tt], writes=[b_sv1])
                        A("act", lambda e: e.activation(out=sv1[:], in_=sv1[:], func=AF.Sqrt, bias=epsT[:], scale=1.0), reads=[b_sv1, b_eps], writes=[b_sv1])
                        A("dve", lambda e: e.reciprocal(out=rsv[:], in_=sv1[:]), reads=[b_sv1], writes=[b_rsv])
                        A("dve", lambda e, tt=tt: e.scalar_tensor_tensor(out=vg[:, tt, :], in0=vg[:, tt, :], scalar=rsv[:, 0:1], in1=gn[:], op0=ALU.mult, op1=ALU.mult),
                          reads=b_vg[tt] + [b_rsv, b_gn], writes=b_vg[tt])
                    pend3 = []
                    for ub in range(4):
                        wb_ = zc % 2; zc += 1
                        c0 = 3072 + ub * 256
                        A("pool", lambda e, wb_=wb_, c0=c0: e.dma_start(out=Wz[wb_][:], in_=w_in_v[:, :, c0:c0 + 256]), writes=[b_Wz[wb_]], dma=True)
                        for tt in range(NT):
                            zb = tt % 2
                            for c in range(16):
                                A("pe", lambda e, zb=zb, c=c, tt=tt, wb_=wb_: e.matmul(pZ[zb][:, 0:256], lhsT=hT[:, c, tt * 128:(tt + 1) * 128], rhs=Wz[wb_][:, c, :], start=(c == 0), stop=(c == 15)),
                                  reads=[b_Wz[wb_], b_hT[tt]], writes=[b_pZ[zb]])
                            for gg in range(2):
                                g = ub * 2 + gg
                                A("pe", lambda e, zb=zb, gg=gg, g=g, tt=tt: e.matmul(pSV[zb][:, gg * 128:(gg + 1) * 128], lhsT=wsT[:, g, :], rhs=vg[:, tt, g * 128:(g + 1) * 128], start=True, stop=True),
                                  reads=[b_wsT, b_vg[tt][g // 2]], writes=[b_pSV[zb]])
                            while pend3:
                                pend3.pop(0)()
                            A("act", lambda e, zb=zb: e.activation(out=ug[zb][:], in_=pZ[zb][:, 0:256], func=AF.Gelu), reads=[b_pZ[zb]], writes=[b_ug[zb]])
                            A("dve", lambda e, zb=zb, ub=ub: e.tensor_tensor(out=tmp[zb][:].rearrange("p (g c) -> p g c", g=2), in0=pSV[zb][:, 0:256].rearrange("p (g c) -> p g c", g=2),
                                                                             in1=bsT[:, ub * 2:(ub + 1) * 2].unsqueeze(2).broadcast_to([128, 2, 128]), op=ALU.add),
                              reads=[b_pSV[zb], b_bsT], writes=[b_tmp[zb]])
                            A("dve", lambda e, zb=zb: e.tensor_tensor(out=obb[zb][:], in0=tmp[zb][:], in1=ug[zb][:], op=ALU.mult), reads=[b_tmp[zb], b_ug[zb]], writes=[b_obb[zb]])
                            def _tr3(zb=zb, ub=ub, tt=tt):
                                for gg in range(2):
                                    A("pe", lambda e, zb=zb, gg=gg: e.transpose(out=pTb[:, gg * 128:(gg + 1) * 128], in_=obb[zb][:, gg * 128:(gg + 1) * 128], identity=identb[:]),
                                      reads=[b_obb[zb], b_identb], writes=[b_pTb])
                                A("act", lambda e, ub=ub, tt=tt: e.activation(out=obT[:, ub * 2:(ub + 1) * 2, tt * 128:(tt + 1) * 128], in_=pTb[:].rearrange("p (g t) -> p g t", g=2), func=AF.Copy),
                                  reads=[b_pTb], writes=[b_obT[ub][tt]])
                            pend3.append(_tr3)
                    while pend3:
                        pend3.pop(0)()
                    if "OBT" in dbg:
                        for g in range(8):
                            A("sp", lambda e, g=g: e.dma_start(out=OBT[:, g, :], in_=obT[:, g, :]), reads=b_obT[g // 2], dma=True)
                  P.barrier()
                P.cur = sc_o
                oaT = P.sb("oaT2", [128, 8, S], BF16); b_oaT2 = [Buf() for _ in range(8)]

                if stop_after >= 4:
                  with ExitStack() as ph:
                    P.cur = ph
                    Wa = [P.sb(f"Wa{i}", [128, 8, 128], BF16) for i in range(2)]; b_Wa = P.bufs(2)
                    Wb = [P.sb(f"Wb{i}", [128, 8, 128], BF16) for i in range(2)]; b_Wb = P.bufs(2)
                    Wga = [P.sb(f"Wga{i}", [128, 16, 128], BF16) for i in range(2)]; b_Wga = P.bufs(2)
                    Wgb = [P.sb(f"Wgb{i}", [128, 16, 128], BF16) for i in range(2)]; b_Wgb = P.bufs(2)
                    sga = [P.sb(f"sga{i}", [128, 512], F32) for i in range(2)]; b_sga = P.bufs(2)
                    sgb = [P.sb(f"sgb{i}", [128, 512], F32) for i in range(2)]; b_sgb = P.bufs(2)
                    mst = [P.sb(f"mst{i}", [128, S], BF16) for i in range(2)]; b_mst = [P.bufs(4) for _ in range(2)]
                    pA = [P.ps(f"pA{i}", [128, 512]) for i in range(2)]; b_pA = P.bufs(2)
                    pB = [P.ps(f"pB{i}", [128, 512]) for i in range(2)]; b_pB = P.bufs(2)
                    pGA = [P.ps(f"pGA{i}", [128, 512]) for i in range(2)]; b_pGA = P.bufs(2)
                    pGB = [P.ps(f"pGB{i}", [128, 512]) for i in range(2)]; b_pGB = P.bufs(2)
                    wa_v = w_a.rearrange("(c p) n -> p c n", p=128)
                    wb_v = w_b.rearrange("(c p) n -> p c n", p=128)
                    for h in range(8):
                        A("sp", lambda e, h=h: e.dma_start(out=oaT[:, h, :], in_=OAT[:, h, :]), reads=[b_OATh[h]], writes=[b_oaT2[h]], dma=True)
                    it = 0
                    for n in range(16):
                        wb_ = n % 2
                        A("pool", lambda e, wb_=wb_, n=n: e.dma_start(out=Wa[wb_][:], in_=wa_v[:, :, n * 128:(n + 1) * 128]), writes=[b_Wa[wb_]], dma=True)
                        A("pool", lambda e, wb_=wb_, n=n: e.dma_start(out=Wb[wb_][:], in_=wb_v[:, :, n * 128:(n + 1) * 128]), writes=[b_Wb[wb_]], dma=True)
                        A("pool", lambda e, wb_=wb_, n=n: e.dma_start(out=Wga[wb_][:], in_=w_in_v[:, :, 5120 + n * 128:5120 + (n + 1) * 128]), writes=[b_Wga[wb_]], dma=True)
                        A("pool", lambda e, wb_=wb_, n=n: e.dma_start(out=Wgb[wb_][:], in_=w_in_v[:, :, 7168 + n * 128:7168 + (n + 1) * 128]), writes=[b_Wgb[wb_]], dma=True)
                        for g in range(4):
                            pb = it % 2; it += 1
                            gs = slice(g * 512, (g + 1) * 512)
                            for c in range(8):
                                A("pe", lambda e, pb=pb, c=c, gs=gs, wb_=wb_: e.matmul(pA[pb][:, :], lhsT=Wa[wb_][:, c, :], rhs=oaT[:, c, gs], start=(c == 0), stop=(c == 7)),
                                  reads=[b_Wa[wb_], b_oaT2[c]], writes=[b_pA[pb]])
                            for c in range(8):
                                A("pe", lambda e, pb=pb, c=c, gs=gs, wb_=wb_: e.matmul(pB[pb][:, :], lhsT=Wb[wb_][:, c, :], rhs=obT[:, c, gs], start=(c == 0), stop=(c == 7)),
                                  reads=[b_Wb[wb_]], writes=[b_pB[pb]])
                            for c in range(16):
                                A("pe", lambda e, pb=pb, c=c, gs=gs, wb_=wb_: e.matmul(pGA[pb][:, :], lhsT=Wga[wb_][:, c, :], rhs=hT[:, c, gs], start=(c == 0), stop=(c == 15)),
                                  reads=[b_Wga[wb_]], writes=[b_pGA[pb]])
                            for c in range(16):
                                A("pe", lambda e, pb=pb, c=c, gs=gs, wb_=wb_: e.matmul(pGB[pb][:, :], lhsT=Wgb[wb_][:, c, :], rhs=hT[:, c, gs], start=(c == 0), stop=(c == 15)),
                                  reads=[b_Wgb[wb_]], writes=[b_pGB[pb]])
                            A("act", lambda e, pb=pb: e.activation(out=sga[pb][:], in_=pGA[pb][:, :], func=AF.Sigmoid), reads=[b_pGA[pb]], writes=[b_sga[pb]])
                            A("act", lambda e, pb=pb: e.activation(out=sgb[pb][:], in_=pGB[pb][:, :], func=AF.Sigmoid), reads=[b_pGB[pb]], writes=[b_sgb[pb]])
                            A("dve", lambda e, pb=pb: e.tensor_tensor(out=sga[pb][:], in0=pA[pb][:, :], in1=sga[pb][:], op=ALU.mult), reads=[b_pA[pb], b_sga[pb]], writes=[b_sga[pb]])
                            A("dve", lambda e, pb=pb: e.tensor_tensor(out=sgb[pb][:], in0=pB[pb][:, :], in1=sgb[pb][:], op=ALU.mult), reads=[b_pB[pb], b_sgb[pb]], writes=[b_sgb[pb]])
                            A("pool", lambda e, pb=pb, wb_=wb_, gs=gs: e.tensor_tensor(out=mst[wb_][:, gs], in0=sga[pb][:], in1=sgb[pb][:], op=ALU.add),
                              reads=[b_sga[pb], b_sgb[pb]], writes=[b_mst[wb_][g]])
                        A("sp", lambda e, wb_=wb_, n=n: e.dma_start(out=MT[:, n, :], in_=mst[wb_][:]), reads=b_mst[wb_], writes=[b_MT[n]], dma=True)
                  P.barrier()
        P.cur = es
        P.barrier()

        if stop_after >= 5:
          with ExitStack() as ph:
            P.cur = ph
            mT = P.sb("mT", [128, 16, S], BF16); b_mT = P.bufs(16)
            Wo = [P.sb(f"Wo{i}", [128, 16, 512], BF16) for i in range(2)]; b_Wo = P.bufs(2)
            xp = [P.sb(f"xp{i}", [128, 512], F32) for i in range(2)]; b_xp = P.bufs(2)
            x2p = [P.sb(f"x2p{i}", [128, 512], F32) for i in range(2)]; b_x2p = P.bufs(2)
            pX = [P.ps(f"pX{i}", [128, 512]) for i in range(2)]; b_pX = P.bufs(2)
            wo_v = w_out.rearrange("(c p) n -> p c n", p=128)
            for c in range(16):
                A("sp", lambda e, c=c: e.dma_start(out=mT[:, c, :], in_=MT[:, c, :]), reads=[b_MT[c]], writes=[b_mT[c]], dma=True)
            seq5 = [(blk, tt) for blk in range(4) for tt in range(NT)]

            def ld5(k):
                blk, tt = seq5[k]
                pb = k % 2
                if tt == 0:
                    wb_ = blk % 2
                    A("pool", lambda e, wb_=wb_, blk=blk: e.dma_start(out=Wo[wb_][:], in_=wo_v[:, :, blk * 512:(blk + 1) * 512]), writes=[b_Wo[wb_]], dma=True)
                A("sp", lambda e, pb=pb, tt=tt, blk=blk: e.dma_start(out=xp[pb][:], in_=x[tt * 128:(tt + 1) * 128, blk * 512:(blk + 1) * 512]), writes=[b_xp[pb]], dma=True)

            ld5(0)
            for k, (blk, tt) in enumerate(seq5):
                pb = k % 2
                wb_ = blk % 2
                if k + 1 < len(seq5):
                    ld5(k + 1)
                for c in range(16):
                    A("pe", lambda e, pb=pb, c=c, tt=tt, wb_=wb_: e.matmul(pX[pb][:, :], lhsT=mT[:, c, tt * 128:(tt + 1) * 128], rhs=Wo[wb_][:, c, :], start=(c == 0), stop=(c == 15)),
                      reads=[b_Wo[wb_], b_mT[c]], writes=[b_pX[pb]])
                A("dve", lambda e, pb=pb: e.tensor_tensor(out=x2p[pb][:], in0=pX[pb][:, :], in1=xp[pb][:], op=ALU.add), reads=[b_pX[pb], b_xp[pb]], writes=[b_x2p[pb]])
                A("sp", lambda e, pb=pb, tt=tt, blk=blk: e.dma_start(out=X2[tt * 128:(tt + 1) * 128, blk * 512:(blk + 1) * 512], in_=x2p[pb][:]),
                  reads=[b_x2p[pb]], writes=[b_X2[tt][blk]], dma=True)
          P.barrier()
          with ExitStack() as ph:
            P.cur = ph
            gF = P.sb("gF", [128, D], F32); b_gF = Buf()
            xt_5 = [P.sb(f"x2t{i}", [128, D], F32) for i in range(3)]; b_xt = P.bufs(3)
            junk_5 = P.sb("junk5", [128, D], BF16); b_junk = Buf()
            ss_5 = [P.sb(f"ss5{i}", [128, 1], F32) for i in range(3)]; b_ss = P.bufs(3)
            rstd_5 = [P.sb(f"rstd5{i}", [128, 1], F32) for i in range(3)]; b_rstd = P.bufs(3)
            hb_5 = [P.sb(f"hb5{i}", [128, D], BF16) for i in range(3)]; b_hb = P.bufs(3)
            hs = [P.sb(f"hs5{i}", [128, 16, 128], BF16) for i in range(3)]; b_hs = P.bufs(3)
            pT_5 = [P.ps(f"pT5{i}", [128, D], BF16) for i in range(3)]; b_pT = P.bufs(3)
            A("sp", lambda e: e.dma_start(out=gF[:], in_=ffn_norm.partition_broadcast(128)), writes=[b_gF], dma=True)
            pend5 = []
            for tt in range(NT):
                b = tt % 3
                A("sp", lambda e, tt=tt, b=b: e.dma_start(out=xt_5[b][:], in_=X2[tt * 128:(tt + 1) * 128, :]), reads=b_X2[tt], writes=[b_xt[b]], dma=True)
                rms_stats(xt_5[b][:], b_xt[b], ss_5[b][:], b_ss[b], rstd_5[b][:], b_rstd[b], junk_5[:], b_junk, D)
                while pend5:
                    pend5.pop(0)()
                A("dve", lambda e, b=b: e.scalar_tensor_tensor(out=hb_5[b][:], in0=xt_5[b][:], scalar=rstd_5[b][:, 0:1], in1=gF[:], op0=ALU.mult, op1=ALU.mult),
                  reads=[b_xt[b], b_rstd[b], b_gF], writes=[b_hb[b]])
                for c in range(16):
                    A("pe", lambda e, b=b, c=c: e.transpose(out=pT_5[b][:, c * 128:(c + 1) * 128], in_=hb_5[b][:, c * 128:(c + 1) * 128], identity=identb[:]),
                      reads=[b_hb[b], b_identb], writes=[b_pT[b]])
                def _cp5(b=b, tt=tt):
                    A("act", lambda e, b=b: e.activation(out=hs[b][:], in_=pT_5[b][:].rearrange("p (c t) -> p c t", c=16), func=AF.Copy), reads=[b_pT[b]], writes=[b_hs[b]])
                    A("sp", lambda e, b=b, tt=tt: e.dma_start(out=H2T[:, :, tt * 128:(tt + 1) * 128], in_=hs[b][:]), reads=[b_hs[b]], writes=[b_H2T[tt]], dma=True)
                pend5.append(_cp5)
            while pend5:
                pend5.pop(0)()
          P.barrier()

        if stop_after >= 6:
          with ExitStack() as sc_q:
            P.cur = sc_q
            qpT = P.sb("qpT", [128, 16, S], BF16); b_qpT = [P.bufs(4) for _ in range(16)]
            with ExitStack() as ph:
                P.cur = ph
                h2T = P.sb("h2T", [128, 16, S], BF16); b_h2T = P.bufs(16)
                Wp = [P.sb(f"Wp{i}", [128, 16, 128], BF16) for i in range(2)]; b_Wp = P.bufs(2)
                pQ_6 = [P.ps(f"pQp{i}", [128, 512]) for i in range(2)]; b_pQ = P.bufs(2)
                wp_v = peer_wq.rearrange("(c p) n -> p c n", p=128)
                for c in range(16):
                    A("sp", lambda e, c=c: e.dma_start(out=h2T[:, c, :], in_=H2T[:, c, :]), reads=b_H2T, writes=[b_h2T[c]], dma=True)
                it = 0
                for n in range(16):
                    wb_ = n % 2
                    A("pool", lambda e, wb_=wb_, n=n: e.dma_start(out=Wp[wb_][:], in_=wp_v[:, :, n * 128:(n + 1) * 128]), writes=[b_Wp[wb_]], dma=True)
                    for g in range(4):
                        pb = it % 2; it += 1
                        for c in range(16):
                            A("pe", lambda e, pb=pb, c=c, g=g, wb_=wb_: e.matmul(pQ_6[pb][:, :], lhsT=Wp[wb_][:, c, :], rhs=h2T[:, c, g * 512:(g + 1) * 512], start=(c == 0), stop=(c == 15)),
                              reads=[b_Wp[wb_], b_h2T[c]], writes=[b_pQ[pb]])
                        if pb == 0:
                            A("act", lambda e, pb=pb, n=n, g=g: e.activation(out=qpT[:, n, g * 512:(g + 1) * 512], in_=pQ_6[pb][:, :], func=AF.Copy), reads=[b_pQ[pb]], writes=[b_qpT[n][g]])
                        else:
                            A("dve", lambda e, pb=pb, n=n, g=g: e.tensor_copy(out=qpT[:, n, g * 512:(g + 1) * 512], in_=pQ_6[pb][:, :]), reads=[b_pQ[pb]], writes=[b_qpT[n][g]])
            P.barrier()
            with ExitStack() as ph:
                P.cur = ph
                skf = P.sb("skf", [128, 2, 128], F32); b_skf = Buf()
                skT = P.sb("skT", [128, 2, 128], BF16); b_skT = Buf()
                sc = [P.sb(f"sc{i}", [128, 16, 128], F32) for i in range(2)]; b_sc = [P.bufs(16) for _ in range(2)]
                v1 = P.sb("v1", [128, 8, 16], F32); b_v1 = P.bufs(8)
                v2 = P.sb("v2", [128, 8, 16], F32); b_v2 = P.bufs(8)
                wk1 = P.sb("wk1", [128, 128], F32); b_wk1 = Buf()
                wk2 = P.sb("wk2", [128, 128], F32); b_wk2 = Buf()
                cand = P.sb("cand", [128, 8, 256], F32); b_cand = P.bufs(8)
                cw = P.sb("cw", [128, 256], F32); b_cw = Buf()
                cw2 = P.sb("cw2", [128, 256], F32); b_cw2 = Buf()
                t17 = P.sb("t17", [128, 8, 8], F32); b_t17 = P.bufs(8)
                thrm = P.sb("thrm", [128, 8], F32); b_thrm = Buf()
                top = P.sb("top", [128, 8, 16], F32); b_top = P.bufs(8)
                negmx = P.sb("negmx", [128, 8], F32); b_negmx = Buf()
                Zt = P.sb("Zt", [128, 8], F32); b_Zt = P.bufs(8)
                rZ = P.sb("rZ", [128, 8], F32); b_rZ = Buf()
                junk6 = P.sb("junk6", [128, 16], F32); b_junk6 = Buf()
                d1 = P.sb("d1", [128, 8, 16], F32); b_d1 = Buf()
                Q4 = P.sb("Q4", [128, 4, 128], F32); b_Q4 = P.bufs(4)
                sT = [P.sb(f"sT{i}", [128, 4, 128], F32) for i in range(2)]; b_sT = P.bufs(2)
                qrep = [P.sb(f"qrep{i}", [128, 16, 2, 128], BF16) for i in range(2)]; b_qrep = [P.bufs(2) for _ in range(2)]
                Eb = [P.sb(f"Eb{i}", [128, 2, 128], F32) for i in range(2)]; b_Eb = [P.bufs(2) for _ in range(2)]
                lhsG = [P.sb(f"lhsG{i}", [128, 2, 128], BF16) for i in range(3)]; b_lhsG = P.bufs(3)
                rhsG = [P.sb(f"rhsG{i}", [128, 2, 128], BF16) for i in range(3)]; b_rhsG = [P.bufs(2) for _ in range(3)]
                SG = [P.sb(f"SG{i}", [128, 128, 128], BF16) for i in range(2)]; b_SG = [P.bufs(32) for _ in range(2)]
                pSc = [P.ps("pSc0", [128, 512])] * 2; b_pSc = [Buf()] * 2
                pT4 = P.ps("pT4", [128, 512]); b_pT4 = Buf()
                pD = [P.ps(f"pD{i}", [128, 512]) for i in range(2)]; b_pD = P.bufs(2)
                pAa = [P.ps(f"pAa{i}", [128, 512]) for i in range(2)]; b_pAa = P.bufs(2)
                pG = [P.ps(f"pG{i}", [128, 512]) for i in range(2)]; b_pG = P.bufs(2)

                A("sp", lambda e: e.dma_start(out=skf[:], in_=peer_sk.rearrange("p n d -> n p d")), writes=[b_skf], dma=True)
                for p_ in range(2):
                    A("pe", lambda e, p_=p_: e.transpose(out=pT4[:, p_ * 128:(p_ + 1) * 128], in_=skf[:, p_, :], identity=identf[:]), reads=[b_skf, b_identf], writes=[b_pT4])
                A("dve", lambda e: e.tensor_copy(out=skT[:], in_=pT4[:, 0:256].rearrange("d (p n) -> d p n", p=2)), reads=[b_pT4], writes=[b_skT])

                def front_scores(tt):
                    ts_ = slice(tt * 128, (tt + 1) * 128)
                    for r in range(4):
                        pb = r % 2
                        for q_ in range(4):
                            hp = r * 4 + q_
                            A("pe", lambda e, pb=pb, q_=q_, hp=hp, ts_=ts_: e.matmul(pSc[pb][:, q_ * 128:(q_ + 1) * 128], lhsT=qpT[:, hp, ts_], rhs=skT[:, hp % 2, :], start=True, stop=True),
                              reads=[b_qpT[hp][tt // 4], b_skT], writes=[b_pSc[pb]])
                        A("act", lambda e, pb=pb, r=r: e.activation(out=sc[tt % 2][:, r * 4:(r + 1) * 4, :], in_=pSc[pb][:].rearrange("p (a n) -> p a n", a=4), func=AF.Copy),
                          reads=[b_pSc[pb]], writes=b_sc[tt % 2][r * 4:(r + 1) * 4])

                def front_topk(tt):
                    for h in range(8):
                        for (src, vv, bvv, wkk, bwk) in ((2 * h, v1, b_v1, wk1, b_wk1), (2 * h + 1, v2, b_v2, wk2, b_wk2)):
                            A("dve", lambda e, src=src, vv=vv, h=h: e.max(out=vv[:, h, 0:8], in_=sc[tt % 2][:, src, :]), reads=[b_sc[tt % 2][src]], writes=[bvv[h]])
                            A("dve", lambda e, src=src, vv=vv, h=h, wkk=wkk: e.match_replace(out=wkk[:], in_to_replace=vv[:, h, 0:8], in_values=sc[tt % 2][:, src, :], imm_value=NEG),
                              reads=[b_sc[tt % 2][src], bvv[h]], writes=[bwk])
                            A("dve", lambda e, vv=vv, h=h, wkk=wkk: e.max(out=vv[:, h, 8:16], in_=wkk[:]), reads=[bwk, bvv[h]], writes=[bvv[h]])
                        A("pool", lambda e, h=h: e.tensor_tensor(out=cand[:, h, :].rearrange("p (a b) -> p a b", a=16), in0=v1[:, h, :].unsqueeze(2).broadcast_to([128, 16, 16]),
                                                                 in1=v2[:, h, :].unsqueeze(1).broadcast_to([128, 16, 16]), op=ALU.add),
                          reads=[b_v1[h], b_v2[h]], writes=[b_cand[h]])
                    for h in range(8):
                        A("dve", lambda e, h=h: e.max(out=top[:, h, 0:8], in_=cand[:, h, :]), reads=[b_cand[h]], writes=[b_top[h]])
                        A("dve", lambda e, h=h: e.match_replace(out=cw[:], in_to_replace=top[:, h, 0:8], in_values=cand[:, h, :], imm_value=NEG),
                          reads=[b_cand[h], b_top[h]], writes=[b_cw])
                        A("dve", lambda e, h=h: e.max(out=top[:, h, 8:16], in_=cw[:]), reads=[b_cw, b_top[h]], writes=[b_top[h]])
                    A("dve", lambda e: e.tensor_scalar(out=negmx[:], in0=top[:, :, 0], scalar1=-1.0, scalar2=None, op0=ALU.mult), reads=b_top, writes=[b_negmx])
                    for h in range(8):
                        A("act", lambda e, h=h: e.activation(out=junk6[:], in_=top[:, h, :], func=AF.Exp, bias=negmx[:, h:h + 1], scale=1.0, accum_out=Zt[:, h:h + 1]),
                          reads=[b_top[h], b_negmx], writes=[b_junk6, b_Zt[h]])
                    A("act", lambda e: e.activation(out=rZ[:], in_=Zt[:], func=AF.Ln), reads=b_Zt, writes=[b_rZ])
                    A("dve", lambda e: e.tensor_tensor(out=rZ[:], in0=rZ[:], in1=v2[:, :, 0], op=ALU.add), reads=[b_rZ] + b_v2, writes=[b_rZ])
                    A("dve", lambda e: e.tensor_tensor(out=d1[:], in0=v1[:], in1=v1[:, :, 0:1].broadcast_to([128, 8, 16]), op=ALU.subtract), reads=b_v1, writes=[b_d1])
                    A("dve", lambda e: e.tensor_copy(out=Q4[:, 0, :].rearrange("p (h a) -> p h a", h=8), in_=v1[:]), reads=b_v1, writes=[b_Q4[0]])
                    A("dve", lambda e: e.tensor_tensor(out=Q4[:, 3, :].rearrange("p (h a) -> p h a", h=8), in0=d1[:], in1=rZ[:].unsqueeze(2).broadcast_to([128, 8, 16]), op=ALU.subtract),
                      reads=[b_d1, b_rZ], writes=[b_Q4[3]])
                    A("dve", lambda e: e.tensor_scalar(out=thrm[:], in0=top[:, :, 15], scalar1=-1.0e-5, scalar2=None, op0=ALU.add), reads=b_top, writes=[b_thrm])
                    A("dve", lambda e: e.tensor_tensor(out=Q4[:, 2, :].rearrange("p (h a) -> p h a", h=8), in0=thrm[:].unsqueeze(2).broadcast_to([128, 8, 16]), in1=v1[:], op=ALU.subtract),
                      reads=[b_thrm] + b_v1, writes=[b_Q4[2]])
                    A("dve", lambda e: e.tensor_tensor(out=Q4[:, 2, :].rearrange("p (h a) -> p h a", h=8), in0=Q4[:, 2, :].rearrange("p (h a) -> p h a", h=8),
                                                       in1=v2[:, :, 15:16].broadcast_to([128, 8, 16]), op=ALU.max),
                      reads=[b_Q4[2]] + b_v2, writes=[b_Q4[2]])
                    tb = tt % 2
                    for k4 in (0, 2, 3):
                        A("pe", lambda e, k4=k4: e.transpose(out=pT4[:, k4 * 128:(k4 + 1) * 128], in_=Q4[:, k4, :], identity=identf[:]), reads=[b_Q4[k4], b_identf], writes=[b_pT4])
                    A("act", lambda e, tb=tb: e.activation(out=sT[tb][:], in_=pT4[:].rearrange("p (k t) -> p k t", k=4), func=AF.Copy), reads=[b_pT4], writes=[b_sT[tb]])

                def issue_qrep(S_):
                    if S_ >= NT * 8:
                        return
                    qb = S_ % 2
                    t0 = S_ * 16
                    ttq = S_ // 8
                    for p_ in range(2):
                        def rep_cp(e, qb=qb, p_=p_, t0=t0):
                            src = qpT[:, p_:16:2, t0:t0 + 16].rearrange("d h t -> d t h").unsqueeze(3).broadcast_to([128, 16, 8, 16])
                            dst = qrep[qb][:, :, p_, :].rearrange("d t (h a) -> d t h a", h=8)
                            if p_ == 0:
                                return e.tensor_copy(out=dst, in_=src)
                            return e.activation(out=dst, in_=src, func=AF.Copy)
                        A("pool" if p_ == 0 else "act", rep_cp, reads=[b_qpT[hp][ttq // 4] for hp in range(p_, 16, 2)], writes=[b_qrep[qb][p_]])

                def gates(tt):
                    tb = tt % 2
                    sgb_ = tt % 2

                    def stageA(g, tt=tt, tb=tb):
                        sub, gl = divmod(g, 8)
                        qb = (tt * 8 + sub) % 2
                        if gl == 0:
                            issue_qrep(tt * 8 + sub + 1)
                        gi = tt * 64 + g
                        sl = gi % 2
                        s3 = gi % 3
                        for q in range(2):
                            tl = gl * 2 + q
                            A("pe", lambda e, sl=sl, qb=qb, tl=tl, q=q: e.matmul(pD[sl][:, q * 128:(q + 1) * 128], lhsT=qrep[qb][:, tl, 0, :], rhs=skT[:, 0, :], start=True, stop=True),
                              reads=[b_qrep[qb][0], b_skT], writes=[b_pD[sl]])
                            A("pe", lambda e, sl=sl, qb=qb, tl=tl, q=q: e.matmul(pD[sl][:, 256 + q * 128:256 + (q + 1) * 128], lhsT=qrep[qb][:, tl, 1, :], rhs=skT[:, 1, :], start=True, stop=True),
                              reads=[b_qrep[qb][1], b_skT], writes=[b_pD[sl]])
                            A("pe", lambda e, sl=sl, qb=qb, tl=tl, q=q: e.matmul(pAa[sl][:, q * 128:(q + 1) * 128], lhsT=qrep[qb][:, tl, 1, :], rhs=skT[:, 1, :], start=True, stop=True),
                              reads=[b_qrep[qb][1], b_skT], writes=[b_pAa[sl]])
                        for q in range(2):
                            t = g * 2 + q
                            A("act", lambda e, sl=sl, tb=tb, t=t, q=q: e.activation(out=Eb[sl][:, q, :], in_=pAa[sl][:, q * 128:(q + 1) * 128], func=AF.Exp, bias=sT[tb][:, 3, t:t + 1], scale=1.0),
                              reads=[b_pAa[sl], b_sT[tb]], writes=[b_Eb[sl][q]])
                        A("dve", lambda e, sl=sl, s3=s3, tb=tb, g=g: e.tensor_tensor(out=lhsG[s3][:], in0=pD[sl][:, 0:256].rearrange("p (q i) -> p q i", q=2),
                                                                                    in1=sT[tb][:, 0, g * 2:g * 2 + 2].unsqueeze(2).broadcast_to([128, 2, 128]), op=ALU.is_equal),
                          reads=[b_pD[sl], b_sT[tb]], writes=[b_lhsG[s3]])
                        for q in range(2):
                            t = g * 2 + q
                            A("dve", lambda e, sl=sl, s3=s3, tb=tb, t=t, q=q: e.scalar_tensor_tensor(out=rhsG[s3][:, q, :], in0=pD[sl][:, 256 + q * 128:256 + (q + 1) * 128], scalar=sT[tb][:, 2, t:t + 1], in1=Eb[sl][:, q, :], op0=ALU.is_ge, op1=ALU.mult),
                              reads=[b_pD[sl], b_sT[tb], b_Eb[sl][q]], writes=[b_rhsG[s3][q]])

                    def stageB(g, tt=tt, sgb_=sgb_):
                        gi = tt * 64 + g
                        s3 = gi % 3
                        gq = (gi // 2) % 2
                        for q in range(2):
                            slot = (g * 2 + q) % 4
                            A("pe", lambda e, s3=s3, gq=gq, q=q, slot=slot: e.matmul(pG[gq][:].rearrange("i (j t) -> i t j", t=4)[:, slot, :], lhsT=lhsG[s3][:, q, :], rhs=rhsG[s3][:, q, :], start=True, stop=True),
                              reads=[b_lhsG[s3], b_rhsG[s3][q]], writes=[b_pG[gq]])
                        if g % 2 == 1:
                            g4 = g // 2
                            A("act", lambda e, gq=gq, sgb_=sgb_, g4=g4: e.activation(out=SG[sgb_][:, :, g4 * 4:g4 * 4 + 4], in_=pG[gq][:].rearrange("i (j t) -> i j t", t=4), func=AF.Copy),
                              reads=[b_pG[gq]], writes=[b_SG[sgb_][g4]])

                    return stageA, stageB

                issue_qrep(0)
                front_scores(0)
                front_topk(0)
                for tt in range(NT):
                    ts_ = slice(tt * 128, (tt + 1) * 128)
                    sgb_ = tt % 2
                    if tt + 1 < NT:
                        front_scores(tt + 1)
                    stageA, stageB = gates(tt)
                    for n_ in range(64 + 1):
                        if n_ == 33 and tt + 1 < NT:
                            front_topk(tt + 1)
                        if n_ < 64:
                            stageA(n_)
                        if n_ >= 1:
                            stageB(n_ - 1)
                    for jb in range(8):
                        A("sp", lambda e, jb=jb, sgb_=sgb_, ts_=ts_: e.dma_start(out=GT[jb * 16:(jb + 1) * 16, :, ts_].rearrange("j i t -> i j t"), in_=SG[sgb_][:, jb * 16:(jb + 1) * 16, :]),
                          reads=b_SG[sgb_], writes=[b_GT[tt]], dma=True)
            P.barrier()
          P.cur = es
          P.barrier()

        if stop_after >= 7:
          u_v = peer_u.rearrange("(i j) d -> j i d", j=128)
          v_v = peer_v.rearrange("(i j) d -> j i d", j=128)
          def _half(hf):
            with ExitStack() as sc_a:
                P.cur = sc_a
                TB = hf * 1024
                h2h = P.sb(f"h2h{hf}", [128, 16, 1024], BF16); b_h2h = P.bufs(16)
                acc = P.sb(f"acc{hf}", [128, 8, D], F32); b_acc = [P.bufs(4) for _ in range(8)]
                with ExitStack() as ph:
                    P.cur = ph
                    Ust = [P.sb(f"Ust{hf}{i}", [128, D], BF16) for i in range(2)]; b_Ust = P.bufs(2)
                    UT = [P.sb(f"UT{hf}{i}", [128, 16, 128], BF16) for i in range(2)]; b_UT = P.bufs(2)
                    Vc = [P.sb(f"Vc{hf}{i}", [128, 4, D], BF16) for i in range(2)]; b_Vc = [P.bufs(4) for _ in range(2)]
                    GTc = [P.sb(f"GTc{hf}{i}", [128, 1024], BF16) for i in range(2)]; b_GTc = P.bufs(2)
                    aT = [P.sb(f"aT{hf}{i}", [128, 4, 1024], BF16) for i in range(2)]; b_aT = [[P.bufs(2) for _ in range(4)] for _ in range(2)]
                    ga = [P.sb(f"ga{hf}{i}", [128, 512], F32) for i in range(2)]; b_ga = P.bufs(2)
                    pU = [P.ps(f"pU{hf}{i}", [128, D], BF16) for i in range(2)]; b_pU = P.bufs(2)
                    pA_7 = [P.ps(f"pAe{hf}{i}", [128, 512]) for i in range(2)]; b_pA = P.bufs(2)
                    pO_7 = [P.ps(f"pOe{hf}{i}", [128, 512]) for i in range(2)]; b_pO = P.bufs(2)
                    for c in range(16):
                        A("sp", lambda e, c=c, TB=TB: e.dma_start(out=h2h[:, c, :], in_=H2T[:, c, TB:TB + 1024]), reads=b_H2T, writes=[b_h2h[c]], dma=True)
                    for tt in range(8):
                        A("sp", lambda e, tt=tt, TB=TB: e.dma_start(out=acc[:, tt, :], in_=X2[TB + tt * 128:TB + (tt + 1) * 128, :]), reads=b_X2[(TB // 128) + tt], writes=b_acc[tt], dma=True)
                    cnt = {"ia": 0, "io": 0}

                    def stT(k):
                        s_, jj = divmod(k, 4)
                        sb_ = s_ % 2
                        cb = k % 2
                        A("pool", lambda e, sb_=sb_, jj=jj, k=k: e.dma_start(out=Vc[sb_][:, jj, :], in_=v_v[k]), writes=[b_Vc[sb_][jj]], dma=True)
                        A("sp", lambda e, cb=cb, k=k: e.dma_start(out=GTc[cb][:], in_=GT[k, :, TB:TB + 1024]), reads=b_GT, writes=[b_GTc[cb]], dma=True)
                        if hf == 1:
                            A("sp", lambda e, cb=cb, k=k: e.dma_start(out=UT[cb][:], in_=UTD[k].rearrange("d (c i) -> d c i", c=16)), reads=[b_UTD[k]], writes=[b_UT[cb]], dma=True)
                            return
                        A("pool", lambda e, cb=cb, k=k: e.dma_start(out=Ust[cb][:], in_=u_v[k]), writes=[b_Ust[cb]], dma=True)
                        for c in range(16):
                            A("pe", lambda e, cb=cb, c=c: e.transpose(out=pU[cb][:, c * 128:(c + 1) * 128], in_=Ust[cb][:, c * 128:(c + 1) * 128], identity=identb[:]),
                              reads=[b_Ust[cb], b_identb], writes=[b_pU[cb]])
                        A("act", lambda e, cb=cb: e.activation(out=UT[cb][:], in_=pU[cb][:].rearrange("p (c i) -> p c i", c=16), func=AF.Copy), reads=[b_pU[cb]], writes=[b_UT[cb]])
                        A("sp", lambda e, cb=cb, k=k: e.dma_start(out=UTD[k].rearrange("d (c i) -> d c i", c=16), in_=UT[cb][:]), reads=[b_UT[cb]], writes=[b_UTD[k]], dma=True)

                    def stM(k):
                        s_, jj = divmod(k, 4)
                        sb_ = s_ % 2
                        cb = k % 2
                        for tg in range(2):
                            ab = cnt["ia"] % 2; cnt["ia"] += 1
                            for c in range(16):
                                A("pe", lambda e, ab=ab, cb=cb, c=c, tg=tg: e.matmul(pA_7[ab][:, :], lhsT=UT[cb][:, c, :], rhs=h2h[:, c, tg * 512:(tg + 1) * 512], start=(c == 0), stop=(c == 15)),
                                  reads=[b_UT[cb], b_h2h[c]], writes=[b_pA[ab]])
                            A("act", lambda e, ab=ab: e.activation(out=ga[ab][:], in_=pA_7[ab][:, :], func=AF.Gelu), reads=[b_pA[ab]], writes=[b_ga[ab]])
                            A("dve", lambda e, ab=ab, sb_=sb_, jj=jj, tg=tg, cb=cb: e.tensor_tensor(out=aT[sb_][:, jj, tg * 512:(tg + 1) * 512], in0=ga[ab][:], in1=GTc[cb][:, tg * 512:(tg + 1) * 512], op=ALU.mult),
                              reads=[b_ga[ab], b_GTc[cb]], writes=[b_aT[sb_][jj][tg]])

                    def stV(s_):
                        sb_ = s_ % 2
                        for tt in range(8):
                            for blk in range(4):
                                ob_ = cnt["io"] % 2; cnt["io"] += 1
                                for jj in range(4):
                                    A("pe", lambda e, ob_=ob_, sb_=sb_, jj=jj, tt=tt, blk=blk: e.matmul(pO_7[ob_][:, :], lhsT=aT[sb_][:, jj, tt * 128:(tt + 1) * 128], rhs=Vc[sb_][:, jj, blk * 512:(blk + 1) * 512], start=(jj == 0), stop=(jj == 3)),
                                      reads=[b_aT[sb_][jj][tt // 4], b_Vc[sb_][jj]], writes=[b_pO[ob_]])
                                A("dve", lambda e, ob_=ob_, tt=tt, blk=blk: e.tensor_tensor(out=acc[:, tt, blk * 512:(blk + 1) * 512], in0=pO_7[ob_][:, :], in1=acc[:, tt, blk * 512:(blk + 1) * 512], op=ALU.add),
                                  reads=[b_pO[ob_], b_acc[tt][blk]], writes=[b_acc[tt][blk]])

                    stT(0)
                    for k in range(128):
                        if k + 1 < 128:
                            stT(k + 1)
                        stM(k)
                        if k % 4 == 0 and k > 0:
                            stV(k // 4 - 1)
                    stV(31)
                P.barrier()
                with ExitStack() as ph:
                    P.cur = ph
                    gO = P.sb(f"gO{hf}", [128, D], F32); b_gO = Buf()
                    junk_7 = P.sb(f"junk7{hf}", [128, D], BF16); b_junk = Buf()
                    ss_7 = [P.sb(f"ss7{hf}{i}", [128, 1], F32) for i in range(2)]; b_ss = P.bufs(2)
                    rstd_7 = [P.sb(f"rstd7{hf}{i}", [128, 1], F32) for i in range(2)]; b_rstd = P.bufs(2)
                    A("sp", lambda e: e.dma_start(out=gO[:], in_=final_norm.partition_broadcast(128)), writes=[b_gO], dma=True)
                    for tt in range(8):
                        b = tt % 2
                        rms_stats(acc[:, tt, :], b_acc[tt], ss_7[b][:], b_ss[b], rstd_7[b][:], b_rstd[b], junk_7[:], b_junk, D)
                        A("dve", lambda e, b=b, tt=tt: e.scalar_tensor_tensor(out=acc[:, tt, :], in0=acc[:, tt, :], scalar=rstd_7[b][:, 0:1], in1=gO[:], op0=ALU.mult, op1=ALU.mult),
                          reads=b_acc[tt] + [b_rstd[b], b_gO], writes=b_acc[tt])
                        fin.append(A("sp", lambda e, tt=tt, TB=TB: e.dma_start(out=out[TB + tt * 128:TB + (tt + 1) * 128, :], in_=acc[:, tt, :]), reads=b_acc[tt], dma=True))
                P.barrier()
            P.cur = es
            P.barrier()
          _half(0)
          _half(1)

        P.emit(final_waits=[op for e_ in P.ENGS for op in P.ops[e_] if op.dma])
        build.nops = P.nops
    return nc


def _consts():
    bf = ml_dtypes.bfloat16
    slopes = 2.0 ** (-np.arange(1, 9, dtype=np.float64))
    pos = np.arange(S)
    kaug = np.zeros((8, 4, S), np.float64)
    qaug = np.zeros((8, 4, S), np.float64)
    for h in range(8):
        kaug[h, 0] = 8 * slopes[h] * 128 * (pos // 128)
        kaug[h, 1] = 8 * slopes[h] * (pos % 128)
        kaug[h, 2] = 1
        kaug[h, 3] = 1
        qaug[h, 0] = 1
        qaug[h, 1] = 1
        qaug[h, 2] = -8 * slopes[h] * 128 * (pos // 128)
        qaug[h, 3] = -8 * slopes[h] * (pos % 128)
    kp = np.arange(128)[:, None]
    qp = np.arange(128)[None, :]
    allowed = (kp // 64) <= (qp // 64)
    bdiag = np.zeros((128, 8, 128), np.float64)
    for h in range(8):
        bdiag[:, h, :] = np.where(allowed, -8 * slopes[h] * np.abs(qp - kp), -240000.0)
    tri = (np.arange(128)[None, :] <= np.arange(128)[:, None]).astype(np.float32)
    return {
        "c_identb": np.eye(128, dtype=np.float32).astype(bf),
        "c_identf": np.eye(128, dtype=np.float32),
        "c_kaug": kaug.astype(np.float32).astype(bf),
        "c_qaug": qaug.astype(np.float32).astype(bf),
        "c_bdiag": bdiag.astype(np.float32).astype(bf),
        "c_tri": tri,
    }


def make_in_maps(inputs, cores):
    f = lambda a: np.ascontiguousarray(np.asarray(a, dtype=np.float32))
    shared = {
        "attn_norm": f(inputs["attn_norm"]).reshape(1, D),
        "w_in": f(inputs["w_in"]).reshape(D, IN_COLS),
        "diff_lambda": f(inputs["diff_lambda"]).reshape(1, 256),
        "diff_subln": f(inputs["diff_subln"]).reshape(1, 128),
        "gmlp_norm": f(inputs["gmlp_norm"]).reshape(1, 1024),
        "gmlp_ws": f(inputs["gmlp_ws"]).reshape(8, 128, 128),
        "gmlp_bs": f(inputs["gmlp_bs"]).reshape(8, 128),
        "w_branch_a": f(inputs["w_branch_a"]).reshape(1024, D),
        "w_branch_b": f(inputs["w_branch_b"]).reshape(1024, D),
        "w_out": f(inputs["w_out"]).reshape(D, D),
        "ffn_norm": f(inputs["ffn_norm"]).reshape(1, D),
        "peer_wq": f(inputs["peer_wq"]).reshape(D, 2048),
        "peer_subkeys": f(inputs["peer_subkeys"]).reshape(2, 128, 128),
        "peer_u": f(inputs["peer_u"]).reshape(16384, D),
        "peer_v": f(inputs["peer_v"]).reshape(16384, D),
        "final_norm": f(inputs["final_norm"]).reshape(1, D),
    }
    shared.update(_consts())
    xs = f(inputs["x"])
    return [dict(shared, x=xs[b]) for b in cores]


def kernel(**inputs):
    nc = build()
    in_maps = make_in_maps(inputs, list(range(8)))
    res = run_bass_kernel_spmd(nc, in_maps, core_ids=list(range(8)))
    return np.stack([np.asarray(r["out"], dtype=np.float32) for r in res.results], axis=0)
```
